# Optimizing a Trainium2 kernel written in Bass

```python
import math
import jax, jax.numpy as jnp
from jax import lax
import numpy as np

D_MODEL = 1024
BATCH = 8
SEQ = 4096
DEPTH = 2

HEAD_DIM = 64
GRID_W = 64
BLOCK_Q = 128
NORM_EPS = 1e-6
N_DIR = 2

A_HEADS = 4
A_WIDTH = A_HEADS * HEAD_DIM
A_DECAY_LORA = 64
A_ICL_LORA = 64
A_GATE_LORA = 128
A_GN_EPS = 64e-5
A_IN = 3 * A_WIDTH + A_DECAY_LORA + A_ICL_LORA + A_GATE_LORA

B_Q_HEADS = 4
B_KV_HEADS = 2
B_WIDTH = B_Q_HEADS * HEAD_DIM
B_IN = (B_Q_HEADS + 2 * B_KV_HEADS) * HEAD_DIM
ROPE_THETA = 10000.0
AXIS_DIM = HEAD_DIM // 2

C_HEADS = 4
C_WIDTH = C_HEADS * HEAD_DIM
C_CONV = 5
C_CHUNK = 64
C_IN = 4 * C_WIDTH + 2 * N_DIR * C_HEADS

D_BRANCHES = ((128, 1), (512, 4), (2048, 16))
D_HEADS_PER_BRANCH = 2
D_HEADS = D_HEADS_PER_BRANCH * len(D_BRANCHES)
D_WIDTH = D_HEADS * HEAD_DIM
D_IN = 3 * D_WIDTH
REL_BUCKETS = 32
REL_MAX_DIST = 1024

P_IN = A_IN + B_IN + C_IN + D_IN
MIX_WIDTH = A_WIDTH + B_WIDTH + C_WIDTH + D_WIDTH

N_EXPERTS = 16
EC_CAPACITY = 2
D_FF_EXPERT = D_MODEL

kernel_name = 'hybrid_parallel_heads_ec_moe_encoder'


def _heads(t):
    return t.reshape(t.shape[:-1] + (t.shape[-1] // HEAD_DIM, HEAD_DIM))


def rms_norm(x, g):
    xf = x.astype(jnp.float32)
    y = xf * lax.rsqrt(jnp.mean(xf * xf, axis=-1, keepdims=True) + NORM_EPS)
    return (y * g.astype(jnp.float32)).astype(x.dtype)


def _l2norm(x):
    return x * lax.rsqrt(jnp.sum(x * x, axis=-1, keepdims=True) + 1e-12)


def rwkv7_mixer(feat, mu_prev, mu_next, w0, w_up, a0, a_up, g_up, k_k, k_a, r_k, ln_w, ln_b):
    dtype = feat.dtype
    f = feat.astype(jnp.float32)
    prev = jnp.pad(f, ((0, 0), (1, 0), (0, 0)))[:, :-1]
    nxt = jnp.pad(f, ((0, 0), (0, 1), (0, 0)))[:, 1:]
    f = f + mu_prev * (prev - f) + mu_next * (nxt - f)
    r, k, v, wd, ad, gd = jnp.split(f, [A_WIDTH, 2 * A_WIDTH, 3 * A_WIDTH, 3 * A_WIDTH + A_DECAY_LORA, 3 * A_WIDTH + A_DECAY_LORA + A_ICL_LORA], axis=-1)
    w_log = -jax.nn.softplus(-(w0[:, None, None, :] + jnp.einsum('bsr,drc->dbsc', jnp.tanh(wd), w_up))) - 0.5
    decay = jnp.exp(-jnp.exp(w_log))
    a = jax.nn.sigmoid(a0[:, None, None, :] + jnp.einsum('bsr,drc->dbsc', ad, a_up))
    g = jnp.einsum('bsr,rc->bsc', jax.nn.sigmoid(gd), g_up)
    kk = _l2norm(_heads(k * k_k))
    k_eff = k[None] * (1.0 + (a - 1.0) * k_a)
    both = lambda t: jnp.stack([t, jnp.flip(t, 1)])
    per_dir = lambda t: jnp.stack([t[0], jnp.flip(t[1], 1)])
    to_time = lambda t: jnp.moveaxis(t, 2, 0)
    xs = (to_time(both(_heads(r))), to_time(per_dir(_heads(decay))), to_time(per_dir(_heads(k_eff))),
          to_time(both(_heads(v))), to_time(both(kk)), to_time(per_dir(_heads(a))))
    bsz, seq = f.shape[0], f.shape[1]
    state0 = jnp.zeros((N_DIR, bsz, A_HEADS, HEAD_DIM, HEAD_DIM), jnp.float32)

    def step(st, inp):
        r_t, w_t, k_t, v_t, kk_t, a_t = inp
        sa = jnp.einsum('dbhvk,dbhk->dbhv', st, -kk_t)
        st = st * w_t[..., None, :] + sa[..., :, None] * (kk_t * a_t)[..., None, :] + v_t[..., :, None] * k_t[..., None, :]
        return st, jnp.einsum('dbhvk,dbhk->dbhv', st, r_t)

    _, ys = lax.scan(step, state0, xs)
    ys = jnp.moveaxis(ys, 0, 2)
    y = ys[0] + jnp.flip(ys[1], 1)
    mean = jnp.mean(y, -1, keepdims=True)
    var = jnp.mean(jnp.square(y - mean), -1, keepdims=True)
    y = ((y - mean) * lax.rsqrt(var + A_GN_EPS)).reshape(bsz, seq, A_WIDTH) * ln_w + ln_b
    bonus = jnp.sum(_heads(r) * _heads(jnp.mean(k_eff, 0)) * r_k, -1, keepdims=True) * _heads(v)
    out = (y + bonus.reshape(bsz, seq, A_WIDTH)) * g
    return out.astype(dtype)


def axial_rope_angles(seq):
    rows = seq // GRID_W
    row_id = jnp.repeat(jnp.arange(rows), GRID_W).astype(jnp.float32)
    col_id = jnp.tile(jnp.arange(GRID_W), rows).astype(jnp.float32)
    inv = ROPE_THETA ** (-jnp.arange(0, AXIS_DIM, 2, dtype=jnp.float32) / AXIS_DIM)
    return row_id[:, None] * inv, col_id[:, None] * inv


def _rotate(x, ang):
    half = AXIS_DIM // 2
    x1, x2 = x[..., :half], x[..., half:]
    cos = jnp.cos(ang)[None, :, None, :].astype(x.dtype)
    sin = jnp.sin(ang)[None, :, None, :].astype(x.dtype)
    return jnp.concatenate([x1 * cos - x2 * sin, x2 * cos + x1 * sin], -1)


def axial_rope(x, ang_r, ang_c):
    return jnp.concatenate([_rotate(x[..., :AXIS_DIM], ang_r), _rotate(x[..., AXIS_DIM:], ang_c)], -1)


def gqa_axial_mixer(feat, q_norm_g, k_norm_g):
    bsz, seq, _ = feat.shape
    q, k, v = jnp.split(feat, [B_WIDTH, B_WIDTH + B_KV_HEADS * HEAD_DIM], axis=-1)
    q = rms_norm(_heads(q), q_norm_g)
    k = rms_norm(_heads(k), k_norm_g)
    v = _heads(v)
    ang_r, ang_c = axial_rope_angles(seq)
    q = axial_rope(q, ang_r, ang_c)
    k = axial_rope(k, ang_r, ang_c)
    rep = B_Q_HEADS // B_KV_HEADS
    nblk = seq // BLOCK_Q
    qb = jnp.moveaxis(q.reshape(bsz, nblk, BLOCK_Q, B_KV_HEADS, rep, HEAD_DIM), 1, 0)
    scale = HEAD_DIM ** -0.5

    def block(qi):
        s = jnp.einsum('bqgrd,bkgd->bgrqk', qi, k).astype(jnp.float32) * scale
        p = jax.nn.softmax(s, axis=-1).astype(v.dtype)
        return jnp.einsum('bgrqk,bkgd->bqgrd', p, v)

    o = lax.map(block, qb)
    return jnp.moveaxis(o, 0, 1).reshape(bsz, seq, B_WIDTH).astype(feat.dtype)


def short_conv(x, w):
    ch = x.shape[-1]
    return lax.conv_general_dilated(x, w[:, None, :].astype(x.dtype), window_strides=(1,),
                                    padding=[(C_CONV // 2, C_CONV // 2)],
                                    dimension_numbers=('NWC', 'WIO', 'NWC'), feature_group_count=ch)


def chunk_gated_delta(q, k, v, beta, g):
    lead = q.shape[:-2]
    seq = q.shape[-2]
    dk, dv = q.shape[-1], v.shape[-1]
    n = seq // C_CHUNK
    q, k, v = (t.astype(jnp.float32).reshape(lead + (n, C_CHUNK, t.shape[-1])) for t in (q, k, v))
    beta = beta.astype(jnp.float32).reshape(lead + (n, C_CHUNK))
    g = jnp.cumsum(g.astype(jnp.float32).reshape(lead + (n, C_CHUNK)), axis=-1)
    incl = jnp.tril(jnp.ones((C_CHUNK, C_CHUNK), bool))
    strict = jnp.tril(jnp.ones((C_CHUNK, C_CHUNK), bool), -1)
    decay = jnp.exp(jnp.where(incl, g[..., :, None] - g[..., None, :], -jnp.inf))
    kb = k * beta[..., None]
    lower = jnp.where(strict, jnp.einsum('...id,...jd->...ij', kb, k) * decay, 0.0)
    eye = jnp.eye(C_CHUNK, dtype=jnp.float32)
    rhs = jnp.concatenate([v * beta[..., None], kb * jnp.exp(g)[..., None]], -1)
    sol = lax.linalg.triangular_solve(lower + eye, rhs, left_side=True, lower=True)
    u, w = sol[..., :dv], sol[..., dv:]
    a_intra = jnp.einsum('...id,...jd->...ij', q, k) * decay
    ax = len(lead)
    mv = lambda t: jnp.moveaxis(t, ax, 0)
    state0 = jnp.zeros(lead + (dk, dv), jnp.float32)

    def step(st, inp):
        qc, kc, uc, wc, gc, ac = inp
        v_new = uc - jnp.einsum('...cd,...dv->...cv', wc, st)
        o = jnp.einsum('...cd,...dv->...cv', qc * jnp.exp(gc)[..., None], st) + jnp.einsum('...ij,...jv->...iv', ac, v_new)
        gl = gc[..., -1:]
        st = st * jnp.exp(gl)[..., None] + jnp.einsum('...cd,...cv->...dv', kc * jnp.exp(gl - gc)[..., None], v_new)
        return st, o

    _, o = lax.scan(step, state0, (mv(q), mv(k), mv(u), mv(w), mv(g), mv(a_intra)))
    return jnp.moveaxis(o, 0, ax).reshape(lead + (seq, dv))


def gated_deltanet_mixer(feat, conv_w, a_log, dt_bias, norm_g):
    bsz, seq, _ = feat.shape
    qkv, z, b, al = jnp.split(feat, [3 * C_WIDTH, 4 * C_WIDTH, 4 * C_WIDTH + N_DIR * C_HEADS], axis=-1)
    qkv = jax.nn.silu(short_conv(qkv, conv_w))
    q, k, v = jnp.split(qkv, [C_WIDTH, 2 * C_WIDTH], axis=-1)
    q = _l2norm(_heads(q).astype(jnp.float32)) * (HEAD_DIM ** -0.5)
    k = _l2norm(_heads(k).astype(jnp.float32))
    v = _heads(v).astype(jnp.float32)
    beta = jax.nn.sigmoid(b.reshape(bsz, seq, N_DIR, C_HEADS).astype(jnp.float32))
    gdec = -jnp.exp(a_log.astype(jnp.float32)) * jax.nn.softplus(al.reshape(bsz, seq, N_DIR, C_HEADS).astype(jnp.float32) + dt_bias)
    both = lambda t: jnp.stack([jnp.swapaxes(t, 1, 2), jnp.flip(jnp.swapaxes(t, 1, 2), 2)])
    per_dir = lambda t: jnp.stack([jnp.transpose(t, (2, 0, 3, 1))[0], jnp.flip(jnp.transpose(t, (2, 0, 3, 1))[1], -1)])
    o = chunk_gated_delta(both(q), both(k), both(v), per_dir(beta), per_dir(gdec))
    o = jnp.swapaxes(o[0] + jnp.flip(o[1], 2), 1, 2)
    o = rms_norm(o, norm_g) * jax.nn.silu(_heads(z).astype(jnp.float32))
    return o.reshape(bsz, seq, C_WIDTH).astype(feat.dtype)


def t5_bucket(rel):
    nb = REL_BUCKETS // 2
    max_exact = nb // 2
    n = jnp.abs(rel)
    large = max_exact + (jnp.log(jnp.maximum(n, 1).astype(jnp.float32) / max_exact)
                         / math.log(REL_MAX_DIST / max_exact) * (nb - max_exact)).astype(jnp.int32)
    large = jnp.minimum(large, nb - 1)
    return jnp.where(rel > 0, nb, 0) + jnp.where(n < max_exact, n, large)


def dilated_window_mixer(feat, rel_bias):
    bsz, seq, _ = feat.shape
    q, k, v = [_heads(t) for t in jnp.split(feat, [D_WIDTH, 2 * D_WIDTH], axis=-1)]
    nblk = seq // BLOCK_Q
    qb = jnp.swapaxes(q.reshape(bsz, nblk, BLOCK_Q, D_HEADS, HEAD_DIM), 0, 1)
    starts = jnp.arange(nblk, dtype=jnp.int32) * BLOCK_Q
    branches = []
    for br, (window, dil) in enumerate(D_BRANCHES):
        half = window // (2 * dil)
        offs = jnp.arange(-half, half + 1, dtype=jnp.int32) * dil
        hs = slice(br * D_HEADS_PER_BRANCH, (br + 1) * D_HEADS_PER_BRANCH)
        bias = rel_bias[t5_bucket(offs)][:, hs].T.astype(jnp.float32)
        branches.append((offs, hs, k[:, :, hs], v[:, :, hs], bias))
    scale = HEAD_DIM ** -0.5

    def block(args):
        qi, start = args
        pos = start + jnp.arange(BLOCK_Q, dtype=jnp.int32)
        ms, ls, outs = [], [], []
        for offs, hs, kbr, vbr, bias in branches:
            idx = pos[:, None] + offs[None, :]
            valid = (idx >= 0) & (idx < seq)
            idx = jnp.clip(idx, 0, seq - 1)
            kg = kbr[:, idx]
            vg = vbr[:, idx]
            s = jnp.einsum('bqhd,bqkhd->bqhk', qi[:, :, hs], kg).astype(jnp.float32) * scale + bias
            s = jnp.where(valid[None, :, None, :], s, -jnp.inf)
            m = jnp.max(s, -1, keepdims=True)
            p = jnp.exp(s - m)
            l = jnp.sum(p, -1, keepdims=True)
            outs.append(jnp.einsum('bqhk,bqkhd->bqhd', (p / l).astype(vg.dtype), vg).astype(jnp.float32))
            ms.append(m)
            ls.append(l)
        m_all = jnp.stack(ms)
        den = jnp.stack(ls) * jnp.exp(m_all - jnp.max(m_all, 0))
        o = jnp.stack(outs) * (den / jnp.sum(den, 0))
        return jnp.moveaxis(o, 0, 2).reshape(bsz, BLOCK_Q, D_HEADS, HEAD_DIM)

    o = lax.map(block, (qb, starts))
    return jnp.swapaxes(o, 0, 1).reshape(bsz, seq, D_WIDTH).astype(feat.dtype)


def expert_choice_ffn(h, router_w, router_b, w_gate, w_up, w_down):
    bsz, seq, _ = h.shape
    cap = EC_CAPACITY * seq // N_EXPERTS
    aff = jax.nn.softmax(jnp.einsum('bsd,de->bse', h, router_w).astype(jnp.float32) + router_b.astype(jnp.float32), axis=-1)
    gate, idx = lax.top_k(jnp.swapaxes(aff, 1, 2), cap)
    bidx = jnp.arange(bsz)[:, None, None]
    xs = h[bidx, idx]
    hid = jax.nn.silu(jnp.einsum('becd,edf->becf', xs, w_gate)) * jnp.einsum('becd,edf->becf', xs, w_up)
    ys = jnp.einsum('becf,efd->becd', hid, w_down) * gate[..., None].astype(h.dtype)
    return jnp.zeros_like(h).at[bidx, idx].add(ys.astype(h.dtype))


def setup_inputs(seed: int = 0) -> dict:
    key = jax.random.key(seed)
    ks = iter(jax.random.split(key, 40))

    def nrm(shape, scale):
        return scale * jax.random.normal(next(ks), shape, jnp.float32)

    def uni(shape, lo, hi):
        return jax.random.uniform(next(ks), shape, jnp.float32, lo, hi)

    L = DEPTH
    dt = jnp.exp(uni((L, N_DIR, C_HEADS), math.log(1e-3), math.log(1e-1)))
    return {
        'x': nrm((BATCH, SEQ, D_MODEL), 1.0),
        'norm_mix_g': 1.0 + nrm((L, D_MODEL), 0.1),
        'norm_ffn_g': 1.0 + nrm((L, D_MODEL), 0.1),
        'norm_final_g': 1.0 + nrm((D_MODEL,), 0.1),
        'w_in': nrm((L, D_MODEL, P_IN), D_MODEL ** -0.5),
        'w_out': nrm((L, MIX_WIDTH, D_MODEL), MIX_WIDTH ** -0.5),
        'rwkv_mu_prev': uni((L, A_IN), 0.0, 0.5),
        'rwkv_mu_next': uni((L, A_IN), 0.0, 0.5),
        'rwkv_w0': nrm((L, N_DIR, A_WIDTH), 0.5) - 0.5,
        'rwkv_w_up': nrm((L, N_DIR, A_DECAY_LORA, A_WIDTH), 0.1),
        'rwkv_a0': nrm((L, N_DIR, A_WIDTH), 0.5),
        'rwkv_a_up': nrm((L, N_DIR, A_ICL_LORA, A_WIDTH), 0.1),
        'rwkv_g_up': nrm((L, A_GATE_LORA, A_WIDTH), A_GATE_LORA ** -0.5),
        'rwkv_k_k': 0.85 + nrm((L, A_WIDTH), 0.05),
        'rwkv_k_a': 1.0 + nrm((L, A_WIDTH), 0.05),
        'rwkv_r_k': nrm((L, A_HEADS, HEAD_DIM), 0.1),
        'rwkv_ln_w': 1.0 + nrm((L, A_WIDTH), 0.1),
        'rwkv_ln_b': nrm((L, A_WIDTH), 0.02),
        'attn_q_norm': 1.0 + nrm((L, HEAD_DIM), 0.1),
        'attn_k_norm': 1.0 + nrm((L, HEAD_DIM), 0.1),
        'gdn_conv': nrm((L, C_CONV, 3 * C_WIDTH), C_CONV ** -0.5),
        'gdn_a_log': jnp.log(uni((L, N_DIR, C_HEADS), 1.0, 16.0)),
        'gdn_dt_bias': dt + jnp.log(-jnp.expm1(-dt)),
        'gdn_norm_g': 1.0 + nrm((L, HEAD_DIM), 0.1),
        'rel_bias': nrm((REL_BUCKETS, D_HEADS), 0.5),
        'router_w': nrm((L, D_MODEL, N_EXPERTS), D_MODEL ** -0.5),
        'router_b': nrm((L, N_EXPERTS), 0.01),
        'expert_w_gate': nrm((L, N_EXPERTS, D_MODEL, D_FF_EXPERT), D_MODEL ** -0.5),
        'expert_w_up': nrm((L, N_EXPERTS, D_MODEL, D_FF_EXPERT), D_MODEL ** -0.5),
        'expert_w_down': nrm((L, N_EXPERTS, D_FF_EXPERT, D_MODEL), D_FF_EXPERT ** -0.5),
    }


def reference(x, norm_mix_g, norm_ffn_g, norm_final_g, w_in, w_out,
              rwkv_mu_prev, rwkv_mu_next, rwkv_w0, rwkv_w_up, rwkv_a0, rwkv_a_up, rwkv_g_up,
              rwkv_k_k, rwkv_k_a, rwkv_r_k, rwkv_ln_w, rwkv_ln_b,
              attn_q_norm, attn_k_norm,
              gdn_conv, gdn_a_log, gdn_dt_bias, gdn_norm_g,
              rel_bias,
              router_w, router_b, expert_w_gate, expert_w_up, expert_w_down):
    for l in range(DEPTH):
        h = rms_norm(x, norm_mix_g[l])
        p = jnp.einsum('bsd,dp->bsp', h, w_in[l])
        fa, fb, fc, fd = jnp.split(p, [A_IN, A_IN + B_IN, A_IN + B_IN + C_IN], axis=-1)
        ya = rwkv7_mixer(fa, rwkv_mu_prev[l], rwkv_mu_next[l], rwkv_w0[l], rwkv_w_up[l], rwkv_a0[l],
                         rwkv_a_up[l], rwkv_g_up[l], rwkv_k_k[l], rwkv_k_a[l], rwkv_r_k[l],
                         rwkv_ln_w[l], rwkv_ln_b[l])
        yb = gqa_axial_mixer(fb, attn_q_norm[l], attn_k_norm[l])
        yc = gated_deltanet_mixer(fc, gdn_conv[l], gdn_a_log[l], gdn_dt_bias[l], gdn_norm_g[l])
        yd = dilated_window_mixer(fd, rel_bias)
        mixed = jnp.concatenate([ya, yb, yc, yd], axis=-1)
        x = x + jnp.einsum('bsm,md->bsd', mixed, w_out[l])
        x = x + expert_choice_ffn(rms_norm(x, norm_ffn_g[l]), router_w[l], router_b[l],
                                  expert_w_gate[l], expert_w_up[l], expert_w_down[l])
    return rms_norm(x, norm_final_g)
```

```python
import math
from contextlib import ExitStack
import numpy as np
import concourse.bass as bass
import concourse.mybir as mybir
from concourse.bass_utils import run_bass_kernel_spmd

F32 = mybir.dt.float32
F32R = mybir.dt.float32r
BF16 = mybir.dt.bfloat16
U32 = mybir.dt.uint32
I32 = mybir.dt.int32
AF = mybir.ActivationFunctionType
ALU = mybir.AluOpType
AX = mybir.AxisListType

T = 4096
NT = 32
D = 1024
L = 2
HD = 64
EPS = 1e-6
A_W = 256
A_IN = 1024
B_IN = 512
C_IN = 1040
D_IN = 1152
P_IN = 3728
OFF_A = 0
OFF_B = 1024
OFF_C = 1536
OFF_D = 2576
MIXW = 1152
NE = 16
CAP = 512
NEG = -30000.0

NDMA = 44
NSW = 12


class Sched:
    def __init__(self, nc, same_engine_sync=True):
        self.nc = nc
        self.eng = {"pe": nc.tensor, "act": nc.scalar, "dve": nc.vector,
                    "pool": nc.gpsimd, "sp": nc.sync}
        self.sem = {}
        for k in self.eng:
            self.sem[k] = nc.alloc_semaphore("sem_" + k)
        self.cnt = {k: 0 for k in self.eng}
        for i in range(NDMA):
            self.sem[("d", i)] = nc.alloc_semaphore("sem_d%d" % i)
        self.dval = [0] * NDMA
        self.dnext = {"hw": 0, "sw": 0}
        self.known = {k: {} for k in self.eng}
        self.w = {}
        self.r = {}
        self.same = same_engine_sync
        self.nwaits = 0
        self.nops = 0

    def _wait(self, e, ev):
        if ev is None:
            return
        key, val = ev
        if key == e and (e == "pe" or not self.same):
            return
        if self.known[e].get(key, 0) >= val:
            return
        self.eng[e].wait_ge(self.sem[key], val)
        self.known[e][key] = val
        self.nwaits += 1

    def _deps(self, e, reads, writes):
        for b in reads:
            self._wait(e, self.w.get(b))
        for b in writes:
            self._wait(e, self.w.get(b))
            for ev in self.r.get(b, ()):
                self._wait(e, ev)

    def _commit(self, ev, reads, writes):
        for b in reads:
            self.r.setdefault(b, []).append(ev)
        for b in writes:
            self.w[b] = ev
            self.r[b] = []

    def op(self, e, fn, reads=(), writes=()):
        self._deps(e, reads, writes)
        ins = fn()
        self.cnt[e] += 1
        ins.then_inc(self.sem[e], 1)
        self._commit((e, self.cnt[e]), reads, writes)
        self.nops += 1
        return ins

    def dma_raw(self, q, fn, reads=(), writes=()):
        self._deps(q, reads, writes)
        if q == "pool":
            s = NDMA - NSW + self.dnext["sw"]
            self.dnext["sw"] = (self.dnext["sw"] + 1) % NSW
        else:
            s = self.dnext["hw"]
            self.dnext["hw"] = (self.dnext["hw"] + 1) % (NDMA - NSW)
        key = ("d", s)
        if self.dval[s] > 0:
            self._wait(q, (key, self.dval[s]))
        ins = fn()
        self.dval[s] += 16
        ins.then_inc(self.sem[key], 16)
        self._commit((key, self.dval[s]), reads, writes)
        self.nops += 1
        return ins

    def dma(self, q, out, in_, reads=(), writes=(), **kw):
        return self.dma_raw(q, lambda: self.eng[q].dma_start(out=out, in_=in_, **kw), reads, writes)

    def barrier(self):
        for e in self.eng:
            for s in range(NDMA):
                if self.dval[s] > 0:
                    self._wait(e, (("d", s), self.dval[s]))
            for o in self.eng:
                if o != e and self.cnt[o] > 0:
                    key, val = o, self.cnt[o]
                    if self.known[e].get(key, 0) < val:
                        self.eng[e].wait_ge(self.sem[key], val)
                        self.known[e][key] = val
                        self.nwaits += 1
        self.w = {}
        self.r = {}


class StopScan(Exception):
    pass


def _stop(tag):
    import os
    return os.environ.get("SCAN_STOP") == tag and _CUR_U[0] >= int(os.environ.get("SCAN_U", "0"))


_CUR_U = [0]


class TB:
    def __init__(self, ap, key):
        self.ap, self.key = ap, key

    def __getitem__(self, idx):
        return TB(self.ap[idx], self.key)

    def v(self, f):
        return TB(f(self.ap), self.key)


class Prog:
    def __init__(self, stop_after=None, dbg=None):
        self.nc = nc = bass.Bass("TRN2", target_bir_lowering=False)
        self.S = Sched(nc)
        self.stop_after = stop_after
        self.dbg = dbg
        dt = nc.dram_tensor
        self.inputs = {}
        self.x = self.inp("x", [T, D])
        self.norm_mix_g = self.inp("norm_mix_g", [L, D])
        self.w_in = self.inp("w_in", [L, D, P_IN])
        self.ident_f = self.inp("ident_f", [128, 128])
        self.P = dt("P_scr", [T, P_IN], F32, kind="Internal").ap()
        self.mixed = dt("mixed_scr", [T, MIXW], F32, kind="Internal").ap()
        self.Dnum = dt("Dnum_scr", [T, 6, 65], F32, kind="Internal").ap()
        self.X2 = dt("X2_scr", [T, D], F32, kind="Internal").ap()
        self.X3s = [dt("X3_scr%d" % i, [T, D], F32, kind="Internal").ap() for i in range(2)]
        self.X3 = self.X3s[0]
        self.norm_final_g = self.inp("norm_final_g", [1, D])
        self.H2 = dt("H2_scr", [T, D], BF16, kind="Internal").ap()
        self.AFFd = dt("AFF_scr", [T, NE], F32, kind="Internal").ap()
        self.IDXd = dt("IDX_scr", [NE, CAP], U32, kind="Internal").ap()
        self.w_out = self.inp("w_out", [L, MIXW, D])
        self.norm_ffn_g = self.inp("norm_ffn_g", [L, D])
        self.router_w = self.inp("router_w", [L, D, NE])
        self.router_b = self.inp("router_b", [L, NE])
        if stop_after not in ("inproj0", "BD", "outproj", "moe_idx", "AC", "prepAC"):
            self.w_gate = self.inp("expert_w_gate", [L, NE, D, D])
            self.w_up = self.inp("expert_w_up", [L, NE, D, D])
            self.w_down = self.inp("expert_w_down", [L, NE, D, D])
        self.ustrict = self.inp("ustrict", [128, 128])
        self.iota_c = self.inp("iota_c", [128, CAP])
        self.tvals = self.inp("tvals", [128, NT, 2])
        self.TM = dt("TM_scr", [T, 2, 2, 6, 256], F32, kind="Internal").ap()
        self.AUX = dt("AUX_scr", [T, 3, 256], F32, kind="Internal").ap()
        self.YAC = dt("YAC_scr", [T, 2, 2, 256], F32, kind="Internal").ap()
        self.rwkv_mu_prev = self.inp("rwkv_mu_prev", [L, 1024])
        self.rwkv_mu_next = self.inp("rwkv_mu_next", [L, 1024])
        self.rwkv_w0 = self.inp("rwkv_w0", [L, 2, 256])
        self.rwkv_w_up = self.inp("rwkv_w_up", [L, 2, 64, 256])
        self.rwkv_a0 = self.inp("rwkv_a0", [L, 2, 256])
        self.rwkv_a_up = self.inp("rwkv_a_up", [L, 2, 64, 256])
        self.rwkv_g_up = self.inp("rwkv_g_up", [L, 128, 256])
        self.rwkv_k_k = self.inp("rwkv_k_k", [L, 256])
        self.rwkv_k_a = self.inp("rwkv_k_a", [L, 256])
        self.rwkv_r_k = self.inp("rwkv_r_k", [L, 4, 64])
        self.rwkv_ln_w = self.inp("rwkv_ln_w", [L, 256])
        self.rwkv_ln_b = self.inp("rwkv_ln_b", [L, 256])
        self.gdn_conv = self.inp("gdn_conv", [L, 5, 768])
        self.gdn_a_log = self.inp("gdn_a_log", [L, 2, 4])
        self.gdn_dt_bias = self.inp("gdn_dt_bias", [L, 2, 4])
        self.gdn_norm_g = self.inp("gdn_norm_g", [L, 64])
        self.c_tri = self.inp("c_tri", [2, 128, 128])
        self.c_msk = self.inp("c_msk", [2, 128, 4, 128])
        self.c_onb = self.inp("c_onb", [128, 128])
        self.attn_q_norm = self.inp("attn_q_norm", [L, 64])
        self.attn_k_norm = self.inp("attn_k_norm", [L, 64])
        self.rope_cos = self.inp("rope_cos", [T, 64])
        self.rope_sin = self.inp("rope_sin", [T, 64])
        self.dbias = self.inp("dbias", [3, 2, 128, 3, 128])
        self.es = ExitStack()
        self.identf = self.sb(self.es, "identf", [128, 128], F32)
        self.identb = self.sb(self.es, "identb", [128, 128], BF16)
        self.AFF = self.sb(self.es, "AFF", [128, NT, NE], F32)
        S = self.S
        S.dma("sp", self.identf, self.ident_f, writes=["identf"])
        S.op("dve", lambda: nc.vector.tensor_copy(out=self.identb, in_=self.identf), reads=["identf"], writes=["identb"])

    def inp(self, name, shape, dtype=F32):
        self.inputs[name] = (tuple(shape), dtype)
        return self.nc.dram_tensor(name, list(shape), dtype, kind="ExternalInput").ap()

    def _uname(self, name):
        self.uid = getattr(self, "uid", 0) + 1
        return "%s_%d" % (name, self.uid)

    def sb(self, es, name, shape, dtype):
        return es.enter_context(self.nc.sbuf_tensor(self._uname(name), list(shape), dtype)).ap()

    def ps(self, es, name, shape, dtype=F32):
        return es.enter_context(self.nc.psum_tensor(self._uname(name), list(shape), dtype)).ap()

    def stage_inproj(self, l, xsrc):
        nc, S = self.nc, self.S
        with ExitStack() as es:
            W = self.sb(es, "Win", [128, 8, P_IN], BF16)
            gB = self.sb(es, "gB", [128, D], F32)
            xts = [self.sb(es, "xt%d" % i, [128, D], F32) for i in range(2)]
            junk = self.sb(es, "junk", [128, D], F32)
            hb = [self.sb(es, "hb%d" % i, [128, D], BF16) for i in range(2)]
            hT = [self.sb(es, "hT%d" % i, [128, 8, 128], BF16) for i in range(2)]
            pts = [self.sb(es, "pt%d" % i, [128, P_IN], F32) for i in range(2)]
            st = self.sb(es, "st", [128, NT, 4], F32)
            pT = self.ps(es, "pT", [128, 8, 128], BF16)
            pss = [self.ps(es, "psA%d" % i, [128, 512], F32) for i in range(4)]
            wv = self.w_in[l].rearrange("(k p) c -> p k c", p=128)
            for k in range(8):
                for c0 in (0, 1864):
                    S.dma("pool", W[:, k, c0:c0 + 1864], wv[:, k, c0:c0 + 1864], writes=[("W", k)])
            S.dma("sp", gB, self.norm_mix_g[l, :].partition_broadcast(128), writes=["gB"])
            nps = 0
            for i in range(NT):
                b = i % 2
                xt = xts[b]
                S.dma("sp", xt, xsrc[i * 128:(i + 1) * 128, :], writes=[("xt", b)])
                S.op("act", lambda: nc.scalar.activation(out=junk, in_=xt, func=AF.Square, accum_out=st[:, i, 0:1]),
                     reads=[("xt", b)], writes=["junk", ("st", i)])
                S.op("dve", lambda: nc.vector.tensor_scalar(out=st[:, i, 1:2], in0=st[:, i, 0:1], scalar1=1.0 / D, scalar2=EPS,
                                                            op0=ALU.mult, op1=ALU.add), reads=[("st", i)], writes=[("st", i)])
                S.op("act", lambda: nc.scalar.activation(out=st[:, i, 2:3], in_=st[:, i, 1:2], func=AF.Sqrt),
                     reads=[("st", i)], writes=[("st", i)])
                S.op("dve", lambda: nc.vector.reciprocal(out=st[:, i, 3:4], in_=st[:, i, 2:3]), reads=[("st", i)], writes=[("st", i)])
                S.op("dve", lambda: nc.vector.scalar_tensor_tensor(out=hb[b], in0=xt, scalar=st[:, i, 3:4], in1=gB,
                                                                   op0=ALU.mult, op1=ALU.mult),
                     reads=[("xt", b), ("st", i), "gB"], writes=[("hb", b)])
                for k in range(8):
                    S.op("pe", lambda: nc.tensor.transpose(out=pT[:, k, :], in_=hb[b][:, k * 128:(k + 1) * 128], identity=self.identb),
                         reads=[("hb", b), "identb"], writes=["pT"])
                S.op("act", lambda: nc.scalar.copy(out=hT[b], in_=pT), reads=["pT"], writes=[("hT", b)])
                for cg in range(8):
                    c0 = cg * 512
                    cw = min(512, P_IN - c0)
                    pb = nps % 4
                    nps += 1
                    for k in range(8):
                        S.op("pe", lambda: nc.tensor.matmul(pss[pb][:, :cw], lhsT=hT[b][:, k, :], rhs=W[:, k, c0:c0 + cw],
                                                            start=(k == 0), stop=(k == 7)),
                             reads=[("hT", b), ("W", k)], writes=[("psA", pb)])
                    if cg % 2 == 0:
                        S.op("dve", lambda: nc.vector.tensor_copy(out=pts[b][:, c0:c0 + cw], in_=pss[pb][:, :cw]),
                             reads=[("psA", pb)], writes=[("pt", b)])
                    else:
                        S.op("act", lambda: nc.scalar.copy(out=pts[b][:, c0:c0 + cw], in_=pss[pb][:, :cw]),
                             reads=[("psA", pb)], writes=[("pt", b)])
                S.dma("sp", self.P[i * 128:(i + 1) * 128, :], pts[b], reads=[("pt", b)])
        S.barrier()

    def rstd_ops(self, ss, tmp, rs, inv_n, eps, r, w):
        nc, S = self.nc, self.S
        S.op("dve", lambda: nc.vector.tensor_scalar(out=tmp, in0=ss, scalar1=inv_n, scalar2=eps, op0=ALU.mult, op1=ALU.add), reads=r, writes=w)
        S.op("act", lambda: nc.scalar.activation(out=tmp, in_=tmp, func=AF.Sqrt), reads=w, writes=w)
        S.op("dve", lambda: nc.vector.reciprocal(out=rs, in_=tmp), reads=w, writes=w)

    def stage_attnB(self, l):
        nc, S = self.nc, self.S
        with ExitStack() as es:
            qT = self.sb(es, "qT", [128, 2, T], BF16)
            kT = self.sb(es, "kT", [128, T], BF16)
            Va = self.sb(es, "Va", [128, NT, 2, 65], BF16)
            gqk = self.sb(es, "gqk", [128, 6, 64], F32)
            fbs = [self.sb(es, "fb%d" % i, [128, 512], F32) for i in range(2)]
            css = [self.sb(es, "cs%d" % i, [128, 2, 64], F32) for i in range(2)]
            sq = self.sb(es, "sqB", [128, 384], F32)
            ss = self.sb(es, "ssB", [128, 6], F32)
            tm = self.sb(es, "tmB", [128, 6], F32)
            rs = self.sb(es, "rsB", [128, 6], F32)
            qn = self.sb(es, "qnB", [128, 6, 64], F32)
            t1 = self.sb(es, "t1B", [128, 6, 64], F32)
            t2 = self.sb(es, "t2B", [128, 6, 64], F32)
            qkb = self.sb(es, "qkb", [128, 6, 64], BF16)
            pTb = [self.sb(es, "pTb%d" % i, [128, 512], BF16) for i in range(2)]
            oT = self.sb(es, "oT", [65, 512], F32)
            rc = self.sb(es, "rcB", [128, 4, 1], F32)
            ybt = [self.sb(es, "ybt%d" % i, [128, 4, 64], F32) for i in range(2)]
            pT3 = self.ps(es, "pT3", [128, 3, 128], BF16)
            ps_s = [self.ps(es, "ps_s%d" % i, [128, 512], F32) for i in range(2)]
            ps_o = self.ps(es, "ps_o", [65, 512], F32)
            ps_t = self.ps(es, "ps_t", [128, 4, 65], F32)
            for h in range(6):
                src = self.attn_q_norm if h < 4 else self.attn_k_norm
                S.dma("sp", gqk[:, h, :], src[l, :].partition_broadcast(128), writes=["gqk"])
            S.op("pool", lambda: nc.gpsimd.memset(Va[:, :, :, 64:65], 1.0), writes=["Va"])
            for i in range(NT):
                b = i % 2
                fb = fbs[b]
                S.dma("sp", fb, self.P[i * 128:(i + 1) * 128, OFF_B:OFF_B + 512], writes=[("fb", b)])
                S.dma("sp", css[b][:, 0, :], self.rope_cos[i * 128:(i + 1) * 128, :], writes=[("cs", b)])
                S.dma("sp", css[b][:, 1, :], self.rope_sin[i * 128:(i + 1) * 128, :], writes=[("cs", b)])
                S.op("dve", lambda: nc.vector.tensor_tensor(out=sq, in0=fb[:, 0:384], in1=fb[:, 0:384], op=ALU.mult), reads=[("fb", b)], writes=["sqB"])
                S.op("dve", lambda: nc.vector.tensor_reduce(out=ss, in_=sq.rearrange("p (h d) -> p h d", d=64), op=ALU.add, axis=AX.X), reads=["sqB"], writes=["ssB"])
                self.rstd_ops(ss, tm, rs, 1.0 / 64, EPS, ["ssB"], ["rsB"])
                f3 = fb[:, 0:384].rearrange("p (h d) -> p h d", d=64)
                S.op("dve", lambda: nc.vector.tensor_tensor(out=qn, in0=f3, in1=rs.unsqueeze(2).to_broadcast([128, 6, 64]), op=ALU.mult),
                     reads=[("fb", b), "rsB"], writes=["qnB"])
                S.op("pool", lambda: nc.gpsimd.tensor_tensor(out=qn, in0=qn, in1=gqk, op=ALU.mult), reads=["qnB", "gqk"], writes=["qnB"])
                cosb = css[b][:, 0, :].unsqueeze(1).to_broadcast([128, 6, 64])
                S.op("dve", lambda: nc.vector.tensor_tensor(out=t1, in0=qn, in1=cosb, op=ALU.mult), reads=["qnB", ("cs", b)], writes=["t1B"])
                q5 = qn.rearrange("p h (a f d) -> p h a f d", a=2, f=2)
                t5 = t2.rearrange("p h (a f d) -> p h a f d", a=2, f=2)
                s5 = css[b][:, 1, :].rearrange("p (a f d) -> p a f d", a=2, f=2)
                for hf in range(2):
                    for a in range(2):
                        S.op("pool", lambda: nc.gpsimd.tensor_tensor(out=t5[:, :, a, hf, :], in0=q5[:, :, a, 1 - hf, :],
                                                                     in1=s5[:, a, hf, :].unsqueeze(1).to_broadcast([128, 6, 16]), op=ALU.mult),
                             reads=["qnB", ("cs", b)], writes=["t2B"])
                S.op("dve", lambda: nc.vector.tensor_tensor(out=qkb[:, 0:4, :].rearrange("p (b a) d -> p a b d", b=2, a=2),
                                                            in0=t1[:, 0:4, :].rearrange("p (a b) d -> p a b d", a=2, b=2),
                                                            in1=t2[:, 0:4, :].rearrange("p (a b) d -> p a b d", a=2, b=2), op=ALU.add),
                     reads=["t1B", "t2B"], writes=["qkb"])
                S.op("dve", lambda: nc.vector.tensor_tensor(out=qkb[:, 4:6, :], in0=t1[:, 4:6, :], in1=t2[:, 4:6, :], op=ALU.add),
                     reads=["t1B", "t2B"], writes=["qkb"])
                S.op("act", lambda: nc.scalar.copy(out=Va[:, i, :, 0:64], in_=fb[:, 384:512].rearrange("p (h d) -> p h d", d=64)),
                     reads=[("fb", b)], writes=["Va"])
                for c in range(3):
                    S.op("pe", lambda: nc.tensor.transpose(out=pT3[:, c, :], in_=qkb[:, 2 * c:2 * c + 2, :], identity=self.identb),
                         reads=["qkb", "identb"], writes=["pT3"])
                S.op("act", lambda: nc.scalar.copy(out=qT[:, :, i * 128:(i + 1) * 128], in_=pT3[:, 0:2, :]), reads=["pT3"], writes=["qT"])
                S.op("act", lambda: nc.scalar.copy(out=kT[:, i * 128:(i + 1) * 128], in_=pT3[:, 2, :]), reads=["pT3"], writes=["kT"])
            n = 0
            for qh in range(4):
                kv = qh // 2
                base = 64 * kv
                ch = qh % 2
                for qc in range(8):
                    for kb in range(NT):
                        pb = n % 2
                        n += 1
                        S.op("pe", lambda: nc.tensor.matmul(ps_s[pb], lhsT=kT[base:base + 64, kb * 128:(kb + 1) * 128],
                                                            rhs=qT[base:base + 64, ch, qc * 512:(qc + 1) * 512], start=True, stop=True),
                             reads=["kT", "qT"], writes=[("ps_s", pb)])
                        S.op("act", lambda: nc.scalar.activation(out=pTb[pb], in_=ps_s[pb], func=AF.Exp, scale=0.125),
                             reads=[("ps_s", pb)], writes=[("pTb", pb)])
                        S.op("pe", lambda: nc.tensor.matmul(ps_o, lhsT=Va[:, kb, kv, :], rhs=pTb[pb], start=(kb == 0), stop=(kb == NT - 1)),
                             reads=["Va", ("pTb", pb)], writes=["ps_o"])
                    S.op("dve", lambda: nc.vector.tensor_copy(out=oT, in_=ps_o), reads=["ps_o"], writes=["oT"])
                    for j in range(4):
                        S.op("pe", lambda: nc.tensor.transpose(out=ps_t[:, j, :], in_=oT[:, j * 128:(j + 1) * 128], identity=self.identf[0:65, 0:65]),
                             reads=["oT", "identf"], writes=["ps_t"])
                    S.op("dve", lambda: nc.vector.reciprocal(out=rc, in_=ps_t[:, :, 64:65]), reads=["ps_t"], writes=["rcB"])
                    yb = ybt[qc % 2]
                    S.op("dve", lambda: nc.vector.tensor_tensor(out=yb, in0=ps_t[:, :, 0:64], in1=rc.to_broadcast([128, 4, 64]), op=ALU.mult),
                         reads=["ps_t", "rcB"], writes=[("ybt", qc % 2)])
                    S.dma("sp", self.mixed[qc * 512:(qc + 1) * 512, 256 + qh * 64:256 + (qh + 1) * 64].rearrange("(j p) d -> p j d", p=128),
                          yb, reads=[("ybt", qc % 2)])
        S.barrier()

    def stage_attnD(self, l):
        nc, S = self.nc, self.S
        with ExitStack() as es:
            dB = self.sb(es, "dB", [128, 6, 3, 128], F32)
            qTd = self.sb(es, "qTd", [128, T], BF16)
            kTd = self.sb(es, "kTd", [128, T], BF16)
            Vd = self.sb(es, "Vd", [128, NT, 2, 65], BF16)
            fds = [self.sb(es, "fd%d" % i, [128, 3, 128], F32) for i in range(2)]
            qkd = self.sb(es, "qkd", [128, 2, 128], BF16)
            sd = [self.sb(es, "sd%d" % i, [128, 3, 128], F32) for i in range(2)]
            pd = [self.sb(es, "pd%d" % i, [128, 3, 128], BF16) for i in range(2)]
            od = [self.sb(es, "od%d" % i, [128, 2, 65], F32) for i in range(2)]
            dn = [self.sb(es, "dn%d" % i, [128, 6, 65], F32) for i in range(2)]
            zs = self.sb(es, "zsD", [128, 2], F32)
            rz = self.sb(es, "rzD", [128, 2], F32)
            yd = [self.sb(es, "yd%d" % i, [128, 6, 64], F32) for i in range(2)]
            pTd = self.ps(es, "pTd", [128, 2, 128], BF16)
            ps_sd = [self.ps(es, "ps_sd%d" % i, [128, 3, 128], F32) for i in range(2)]
            ps_od = [self.ps(es, "ps_od%d" % i, [128, 2, 65], F32) for i in range(2)]
            for br in range(3):
                for j in range(2):
                    S.dma("sp", dB[:, br * 2 + j, :, :], self.dbias[br, j], writes=["dB"])
            S.op("pool", lambda: nc.gpsimd.memset(Vd[:, :, :, 64:65], 1.0), writes=["Vd"])
            Pd = self.P[:, OFF_D:OFF_D + D_IN].rearrange("r (t x) -> r t x", t=3)
            for br, dil in enumerate((1, 4, 16)):
                nb = NT // dil
                Pv = Pd.rearrange("(n m d) t x -> d n m t x", m=128, d=dil)
                Dv = self.Dnum.rearrange("(n m d) s c -> d n m s c", m=128, d=dil)
                for r in range(dil):
                    for b in range(nb):
                        ti = r * nb + b
                        fb = ti % 2
                        S.dma("sp", fds[fb], Pv[r, b][:, :, br * 128:(br + 1) * 128], writes=[("fd", fb)])
                        S.op("act", lambda: nc.scalar.copy(out=qkd, in_=fds[fb][:, 0:2, :]), reads=[("fd", fb)], writes=["qkd"])
                        S.op("pool", lambda: nc.gpsimd.tensor_copy(out=Vd[:, ti, :, 0:64], in_=fds[fb][:, 2, :].rearrange("p (h d) -> p h d", d=64)),
                             reads=[("fd", fb)], writes=["Vd"])
                        for c in range(2):
                            S.op("pe", lambda: nc.tensor.transpose(out=pTd[:, c, :], in_=qkd[:, c, :], identity=self.identb),
                                 reads=["qkd", "identb"], writes=["pTd"])
                        S.op("dve", lambda: nc.vector.tensor_copy(out=qTd[:, ti * 128:(ti + 1) * 128], in_=pTd[:, 0, :]), reads=["pTd"], writes=["qTd"])
                        S.op("dve", lambda: nc.vector.tensor_copy(out=kTd[:, ti * 128:(ti + 1) * 128], in_=pTd[:, 1, :]), reads=["pTd"], writes=["kTd"])
                for r in range(dil):
                    for b in range(nb):
                        ti = r * nb + b
                        ob = ti % 2
                        for j in range(2):
                            base = 64 * j
                            rels = [rel for rel in range(3) if 0 <= b + rel - 1 < nb]
                            r0, r1 = rels[0], rels[-1] + 1
                            for rel in rels:
                                kt = ti + rel - 1
                                S.op("pe", lambda: nc.tensor.matmul(ps_sd[j][:, rel, :], lhsT=kTd[base:base + 64, kt * 128:(kt + 1) * 128],
                                                                    rhs=qTd[base:base + 64, ti * 128:(ti + 1) * 128], start=True, stop=True),
                                     reads=["kTd", "qTd"], writes=[("ps_sd", j)])
                            S.op("dve", lambda: nc.vector.scalar_tensor_tensor(out=sd[j][:, r0:r1, :], in0=ps_sd[j][:, r0:r1, :], scalar=0.125,
                                                                               in1=dB[:, br * 2 + j, r0:r1, :], op0=ALU.mult, op1=ALU.add),
                                 reads=[("ps_sd", j), "dB"], writes=[("sd", j)])
                            S.op("act", lambda: nc.scalar.activation(out=pd[j][:, r0:r1, :], in_=sd[j][:, r0:r1, :], func=AF.Exp),
                                 reads=[("sd", j)], writes=[("pd", j)])
                            for rel in rels:
                                kt = ti + rel - 1
                                S.op("pe", lambda: nc.tensor.matmul(ps_od[ob][:, j, :], lhsT=pd[j][:, rel, :], rhs=Vd[:, kt, j, :],
                                                                    start=(rel == rels[0]), stop=(rel == rels[-1])),
                                     reads=[("pd", j), "Vd"], writes=[("ps_od", ob)])
                        S.op("dve", lambda: nc.vector.tensor_copy(out=od[ob], in_=ps_od[ob]), reads=[("ps_od", ob)], writes=[("od", ob)])
                        S.dma("sp", Dv[r, b][:, br * 2:br * 2 + 2, :], od[ob], reads=[("od", ob)])
            S.barrier()
            for i in range(NT):
                b = i % 2
                S.dma("sp", dn[b], self.Dnum[i * 128:(i + 1) * 128], writes=[("dn", b)])
                z3 = dn[b][:, :, 64].rearrange("p (r j) -> p r j", j=2)
                S.op("dve", lambda: nc.vector.tensor_tensor(out=zs, in0=z3[:, 0, :], in1=z3[:, 1, :], op=ALU.add), reads=[("dn", b)], writes=["zsD"])
                S.op("dve", lambda: nc.vector.tensor_tensor(out=zs, in0=zs, in1=z3[:, 2, :], op=ALU.add), reads=[("dn", b), "zsD"], writes=["zsD"])
                S.op("dve", lambda: nc.vector.reciprocal(out=rz, in_=zs), reads=["zsD"], writes=["rzD"])
                for br in range(3):
                    S.op("dve", lambda: nc.vector.tensor_tensor(out=yd[b][:, br * 2:br * 2 + 2, :], in0=dn[b][:, br * 2:br * 2 + 2, 0:64],
                                                                in1=rz.unsqueeze(2).to_broadcast([128, 2, 64]), op=ALU.mult),
                         reads=[("dn", b), "rzD"], writes=[("yd", b)])
                S.dma("sp", self.mixed[i * 128:(i + 1) * 128, 768:1152], yd[b], reads=[("yd", b)])
        S.barrier()

    def stage_outproj(self, l, xsrc):
        nc, S = self.nc, self.S
        with ExitStack() as es:
            Wo = self.sb(es, "Wo", [128, 9, D], BF16)
            g2 = self.sb(es, "g2B", [128, D], F32)
            Wr = self.sb(es, "Wr", [128, 8, NE], F32)
            rbB = self.sb(es, "rbB", [128, NE], F32)
            mts = [self.sb(es, "mt%d" % i, [128, MIXW], F32) for i in range(2)]
            mb = self.sb(es, "mb", [128, MIXW], BF16)
            mT = self.sb(es, "mT", [128, 9, 128], BF16)
            xts = [self.sb(es, "xo%d" % i, [128, D], F32) for i in range(2)]
            x2s = [self.sb(es, "x2t%d" % i, [128, D], F32) for i in range(2)]
            junk = self.sb(es, "junkO", [128, D], F32)
            st = self.sb(es, "stO", [128, NT, 4], F32)
            h2f = self.sb(es, "h2f", [128, D], F32)
            h2b = [self.sb(es, "h2b%d" % i, [128, D], BF16) for i in range(2)]
            h2T = self.sb(es, "h2T", [128, 8, 128], F32)
            lg = self.sb(es, "lgO", [128, NE], F32)
            mx = self.sb(es, "mxO", [128, 4], F32)
            ex = self.sb(es, "exO", [128, NE], F32)
            pTa = self.ps(es, "pTa", [128, 8, 128], BF16)
            pTc = self.ps(es, "pTc", [128, 1, 128], BF16)
            pso = [self.ps(es, "pso%d" % i, [128, 512], F32) for i in range(2)]
            pT4 = [self.ps(es, "pT4%d" % i, [128, 4, 128], F32) for i in range(2)]
            psr = self.ps(es, "psr", [128, NE], F32)
            wv = self.w_out[l].rearrange("(k p) c -> p k c", p=128)
            for k in range(9):
                S.dma("pool", Wo[:, k, :], wv[:, k, :], writes=["Wo"])
            S.dma("sp", g2, self.norm_ffn_g[l, :].partition_broadcast(128), writes=["g2B"])
            S.dma("sp", Wr, self.router_w[l].rearrange("(k p) e -> p k e", p=128), writes=["Wr"])
            S.dma("sp", rbB, self.router_b[l, :].partition_broadcast(128), writes=["rbB"])
            for i in range(NT):
                b = i % 2
                rows = slice(i * 128, (i + 1) * 128)
                S.dma("sp", mts[b], self.mixed[rows, :], writes=[("mt", b)])
                S.dma("sp", xts[b], xsrc[rows, :], writes=[("xo", b)])
                S.op("act", lambda: nc.scalar.copy(out=mb, in_=mts[b]), reads=[("mt", b)], writes=["mb"])
                for k in range(9):
                    dst = pTa[:, k, :] if k < 8 else pTc[:, 0, :]
                    S.op("pe", lambda: nc.tensor.transpose(out=dst, in_=mb[:, k * 128:(k + 1) * 128], identity=self.identb),
                         reads=["mb", "identb"], writes=["pTa" if k < 8 else "pTc"])
                S.op("act", lambda: nc.scalar.copy(out=mT[:, 0:8, :], in_=pTa), reads=["pTa"], writes=["mT"])
                S.op("dve", lambda: nc.vector.tensor_copy(out=mT[:, 8:9, :], in_=pTc), reads=["pTc"], writes=["mT"])
                x2 = x2s[b]
                for hf in range(2):
                    for k in range(9):
                        S.op("pe", lambda: nc.tensor.matmul(pso[hf], lhsT=mT[:, k, :], rhs=Wo[:, k, hf * 512:(hf + 1) * 512], start=(k == 0), stop=(k == 8)),
                             reads=["mT", "Wo"], writes=[("pso", hf)])
                    S.op("dve", lambda: nc.vector.tensor_tensor(out=x2[:, hf * 512:(hf + 1) * 512], in0=pso[hf], in1=xts[b][:, hf * 512:(hf + 1) * 512], op=ALU.add),
                         reads=[("pso", hf), ("xo", b)], writes=[("x2t", b)])
                S.dma("sp", self.X2[rows, :], x2, reads=[("x2t", b)])
                S.dma("sp", self.X3[rows, :], x2, reads=[("x2t", b)])
                S.op("act", lambda: nc.scalar.activation(out=junk, in_=x2, func=AF.Square, accum_out=st[:, i, 0:1]), reads=[("x2t", b)], writes=["junkO", ("stO", i)])
                self.rstd_ops(st[:, i, 0:1], st[:, i, 1:2], st[:, i, 2:3], 1.0 / D, EPS, [("stO", i)], [("stO", i)])
                S.op("dve", lambda: nc.vector.scalar_tensor_tensor(out=h2f, in0=x2, scalar=st[:, i, 2:3], in1=g2, op0=ALU.mult, op1=ALU.mult),
                     reads=[("x2t", b), ("stO", i), "g2B"], writes=["h2f"])
                S.op("act", lambda: nc.scalar.copy(out=h2b[b], in_=h2f), reads=["h2f"], writes=[("h2b", b)])
                S.dma("sp", self.H2[rows, :], h2b[b], reads=[("h2b", b)])
                for k in range(8):
                    S.op("pe", lambda: nc.tensor.transpose(out=pT4[k // 4][:, k % 4, :], in_=h2f[:, k * 128:(k + 1) * 128], identity=self.identf),
                         reads=["h2f", "identf"], writes=[("pT4", k // 4)])
                S.op("act", lambda: nc.scalar.copy(out=h2T[:, 0:4, :], in_=pT4[0]), reads=[("pT4", 0)], writes=["h2T"])
                S.op("dve", lambda: nc.vector.tensor_copy(out=h2T[:, 4:8, :], in_=pT4[1]), reads=[("pT4", 1)], writes=["h2T"])
                for k in range(8):
                    S.op("pe", lambda: nc.tensor.matmul(psr, lhsT=h2T[:, k, :], rhs=Wr[:, k, :], start=(k == 0), stop=(k == 7)),
                         reads=["h2T", "Wr"], writes=["psr"])
                S.op("dve", lambda: nc.vector.tensor_tensor(out=lg, in0=psr, in1=rbB, op=ALU.add), reads=["psr", "rbB"], writes=["lgO"])
                S.op("dve", lambda: nc.vector.tensor_reduce(out=mx[:, 0:1], in_=lg, op=ALU.max, axis=AX.X), reads=["lgO"], writes=["mxO"])
                S.op("dve", lambda: nc.vector.tensor_scalar(out=lg, in0=lg, scalar1=mx[:, 0:1], scalar2=None, op0=ALU.subtract), reads=["lgO", "mxO"], writes=["lgO"])
                S.op("act", lambda: nc.scalar.activation(out=ex, in_=lg, func=AF.Exp), reads=["lgO"], writes=["exO"])
                S.op("dve", lambda: nc.vector.tensor_reduce(out=mx[:, 2:3], in_=ex, op=ALU.add, axis=AX.X), reads=["exO"], writes=["mxO"])
                S.op("dve", lambda: nc.vector.reciprocal(out=mx[:, 3:4], in_=mx[:, 2:3]), reads=["mxO"], writes=["mxO"])
                S.op("dve", lambda: nc.vector.tensor_scalar(out=self.AFF[:, i, :], in0=ex, scalar1=mx[:, 3:4], scalar2=None, op0=ALU.mult),
                     reads=["exO", "mxO"], writes=["AFF"])
                S.dma("sp", self.AFFd[rows, :], self.AFF[:, i, :], reads=["AFF"])
        S.barrier()

    def stage_moe(self, l):
        nc, S = self.nc, self.S
        AFF = self.AFF
        with ExitStack() as es:
            IDX = self.sb(es, "IDX", [128, NE, 4], U32)
            es1 = ExitStack()
            es_outer = es
            es = es1
            lo = self.sb(es, "loM", [128, NE], F32)
            mid = self.sb(es, "midM", [128, NE], F32)
            cmp_ = self.sb(es, "cmpM", [128, NT, NE], F32)
            pc = self.sb(es, "pcM", [128, NE], F32)
            ge = self.sb(es, "geM", [128, NE], F32)
            onesb = self.sb(es, "onesb", [128, 128], BF16)
            onesf = self.sb(es, "onesf", [128, 128], F32)
            Ub = self.sb(es, "Ub", [128, 128], BF16)
            selb = self.sb(es, "selb", [128, NT, NE], BF16)
            Uf = self.sb(es, "Uf", [128, 128], F32)
            self_f = self.sb(es, "self", [128, NT, NE], F32)
            tot = [self.sb(es, "totM%d" % i, [128, NT, NE], F32) for i in range(2)]
            tot0 = self.sb(es, "tot0", [128, NT, NE], F32)
            rank = self.sb(es, "rankM", [128, NT, NE], F32)
            iotaC = self.sb(es, "iotaC", [128, CAP], F32)
            tvf = self.sb(es, "tvf", [128, NT, 2], F32)
            oh = [self.sb(es, "oh%d" % i, [128, CAP], F32) for i in range(3)]
            rws = self.sb(es, "rws", [2, CAP], F32)
            rwu = self.sb(es, "rwu", [2, CAP], U32)
            IDXf = self.sb(es, "IDXf", [128, NE, 4], F32)
            psc = self.ps(es, "psc", [128, NE], F32)
            psp = self.ps(es, "psp", [128, 512], F32)
            pst = self.ps(es, "pst", [128, 512], F32)
            psid = [self.ps(es, "psid%d" % i, [128, NE, 2], F32) for i in range(4)]
            IDX2 = self.sb(es, "IDX2", [128, NE, 4, 2], F32)
            S.dma("sp", Uf, self.ustrict, writes=["Uf"])
            S.op("dve", lambda: nc.vector.tensor_copy(out=Ub, in_=Uf), reads=["Uf"], writes=["Ub"])
            S.op("dve", lambda: nc.vector.memset(onesb, 1.0), writes=["onesb"])
            S.op("dve", lambda: nc.vector.memset(onesf, 1.0), writes=["onesf"])
            S.dma("sp", iotaC, self.iota_c, writes=["iotaC"])
            S.dma("sp", tvf, self.tvals, writes=["tvf"])
            S.op("dve", lambda: nc.vector.memset(lo, 0.0), writes=["lo"])
            for it in range(32):
                c = 2.0 ** -(it + 1)
                S.op("dve", lambda: nc.vector.tensor_scalar(out=mid, in0=lo, scalar1=c, scalar2=None, op0=ALU.add), reads=["lo"], writes=["mid"])
                S.op("dve", lambda: nc.vector.tensor_tensor(out=cmp_, in0=AFF, in1=mid.unsqueeze(1).to_broadcast([128, NT, NE]), op=ALU.is_ge),
                     reads=["AFF", "mid"], writes=["cmp"])
                S.op("dve", lambda: nc.vector.tensor_reduce(out=pc, in_=cmp_.rearrange("p t e -> p e t"), op=ALU.add, axis=AX.X), reads=["cmp"], writes=["pc"])
                S.op("pe", lambda: nc.tensor.matmul(psc, lhsT=onesf, rhs=pc, start=True, stop=True), reads=["onesf", "pc"], writes=["psc"])
                S.op("dve", lambda: nc.vector.tensor_single_scalar(out=ge, in_=psc, scalar=CAP - 0.5, op=ALU.is_ge), reads=["psc"], writes=["ge"])
                S.op("dve", lambda: nc.vector.scalar_tensor_tensor(out=lo, in0=ge, scalar=c, in1=lo, op0=ALU.mult, op1=ALU.add), reads=["ge", "lo"], writes=["lo"])
            import os
            if os.environ.get("MOE_STOP") == "1":
                S.dma("sp", self.dbg_small[:, 64:80], lo, reads=["lo"])
                S.barrier()
                es1.close()
                return
            S.op("dve", lambda: nc.vector.tensor_tensor(out=self_f, in0=AFF, in1=lo.unsqueeze(1).to_broadcast([128, NT, NE]), op=ALU.is_ge),
                 reads=["AFF", "lo"], writes=["self"])
            S.op("dve", lambda: nc.vector.tensor_tensor(out=selb, in0=AFF, in1=lo.unsqueeze(1).to_broadcast([128, NT, NE]), op=ALU.is_ge),
                 reads=["AFF", "lo"], writes=["selb"])
            sel2 = selb.rearrange("p t e -> p (t e)")
            S.op("pe", lambda: nc.tensor.matmul(psp, lhsT=Ub, rhs=sel2, start=True, stop=True), reads=["Ub", "selb"], writes=["psp"])
            S.op("pe", lambda: nc.tensor.matmul(pst, lhsT=onesb, rhs=sel2, start=True, stop=True), reads=["onesb", "selb"], writes=["pst"])
            S.op("dve", lambda: nc.vector.tensor_copy(out=tot0.rearrange("p t e -> p (t e)"), in_=pst), reads=["pst"], writes=["tot0"])
            S.op("dve", lambda: nc.vector.tensor_copy(out=tot[0].rearrange("p t e -> p (t e)"), in_=pst), reads=["pst"], writes=[("tot", 0)])
            if os.environ.get("MOE_STOP") == "1b":
                S.dma("sp", self.dbg_small[:, 0:16], tot[0][:, 5, :], reads=[("tot", 0)])
                S.barrier()
                es1.close()
                return
            cur = 0
            for sft in (1, 2, 4, 8, 16):
                a, bb = tot[cur], tot[1 - cur]
                S.op("pool", lambda: nc.gpsimd.tensor_copy(out=bb[:, 0:sft, :], in_=a[:, 0:sft, :]), reads=[("tot", cur)], writes=[("tot", 1 - cur)])
                S.op("dve", lambda: nc.vector.tensor_tensor(out=bb[:, sft:NT, :], in0=a[:, sft:NT, :], in1=a[:, 0:NT - sft, :], op=ALU.add),
                     reads=[("tot", cur)], writes=[("tot", 1 - cur)])
                cur = 1 - cur
            inc = tot[cur]
            if os.environ.get("MOE_STOP") == "1c":
                S.dma("sp", self.dbg_small[:, 0:16], inc[:, 5, :], reads=[("tot", cur)])
                S.barrier()
                es1.close()
                return
            S.op("dve", lambda: nc.vector.tensor_tensor(out=rank, in0=inc, in1=tot0, op=ALU.subtract), reads=[("tot", cur), "tot0"], writes=["rank"])
            S.op("dve", lambda: nc.vector.tensor_tensor(out=rank.rearrange("p t e -> p (t e)"), in0=rank.rearrange("p t e -> p (t e)"), in1=psp, op=ALU.add),
                 reads=["rank", "psp"], writes=["rank"])
            S.op("dve", lambda: nc.vector.scalar_tensor_tensor(out=rank, in0=rank, scalar=-9999.0, in1=self_f, op0=ALU.add, op1=ALU.mult),
                 reads=["rank", "self"], writes=["rank"])
            S.op("dve", lambda: nc.vector.tensor_scalar(out=rank, in0=rank, scalar1=9999.0, scalar2=None, op0=ALU.add), reads=["rank"], writes=["rank"])
            if os.environ.get("MOE_STOP") == "2":
                S.dma("sp", self.dbg_small[:, 0:16], rank[:, 5, :], reads=["rank"])
                S.barrier()
                es1.close()
                return
            n = 0
            for e in range(NE):
                for i in range(NT):
                    ob = n % 3
                    n += 1
                    S.op("dve", lambda: nc.vector.tensor_scalar(out=oh[ob], in0=iotaC, scalar1=rank[:, i, e:e + 1], scalar2=None, op0=ALU.is_equal),
                         reads=["iotaC", "rank"], writes=[("oh", ob)])
                    for cc in range(4):
                        S.op("pe", lambda: nc.tensor.matmul(psid[cc][:, e, :], lhsT=oh[ob][:, cc * 128:(cc + 1) * 128], rhs=tvf[:, i, :],
                                                            start=(i == 0), stop=(i == NT - 1)),
                             reads=["tvf", ("oh", ob)], writes=[("psid", cc)])
            for cc in range(4):
                S.op("dve", lambda: nc.vector.tensor_tensor(out=IDXf[:, :, cc], in0=psid[cc][:, :, 0], in1=psid[cc][:, :, 1], op=ALU.add) if False else
                     nc.vector.tensor_copy(out=IDX2[:, :, cc, :], in_=psid[cc]), reads=[("psid", cc)], writes=["IDX2"])
            S.op("dve", lambda: nc.vector.tensor_tensor(out=IDXf, in0=IDX2[:, :, :, 0], in1=IDX2[:, :, :, 1], op=ALU.add), reads=["IDX2"], writes=["IDXf"])
            S.op("dve", lambda: nc.vector.tensor_copy(out=IDX, in_=IDXf), reads=["IDXf"], writes=["IDX"])
            if self.stop_after == "moe_idx":
                S.dma("sp", self.dbg_small[:, 0:64], IDXf.rearrange("p e c -> p (e c)"), reads=["IDXf"])
                S.dma("sp", self.dbg_small[:, 64:80], lo, reads=["lo"])
                S.barrier()
                es1.close()
                return
            S.barrier()
            es1.close()
            es = es_outer
            Wg = [self.sb(es, "Wg%d" % i, [128, 8, D], BF16) for i in range(2)]
            Wu = [self.sb(es, "Wu%d" % i, [128, 8, D], BF16) for i in range(2)]
            Wd = [self.sb(es, "Wd%d" % i, [128, 8, D], BF16) for i in range(2)]
            xs = [self.sb(es, "xs%d" % i, [128, 4, D], BF16) for i in range(2)]
            gt = [self.sb(es, "gt%d" % i, [128, 4, NE], F32) for i in range(2)]
            xsT = self.sb(es, "xsT", [128, 8, CAP], BF16)
            sg = [self.sb(es, "sg%d" % i, [128, CAP], F32) for i in range(2)]
            hid = self.sb(es, "hid", [128, 8, CAP], BF16)
            yt = [self.sb(es, "yt%d" % i, [128, D], F32) for i in range(2)]
            psx = self.ps(es, "psx", [128, 8, 128], BF16)
            psg = self.ps(es, "psg", [128, CAP], F32)
            psu = self.ps(es, "psu", [128, CAP], F32)
            psy = [self.ps(es, "psy%d" % i, [128, 512], F32) for i in range(2)]

            def load_w(e):
                wb = e % 2
                for (dst, src, nm) in ((Wg[wb], self.w_gate, "Wg"), (Wu[wb], self.w_up, "Wu"), (Wd[wb], self.w_down, "Wd")):
                    sv = src[l, e].rearrange("(k p) f -> p k f", p=128)
                    for k0 in (0, 4):
                        S.dma("pool", dst[:, k0:k0 + 4, :], sv[:, k0:k0 + 4, :], writes=[(nm, wb)])

            def gather(e):
                wb = e % 2
                for cc in range(4):
                    S.dma_raw("pool", lambda: nc.gpsimd.indirect_dma_start(out=xs[wb][:, cc, :], out_offset=None, in_=self.H2,
                                                                           in_offset=bass.IndirectOffsetOnAxis(ap=IDX[:, e, cc:cc + 1], axis=0)),
                              reads=["IDX"], writes=[("xs", wb)])
                    S.dma_raw("pool", lambda: nc.gpsimd.indirect_dma_start(out=gt[wb][:, cc, :], out_offset=None, in_=self.AFFd,
                                                                           in_offset=bass.IndirectOffsetOnAxis(ap=IDX[:, e, cc:cc + 1], axis=0)),
                              reads=["IDX"], writes=[("gt", wb)])

            load_w(0)
            gather(0)
            ny = 0
            for e in range(NE):
                wb = e % 2
                if e + 1 < NE:
                    load_w(e + 1)
                    gather(e + 1)
                for cc in range(4):
                    for k in range(8):
                        S.op("pe", lambda: nc.tensor.transpose(out=psx[:, k, :], in_=xs[wb][:, cc, k * 128:(k + 1) * 128], identity=self.identb),
                             reads=[("xs", wb), "identb"], writes=["psx"])
                    S.op("act", lambda: nc.scalar.copy(out=xsT[:, :, cc * 128:(cc + 1) * 128], in_=psx), reads=["psx"], writes=["xsT"])
                for f in range(8):
                    for k in range(8):
                        S.op("pe", lambda: nc.tensor.matmul(psg, lhsT=Wg[wb][:, k, f * 128:(f + 1) * 128], rhs=xsT[:, k, :], start=(k == 0), stop=(k == 7)),
                             reads=[("Wg", wb), "xsT"], writes=["psg"])
                    for k in range(8):
                        S.op("pe", lambda: nc.tensor.matmul(psu, lhsT=Wu[wb][:, k, f * 128:(f + 1) * 128], rhs=xsT[:, k, :], start=(k == 0), stop=(k == 7)),
                             reads=[("Wu", wb), "xsT"], writes=["psu"])
                    S.op("act", lambda: nc.scalar.activation(out=sg[f % 2], in_=psg, func=AF.Silu), reads=["psg"], writes=[("sg", f % 2)])
                    S.op("dve", lambda: nc.vector.tensor_tensor(out=hid[:, f, :], in0=sg[f % 2], in1=psu, op=ALU.mult), reads=[("sg", f % 2), "psu"], writes=["hid"])
                for cc in range(4):
                    yb = ny % 2
                    ny += 1
                    for hf in range(2):
                        for f in range(8):
                            S.op("pe", lambda: nc.tensor.matmul(psy[hf], lhsT=hid[:, f, cc * 128:(cc + 1) * 128], rhs=Wd[wb][:, f, hf * 512:(hf + 1) * 512],
                                                                start=(f == 0), stop=(f == 7)), reads=["hid", ("Wd", wb)], writes=[("psy", hf)])
                        S.op("dve", lambda: nc.vector.tensor_scalar(out=yt[yb][:, hf * 512:(hf + 1) * 512], in0=psy[hf], scalar1=gt[wb][:, cc, e:e + 1], scalar2=None, op0=ALU.mult),
                             reads=[("psy", hf), ("gt", wb)], writes=[("yt", yb)])
                    S.dma_raw("pool", lambda: nc.gpsimd.indirect_dma_start(out=self.X3, out_offset=bass.IndirectOffsetOnAxis(ap=IDX[:, e, cc:cc + 1], axis=0),
                                                                           in_=yt[yb], in_offset=None, compute_op=ALU.add),
                              reads=[("yt", yb), "IDX", "X3"], writes=["X3"])
        S.barrier()

    def _e(self, eng):
        return self.S.eng[eng]

    def tt(self, eng, out, a, b, op):
        self.S.op(eng, lambda: self._e(eng).tensor_tensor(out=out.ap, in0=a.ap, in1=b.ap, op=op), reads=[a.key, b.key], writes=[out.key])

    def ts(self, eng, out, a, s1, op0, s2=None, op1=None):
        rd = [a.key]
        v1 = s1
        if isinstance(s1, TB):
            rd.append(s1.key)
            v1 = s1.ap
        kw = {}
        if op1 is not None:
            kw["op1"] = op1
        self.S.op(eng, lambda: self._e(eng).tensor_scalar(out=out.ap, in0=a.ap, scalar1=v1, scalar2=s2, op0=op0, **kw), reads=rd, writes=[out.key])

    def stt(self, out, a, scalar, b, op0, op1):
        rd = [a.key, b.key]
        sv = scalar
        if isinstance(scalar, TB):
            rd.append(scalar.key)
            sv = scalar.ap
        self.S.op("dve", lambda: self.nc.vector.scalar_tensor_tensor(out=out.ap, in0=a.ap, scalar=sv, in1=b.ap, op0=op0, op1=op1), reads=rd, writes=[out.key])

    def act(self, out, a, func, scale=1.0, bias=None):
        rd = [a.key]
        kw = {}
        if isinstance(bias, TB):
            rd.append(bias.key)
            kw["bias"] = bias.ap
        elif bias is not None:
            kw["bias"] = bias
        self.S.op("act", lambda: self.nc.scalar.activation(out=out.ap, in_=a.ap, func=func, scale=scale, **kw), reads=rd, writes=[out.key])

    def cp(self, eng, out, a):
        if eng == "act":
            self.S.op("act", lambda: self.nc.scalar.copy(out=out.ap, in_=a.ap), reads=[a.key], writes=[out.key])
        else:
            self.S.op(eng, lambda: self._e(eng).tensor_copy(out=out.ap, in_=a.ap), reads=[a.key], writes=[out.key])

    def red(self, out, a, op=None):
        self.S.op("dve", lambda: self.nc.vector.tensor_reduce(out=out.ap, in_=a.ap, op=(op or ALU.add), axis=AX.X), reads=[a.key], writes=[out.key])

    def rcp(self, out, a):
        self.S.op("dve", lambda: self.nc.vector.reciprocal(out=out.ap, in_=a.ap), reads=[a.key], writes=[out.key])

    def mm(self, out, lhsT, rhs, start=True, stop=True):
        self.S.op("pe", lambda: self.nc.tensor.matmul(out.ap, lhsT=lhsT.ap, rhs=rhs.ap, start=start, stop=stop), reads=[lhsT.key, rhs.key], writes=[out.key])

    def tp(self, out, a, ident):
        self.S.op("pe", lambda: self.nc.tensor.transpose(out=out.ap, in_=a.ap, identity=ident.ap), reads=[a.key, ident.key], writes=[out.key])

    def ld(self, out, src, q="sp"):
        self.S.dma(q, out.ap, src, writes=[out.key])

    def st_(self, dst, a, q="sp"):
        self.S.dma(q, dst, a.ap, reads=[a.key])

    def tb(self, es, name, shape, dtype=F32):
        return TB(self.sb(es, name, shape, dtype), name)

    def rstd(self, out, ss, tmp, inv_n, eps):
        self.ts("dve", tmp, ss, inv_n, ALU.mult, eps, ALU.add)
        self.act(tmp, tmp, AF.Sqrt)
        self.rcp(out, tmp)

    def bank(self):
        b = self.banks[self.nbank % 8]
        self.nbank += 1
        return b

    def bcast_row(self, es, name, src_row, n):
        t = self.tb(es, name, [128, n])
        self.ld(t, src_row.partition_broadcast(128))
        return t

    def stage_prepA(self, l):
        nc, S = self.nc, self.S
        with ExitStack() as es:
            self.banks = [TB(self.ps(es, "bk%d" % i, [128, 512], F32), ("bk", i)) for i in range(8)]
            self.nbank = 0
            identf = TB(self.identf, "identf")
            mpB = self.bcast_row(es, "mpB", self.rwkv_mu_prev[l, :], 1024)
            mnB = self.bcast_row(es, "mnB", self.rwkv_mu_next[l, :], 1024)
            c0B = self.tb(es, "c0B", [128, 1024])
            self.tt("dve", c0B, mpB, mnB, ALU.add)
            self.ts("dve", c0B, c0B, -1.0, ALU.mult, 1.0, ALU.add)
            kkB = self.bcast_row(es, "kkB", self.rwkv_k_k[l, :], 256)
            kaB = self.bcast_row(es, "kaB", self.rwkv_k_a[l, :], 256)
            omk = self.tb(es, "omk", [128, 256])
            self.ts("dve", omk, kaB, -1.0, ALU.mult, 1.0, ALU.add)
            rkB = self.bcast_row(es, "rkB", self.rwkv_r_k[l].rearrange("h d -> (h d)"), 256)
            w0B = self.tb(es, "w0B", [128, 2, 256])
            a0B = self.tb(es, "a0B", [128, 2, 256])
            for d in range(2):
                self.ld(w0B[:, d, :], self.rwkv_w0[l, d, :].partition_broadcast(128))
                self.ld(a0B[:, d, :], self.rwkv_a0[l, d, :].partition_broadcast(128))
            Wl = self.tb(es, "Wl", [128, 2, 256])
            for d in range(2):
                self.ld(Wl[0:64, d, :], self.rwkv_w_up[l, d])
                self.ld(Wl[64:128, d, :], self.rwkv_a_up[l, d])
            Wg = self.tb(es, "WgA", [128, 256])
            self.ld(Wg, self.rwkv_g_up[l])
            cur = [self.tb(es, "curA%d" % i, [128, 1024]) for i in range(2)]
            prv = [self.tb(es, "prvA%d" % i, [128, 1024]) for i in range(2)]
            nxt = [self.tb(es, "nxtA%d" % i, [128, 1024]) for i in range(2)]
            f = self.tb(es, "fA", [128, 1024])
            t2 = self.tb(es, "t2A", [128, 1024])
            lin = self.tb(es, "linA", [128, 256])
            linT = self.tb(es, "linT", [128, 2, 128])
            zw = self.tb(es, "zwA", [128, 2, 256])
            al = self.tb(es, "alA", [128, 2, 256])
            kkr = self.tb(es, "kkr", [128, 256])
            sq = self.tb(es, "sqA", [128, 256])
            ss = self.tb(es, "ssA", [128, 4])
            tm4 = self.tb(es, "tm4A", [128, 4])
            rs4 = self.tb(es, "rs4A", [128, 4])
            kk = self.tb(es, "kkA", [128, 256])
            tk = self.tb(es, "tkA", [128, 256])
            km = self.tb(es, "kmA", [128, 256])
            TMt = [self.tb(es, "TMtA%d" % i, [128, 2, 6, 256]) for i in range(2)]
            for t_ in TMt:
                S.op("pool", lambda: nc.gpsimd.memset(t_.ap, 0.0), writes=[t_.key])
            aux = [self.tb(es, "auxA%d" % i, [128, 2, 256]) for i in range(2)]
            P = self.P
            for i in range(NT):
                b = i % 2
                r0 = i * 128
                self.ld(cur[b], P[r0:r0 + 128, 0:1024])
                if i == 0:
                    S.op("pool", lambda: nc.gpsimd.memset(prv[b].ap, 0.0), writes=[prv[b].key])
                    S.dma("sp", prv[b].ap[1:128, :], P[0:127, 0:1024], writes=[prv[b].key])
                else:
                    self.ld(prv[b], P[r0 - 1:r0 + 127, 0:1024])
                if i == NT - 1:
                    S.op("pool", lambda: nc.gpsimd.memset(nxt[b].ap, 0.0), writes=[nxt[b].key])
                    S.dma("sp", nxt[b].ap[0:127, :], P[r0 + 1:r0 + 128, 0:1024], writes=[nxt[b].key])
                else:
                    self.ld(nxt[b], P[r0 + 1:r0 + 129, 0:1024])
                self.tt("dve", f, cur[b], c0B, ALU.mult)
                self.tt("pool", t2, prv[b], mpB, ALU.mult)
                self.tt("dve", f, f, t2, ALU.add)
                self.tt("pool", t2, nxt[b], mnB, ALU.mult)
                self.tt("dve", f, f, t2, ALU.add)
                T_ = TMt[b]
                r_, k_, v_ = f[:, 0:256], f[:, 256:512], f[:, 512:768]
                self.act(lin[:, 0:64], f[:, 768:832], AF.Tanh)
                self.cp("act", lin[:, 64:128], f[:, 832:896])
                self.act(lin[:, 128:256], f[:, 896:1024], AF.Sigmoid)
                bkT = self.bank()
                for c in range(2):
                    self.tp(bkT[:, c * 128:(c + 1) * 128], lin[:, c * 128:(c + 1) * 128], identf)
                self.cp("dve", linT.v(lambda a: a.rearrange("p c t -> p (c t)")), bkT[:, 0:256])
                bw = self.bank()
                ba = self.bank()
                for d in range(2):
                    self.mm(bw[:, d * 256:(d + 1) * 256], linT[0:64, 0, :], Wl[0:64, d, :])
                for d in range(2):
                    self.mm(ba[:, d * 256:(d + 1) * 256], linT[64:128, 0, :], Wl[64:128, d, :])
                bg = self.bank()
                self.mm(bg[:, 0:256], linT[:, 1, :], Wg)
                self.tt("dve", zw.v(lambda a: a.rearrange("p d c -> p (d c)")), bw, w0B.v(lambda a: a.rearrange("p d c -> p (d c)")), ALU.add)
                self.tt("dve", al.v(lambda a: a.rearrange("p d c -> p (d c)")), ba, a0B.v(lambda a: a.rearrange("p d c -> p (d c)")), ALU.add)
                self.act(zw, zw, AF.Sigmoid)
                self.act(al, al, AF.Sigmoid)
                self.cp("act", aux[b][:, 1, :], bg[:, 0:256])
                self.tt("dve", kkr, k_, kkB, ALU.mult)
                self.tt("pool", sq, kkr, kkr, ALU.mult)
                self.red(ss, sq.v(lambda a: a.rearrange("p (h d) -> p h d", d=64)))
                self.rstd(rs4, ss, tm4, 1.0, 1e-12)
                self.tt("dve", kk.v(lambda a: a.rearrange("p (h d) -> p h d", d=64)), kkr.v(lambda a: a.rearrange("p (h d) -> p h d", d=64)),
                        rs4.v(lambda a: a.unsqueeze(2).to_broadcast([128, 4, 64])), ALU.mult)
                for d in range(2):
                    self.ts("dve", T_[:, d, 0, :], zw[:, d, :], -0.6065306597126334, ALU.mult)
                    self.tt("pool", T_[:, d, 2, :], kk, al[:, d, :], ALU.mult)
                    self.tt("dve", tk, al[:, d, :], kaB, ALU.mult)
                    self.tt("pool", tk, tk, omk, ALU.add)
                    self.tt("dve", T_[:, d, 3, :], k_, tk, ALU.mult)
                self.ts("dve", T_[:, 0, 1, :], kk, -1.0, ALU.mult)
                self.cp("act", T_[:, 0, 4, :], r_)
                self.cp("act", T_[:, 0, 5, :], v_)
                self.tt("pool", km, T_[:, 0, 3, :], T_[:, 1, 3, :], ALU.add)
                self.tt("dve", km, km, r_, ALU.mult)
                self.tt("pool", km, km, rkB, ALU.mult)
                self.red(ss, km.v(lambda a: a.rearrange("p (h d) -> p h d", d=64)))
                self.ts("dve", ss, ss, 0.5, ALU.mult)
                self.tt("dve", aux[b][:, 0, :].v(lambda a: a.rearrange("p (h d) -> p h d", d=64)), v_.v(lambda a: a.rearrange("p (h d) -> p h d", d=64)),
                        ss.v(lambda a: a.unsqueeze(2).to_broadcast([128, 4, 64])), ALU.mult)
                self.st_(self.TM[r0:r0 + 128, 0], T_)
                self.st_(self.AUX[r0:r0 + 128, 0:2, :], aux[b])
        S.barrier()

    def stage_prepC(self, l):
        nc, S = self.nc, self.S
        with ExitStack() as es:
            cwB = self.tb(es, "cwB", [128, 5, 768])
            for j in range(5):
                self.ld(cwB[:, j, :], self.gdn_conv[l, j, :].partition_broadcast(128))
            alB = self.bcast_row(es, "alogB", self.gdn_a_log[l].rearrange("d h -> (d h)"), 8)
            dtB = self.bcast_row(es, "dtB", self.gdn_dt_bias[l].rearrange("d h -> (d h)"), 8)
            negA = self.tb(es, "negA", [128, 8])
            self.act(negA, alB, AF.Exp)
            self.ts("dve", negA, negA, -1.0, ALU.mult)
            xs = [[self.tb(es, "xc%d_%d" % (j, i), [128, 768]) for j in range(5)] for i in range(2)]
            zt = [self.tb(es, "ztC%d" % i, [128, 272]) for i in range(2)]
            cv = self.tb(es, "cvC", [128, 768])
            t2 = self.tb(es, "t2C", [128, 768])
            sq = self.tb(es, "sqC", [128, 512])
            ss = self.tb(es, "ssC", [128, 8])
            tm8 = self.tb(es, "tm8C", [128, 8])
            rs8 = self.tb(es, "rs8C", [128, 8])
            bt = self.tb(es, "btC", [128, 8])
            nbt = self.tb(es, "nbtC", [128, 8])
            gg = self.tb(es, "ggC", [128, 8])
            TMt = [self.tb(es, "TMtC%d" % i, [128, 2, 6, 256]) for i in range(2)]
            for t_ in TMt:
                S.op("pool", lambda: nc.gpsimd.memset(t_.ap, 0.0), writes=[t_.key])
            aux = [self.tb(es, "auxC%d" % i, [128, 256]) for i in range(2)]
            P = self.P
            h3 = lambda a: a.rearrange("p (h d) -> p h d", d=64)
            for i in range(NT):
                b = i % 2
                r0 = i * 128
                for j in range(5):
                    sh = j - 2
                    lo_, hi_ = r0 + sh, r0 + sh + 128
                    x = xs[b][j]
                    if lo_ < 0 or hi_ > T:
                        S.op("pool", lambda: nc.gpsimd.memset(x.ap, 0.0), writes=[x.key])
                        a0, a1 = max(lo_, 0), min(hi_, T)
                        S.dma("sp", x.ap[a0 - lo_:a1 - lo_, :], P[a0:a1, OFF_C:OFF_C + 768], writes=[x.key])
                    else:
                        self.ld(x, P[lo_:hi_, OFF_C:OFF_C + 768])
                self.ld(zt[b], P[r0:r0 + 128, OFF_C + 768:OFF_C + 1040])
                self.tt("dve", cv, xs[b][0], cwB[:, 0, :], ALU.mult)
                for j in range(1, 5):
                    self.tt("pool", t2, xs[b][j], cwB[:, j, :], ALU.mult)
                    self.tt("dve", cv, cv, t2, ALU.add)
                self.act(cv, cv, AF.Silu)
                T_ = TMt[b]
                self.tt("pool", sq, cv[:, 0:512], cv[:, 0:512], ALU.mult)
                self.red(ss, sq.v(h3))
                self.rstd(rs8, ss, tm8, 1.0, 1e-12)
                self.ts("dve", rs8[:, 0:4], rs8[:, 0:4], 0.125, ALU.mult)
                self.tt("dve", T_[:, 0, 4, :].v(h3), cv[:, 0:256].v(h3), rs8[:, 0:4].v(lambda a: a.unsqueeze(2).to_broadcast([128, 4, 64])), ALU.mult)
                self.tt("dve", T_[:, 0, 2, :].v(h3), cv[:, 256:512].v(h3), rs8[:, 4:8].v(lambda a: a.unsqueeze(2).to_broadcast([128, 4, 64])), ALU.mult)
                self.cp("act", T_[:, 0, 3, :], T_[:, 0, 2, :])
                self.act(bt, zt[b][:, 256:264], AF.Sigmoid)
                self.ts("dve", nbt, bt, -1.0, ALU.mult)
                self.tt("dve", gg, zt[b][:, 264:272], dtB, ALU.add)
                self.act(gg, gg, AF.Exp)
                self.act(gg, gg, AF.Ln, bias=1.0)
                self.tt("dve", gg, gg, negA, ALU.mult)
                for d in range(2):
                    bc = lambda t_: t_[:, d * 4:(d + 1) * 4].v(lambda a: a.unsqueeze(2).to_broadcast([128, 4, 64]))
                    self.cp("pool", T_[:, d, 0, :].v(h3), bc(gg))
                    self.tt("dve", T_[:, d, 1, :].v(h3), T_[:, 0, 2, :].v(h3), bc(nbt), ALU.mult)
                    self.tt("pool", T_[:, d, 5, :].v(h3), cv[:, 512:768].v(h3), bc(bt), ALU.mult)
                self.act(aux[b], zt[b][:, 0:256], AF.Silu)
                self.st_(self.TM[r0:r0 + 128, 1], T_)
                self.st_(self.AUX[r0:r0 + 128, 2, :], aux[b])
        S.barrier()

    def stage_scan(self, l):
        nc, S = self.nc, self.S
        with ExitStack() as es:
            self.banks = [TB(self.ps(es, "bk%d" % i, [128, 512], F32), ("bk", i)) for i in range(8)]
            self.nbank = 0
            identf = TB(self.identf, "identf")
            tri = self.tb(es, "triS", [128, 2, 128])
            msk = self.tb(es, "mskS", [128, 2, 4, 128])
            onb = self.tb(es, "onbS", [128, 128])
            for d in range(2):
                self.ld(tri[:, d, :], self.c_tri[d])
                self.ld(msk[:, d, :, :], self.c_msk[d])
            self.ld(onb, self.c_onb)
            ST = [self.tb(es, "ST%d" % i, [64, 16, 64]) for i in range(2)]
            S.op("pool", lambda: nc.gpsimd.memset(ST[0].ap, 0.0), writes=[ST[0].key])
            U4 = range(4)
            BP = [self.tb(es, "BP%d" % u, [128, 256]) for u in U4]
            KP = [self.tb(es, "KP%d" % u, [128, 256]) for u in U4]
            VV = [self.tb(es, "VV%d" % u, [128, 256]) for u in U4]
            VP = [self.tb(es, "VP%d" % u, [128, 256]) for u in U4]
            G4 = [self.tb(es, "G4%d" % u, [128, 4, 4, 128]) for u in U4]
            APT = [self.tb(es, "APT%d" % u, [64, 4, 128]) for u in U4]
            RST = [self.tb(es, "RST%d" % u, [64, 4, 128]) for u in U4]
            PC = [self.tb(es, "PC%d" % u, [64, 4, 2]) for u in U4]
            def mk_lane(tg):
                lds = [self.tb(es, "ld%d_%s" % (q, tg), [128, 256]) for q in range(5)]
                c0t = self.tb(es, "c0t_" + tg, [128, 256])
                a1 = self.tb(es, "a1_" + tg, [128, 256])
                a1a = self.tb(es, "a1a_" + tg, [128, 256])
                X1 = self.tb(es, "X1_" + tg, [128, 256])
                X1a = self.tb(es, "X1a_" + tg, [128, 256])
                X2 = self.tb(es, "X2_" + tg, [128, 256])
                Ec = self.tb(es, "Ec_" + tg, [128, 256])
                Ag = self.tb(es, "Ag_" + tg, [128, 256])
                Rg = self.tb(es, "Rg_" + tg, [128, 256])
                Bg = self.tb(es, "Bg_" + tg, [128, 256])
                Kg = self.tb(es, "Kg_" + tg, [128, 256])
                As = self.tb(es, "As_" + tg, [128, 256])
                Rs = self.tb(es, "Rs_" + tg, [128, 256])
                FMar = self.tb(es, "FMar_" + tg, [64, 4, 2, 128])
                FMbg = self.tb(es, "FMbg_" + tg, [64, 4, 128])
                FMkg = self.tb(es, "FMkg_" + tg, [64, 4, 128])
                FMcum = self.tb(es, "FMcum_" + tg, [64, 4, 128])
                cumS = self.tb(es, "cumS_" + tg, [128, 256])
                Dm = self.tb(es, "DmS_" + tg, [128, 4, 128])
                mskD = self.tb(es, "mskD_" + tg, [128, 4, 2, 128])
                Xb = [self.tb(es, ("Xb%d_" % i) + tg, [128, 4, 128], F32R) for i in range(2)]
                XTb = [self.tb(es, ("XTb%d_" % i) + tg, [128, 4, 128], F32R) for i in range(2)]
                Wb = [self.tb(es, ("Wb%d_" % i) + tg, [128, 4, 128], F32R) for i in range(2)]
                Zs = self.tb(es, "Zs_" + tg, [128, 256])
                return dict(lds=lds, c0t=c0t, a1=a1, a1a=a1a, X1=X1, X1a=X1a, X2=X2, Ec=Ec, Ag=Ag, Rg=Rg, Bg=Bg, Kg=Kg, As=As, Rs=Rs, FMar=FMar, FMbg=FMbg, FMkg=FMkg, FMcum=FMcum, cumS=cumS, Dm=Dm, mskD=mskD, Xb=Xb, XTb=XTb, Wb=Wb, Zs=Zs)
            avg64 = self.tb(es, "avg64", [64, 128])
            S.op("pool", lambda: nc.gpsimd.memset(avg64.ap, 1.0 / 64), writes=[avg64.key])
            lanes = [mk_lane("L0"), mk_lane("L1")]
            Usb = self.tb(es, "Usb", [128, 2, 512])
            tmpS = self.tb(es, "tmpS", [64, 16, 64])
            Y1s = self.tb(es, "Y1s", [128, 2, 512])
            yt = [self.tb(es, "ytS%d" % d, [128, 512]) for d in range(2)]
            flat = lambda a: a.rearrange("p h t -> p (h t)")
            nld = 0
            cur = 0
            def unit_gen(m, d, j, Lz):
                lds = Lz["lds"]
                c0t = Lz["c0t"]
                a1 = Lz["a1"]
                a1a = Lz["a1a"]
                X1 = Lz["X1"]
                X1a = Lz["X1a"]
                X2 = Lz["X2"]
                Ec = Lz["Ec"]
                Ag = Lz["Ag"]
                Rg = Lz["Rg"]
                Bg = Lz["Bg"]
                Kg = Lz["Kg"]
                As = Lz["As"]
                Rs = Lz["Rs"]
                FMar = Lz["FMar"]
                FMbg = Lz["FMbg"]
                FMkg = Lz["FMkg"]
                FMcum = Lz["FMcum"]
                cumS = Lz["cumS"]
                Dm = Lz["Dm"]
                mskD = Lz["mskD"]
                Xb = Lz["Xb"]
                XTb = Lz["XTb"]
                Wb = Lz["Wb"]
                Zs = Lz["Zs"]
                u = m * 2 + d
                _CUR_U[0] = u
                tile_ = j if d == 0 else NT - 1 - j
                r0 = tile_ * 128
                L_ = lds
                dsrc = {0: d, 1: (0 if m == 0 else d), 2: (d if m == 0 else 0), 3: (d if m == 0 else 0), 4: 0, 5: (0 if m == 0 else d)}
                LW, Aa, Bb, Kk, Rr = L_
                for q, dst in ((0, LW), (1, Aa), (2, Bb), (3, Kk), (4, Rr), (5, VV[u])):
                    self.ld(dst, self.TM[r0:r0 + 128, m, dsrc[q], q, :])
                bc = self.bank()
                self.mm(bc[:, 0:256], tri[:, d, :], LW)
                self.mm(bc[:, 256:512], onb, LW)
                self.ts("dve", c0t, bc[:, 256:512], 0.5, ALU.mult)
                if m == 0:
                    self.tt("dve", a1, bc[:, 0:256], c0t, ALU.subtract)
                    self.act(X1, a1, AF.Exp)
                    self.act(X2, a1, AF.Exp, scale=-1.0)
                    self.act(Ec, c0t, AF.Exp)
                    self.tt("pool", a1a, a1, LW, ALU.subtract)
                    self.act(X1a, a1a, AF.Exp)
                    xa = X1a
                    self.tt("dve", Ag, Aa, xa, ALU.mult)
                    self.tt("pool", Rg, Rr, X1, ALU.mult)
                    self.tt("dve", Bg, Bb, X2, ALU.mult)
                    self.tt("pool", Kg, Kk, X2, ALU.mult)
                    self.tt("dve", As, Ag, Ec, ALU.mult)
                    self.tt("pool", Rs, Rg, Ec, ALU.mult)
                    self.tt("dve", BP[u], Bg, Ec, ALU.mult)
                    self.tt("pool", KP[u], Kg, Ec, ALU.mult)
                    gA, gR, gB_ = Ag, Rg, Bg
                else:
                    cops = [
                        lambda: self.cp("dve", cumS, bc[:, 0:256]),
                        lambda: self.act(X1, cumS, AF.Exp),
                        lambda: self.tt("dve", a1, c0t, cumS, ALU.subtract),
                        lambda: self.tt("dve", a1, a1, c0t, ALU.add),
                        lambda: self.act(X2, a1, AF.Exp),
                        lambda: self.tt("dve", As, Aa, X1, ALU.mult),
                        lambda: self.tt("pool", Rs, Rr, X1, ALU.mult),
                        lambda: self.tt("dve", BP[u], Bb, X2, ALU.mult),
                        lambda: self.cp("pool", KP[u], BP[u]),
                    ]
                    import os as _os
                    for ci, cop in enumerate(cops):
                        if _os.environ.get("SCAN_STOP") == "cn" and ci == int(_os.environ.get("SCAN_N", "0")):
                            S.barrier()
                            return True
                        cop()
                    gA, gR, gB_ = Aa, Rr, Bb
                if _stop("a"):
                    S.barrier()
                    return True
                yield
                bpc = self.bank()
                on2 = onb.v(lambda a: a.rearrange("p (c t) -> p c t", t=64)[:, :, 0])
                for h in range(4):
                    self.mm(bpc[0:64, h * 2:(h + 1) * 2], LW[:, h * 64:(h + 1) * 64], on2)
                self.act(PC[u].v(lambda a: a.rearrange("p h c -> p (h c)")), bpc[0:64, 0:8], AF.Exp)
                if _stop("b"):
                    S.barrier()
                    return True
                yield
                fm_list = [(gA, FMar[:, :, 0, :]), (gR, FMar[:, :, 1, :]), (gB_, FMbg), (Rs, RST[u])]
                fm_list.append((Kg, FMkg) if m == 0 else (cumS, FMcum))
                for (src, dst) in fm_list:
                    bt_ = self.bank()
                    for h in range(4):
                        self.mm(bt_[0:64, h * 128:(h + 1) * 128], src[:, h * 64:(h + 1) * 64], identf)
                    self.cp("act", dst, bt_[0:64, :].v(lambda a: a.rearrange("p (h t) -> p h t", t=128)))
                if _stop("c"):
                    S.barrier()
                    return True
                yield
                nblk = 4 if m == 0 else 2
                if m == 1:
                    bd_ = self.bank()
                    for h in range(4):
                        self.mm(bd_[:, h * 128:(h + 1) * 128], avg64, FMcum[:, h, :])
                    for h in range(4):
                        self.ts("dve", Dm[:, h, :], bd_[:, h * 128:(h + 1) * 128], cumS[:, h * 64:h * 64 + 1], ALU.subtract)
                    self.ts("dve", Dm, Dm, 0.0, ALU.min)
                    if True:
                        pass
                    self.act(Dm, Dm, AF.Exp)
                    for h in range(4):
                        self.tt("pool", mskD[:, h, :, :], msk[:, d, 0:2, :], Dm[:, h, :].v(lambda a: a.unsqueeze(1).to_broadcast([128, 2, 128])), ALU.mult)
                for h in range(4):
                    bg_ = self.bank()
                    rhs = FMar[:, h, :, :].v(lambda a: a.rearrange("p c t -> p (c t)"))
                    self.mm(bg_[:, 0:256], FMbg[:, h, :], rhs)
                    if m == 0:
                        self.mm(bg_[:, 256:512], FMkg[:, h, :], rhs)
                    if _stop("c2"):
                        S.barrier()
                        return True
                    mk_ = msk[:, d, 0:nblk, :] if m == 0 else mskD[:, h, :, :]
                    self.tt("dve", G4[u][:, h, 0:nblk, :].v(lambda a: a.rearrange("p b t -> p (b t)")), bg_[:, 0:nblk * 128],
                            mk_.v(lambda a: a.rearrange("p b t -> p (b t)")), ALU.mult)
                    import os as _os
                    if _stop("c3") and h == int(_os.environ.get("SCAN_H", "0")):
                        S.barrier()
                        return True
                mak_i = 2 if m == 0 else 0
                nrk_i = 3 if m == 0 else 1
                if _stop("d"):
                    S.barrier()
                    return True
                yield
                xi = 0
                Xc, XTc, Wc = Xb[0], XTb[0], Wb[0]
                self.cp("act", Xc, G4[u][:, :, 0, :])
                bt_ = self.bank()
                for h in range(4):
                    self.tp(bt_[:, h * 128:(h + 1) * 128], Xc[:, h, :].v(lambda a: a.bitcast(F32)), identf)
                self.cp("act", XTc.v(flat), bt_)
                self.tt("dve", Wc, Xc.v(lambda a: a.bitcast(F32)), identf.v(lambda a: a.unsqueeze(1).to_broadcast([128, 4, 128])), ALU.add)
                for lv in range(1, 6):
                    Xn, XTn, Wn = Xb[1 - xi], XTb[1 - xi], Wb[1 - xi]
                    bx = self.bank()
                    for h in range(4):
                        self.mm(bx[:, h * 128:(h + 1) * 128], Xc[:, h, :], XTc[:, h, :])
                    self.cp("act", XTn.v(flat), bx)
                    if lv < 5:
                        by = self.bank()
                        for h in range(4):
                            self.mm(by[:, h * 128:(h + 1) * 128], XTc[:, h, :], Xc[:, h, :])
                        self.cp("act", Xn.v(flat), by)
                    bw = self.bank()
                    for h in range(4):
                        self.mm(bw[:, h * 128:(h + 1) * 128], XTn[:, h, :], Wc[:, h, :])
                    self.tt("dve", Wn.v(flat), Wc.v(flat).v(lambda a: a.bitcast(F32)), bw, ALU.add)
                    xi = 1 - xi
                    Xc, XTc, Wc = Xn, XTn, Wn
                    yield
                if _stop("e"):
                    S.barrier()
                    return True
                yield
                bz = self.bank()
                for h in range(4):
                    self.mm(bz[:, h * 64:(h + 1) * 64], G4[u][:, h, mak_i, :], VV[u][:, h * 64:(h + 1) * 64])
                self.cp("act", Zs, bz[:, 0:256])
                yield
                bv = self.bank()
                for h in range(4):
                    self.mm(bv[:, h * 64:(h + 1) * 64], Wc[:, h, :].v(lambda a: a.bitcast(F32)), Zs[:, h * 64:(h + 1) * 64])
                self.cp("act", VP[u], bv[:, 0:256])
                yield
                ba = self.bank()
                for h in range(4):
                    self.mm(ba[0:64, h * 128:(h + 1) * 128], As[:, h * 64:(h + 1) * 64], Wc[:, h, :].v(lambda a: a.bitcast(F32)))
                self.cp("act", APT[u].v(flat), ba[0:64, :])
                import os as _os
                if _stop("u") and u == int(_os.environ.get("SCAN_U", "0")):
                    S.barrier()
                    return True
                yield
            for j in range(NT):
                for m in range(2):
                    gens = [unit_gen(m, 0, j, lanes[0]), unit_gen(m, 1, j, lanes[1])]
                    while gens:
                        for g_ in list(gens):
                            try:
                                next(g_)
                            except StopIteration:
                                gens.remove(g_)
                if _stop("f"):
                    S.barrier()
                    return True
                for sub in range(2):
                    STc, STn = ST[cur], ST[1 - cur]
                    par = [sub, 1 - sub]
                    rows = [slice(par[d] * 64, par[d] * 64 + 64) for d in range(2)]
                    bu = [self.bank(), self.bank()]
                    for d in range(2):
                        for m in range(2):
                            u = m * 2 + d
                            for h in range(4):
                                c0_ = (m * 4 + h) * 64
                                self.mm(bu[d][rows[d], c0_:c0_ + 64], APT[u][:, h, rows[d]], STc[:, d * 8 + m * 4 + h, :])
                    for d in range(2):
                        for m in range(2):
                            u = m * 2 + d
                            self.tt("dve", Usb[rows[d], d, m * 256:(m + 1) * 256], bu[d][rows[d], m * 256:(m + 1) * 256], VP[u][rows[d], :], ALU.add)
                    bs = [self.bank(), self.bank()]
                    for d in range(2):
                        for m in range(2):
                            u = m * 2 + d
                            for h in range(4):
                                c0_ = (m * 4 + h) * 64
                                self.mm(bs[d][0:64, c0_:c0_ + 64], BP[u][rows[d], h * 64:(h + 1) * 64], Usb[rows[d], d, c0_:c0_ + 64], start=True, stop=False)
                                self.mm(bs[d][0:64, c0_:c0_ + 64], KP[u][rows[d], h * 64:(h + 1) * 64], VV[u][rows[d], h * 64:(h + 1) * 64], start=False, stop=True)
                    by1 = [self.bank(), self.bank()]
                    by2 = [self.bank(), self.bank()]
                    for d in range(2):
                        for m in range(2):
                            u = m * 2 + d
                            nrk_i = 3 if m == 0 else 1
                            for h in range(4):
                                c0_ = (m * 4 + h) * 64
                                self.mm(by1[d][rows[d], c0_:c0_ + 64], RST[u][:, h, rows[d]], STc[:, d * 8 + m * 4 + h, :])
                                self.mm(by2[d][rows[d], c0_:c0_ + 64], G4[u][rows[d], h, 1, rows[d]], Usb[rows[d], d, c0_:c0_ + 64], start=True, stop=False)
                                self.mm(by2[d][rows[d], c0_:c0_ + 64], G4[u][rows[d], h, nrk_i, rows[d]], VV[u][rows[d], h * 64:(h + 1) * 64], start=False, stop=True)
                    for d in range(2):
                        for m in range(2):
                            u = m * 2 + d
                            sl_ = slice(d * 8 + m * 4, d * 8 + m * 4 + 4)
                            self.tt("pool", tmpS[:, sl_, :], STc[:, sl_, :], PC[u][:, :, par[d]].v(lambda a: a.unsqueeze(2).to_broadcast([64, 4, 64])), ALU.mult)
                        sl8 = slice(d * 8, d * 8 + 8)
                        self.tt("dve", STn[:, sl8, :].v(lambda a: a.rearrange("p s v -> p (s v)")), tmpS[:, sl8, :].v(lambda a: a.rearrange("p s v -> p (s v)")),
                                bs[d][0:64, :], ALU.add)
                        self.cp("act", Y1s[rows[d], d, :], by1[d][rows[d], :])
                        self.tt("dve", yt[d][rows[d], :], Y1s[rows[d], d, :], by2[d][rows[d], :], ALU.add)
                    cur = 1 - cur
                    if _stop("g"):
                        S.barrier()
                        return True
                for d in range(2):
                    tile_ = j if d == 0 else NT - 1 - j
                    self.st_(self.YAC[tile_ * 128:(tile_ + 1) * 128, :, d, :], yt[d].v(lambda a: a.rearrange("p (m c) -> p m c", m=2)))
        S.barrier()

    def stage_postAC(self, l):
        nc, S = self.nc, self.S
        with ExitStack() as es:
            lnw = self.bcast_row(es, "lnwB", self.rwkv_ln_w[l, :], 256)
            lnb = self.bcast_row(es, "lnbB", self.rwkv_ln_b[l, :], 256)
            gn = self.tb(es, "gnB", [128, 4, 64])
            for h in range(4):
                self.ld(gn[:, h, :], self.gdn_norm_g[l, :].partition_broadcast(128))
            ys = [self.tb(es, "ysP%d" % i, [128, 2, 2, 256]) for i in range(2)]
            ax = [self.tb(es, "axP%d" % i, [128, 3, 256]) for i in range(2)]
            y = self.tb(es, "yP", [128, 256])
            yc = self.tb(es, "ycP", [128, 256])
            sq = self.tb(es, "sqP", [128, 256])
            s4 = self.tb(es, "s4P", [128, 4])
            t4 = self.tb(es, "t4P", [128, 4])
            r4 = self.tb(es, "r4P", [128, 4])
            oa = [self.tb(es, "oaP%d" % i, [128, 256]) for i in range(2)]
            oc = [self.tb(es, "ocP%d" % i, [128, 256]) for i in range(2)]
            h3 = lambda a: a.rearrange("p (h d) -> p h d", d=64)
            b4 = lambda t_: t_.v(lambda a: a.unsqueeze(2).to_broadcast([128, 4, 64]))
            for i in range(NT):
                b = i % 2
                r0 = i * 128
                self.ld(ys[b], self.YAC[r0:r0 + 128])
                self.ld(ax[b], self.AUX[r0:r0 + 128])
                self.tt("dve", y, ys[b][:, 0, 0, :], ys[b][:, 0, 1, :], ALU.add)
                self.red(s4, y.v(h3))
                self.ts("dve", s4, s4, 1.0 / 64, ALU.mult)
                self.tt("dve", yc.v(h3), y.v(h3), b4(s4), ALU.subtract)
                self.tt("pool", sq, yc, yc, ALU.mult)
                self.red(s4, sq.v(h3))
                self.rstd(r4, s4, t4, 1.0 / 64, 64e-5)
                self.tt("dve", yc.v(h3), yc.v(h3), b4(r4), ALU.mult)
                self.tt("pool", yc, yc, lnw, ALU.mult)
                self.tt("dve", yc, yc, lnb, ALU.add)
                self.tt("pool", yc, yc, ax[b][:, 0, :], ALU.add)
                self.tt("dve", oa[b], yc, ax[b][:, 1, :], ALU.mult)
                self.st_(self.mixed[r0:r0 + 128, 0:256], oa[b])
                self.tt("dve", y, ys[b][:, 1, 0, :], ys[b][:, 1, 1, :], ALU.add)
                self.tt("pool", sq, y, y, ALU.mult)
                self.red(s4, sq.v(h3))
                self.rstd(r4, s4, t4, 1.0 / 64, EPS)
                self.tt("dve", yc.v(h3), y.v(h3), b4(r4), ALU.mult)
                self.tt("pool", yc.v(h3), yc.v(h3), gn, ALU.mult)
                self.tt("dve", oc[b], yc, ax[b][:, 2, :], ALU.mult)
                self.st_(self.mixed[r0:r0 + 128, 512:768], oc[b])
        S.barrier()

    def stage_final(self, xsrc):
        nc, S = self.nc, self.S
        with ExitStack() as es:
            gB = self.bcast_row(es, "gFB", self.norm_final_g[0, :], D)
            xt = [self.tb(es, "xF%d" % i, [128, D]) for i in range(2)]
            ot = [self.tb(es, "oF%d" % i, [128, D]) for i in range(2)]
            junk = self.tb(es, "junkF", [128, D])
            st = self.tb(es, "stF", [128, NT, 4])
            for i in range(NT):
                b = i % 2
                self.ld(xt[b], xsrc[i * 128:(i + 1) * 128, :])
                S.op("act", lambda: nc.scalar.activation(out=junk.ap, in_=xt[b].ap, func=AF.Square, accum_out=st.ap[:, i, 0:1]),
                     reads=[xt[b].key], writes=[junk.key, st.key])
                self.rstd(st[:, i, 2:3], st[:, i, 0:1], st[:, i, 1:2], 1.0 / D, EPS)
                self.stt(ot[b], xt[b], st[:, i, 2:3], gB, ALU.mult, ALU.mult)
                self.st_(self.out[i * 128:(i + 1) * 128, :], ot[b])
        S.barrier()

    def build(self):
        nc, S = self.nc, self.S
        self.stage_inproj(0, self.x)
        if self.stop_after == "inproj0":
            return self.finish_debug(self.P, [T, P_IN])
        if self.stop_after in ("moe_idx", "moe", "outproj"):
            self.mixed = self.inp("mixed_in", [T, MIXW])
            self.stage_outproj(0, self.x)
            if self.stop_after == "outproj":
                return self.finish_debug(self.X2, [T, D])
            if self.stop_after == "moe_idx":
                self.dbg_small = nc.dram_tensor("dbg", [128, 80], F32, kind="ExternalOutput").ap()
                self.stage_moe(0)
                self.es.close()
                return nc
            self.stage_moe(0)
            return self.finish_debug(self.X3, [T, D])
        if self.stop_after == "AC":
            import os
            la = int(os.environ.get("ACL", "0"))
            if la:
                self.stage_inproj(la, self.x)
            self.stage_prepA(la)
            self.stage_prepC(la)
            if self.stage_scan(la):
                return self.finish_debug(self.AUX.rearrange("t a c -> t (a c)"), [T, 768])
            if os.environ.get("SCAN_STOP") == "h":
                return self.finish_debug(self.YAC.rearrange("t a b c -> t (a b c)"), [T, 1024])
            self.stage_postAC(la)
            return self.finish_debug(self.mixed, [T, MIXW], cols=[(0, 256), (512, 768)])
        if self.stop_after == "prepAC":
            self.stage_prepA(0)
            self.stage_prepC(0)
            return self.finish_debug(self.TM.rearrange("t m d q c -> t (m d q c)"), [T, 2 * 2 * 6 * 256])
        if self.stop_after == "BD":
            self.stage_attnB(0)
            self.stage_attnD(0)
            return self.finish_debug(self.mixed, [T, MIXW])
        self.out = nc.dram_tensor("out", [T, D], F32, kind="ExternalOutput").ap()
        xsrc = self.x
        for l in range(L):
            if l > 0:
                self.stage_inproj(l, xsrc)
            self.X3 = self.X3s[l % 2]
            self.stage_prepA(l)
            self.stage_prepC(l)
            self.stage_scan(l)
            self.stage_postAC(l)
            self.stage_attnB(l)
            self.stage_attnD(l)
            self.stage_outproj(l, xsrc)
            self.stage_moe(l)
            xsrc = self.X3
        self.stage_final(xsrc)
        self.es.close()
        return nc

    def finish_debug(self, src, shape, cols=None):
        nc, S = self.nc, self.S
        dbg = nc.dram_tensor("dbg", list(shape), F32, kind="ExternalOutput").ap()
        with ExitStack() as es:
            tb = [self.sb(es, "dbgt%d" % i, [128, shape[1]], F32) for i in range(2)]
            for i in range(shape[0] // 128):
                for (c0, c1) in (cols or [(0, shape[1])]):
                    S.dma("sp", tb[i % 2][:, c0:c1], src[i * 128:(i + 1) * 128, c0:c1], writes=[("dbgt", i % 2)])
                    S.dma("sp", dbg[i * 128:(i + 1) * 128, c0:c1], tb[i % 2][:, c0:c1], reads=[("dbgt", i % 2)])
        S.barrier()
        self.es.close()
        return nc


def _t5_bucket(rel):
    nb = 16
    max_exact = 8
    n = np.abs(rel)
    large = max_exact + (np.log(np.maximum(n, 1).astype(np.float32) / np.float32(max_exact))
                         / np.float32(math.log(1024 / max_exact)) * np.float32(nb - max_exact)).astype(np.int32)
    large = np.minimum(large, nb - 1)
    return np.where(rel > 0, nb, 0) + np.where(n < max_exact, n, large)


def host_consts(inputs=None):
    c = {}
    c["ident_f"] = np.eye(128, dtype=np.float32)
    t = np.arange(T)
    inv = (np.float32(10000.0) ** (-np.arange(0, 32, 2, dtype=np.float32) / np.float32(32))).astype(np.float32)
    ar = (t // 64).astype(np.float32)[:, None] * inv
    ac = (t % 64).astype(np.float32)[:, None] * inv
    c["rope_cos"] = np.concatenate([np.cos(ar), np.cos(ar), np.cos(ac), np.cos(ac)], 1).astype(np.float32)
    c["rope_sin"] = np.concatenate([-np.sin(ar), np.sin(ar), -np.sin(ac), np.sin(ac)], 1).astype(np.float32)
    ch = np.arange(128) // 64
    same = (ch[:, None] == ch[None, :])
    sI, tI = np.arange(128)[:, None], np.arange(128)[None, :]
    c["c_onb"] = same.astype(np.float32)
    c["c_tri"] = np.stack([(same & (sI <= tI)), (same & (sI >= tI))]).astype(np.float32)
    mk = np.zeros((2, 128, 4, 128), np.float32)
    for blk in range(4):
        strict = blk in (0, 2)
        mk[0, :, blk, :] = same & ((sI < tI) if strict else (sI <= tI))
        mk[1, :, blk, :] = same & ((sI > tI) if strict else (sI >= tI))
    c["c_msk"] = mk
    c["ustrict"] = np.triu(np.ones((128, 128), np.float32), 1)
    c["iota_c"] = np.tile(np.arange(CAP, dtype=np.float32)[None, :], (128, 1))
    tv = np.zeros((128, NT, 2), np.float32)
    tv[:, :, 0] = np.arange(128)[:, None]
    tv[:, :, 1] = 128.0 * np.arange(NT)[None, :]
    c["tvals"] = tv
    c["tvals_"] = tv
    c["coef2"] = np.array([[1.0], [128.0]], np.float32)
    if inputs is not None:
        rb = np.asarray(inputs["rel_bias"], np.float32)
        kp = np.arange(128)[:, None, None]
        rel = np.arange(3)[None, :, None]
        qp = np.arange(128)[None, None, :]
        o = 128 * (rel - 1) + kp - qp
        valid = np.abs(o) <= 64
        db = np.full((3, 2, 128, 3, 128), NEG, np.float32)
        for br, dil in enumerate((1, 4, 16)):
            bk = _t5_bucket(o * dil)
            for j in range(2):
                db[br, j] = np.where(valid, rb[bk, br * 2 + j], np.float32(NEG))
        c["dbias"] = db
    return c


_PROG = None


def kernel(**inputs):
    global _PROG
    if _PROG is None:
        pr = Prog()
        _PROG = (pr, pr.build())
    pr, nc = _PROG
    consts = host_consts(inputs)
    x = np.asarray(inputs["x"], np.float32)
    nb = x.shape[0]
    in_maps = []
    for b in range(nb):
        m = {}
        for name, (shape, dt) in pr.inputs.items():
            if name == "x":
                m[name] = np.ascontiguousarray(x[b])
            elif name in consts:
                m[name] = consts[name]
            else:
                m[name] = np.ascontiguousarray(np.asarray(inputs[name], np.float32)).reshape(shape)
        in_maps.append(m)
    res = run_bass_kernel_spmd(nc, in_maps, core_ids=list(range(nb)))
    return np.stack([np.asarray(res.results[b]["out"], np.float32) for b in range(nb)])
```

```python
import math
from contextlib import ExitStack
import numpy as np
import concourse.bass as bass
import concourse.mybir as mybir
from concourse.bass_utils import run_bass_kernel_spmd

F32 = mybir.dt.float32
F32R = mybir.dt.float32r
BF16 = mybir.dt.bfloat16
U32 = mybir.dt.uint32
I32 = mybir.dt.int32
AF = mybir.ActivationFunctionType
ALU = mybir.AluOpType
AX = mybir.AxisListType

T = 4096
NT = 32
D = 1024
L = 2
HD = 64
EPS = 1e-6
A_W = 256
A_IN = 1024
B_IN = 512
C_IN = 1040
D_IN = 1152
P_IN = 3728
OFF_A = 0
OFF_B = 1024
OFF_C = 1536
OFF_D = 2576
MIXW = 1152
NE = 16
CAP = 512
NEG = -30000.0

NDMA = 44
NSW = 12


class Sched:
    def __init__(self, nc, same_engine_sync=True):
        self.nc = nc
        self.eng = {"pe": nc.tensor, "act": nc.scalar, "dve": nc.vector,
                    "pool": nc.gpsimd, "sp": nc.sync}
        self.sem = {}
        for k in self.eng:
            self.sem[k] = nc.alloc_semaphore("sem_" + k)
        self.cnt = {k: 0 for k in self.eng}
        for i in range(NDMA):
            self.sem[("d", i)] = nc.alloc_semaphore("sem_d%d" % i)
        self.dval = [0] * NDMA
        self.dnext = {"hw": 0, "sw": 0}
        self.known = {k: {} for k in self.eng}
        self.w = {}
        self.r = {}
        self.same = same_engine_sync
        self.nwaits = 0
        self.nops = 0

    def _wait(self, e, ev):
        if ev is None:
            return
        key, val = ev
        if key == e and (e == "pe" or not self.same):
            return
        if self.known[e].get(key, 0) >= val:
            return
        self.eng[e].wait_ge(self.sem[key], val)
        self.known[e][key] = val
        self.nwaits += 1

    def _deps(self, e, reads, writes):
        for b in reads:
            self._wait(e, self.w.get(b))
        for b in writes:
            self._wait(e, self.w.get(b))
            for ev in self.r.get(b, ()):
                self._wait(e, ev)

    def _commit(self, ev, reads, writes):
        for b in reads:
            self.r.setdefault(b, []).append(ev)
        for b in writes:
            self.w[b] = ev
            self.r[b] = []

    def op(self, e, fn, reads=(), writes=()):
        self._deps(e, reads, writes)
        ins = fn()
        self.cnt[e] += 1
        ins.then_inc(self.sem[e], 1)
        self._commit((e, self.cnt[e]), reads, writes)
        self.nops += 1
        return ins

    def dma_raw(self, q, fn, reads=(), writes=()):
        self._deps(q, reads, writes)
        if q == "pool":
            s = NDMA - NSW + self.dnext["sw"]
            self.dnext["sw"] = (self.dnext["sw"] + 1) % NSW
        else:
            s = self.dnext["hw"]
            self.dnext["hw"] = (self.dnext["hw"] + 1) % (NDMA - NSW)
        key = ("d", s)
        if self.dval[s] > 0:
            self._wait(q, (key, self.dval[s]))
        ins = fn()
        self.dval[s] += 16
        ins.then_inc(self.sem[key], 16)
        self._commit((key, self.dval[s]), reads, writes)
        self.nops += 1
        return ins

    def dma(self, q, out, in_, reads=(), writes=(), **kw):
        return self.dma_raw(q, lambda: self.eng[q].dma_start(out=out, in_=in_, **kw), reads, writes)

    def barrier(self):
        for e in self.eng:
            for s in range(NDMA):
                if self.dval[s] > 0:
                    self._wait(e, (("d", s), self.dval[s]))
            for o in self.eng:
                if o != e and self.cnt[o] > 0:
                    key, val = o, self.cnt[o]
                    if self.known[e].get(key, 0) < val:
                        self.eng[e].wait_ge(self.sem[key], val)
                        self.known[e][key] = val
                        self.nwaits += 1
        self.w = {}
        self.r = {}


class StopScan(Exception):
    pass


def _stop(tag):
    import os
    return os.environ.get("SCAN_STOP") == tag and _CUR_U[0] >= int(os.environ.get("SCAN_U", "0"))


_CUR_U = [0]


class TB:
    def __init__(self, ap, key):
        self.ap, self.key = ap, key

    def __getitem__(self, idx):
        return TB(self.ap[idx], self.key)

    def v(self, f):
        return TB(f(self.ap), self.key)


class Prog:
    def __init__(self, stop_after=None, dbg=None):
        self.nc = nc = bass.Bass("TRN2", target_bir_lowering=False)
        self.S = Sched(nc)
        self.stop_after = stop_after
        self.dbg = dbg
        dt = nc.dram_tensor
        self.inputs = {}
        self.x = self.inp("x", [T, D])
        self.norm_mix_g = self.inp("norm_mix_g", [L, D])
        self.w_in = self.inp("w_in", [L, D, P_IN])
        self.ident_f = self.inp("ident_f", [128, 128])
        self.P = dt("P_scr", [T, P_IN], F32, kind="Internal").ap()
        self.mixed = dt("mixed_scr", [T, MIXW], F32, kind="Internal").ap()
        self.Dnum = dt("Dnum_scr", [T, 6, 65], F32, kind="Internal").ap()
        self.X2 = dt("X2_scr", [T, D], F32, kind="Internal").ap()
        self.X3s = [dt("X3_scr%d" % i, [T, D], F32, kind="Internal").ap() for i in range(2)]
        self.X3 = self.X3s[0]
        self.norm_final_g = self.inp("norm_final_g", [1, D])
        self.H2 = dt("H2_scr", [T, D], BF16, kind="Internal").ap()
        self.AFFd = dt("AFF_scr", [T, NE], F32, kind="Internal").ap()
        self.IDXd = dt("IDX_scr", [NE, CAP], U32, kind="Internal").ap()
        self.w_out = self.inp("w_out", [L, MIXW, D])
        self.norm_ffn_g = self.inp("norm_ffn_g", [L, D])
        self.router_w = self.inp("router_w", [L, D, NE])
        self.router_b = self.inp("router_b", [L, NE])
        if stop_after not in ("inproj0", "BD", "outproj", "moe_idx", "AC", "prepAC"):
            self.w_gate = self.inp("expert_w_gate", [L, NE, D, D])
            self.w_up = self.inp("expert_w_up", [L, NE, D, D])
            self.w_down = self.inp("expert_w_down", [L, NE, D, D])
        self.ustrict = self.inp("ustrict", [128, 128])
        self.iota_c = self.inp("iota_c", [128, CAP])
        self.tvals = self.inp("tvals", [128, NT, 2])
        self.TM = dt("TM_scr", [T, 2, 2, 6, 256], F32, kind="Internal").ap()
        self.AUX = dt("AUX_scr", [T, 3, 256], F32, kind="Internal").ap()
        self.YAC = dt("YAC_scr", [T, 2, 2, 256], F32, kind="Internal").ap()
        self.rwkv_mu_prev = self.inp("rwkv_mu_prev", [L, 1024])
        self.rwkv_mu_next = self.inp("rwkv_mu_next", [L, 1024])
        self.rwkv_w0 = self.inp("rwkv_w0", [L, 2, 256])
        self.rwkv_w_up = self.inp("rwkv_w_up", [L, 2, 64, 256])
        self.rwkv_a0 = self.inp("rwkv_a0", [L, 2, 256])
        self.rwkv_a_up = self.inp("rwkv_a_up", [L, 2, 64, 256])
        self.rwkv_g_up = self.inp("rwkv_g_up", [L, 128, 256])
        self.rwkv_k_k = self.inp("rwkv_k_k", [L, 256])
        self.rwkv_k_a = self.inp("rwkv_k_a", [L, 256])
        self.rwkv_r_k = self.inp("rwkv_r_k", [L, 4, 64])
        self.rwkv_ln_w = self.inp("rwkv_ln_w", [L, 256])
        self.rwkv_ln_b = self.inp("rwkv_ln_b", [L, 256])
        self.gdn_conv = self.inp("gdn_conv", [L, 5, 768])
        self.gdn_a_log = self.inp("gdn_a_log", [L, 2, 4])
        self.gdn_dt_bias = self.inp("gdn_dt_bias", [L, 2, 4])
        self.gdn_norm_g = self.inp("gdn_norm_g", [L, 64])
        self.c_tri = self.inp("c_tri", [2, 128, 128])
        self.c_msk = self.inp("c_msk", [2, 128, 4, 128])
        self.c_onb = self.inp("c_onb", [128, 128])
        self.attn_q_norm = self.inp("attn_q_norm", [L, 64])
        self.attn_k_norm = self.inp("attn_k_norm", [L, 64])
        self.rope_cos = self.inp("rope_cos", [T, 64])
        self.rope_sin = self.inp("rope_sin", [T, 64])
        self.dbias = self.inp("dbias", [3, 2, 128, 3, 128])
        self.es = ExitStack()
        self.identf = self.sb(self.es, "identf", [128, 128], F32)
        self.identb = self.sb(self.es, "identb", [128, 128], BF16)
        self.AFF = self.sb(self.es, "AFF", [128, NT, NE], F32)
        S = self.S
        S.dma("sp", self.identf, self.ident_f, writes=["identf"])
        S.op("dve", lambda: nc.vector.tensor_copy(out=self.identb, in_=self.identf), reads=["identf"], writes=["identb"])

    def inp(self, name, shape, dtype=F32):
        self.inputs[name] = (tuple(shape), dtype)
        return self.nc.dram_tensor(name, list(shape), dtype, kind="ExternalInput").ap()

    def _uname(self, name):
        self.uid = getattr(self, "uid", 0) + 1
        return "%s_%d" % (name, self.uid)

    def sb(self, es, name, shape, dtype):
        return es.enter_context(self.nc.sbuf_tensor(self._uname(name), list(shape), dtype)).ap()

    def ps(self, es, name, shape, dtype=F32):
        return es.enter_context(self.nc.psum_tensor(self._uname(name), list(shape), dtype)).ap()

    def stage_inproj(self, l, xsrc):
        nc, S = self.nc, self.S
        with ExitStack() as es:
            W = self.sb(es, "Win", [128, 8, P_IN], BF16)
            gB = self.sb(es, "gB", [128, D], F32)
            xts = [self.sb(es, "xt%d" % i, [128, D], F32) for i in range(2)]
            junk = self.sb(es, "junk", [128, D], F32)
            hb = [self.sb(es, "hb%d" % i, [128, D], BF16) for i in range(2)]
            hT = [self.sb(es, "hT%d" % i, [128, 8, 128], BF16) for i in range(2)]
            pts = [self.sb(es, "pt%d" % i, [128, P_IN], F32) for i in range(2)]
            st = self.sb(es, "st", [128, NT, 4], F32)
            pT = self.ps(es, "pT", [128, 8, 128], BF16)
            pss = [self.ps(es, "psA%d" % i, [128, 512], F32) for i in range(4)]
            wv = self.w_in[l].rearrange("(k p) c -> p k c", p=128)
            for k in range(8):
                for c0 in (0, 1864):
                    S.dma("pool", W[:, k, c0:c0 + 1864], wv[:, k, c0:c0 + 1864], writes=[("W", k)])
            S.dma("sp", gB, self.norm_mix_g[l, :].partition_broadcast(128), writes=["gB"])
            nps = 0
            for i in range(NT):
                b = i % 2
                xt = xts[b]
                S.dma("sp", xt, xsrc[i * 128:(i + 1) * 128, :], writes=[("xt", b)])
                S.op("act", lambda: nc.scalar.activation(out=junk, in_=xt, func=AF.Square, accum_out=st[:, i, 0:1]),
                     reads=[("xt", b)], writes=["junk", ("st", i)])
                S.op("dve", lambda: nc.vector.tensor_scalar(out=st[:, i, 1:2], in0=st[:, i, 0:1], scalar1=1.0 / D, scalar2=EPS,
                                                            op0=ALU.mult, op1=ALU.add), reads=[("st", i)], writes=[("st", i)])
                S.op("act", lambda: nc.scalar.activation(out=st[:, i, 2:3], in_=st[:, i, 1:2], func=AF.Sqrt),
                     reads=[("st", i)], writes=[("st", i)])
                S.op("dve", lambda: nc.vector.reciprocal(out=st[:, i, 3:4], in_=st[:, i, 2:3]), reads=[("st", i)], writes=[("st", i)])
                S.op("dve", lambda: nc.vector.scalar_tensor_tensor(out=hb[b], in0=xt, scalar=st[:, i, 3:4], in1=gB,
                                                                   op0=ALU.mult, op1=ALU.mult),
                     reads=[("xt", b), ("st", i), "gB"], writes=[("hb", b)])
                for k in range(8):
                    S.op("pe", lambda: nc.tensor.transpose(out=pT[:, k, :], in_=hb[b][:, k * 128:(k + 1) * 128], identity=self.identb),
                         reads=[("hb", b), "identb"], writes=["pT"])
                S.op("act", lambda: nc.scalar.copy(out=hT[b], in_=pT), reads=["pT"], writes=[("hT", b)])
                for cg in range(8):
                    c0 = cg * 512
                    cw = min(512, P_IN - c0)
                    pb = nps % 4
                    nps += 1
                    for k in range(8):
                        S.op("pe", lambda: nc.tensor.matmul(pss[pb][:, :cw], lhsT=hT[b][:, k, :], rhs=W[:, k, c0:c0 + cw],
                                                            start=(k == 0), stop=(k == 7)),
                             reads=[("hT", b), ("W", k)], writes=[("psA", pb)])
                    if cg % 2 == 0:
                        S.op("dve", lambda: nc.vector.tensor_copy(out=pts[b][:, c0:c0 + cw], in_=pss[pb][:, :cw]),
                             reads=[("psA", pb)], writes=[("pt", b)])
                    else:
                        S.op("act", lambda: nc.scalar.copy(out=pts[b][:, c0:c0 + cw], in_=pss[pb][:, :cw]),
                             reads=[("psA", pb)], writes=[("pt", b)])
                S.dma("sp", self.P[i * 128:(i + 1) * 128, :], pts[b], reads=[("pt", b)])
        S.barrier()

    def rstd_ops(self, ss, tmp, rs, inv_n, eps, r, w):
        nc, S = self.nc, self.S
        S.op("dve", lambda: nc.vector.tensor_scalar(out=tmp, in0=ss, scalar1=inv_n, scalar2=eps, op0=ALU.mult, op1=ALU.add), reads=r, writes=w)
        S.op("act", lambda: nc.scalar.activation(out=tmp, in_=tmp, func=AF.Sqrt), reads=w, writes=w)
        S.op("dve", lambda: nc.vector.reciprocal(out=rs, in_=tmp), reads=w, writes=w)

    def stage_attnB(self, l):
        nc, S = self.nc, self.S
        with ExitStack() as es:
            qT = self.sb(es, "qT", [128, 2, T], BF16)
            kT = self.sb(es, "kT", [128, T], BF16)
            Va = self.sb(es, "Va", [128, NT, 2, 65], BF16)
            gqk = self.sb(es, "gqk", [128, 6, 64], F32)
            fbs = [self.sb(es, "fb%d" % i, [128, 512], F32) for i in range(2)]
            css = [self.sb(es, "cs%d" % i, [128, 2, 64], F32) for i in range(2)]
            sq = self.sb(es, "sqB", [128, 384], F32)
            ss = self.sb(es, "ssB", [128, 6], F32)
            tm = self.sb(es, "tmB", [128, 6], F32)
            rs = self.sb(es, "rsB", [128, 6], F32)
            qn = self.sb(es, "qnB", [128, 6, 64], F32)
            t1 = self.sb(es, "t1B", [128, 6, 64], F32)
            t2 = self.sb(es, "t2B", [128, 6, 64], F32)
            qkb = self.sb(es, "qkb", [128, 6, 64], BF16)
            pTb = [self.sb(es, "pTb%d" % i, [128, 512], BF16) for i in range(2)]
            oT = self.sb(es, "oT", [65, 512], F32)
            rc = self.sb(es, "rcB", [128, 4, 1], F32)
            ybt = [self.sb(es, "ybt%d" % i, [128, 4, 64], F32) for i in range(2)]
            pT3 = self.ps(es, "pT3", [128, 3, 128], BF16)
            ps_s = [self.ps(es, "ps_s%d" % i, [128, 512], F32) for i in range(2)]
            ps_o = self.ps(es, "ps_o", [65, 512], F32)
            ps_t = self.ps(es, "ps_t", [128, 4, 65], F32)
            for h in range(6):
                src = self.attn_q_norm if h < 4 else self.attn_k_norm
                S.dma("sp", gqk[:, h, :], src[l, :].partition_broadcast(128), writes=["gqk"])
            S.op("pool", lambda: nc.gpsimd.memset(Va[:, :, :, 64:65], 1.0), writes=["Va"])
            for i in range(NT):
                b = i % 2
                fb = fbs[b]
                S.dma("sp", fb, self.P[i * 128:(i + 1) * 128, OFF_B:OFF_B + 512], writes=[("fb", b)])
                S.dma("sp", css[b][:, 0, :], self.rope_cos[i * 128:(i + 1) * 128, :], writes=[("cs", b)])
                S.dma("sp", css[b][:, 1, :], self.rope_sin[i * 128:(i + 1) * 128, :], writes=[("cs", b)])
                S.op("dve", lambda: nc.vector.tensor_tensor(out=sq, in0=fb[:, 0:384], in1=fb[:, 0:384], op=ALU.mult), reads=[("fb", b)], writes=["sqB"])
                S.op("dve", lambda: nc.vector.tensor_reduce(out=ss, in_=sq.rearrange("p (h d) -> p h d", d=64), op=ALU.add, axis=AX.X), reads=["sqB"], writes=["ssB"])
                self.rstd_ops(ss, tm, rs, 1.0 / 64, EPS, ["ssB"], ["rsB"])
                f3 = fb[:, 0:384].rearrange("p (h d) -> p h d", d=64)
                S.op("dve", lambda: nc.vector.tensor_tensor(out=qn, in0=f3, in1=rs.unsqueeze(2).to_broadcast([128, 6, 64]), op=ALU.mult),
                     reads=[("fb", b), "rsB"], writes=["qnB"])
                S.op("pool", lambda: nc.gpsimd.tensor_tensor(out=qn, in0=qn, in1=gqk, op=ALU.mult), reads=["qnB", "gqk"], writes=["qnB"])
                cosb = css[b][:, 0, :].unsqueeze(1).to_broadcast([128, 6, 64])
                S.op("dve", lambda: nc.vector.tensor_tensor(out=t1, in0=qn, in1=cosb, op=ALU.mult), reads=["qnB", ("cs", b)], writes=["t1B"])
                q5 = qn.rearrange("p h (a f d) -> p h a f d", a=2, f=2)
                t5 = t2.rearrange("p h (a f d) -> p h a f d", a=2, f=2)
                s5 = css[b][:, 1, :].rearrange("p (a f d) -> p a f d", a=2, f=2)
                for hf in range(2):
                    for a in range(2):
                        S.op("pool", lambda: nc.gpsimd.tensor_tensor(out=t5[:, :, a, hf, :], in0=q5[:, :, a, 1 - hf, :],
                                                                     in1=s5[:, a, hf, :].unsqueeze(1).to_broadcast([128, 6, 16]), op=ALU.mult),
                             reads=["qnB", ("cs", b)], writes=["t2B"])
                S.op("dve", lambda: nc.vector.tensor_tensor(out=qkb[:, 0:4, :].rearrange("p (b a) d -> p a b d", b=2, a=2),
                                                            in0=t1[:, 0:4, :].rearrange("p (a b) d -> p a b d", a=2, b=2),
                                                            in1=t2[:, 0:4, :].rearrange("p (a b) d -> p a b d", a=2, b=2), op=ALU.add),
                     reads=["t1B", "t2B"], writes=["qkb"])
                S.op("dve", lambda: nc.vector.tensor_tensor(out=qkb[:, 4:6, :], in0=t1[:, 4:6, :], in1=t2[:, 4:6, :], op=ALU.add),
                     reads=["t1B", "t2B"], writes=["qkb"])
                S.op("act", lambda: nc.scalar.copy(out=Va[:, i, :, 0:64], in_=fb[:, 384:512].rearrange("p (h d) -> p h d", d=64)),
                     reads=[("fb", b)], writes=["Va"])
                for c in range(3):
                    S.op("pe", lambda: nc.tensor.transpose(out=pT3[:, c, :], in_=qkb[:, 2 * c:2 * c + 2, :], identity=self.identb),
                         reads=["qkb", "identb"], writes=["pT3"])
                S.op("act", lambda: nc.scalar.copy(out=qT[:, :, i * 128:(i + 1) * 128], in_=pT3[:, 0:2, :]), reads=["pT3"], writes=["qT"])
                S.op("act", lambda: nc.scalar.copy(out=kT[:, i * 128:(i + 1) * 128], in_=pT3[:, 2, :]), reads=["pT3"], writes=["kT"])
            n = 0
            for qh in range(4):
                kv = qh // 2
                base = 64 * kv
                ch = qh % 2
                for qc in range(8):
                    def s_mm(kb_):
                        pb_ = kb_ % 2
                        S.op("pe", lambda: nc.tensor.matmul(ps_s[pb_], lhsT=kT[base:base + 64, kb_ * 128:(kb_ + 1) * 128],
                                                            rhs=qT[base:base + 64, ch, qc * 512:(qc + 1) * 512], start=True, stop=True),
                             reads=["kT", "qT"], writes=[("ps_s", pb_)])
                    s_mm(0)
                    for kb in range(NT):
                        pb = kb % 2
                        if kb + 1 < NT:
                            s_mm(kb + 1)
                        S.op("act", lambda: nc.scalar.activation(out=pTb[pb], in_=ps_s[pb], func=AF.Exp, scale=0.125),
                             reads=[("ps_s", pb)], writes=[("pTb", pb)])
                        S.op("pe", lambda: nc.tensor.matmul(ps_o, lhsT=Va[:, kb, kv, :], rhs=pTb[pb], start=(kb == 0), stop=(kb == NT - 1)),
                             reads=["Va", ("pTb", pb)], writes=["ps_o"])
                    S.op("dve", lambda: nc.vector.tensor_copy(out=oT, in_=ps_o), reads=["ps_o"], writes=["oT"])
                    for j in range(4):
                        S.op("pe", lambda: nc.tensor.transpose(out=ps_t[:, j, :], in_=oT[:, j * 128:(j + 1) * 128], identity=self.identf[0:65, 0:65]),
                             reads=["oT", "identf"], writes=["ps_t"])
                    S.op("dve", lambda: nc.vector.reciprocal(out=rc, in_=ps_t[:, :, 64:65]), reads=["ps_t"], writes=["rcB"])
                    yb = ybt[qc % 2]
                    S.op("dve", lambda: nc.vector.tensor_tensor(out=yb, in0=ps_t[:, :, 0:64], in1=rc.to_broadcast([128, 4, 64]), op=ALU.mult),
                         reads=["ps_t", "rcB"], writes=[("ybt", qc % 2)])
                    S.dma("sp", self.mixed[qc * 512:(qc + 1) * 512, 256 + qh * 64:256 + (qh + 1) * 64].rearrange("(j p) d -> p j d", p=128),
                          yb, reads=[("ybt", qc % 2)])
        S.barrier()

    def stage_attnD(self, l):
        nc, S = self.nc, self.S
        with ExitStack() as es:
            dB = self.sb(es, "dB", [128, 6, 3, 128], F32)
            qTd = self.sb(es, "qTd", [128, T], BF16)
            kTd = self.sb(es, "kTd", [128, T], BF16)
            Vd = self.sb(es, "Vd", [128, NT, 2, 65], BF16)
            fds = [self.sb(es, "fd%d" % i, [128, 3, 128], F32) for i in range(2)]
            qkd = self.sb(es, "qkd", [128, 2, 128], BF16)
            sd = [self.sb(es, "sd%d" % i, [128, 3, 128], F32) for i in range(2)]
            pd = [self.sb(es, "pd%d" % i, [128, 3, 128], BF16) for i in range(2)]
            od = [self.sb(es, "od%d" % i, [128, 2, 65], F32) for i in range(2)]
            dn = [self.sb(es, "dn%d" % i, [128, 6, 65], F32) for i in range(2)]
            zs = self.sb(es, "zsD", [128, 2], F32)
            rz = self.sb(es, "rzD", [128, 2], F32)
            yd = [self.sb(es, "yd%d" % i, [128, 6, 64], F32) for i in range(2)]
            pTd = self.ps(es, "pTd", [128, 2, 128], BF16)
            ps_sd = [self.ps(es, "ps_sd%d" % i, [128, 3, 128], F32) for i in range(2)]
            ps_od = [self.ps(es, "ps_od%d" % i, [128, 2, 65], F32) for i in range(2)]
            for br in range(3):
                for j in range(2):
                    S.dma("sp", dB[:, br * 2 + j, :, :], self.dbias[br, j], writes=["dB"])
            S.op("pool", lambda: nc.gpsimd.memset(Vd[:, :, :, 64:65], 1.0), writes=["Vd"])
            Pd = self.P[:, OFF_D:OFF_D + D_IN].rearrange("r (t x) -> r t x", t=3)
            for br, dil in enumerate((1, 4, 16)):
                nb = NT // dil
                Pv = Pd.rearrange("(n m d) t x -> d n m t x", m=128, d=dil)
                Dv = self.Dnum.rearrange("(n m d) s c -> d n m s c", m=128, d=dil)
                for r in range(dil):
                    for b in range(nb):
                        ti = r * nb + b
                        fb = ti % 2
                        S.dma("sp", fds[fb], Pv[r, b][:, :, br * 128:(br + 1) * 128], writes=[("fd", fb)])
                        S.op("act", lambda: nc.scalar.copy(out=qkd, in_=fds[fb][:, 0:2, :]), reads=[("fd", fb)], writes=["qkd"])
                        S.op("pool", lambda: nc.gpsimd.tensor_copy(out=Vd[:, ti, :, 0:64], in_=fds[fb][:, 2, :].rearrange("p (h d) -> p h d", d=64)),
                             reads=[("fd", fb)], writes=["Vd"])
                        for c in range(2):
                            S.op("pe", lambda: nc.tensor.transpose(out=pTd[:, c, :], in_=qkd[:, c, :], identity=self.identb),
                                 reads=["qkd", "identb"], writes=["pTd"])
                        S.op("dve", lambda: nc.vector.tensor_copy(out=qTd[:, ti * 128:(ti + 1) * 128], in_=pTd[:, 0, :]), reads=["pTd"], writes=["qTd"])
                        S.op("dve", lambda: nc.vector.tensor_copy(out=kTd[:, ti * 128:(ti + 1) * 128], in_=pTd[:, 1, :]), reads=["pTd"], writes=["kTd"])
                for r in range(dil):
                    for b in range(nb):
                        ti = r * nb + b
                        ob = ti % 2
                        for j in range(2):
                            base = 64 * j
                            rels = [rel for rel in range(3) if 0 <= b + rel - 1 < nb]
                            r0, r1 = rels[0], rels[-1] + 1
                            for rel in rels:
                                kt = ti + rel - 1
                                S.op("pe", lambda: nc.tensor.matmul(ps_sd[j][:, rel, :], lhsT=kTd[base:base + 64, kt * 128:(kt + 1) * 128],
                                                                    rhs=qTd[base:base + 64, ti * 128:(ti + 1) * 128], start=True, stop=True),
                                     reads=["kTd", "qTd"], writes=[("ps_sd", j)])
                            S.op("dve", lambda: nc.vector.scalar_tensor_tensor(out=sd[j][:, r0:r1, :], in0=ps_sd[j][:, r0:r1, :], scalar=0.125,
                                                                               in1=dB[:, br * 2 + j, r0:r1, :], op0=ALU.mult, op1=ALU.add),
                                 reads=[("ps_sd", j), "dB"], writes=[("sd", j)])
                            S.op("act", lambda: nc.scalar.activation(out=pd[j][:, r0:r1, :], in_=sd[j][:, r0:r1, :], func=AF.Exp),
                                 reads=[("sd", j)], writes=[("pd", j)])
                            for rel in rels:
                                kt = ti + rel - 1
                                S.op("pe", lambda: nc.tensor.matmul(ps_od[ob][:, j, :], lhsT=pd[j][:, rel, :], rhs=Vd[:, kt, j, :],
                                                                    start=(rel == rels[0]), stop=(rel == rels[-1])),
                                     reads=[("pd", j), "Vd"], writes=[("ps_od", ob)])
                        S.op("dve", lambda: nc.vector.tensor_copy(out=od[ob], in_=ps_od[ob]), reads=[("ps_od", ob)], writes=[("od", ob)])
                        S.dma("sp", Dv[r, b][:, br * 2:br * 2 + 2, :], od[ob], reads=[("od", ob)])
            S.barrier()
            for i in range(NT):
                b = i % 2
                S.dma("sp", dn[b], self.Dnum[i * 128:(i + 1) * 128], writes=[("dn", b)])
                z3 = dn[b][:, :, 64].rearrange("p (r j) -> p r j", j=2)
                S.op("dve", lambda: nc.vector.tensor_tensor(out=zs, in0=z3[:, 0, :], in1=z3[:, 1, :], op=ALU.add), reads=[("dn", b)], writes=["zsD"])
                S.op("dve", lambda: nc.vector.tensor_tensor(out=zs, in0=zs, in1=z3[:, 2, :], op=ALU.add), reads=[("dn", b), "zsD"], writes=["zsD"])
                S.op("dve", lambda: nc.vector.reciprocal(out=rz, in_=zs), reads=["zsD"], writes=["rzD"])
                for br in range(3):
                    S.op("dve", lambda: nc.vector.tensor_tensor(out=yd[b][:, br * 2:br * 2 + 2, :], in0=dn[b][:, br * 2:br * 2 + 2, 0:64],
                                                                in1=rz.unsqueeze(2).to_broadcast([128, 2, 64]), op=ALU.mult),
                         reads=[("dn", b), "rzD"], writes=[("yd", b)])
                S.dma("sp", self.mixed[i * 128:(i + 1) * 128, 768:1152], yd[b], reads=[("yd", b)])
        S.barrier()

    def stage_outproj(self, l, xsrc):
        nc, S = self.nc, self.S
        with ExitStack() as es:
            Wo = self.sb(es, "Wo", [128, 9, D], BF16)
            g2 = self.sb(es, "g2B", [128, D], F32)
            Wr = self.sb(es, "Wr", [128, 8, NE], F32)
            rbB = self.sb(es, "rbB", [128, NE], F32)
            mts = [self.sb(es, "mt%d" % i, [128, MIXW], F32) for i in range(2)]
            mb = self.sb(es, "mb", [128, MIXW], BF16)
            mT = self.sb(es, "mT", [128, 9, 128], BF16)
            xts = [self.sb(es, "xo%d" % i, [128, D], F32) for i in range(2)]
            x2s = [self.sb(es, "x2t%d" % i, [128, D], F32) for i in range(2)]
            junk = self.sb(es, "junkO", [128, D], F32)
            st = self.sb(es, "stO", [128, NT, 4], F32)
            h2f = self.sb(es, "h2f", [128, D], F32)
            h2b = [self.sb(es, "h2b%d" % i, [128, D], BF16) for i in range(2)]
            h2T = self.sb(es, "h2T", [128, 8, 128], F32)
            lg = self.sb(es, "lgO", [128, NE], F32)
            mx = self.sb(es, "mxO", [128, 4], F32)
            ex = self.sb(es, "exO", [128, NE], F32)
            pTa = self.ps(es, "pTa", [128, 8, 128], BF16)
            pTc = self.ps(es, "pTc", [128, 1, 128], BF16)
            pso = [self.ps(es, "pso%d" % i, [128, 512], F32) for i in range(2)]
            pT4 = [self.ps(es, "pT4%d" % i, [128, 4, 128], F32) for i in range(2)]
            psr = self.ps(es, "psr", [128, NE], F32)
            wv = self.w_out[l].rearrange("(k p) c -> p k c", p=128)
            for k in range(9):
                S.dma("pool", Wo[:, k, :], wv[:, k, :], writes=["Wo"])
            S.dma("sp", g2, self.norm_ffn_g[l, :].partition_broadcast(128), writes=["g2B"])
            S.dma("sp", Wr, self.router_w[l].rearrange("(k p) e -> p k e", p=128), writes=["Wr"])
            S.dma("sp", rbB, self.router_b[l, :].partition_broadcast(128), writes=["rbB"])
            for i in range(NT):
                b = i % 2
                rows = slice(i * 128, (i + 1) * 128)
                S.dma("sp", mts[b], self.mixed[rows, :], writes=[("mt", b)])
                S.dma("sp", xts[b], xsrc[rows, :], writes=[("xo", b)])
                S.op("act", lambda: nc.scalar.copy(out=mb, in_=mts[b]), reads=[("mt", b)], writes=["mb"])
                for k in range(9):
                    dst = pTa[:, k, :] if k < 8 else pTc[:, 0, :]
                    S.op("pe", lambda: nc.tensor.transpose(out=dst, in_=mb[:, k * 128:(k + 1) * 128], identity=self.identb),
                         reads=["mb", "identb"], writes=["pTa" if k < 8 else "pTc"])
                S.op("act", lambda: nc.scalar.copy(out=mT[:, 0:8, :], in_=pTa), reads=["pTa"], writes=["mT"])
                S.op("dve", lambda: nc.vector.tensor_copy(out=mT[:, 8:9, :], in_=pTc), reads=["pTc"], writes=["mT"])
                x2 = x2s[b]
                for hf in range(2):
                    for k in range(9):
                        S.op("pe", lambda: nc.tensor.matmul(pso[hf], lhsT=mT[:, k, :], rhs=Wo[:, k, hf * 512:(hf + 1) * 512], start=(k == 0), stop=(k == 8)),
                             reads=["mT", "Wo"], writes=[("pso", hf)])
                    S.op("dve", lambda: nc.vector.tensor_tensor(out=x2[:, hf * 512:(hf + 1) * 512], in0=pso[hf], in1=xts[b][:, hf * 512:(hf + 1) * 512], op=ALU.add),
                         reads=[("pso", hf), ("xo", b)], writes=[("x2t", b)])
                S.dma("sp", self.X2[rows, :], x2, reads=[("x2t", b)])
                S.dma("sp", self.X3[rows, :], x2, reads=[("x2t", b)])
                S.op("act", lambda: nc.scalar.activation(out=junk, in_=x2, func=AF.Square, accum_out=st[:, i, 0:1]), reads=[("x2t", b)], writes=["junkO", ("stO", i)])
                self.rstd_ops(st[:, i, 0:1], st[:, i, 1:2], st[:, i, 2:3], 1.0 / D, EPS, [("stO", i)], [("stO", i)])
                S.op("dve", lambda: nc.vector.scalar_tensor_tensor(out=h2f, in0=x2, scalar=st[:, i, 2:3], in1=g2, op0=ALU.mult, op1=ALU.mult),
                     reads=[("x2t", b), ("stO", i), "g2B"], writes=["h2f"])
                S.op("act", lambda: nc.scalar.copy(out=h2b[b], in_=h2f), reads=["h2f"], writes=[("h2b", b)])
                S.dma("sp", self.H2[rows, :], h2b[b], reads=[("h2b", b)])
                for k in range(8):
                    S.op("pe", lambda: nc.tensor.transpose(out=pT4[k // 4][:, k % 4, :], in_=h2f[:, k * 128:(k + 1) * 128], identity=self.identf),
                         reads=["h2f", "identf"], writes=[("pT4", k // 4)])
                S.op("act", lambda: nc.scalar.copy(out=h2T[:, 0:4, :], in_=pT4[0]), reads=[("pT4", 0)], writes=["h2T"])
                S.op("dve", lambda: nc.vector.tensor_copy(out=h2T[:, 4:8, :], in_=pT4[1]), reads=[("pT4", 1)], writes=["h2T"])
                for k in range(8):
                    S.op("pe", lambda: nc.tensor.matmul(psr, lhsT=h2T[:, k, :], rhs=Wr[:, k, :], start=(k == 0), stop=(k == 7)),
                         reads=["h2T", "Wr"], writes=["psr"])
                S.op("dve", lambda: nc.vector.tensor_tensor(out=lg, in0=psr, in1=rbB, op=ALU.add), reads=["psr", "rbB"], writes=["lgO"])
                S.op("dve", lambda: nc.vector.tensor_reduce(out=mx[:, 0:1], in_=lg, op=ALU.max, axis=AX.X), reads=["lgO"], writes=["mxO"])
                S.op("dve", lambda: nc.vector.tensor_scalar(out=lg, in0=lg, scalar1=mx[:, 0:1], scalar2=None, op0=ALU.subtract), reads=["lgO", "mxO"], writes=["lgO"])
                S.op("act", lambda: nc.scalar.activation(out=ex, in_=lg, func=AF.Exp), reads=["lgO"], writes=["exO"])
                S.op("dve", lambda: nc.vector.tensor_reduce(out=mx[:, 2:3], in_=ex, op=ALU.add, axis=AX.X), reads=["exO"], writes=["mxO"])
                S.op("dve", lambda: nc.vector.reciprocal(out=mx[:, 3:4], in_=mx[:, 2:3]), reads=["mxO"], writes=["mxO"])
                S.op("dve", lambda: nc.vector.tensor_scalar(out=self.AFF[:, i, :], in0=ex, scalar1=mx[:, 3:4], scalar2=None, op0=ALU.mult),
                     reads=["exO", "mxO"], writes=["AFF"])
                S.dma("sp", self.AFFd[rows, :], self.AFF[:, i, :], reads=["AFF"])
        S.barrier()

    def stage_moe(self, l):
        nc, S = self.nc, self.S
        AFF = self.AFF
        with ExitStack() as es:
            IDX = self.sb(es, "IDX", [128, NE, 4], U32)
            es1 = ExitStack()
            es_outer = es
            es = es1
            lo = self.sb(es, "loM", [128, NE], F32)
            mid = self.sb(es, "midM", [128, NE], F32)
            cmp_ = self.sb(es, "cmpM", [128, NT, NE], F32)
            pc = self.sb(es, "pcM", [128, NE], F32)
            ge = self.sb(es, "geM", [128, NE], F32)
            onesb = self.sb(es, "onesb", [128, 128], BF16)
            onesf = self.sb(es, "onesf", [128, 128], F32)
            Ub = self.sb(es, "Ub", [128, 128], BF16)
            selb = self.sb(es, "selb", [128, NT, NE], BF16)
            Uf = self.sb(es, "Uf", [128, 128], F32)
            self_f = self.sb(es, "self", [128, NT, NE], F32)
            tot = [self.sb(es, "totM%d" % i, [128, NT, NE], F32) for i in range(2)]
            tot0 = self.sb(es, "tot0", [128, NT, NE], F32)
            rank = self.sb(es, "rankM", [128, NT, NE], F32)
            iotaC = self.sb(es, "iotaC", [128, CAP], F32)
            tvf = self.sb(es, "tvf", [128, NT, 2], F32)
            oh = [self.sb(es, "oh%d" % i, [128, CAP], F32) for i in range(3)]
            rws = self.sb(es, "rws", [2, CAP], F32)
            rwu = self.sb(es, "rwu", [2, CAP], U32)
            IDXf = self.sb(es, "IDXf", [128, NE, 4], F32)
            psc = self.ps(es, "psc", [128, NE], F32)
            psp = self.ps(es, "psp", [128, 512], F32)
            pst = self.ps(es, "pst", [128, 512], F32)
            psid = [self.ps(es, "psid%d" % i, [128, NE, 2], F32) for i in range(4)]
            IDX2 = self.sb(es, "IDX2", [128, NE, 4, 2], F32)
            S.dma("sp", Uf, self.ustrict, writes=["Uf"])
            S.op("dve", lambda: nc.vector.tensor_copy(out=Ub, in_=Uf), reads=["Uf"], writes=["Ub"])
            S.op("dve", lambda: nc.vector.memset(onesb, 1.0), writes=["onesb"])
            S.op("dve", lambda: nc.vector.memset(onesf, 1.0), writes=["onesf"])
            S.dma("sp", iotaC, self.iota_c, writes=["iotaC"])
            S.dma("sp", tvf, self.tvals, writes=["tvf"])
            S.op("dve", lambda: nc.vector.memset(lo, 0.0), writes=["lo"])
            for it in range(32):
                c = 2.0 ** -(it + 1)
                S.op("dve", lambda: nc.vector.tensor_scalar(out=mid, in0=lo, scalar1=c, scalar2=None, op0=ALU.add), reads=["lo"], writes=["mid"])
                S.op("dve", lambda: nc.vector.tensor_tensor(out=cmp_, in0=AFF, in1=mid.unsqueeze(1).to_broadcast([128, NT, NE]), op=ALU.is_ge),
                     reads=["AFF", "mid"], writes=["cmp"])
                S.op("dve", lambda: nc.vector.tensor_reduce(out=pc, in_=cmp_.rearrange("p t e -> p e t"), op=ALU.add, axis=AX.X), reads=["cmp"], writes=["pc"])
                S.op("pe", lambda: nc.tensor.matmul(psc, lhsT=onesf, rhs=pc, start=True, stop=True), reads=["onesf", "pc"], writes=["psc"])
                S.op("dve", lambda: nc.vector.tensor_single_scalar(out=ge, in_=psc, scalar=CAP - 0.5, op=ALU.is_ge), reads=["psc"], writes=["ge"])
                S.op("dve", lambda: nc.vector.scalar_tensor_tensor(out=lo, in0=ge, scalar=c, in1=lo, op0=ALU.mult, op1=ALU.add), reads=["ge", "lo"], writes=["lo"])
            import os
            if os.environ.get("MOE_STOP") == "1":
                S.dma("sp", self.dbg_small[:, 64:80], lo, reads=["lo"])
                S.barrier()
                es1.close()
                return
            S.op("dve", lambda: nc.vector.tensor_tensor(out=self_f, in0=AFF, in1=lo.unsqueeze(1).to_broadcast([128, NT, NE]), op=ALU.is_ge),
                 reads=["AFF", "lo"], writes=["self"])
            S.op("dve", lambda: nc.vector.tensor_tensor(out=selb, in0=AFF, in1=lo.unsqueeze(1).to_broadcast([128, NT, NE]), op=ALU.is_ge),
                 reads=["AFF", "lo"], writes=["selb"])
            sel2 = selb.rearrange("p t e -> p (t e)")
            S.op("pe", lambda: nc.tensor.matmul(psp, lhsT=Ub, rhs=sel2, start=True, stop=True), reads=["Ub", "selb"], writes=["psp"])
            S.op("pe", lambda: nc.tensor.matmul(pst, lhsT=onesb, rhs=sel2, start=True, stop=True), reads=["onesb", "selb"], writes=["pst"])
            S.op("dve", lambda: nc.vector.tensor_copy(out=tot0.rearrange("p t e -> p (t e)"), in_=pst), reads=["pst"], writes=["tot0"])
            S.op("dve", lambda: nc.vector.tensor_copy(out=tot[0].rearrange("p t e -> p (t e)"), in_=pst), reads=["pst"], writes=[("tot", 0)])
            if os.environ.get("MOE_STOP") == "1b":
                S.dma("sp", self.dbg_small[:, 0:16], tot[0][:, 5, :], reads=[("tot", 0)])
                S.barrier()
                es1.close()
                return
            cur = 0
            for sft in (1, 2, 4, 8, 16):
                a, bb = tot[cur], tot[1 - cur]
                S.op("pool", lambda: nc.gpsimd.tensor_copy(out=bb[:, 0:sft, :], in_=a[:, 0:sft, :]), reads=[("tot", cur)], writes=[("tot", 1 - cur)])
                S.op("dve", lambda: nc.vector.tensor_tensor(out=bb[:, sft:NT, :], in0=a[:, sft:NT, :], in1=a[:, 0:NT - sft, :], op=ALU.add),
                     reads=[("tot", cur)], writes=[("tot", 1 - cur)])
                cur = 1 - cur
            inc = tot[cur]
            if os.environ.get("MOE_STOP") == "1c":
                S.dma("sp", self.dbg_small[:, 0:16], inc[:, 5, :], reads=[("tot", cur)])
                S.barrier()
                es1.close()
                return
            S.op("dve", lambda: nc.vector.tensor_tensor(out=rank, in0=inc, in1=tot0, op=ALU.subtract), reads=[("tot", cur), "tot0"], writes=["rank"])
            S.op("dve", lambda: nc.vector.tensor_tensor(out=rank.rearrange("p t e -> p (t e)"), in0=rank.rearrange("p t e -> p (t e)"), in1=psp, op=ALU.add),
                 reads=["rank", "psp"], writes=["rank"])
            S.op("dve", lambda: nc.vector.scalar_tensor_tensor(out=rank, in0=rank, scalar=-9999.0, in1=self_f, op0=ALU.add, op1=ALU.mult),
                 reads=["rank", "self"], writes=["rank"])
            S.op("dve", lambda: nc.vector.tensor_scalar(out=rank, in0=rank, scalar1=9999.0, scalar2=None, op0=ALU.add), reads=["rank"], writes=["rank"])
            if os.environ.get("MOE_STOP") == "2":
                S.dma("sp", self.dbg_small[:, 0:16], rank[:, 5, :], reads=["rank"])
                S.barrier()
                es1.close()
                return
            n = 0
            for e in range(NE):
                for i in range(NT):
                    ob = n % 3
                    n += 1
                    S.op("dve", lambda: nc.vector.tensor_scalar(out=oh[ob], in0=iotaC, scalar1=rank[:, i, e:e + 1], scalar2=None, op0=ALU.is_equal),
                         reads=["iotaC", "rank"], writes=[("oh", ob)])
                    for cc in range(4):
                        S.op("pe", lambda: nc.tensor.matmul(psid[cc][:, e, :], lhsT=oh[ob][:, cc * 128:(cc + 1) * 128], rhs=tvf[:, i, :],
                                                            start=(i == 0), stop=(i == NT - 1)),
                             reads=["tvf", ("oh", ob)], writes=[("psid", cc)])
            for cc in range(4):
                S.op("dve", lambda: nc.vector.tensor_tensor(out=IDXf[:, :, cc], in0=psid[cc][:, :, 0], in1=psid[cc][:, :, 1], op=ALU.add) if False else
                     nc.vector.tensor_copy(out=IDX2[:, :, cc, :], in_=psid[cc]), reads=[("psid", cc)], writes=["IDX2"])
            S.op("dve", lambda: nc.vector.tensor_tensor(out=IDXf, in0=IDX2[:, :, :, 0], in1=IDX2[:, :, :, 1], op=ALU.add), reads=["IDX2"], writes=["IDXf"])
            S.op("dve", lambda: nc.vector.tensor_copy(out=IDX, in_=IDXf), reads=["IDXf"], writes=["IDX"])
            if self.stop_after == "moe_idx":
                S.dma("sp", self.dbg_small[:, 0:64], IDXf.rearrange("p e c -> p (e c)"), reads=["IDXf"])
                S.dma("sp", self.dbg_small[:, 64:80], lo, reads=["lo"])
                S.barrier()
                es1.close()
                return
            S.barrier()
            es1.close()
            es = es_outer
            Wg = [self.sb(es, "Wg%d" % i, [128, 8, D], BF16) for i in range(2)]
            Wu = [self.sb(es, "Wu%d" % i, [128, 8, D], BF16) for i in range(2)]
            Wd = [self.sb(es, "Wd%d" % i, [128, 8, D], BF16) for i in range(2)]
            xs = [self.sb(es, "xs%d" % i, [128, 4, D], BF16) for i in range(2)]
            gt = [self.sb(es, "gt%d" % i, [128, 4, NE], F32) for i in range(2)]
            xsT = self.sb(es, "xsT", [128, 8, CAP], BF16)
            sg = [self.sb(es, "sg%d" % i, [128, CAP], F32) for i in range(2)]
            hid = self.sb(es, "hid", [128, 8, CAP], BF16)
            yt = [self.sb(es, "yt%d" % i, [128, D], F32) for i in range(2)]
            psx = self.ps(es, "psx", [128, 8, 128], BF16)
            psg = self.ps(es, "psg", [128, CAP], F32)
            psu = self.ps(es, "psu", [128, CAP], F32)
            psy = [self.ps(es, "psy%d" % i, [128, 512], F32) for i in range(2)]

            def load_w(e):
                wb = e % 2
                for (dst, src, nm) in ((Wg[wb], self.w_gate, "Wg"), (Wu[wb], self.w_up, "Wu"), (Wd[wb], self.w_down, "Wd")):
                    sv = src[l, e].rearrange("(k p) f -> p k f", p=128)
                    for k0 in (0, 4):
                        S.dma("pool", dst[:, k0:k0 + 4, :], sv[:, k0:k0 + 4, :], writes=[(nm, wb)])

            def gather(e):
                wb = e % 2
                for cc in range(4):
                    S.dma_raw("pool", lambda: nc.gpsimd.indirect_dma_start(out=xs[wb][:, cc, :], out_offset=None, in_=self.H2,
                                                                           in_offset=bass.IndirectOffsetOnAxis(ap=IDX[:, e, cc:cc + 1], axis=0)),
                              reads=["IDX"], writes=[("xs", wb)])
                    S.dma_raw("pool", lambda: nc.gpsimd.indirect_dma_start(out=gt[wb][:, cc, :], out_offset=None, in_=self.AFFd,
                                                                           in_offset=bass.IndirectOffsetOnAxis(ap=IDX[:, e, cc:cc + 1], axis=0)),
                              reads=["IDX"], writes=[("gt", wb)])

            load_w(0)
            gather(0)
            ny = 0
            for e in range(NE):
                wb = e % 2
                if e + 1 < NE:
                    load_w(e + 1)
                    gather(e + 1)
                for cc in range(4):
                    for k in range(8):
                        S.op("pe", lambda: nc.tensor.transpose(out=psx[:, k, :], in_=xs[wb][:, cc, k * 128:(k + 1) * 128], identity=self.identb),
                             reads=[("xs", wb), "identb"], writes=["psx"])
                    S.op("act", lambda: nc.scalar.copy(out=xsT[:, :, cc * 128:(cc + 1) * 128], in_=psx), reads=["psx"], writes=["xsT"])
                for f in range(8):
                    for k in range(8):
                        S.op("pe", lambda: nc.tensor.matmul(psg, lhsT=Wg[wb][:, k, f * 128:(f + 1) * 128], rhs=xsT[:, k, :], start=(k == 0), stop=(k == 7)),
                             reads=[("Wg", wb), "xsT"], writes=["psg"])
                    for k in range(8):
                        S.op("pe", lambda: nc.tensor.matmul(psu, lhsT=Wu[wb][:, k, f * 128:(f + 1) * 128], rhs=xsT[:, k, :], start=(k == 0), stop=(k == 7)),
                             reads=[("Wu", wb), "xsT"], writes=["psu"])
                    S.op("act", lambda: nc.scalar.activation(out=sg[f % 2], in_=psg, func=AF.Silu), reads=["psg"], writes=[("sg", f % 2)])
                    S.op("dve", lambda: nc.vector.tensor_tensor(out=hid[:, f, :], in0=sg[f % 2], in1=psu, op=ALU.mult), reads=[("sg", f % 2), "psu"], writes=["hid"])
                for cc in range(4):
                    yb = ny % 2
                    ny += 1
                    for hf in range(2):
                        for f in range(8):
                            S.op("pe", lambda: nc.tensor.matmul(psy[hf], lhsT=hid[:, f, cc * 128:(cc + 1) * 128], rhs=Wd[wb][:, f, hf * 512:(hf + 1) * 512],
                                                                start=(f == 0), stop=(f == 7)), reads=["hid", ("Wd", wb)], writes=[("psy", hf)])
                        S.op("dve", lambda: nc.vector.tensor_scalar(out=yt[yb][:, hf * 512:(hf + 1) * 512], in0=psy[hf], scalar1=gt[wb][:, cc, e:e + 1], scalar2=None, op0=ALU.mult),
                             reads=[("psy", hf), ("gt", wb)], writes=[("yt", yb)])
                    S.dma_raw("pool", lambda: nc.gpsimd.indirect_dma_start(out=self.X3, out_offset=bass.IndirectOffsetOnAxis(ap=IDX[:, e, cc:cc + 1], axis=0),
                                                                           in_=yt[yb], in_offset=None, compute_op=ALU.add),
                              reads=[("yt", yb), "IDX", "X3"], writes=["X3"])
        S.barrier()

    def _e(self, eng):
        return self.S.eng[eng]

    def tt(self, eng, out, a, b, op):
        self.S.op(eng, lambda: self._e(eng).tensor_tensor(out=out.ap, in0=a.ap, in1=b.ap, op=op), reads=[a.key, b.key], writes=[out.key])

    def ts(self, eng, out, a, s1, op0, s2=None, op1=None):
        rd = [a.key]
        v1 = s1
        if isinstance(s1, TB):
            rd.append(s1.key)
            v1 = s1.ap
        kw = {}
        if op1 is not None:
            kw["op1"] = op1
        self.S.op(eng, lambda: self._e(eng).tensor_scalar(out=out.ap, in0=a.ap, scalar1=v1, scalar2=s2, op0=op0, **kw), reads=rd, writes=[out.key])

    def stt(self, out, a, scalar, b, op0, op1):
        rd = [a.key, b.key]
        sv = scalar
        if isinstance(scalar, TB):
            rd.append(scalar.key)
            sv = scalar.ap
        self.S.op("dve", lambda: self.nc.vector.scalar_tensor_tensor(out=out.ap, in0=a.ap, scalar=sv, in1=b.ap, op0=op0, op1=op1), reads=rd, writes=[out.key])

    def act(self, out, a, func, scale=1.0, bias=None):
        rd = [a.key]
        kw = {}
        if isinstance(bias, TB):
            rd.append(bias.key)
            kw["bias"] = bias.ap
        elif bias is not None:
            kw["bias"] = bias
        self.S.op("act", lambda: self.nc.scalar.activation(out=out.ap, in_=a.ap, func=func, scale=scale, **kw), reads=rd, writes=[out.key])

    def cp(self, eng, out, a):
        if eng == "act":
            self.S.op("act", lambda: self.nc.scalar.copy(out=out.ap, in_=a.ap), reads=[a.key], writes=[out.key])
        else:
            self.S.op(eng, lambda: self._e(eng).tensor_copy(out=out.ap, in_=a.ap), reads=[a.key], writes=[out.key])

    def red(self, out, a, op=None):
        self.S.op("dve", lambda: self.nc.vector.tensor_reduce(out=out.ap, in_=a.ap, op=(op or ALU.add), axis=AX.X), reads=[a.key], writes=[out.key])

    def rcp(self, out, a):
        self.S.op("dve", lambda: self.nc.vector.reciprocal(out=out.ap, in_=a.ap), reads=[a.key], writes=[out.key])

    def mm(self, out, lhsT, rhs, start=True, stop=True):
        self.S.op("pe", lambda: self.nc.tensor.matmul(out.ap, lhsT=lhsT.ap, rhs=rhs.ap, start=start, stop=stop), reads=[lhsT.key, rhs.key], writes=[out.key])

    def tp(self, out, a, ident):
        self.S.op("pe", lambda: self.nc.tensor.transpose(out=out.ap, in_=a.ap, identity=ident.ap), reads=[a.key, ident.key], writes=[out.key])

    def ld(self, out, src, q="sp"):
        self.S.dma(q, out.ap, src, writes=[out.key])

    def st_(self, dst, a, q="sp"):
        self.S.dma(q, dst, a.ap, reads=[a.key])

    def tb(self, es, name, shape, dtype=F32):
        return TB(self.sb(es, name, shape, dtype), name)

    def rstd(self, out, ss, tmp, inv_n, eps):
        self.ts("dve", tmp, ss, inv_n, ALU.mult, eps, ALU.add)
        self.act(tmp, tmp, AF.Sqrt)
        self.rcp(out, tmp)

    def bank(self):
        b = self.banks[self.nbank % 8]
        self.nbank += 1
        return b

    def bcast_row(self, es, name, src_row, n):
        t = self.tb(es, name, [128, n])
        self.ld(t, src_row.partition_broadcast(128))
        return t

    def stage_prepA(self, l):
        nc, S = self.nc, self.S
        with ExitStack() as es:
            self.banks = [TB(self.ps(es, "bk%d" % i, [128, 512], F32), ("bk", i)) for i in range(8)]
            self.nbank = 0
            identf = TB(self.identf, "identf")
            mpB = self.bcast_row(es, "mpB", self.rwkv_mu_prev[l, :], 1024)
            mnB = self.bcast_row(es, "mnB", self.rwkv_mu_next[l, :], 1024)
            c0B = self.tb(es, "c0B", [128, 1024])
            self.tt("dve", c0B, mpB, mnB, ALU.add)
            self.ts("dve", c0B, c0B, -1.0, ALU.mult, 1.0, ALU.add)
            kkB = self.bcast_row(es, "kkB", self.rwkv_k_k[l, :], 256)
            kaB = self.bcast_row(es, "kaB", self.rwkv_k_a[l, :], 256)
            omk = self.tb(es, "omk", [128, 256])
            self.ts("dve", omk, kaB, -1.0, ALU.mult, 1.0, ALU.add)
            rkB = self.bcast_row(es, "rkB", self.rwkv_r_k[l].rearrange("h d -> (h d)"), 256)
            w0B = self.tb(es, "w0B", [128, 2, 256])
            a0B = self.tb(es, "a0B", [128, 2, 256])
            for d in range(2):
                self.ld(w0B[:, d, :], self.rwkv_w0[l, d, :].partition_broadcast(128))
                self.ld(a0B[:, d, :], self.rwkv_a0[l, d, :].partition_broadcast(128))
            Wl = self.tb(es, "Wl", [128, 2, 256])
            for d in range(2):
                self.ld(Wl[0:64, d, :], self.rwkv_w_up[l, d])
                self.ld(Wl[64:128, d, :], self.rwkv_a_up[l, d])
            Wg = self.tb(es, "WgA", [128, 256])
            self.ld(Wg, self.rwkv_g_up[l])
            cur = [self.tb(es, "curA%d" % i, [128, 1024]) for i in range(2)]
            prv = [self.tb(es, "prvA%d" % i, [128, 1024]) for i in range(2)]
            nxt = [self.tb(es, "nxtA%d" % i, [128, 1024]) for i in range(2)]
            f = self.tb(es, "fA", [128, 1024])
            t2 = self.tb(es, "t2A", [128, 1024])
            lin = self.tb(es, "linA", [128, 256])
            linT = self.tb(es, "linT", [128, 2, 128])
            zw = self.tb(es, "zwA", [128, 2, 256])
            al = self.tb(es, "alA", [128, 2, 256])
            kkr = self.tb(es, "kkr", [128, 256])
            sq = self.tb(es, "sqA", [128, 256])
            ss = self.tb(es, "ssA", [128, 4])
            tm4 = self.tb(es, "tm4A", [128, 4])
            rs4 = self.tb(es, "rs4A", [128, 4])
            kk = self.tb(es, "kkA", [128, 256])
            tk = self.tb(es, "tkA", [128, 256])
            km = self.tb(es, "kmA", [128, 256])
            TMt = [self.tb(es, "TMtA%d" % i, [128, 2, 6, 256]) for i in range(2)]
            for t_ in TMt:
                S.op("pool", lambda: nc.gpsimd.memset(t_.ap, 0.0), writes=[t_.key])
            aux = [self.tb(es, "auxA%d" % i, [128, 2, 256]) for i in range(2)]
            P = self.P
            for i in range(NT):
                b = i % 2
                r0 = i * 128
                self.ld(cur[b], P[r0:r0 + 128, 0:1024])
                if i == 0:
                    S.op("pool", lambda: nc.gpsimd.memset(prv[b].ap, 0.0), writes=[prv[b].key])
                    S.dma("sp", prv[b].ap[1:128, :], P[0:127, 0:1024], writes=[prv[b].key])
                else:
                    self.ld(prv[b], P[r0 - 1:r0 + 127, 0:1024])
                if i == NT - 1:
                    S.op("pool", lambda: nc.gpsimd.memset(nxt[b].ap, 0.0), writes=[nxt[b].key])
                    S.dma("sp", nxt[b].ap[0:127, :], P[r0 + 1:r0 + 128, 0:1024], writes=[nxt[b].key])
                else:
                    self.ld(nxt[b], P[r0 + 1:r0 + 129, 0:1024])
                self.tt("dve", f, cur[b], c0B, ALU.mult)
                self.tt("pool", t2, prv[b], mpB, ALU.mult)
                self.tt("dve", f, f, t2, ALU.add)
                self.tt("pool", t2, nxt[b], mnB, ALU.mult)
                self.tt("dve", f, f, t2, ALU.add)
                T_ = TMt[b]
                r_, k_, v_ = f[:, 0:256], f[:, 256:512], f[:, 512:768]
                self.act(lin[:, 0:64], f[:, 768:832], AF.Tanh)
                self.cp("act", lin[:, 64:128], f[:, 832:896])
                self.act(lin[:, 128:256], f[:, 896:1024], AF.Sigmoid)
                bkT = self.bank()
                for c in range(2):
                    self.tp(bkT[:, c * 128:(c + 1) * 128], lin[:, c * 128:(c + 1) * 128], identf)
                self.cp("dve", linT.v(lambda a: a.rearrange("p c t -> p (c t)")), bkT[:, 0:256])
                bw = self.bank()
                ba = self.bank()
                for d in range(2):
                    self.mm(bw[:, d * 256:(d + 1) * 256], linT[0:64, 0, :], Wl[0:64, d, :])
                for d in range(2):
                    self.mm(ba[:, d * 256:(d + 1) * 256], linT[64:128, 0, :], Wl[64:128, d, :])
                bg = self.bank()
                self.mm(bg[:, 0:256], linT[:, 1, :], Wg)
                self.tt("dve", zw.v(lambda a: a.rearrange("p d c -> p (d c)")), bw, w0B.v(lambda a: a.rearrange("p d c -> p (d c)")), ALU.add)
                self.tt("dve", al.v(lambda a: a.rearrange("p d c -> p (d c)")), ba, a0B.v(lambda a: a.rearrange("p d c -> p (d c)")), ALU.add)
                self.act(zw, zw, AF.Sigmoid)
                self.act(al, al, AF.Sigmoid)
                self.cp("act", aux[b][:, 1, :], bg[:, 0:256])
                self.tt("dve", kkr, k_, kkB, ALU.mult)
                self.tt("pool", sq, kkr, kkr, ALU.mult)
                self.red(ss, sq.v(lambda a: a.rearrange("p (h d) -> p h d", d=64)))
                self.rstd(rs4, ss, tm4, 1.0, 1e-12)
                self.tt("dve", kk.v(lambda a: a.rearrange("p (h d) -> p h d", d=64)), kkr.v(lambda a: a.rearrange("p (h d) -> p h d", d=64)),
                        rs4.v(lambda a: a.unsqueeze(2).to_broadcast([128, 4, 64])), ALU.mult)
                for d in range(2):
                    self.ts("dve", T_[:, d, 0, :], zw[:, d, :], -0.6065306597126334, ALU.mult)
                    self.tt("pool", T_[:, d, 2, :], kk, al[:, d, :], ALU.mult)
                    self.tt("dve", tk, al[:, d, :], kaB, ALU.mult)
                    self.tt("pool", tk, tk, omk, ALU.add)
                    self.tt("dve", T_[:, d, 3, :], k_, tk, ALU.mult)
                self.ts("dve", T_[:, 0, 1, :], kk, -1.0, ALU.mult)
                self.cp("act", T_[:, 0, 4, :], r_)
                self.cp("act", T_[:, 0, 5, :], v_)
                self.tt("pool", km, T_[:, 0, 3, :], T_[:, 1, 3, :], ALU.add)
                self.tt("dve", km, km, r_, ALU.mult)
                self.tt("pool", km, km, rkB, ALU.mult)
                self.red(ss, km.v(lambda a: a.rearrange("p (h d) -> p h d", d=64)))
                self.ts("dve", ss, ss, 0.5, ALU.mult)
                self.tt("dve", aux[b][:, 0, :].v(lambda a: a.rearrange("p (h d) -> p h d", d=64)), v_.v(lambda a: a.rearrange("p (h d) -> p h d", d=64)),
                        ss.v(lambda a: a.unsqueeze(2).to_broadcast([128, 4, 64])), ALU.mult)
                self.st_(self.TM[r0:r0 + 128, 0], T_)
                self.st_(self.AUX[r0:r0 + 128, 0:2, :], aux[b])
        S.barrier()

    def stage_prepC(self, l):
        nc, S = self.nc, self.S
        with ExitStack() as es:
            cwB = self.tb(es, "cwB", [128, 5, 768])
            for j in range(5):
                self.ld(cwB[:, j, :], self.gdn_conv[l, j, :].partition_broadcast(128))
            alB = self.bcast_row(es, "alogB", self.gdn_a_log[l].rearrange("d h -> (d h)"), 8)
            dtB = self.bcast_row(es, "dtB", self.gdn_dt_bias[l].rearrange("d h -> (d h)"), 8)
            negA = self.tb(es, "negA", [128, 8])
            self.act(negA, alB, AF.Exp)
            self.ts("dve", negA, negA, -1.0, ALU.mult)
            xs = [[self.tb(es, "xc%d_%d" % (j, i), [128, 768]) for j in range(5)] for i in range(2)]
            zt = [self.tb(es, "ztC%d" % i, [128, 272]) for i in range(2)]
            cv = self.tb(es, "cvC", [128, 768])
            t2 = self.tb(es, "t2C", [128, 768])
            sq = self.tb(es, "sqC", [128, 512])
            ss = self.tb(es, "ssC", [128, 8])
            tm8 = self.tb(es, "tm8C", [128, 8])
            rs8 = self.tb(es, "rs8C", [128, 8])
            bt = self.tb(es, "btC", [128, 8])
            nbt = self.tb(es, "nbtC", [128, 8])
            gg = self.tb(es, "ggC", [128, 8])
            TMt = [self.tb(es, "TMtC%d" % i, [128, 2, 6, 256]) for i in range(2)]
            for t_ in TMt:
                S.op("pool", lambda: nc.gpsimd.memset(t_.ap, 0.0), writes=[t_.key])
            aux = [self.tb(es, "auxC%d" % i, [128, 256]) for i in range(2)]
            P = self.P
            h3 = lambda a: a.rearrange("p (h d) -> p h d", d=64)
            for i in range(NT):
                b = i % 2
                r0 = i * 128
                for j in range(5):
                    sh = j - 2
                    lo_, hi_ = r0 + sh, r0 + sh + 128
                    x = xs[b][j]
                    if lo_ < 0 or hi_ > T:
                        S.op("pool", lambda: nc.gpsimd.memset(x.ap, 0.0), writes=[x.key])
                        a0, a1 = max(lo_, 0), min(hi_, T)
                        S.dma("sp", x.ap[a0 - lo_:a1 - lo_, :], P[a0:a1, OFF_C:OFF_C + 768], writes=[x.key])
                    else:
                        self.ld(x, P[lo_:hi_, OFF_C:OFF_C + 768])
                self.ld(zt[b], P[r0:r0 + 128, OFF_C + 768:OFF_C + 1040])
                self.tt("dve", cv, xs[b][0], cwB[:, 0, :], ALU.mult)
                for j in range(1, 5):
                    self.tt("pool", t2, xs[b][j], cwB[:, j, :], ALU.mult)
                    self.tt("dve", cv, cv, t2, ALU.add)
                self.act(cv, cv, AF.Silu)
                T_ = TMt[b]
                self.tt("pool", sq, cv[:, 0:512], cv[:, 0:512], ALU.mult)
                self.red(ss, sq.v(h3))
                self.rstd(rs8, ss, tm8, 1.0, 1e-12)
                self.ts("dve", rs8[:, 0:4], rs8[:, 0:4], 0.125, ALU.mult)
                self.tt("dve", T_[:, 0, 4, :].v(h3), cv[:, 0:256].v(h3), rs8[:, 0:4].v(lambda a: a.unsqueeze(2).to_broadcast([128, 4, 64])), ALU.mult)
                self.tt("dve", T_[:, 0, 2, :].v(h3), cv[:, 256:512].v(h3), rs8[:, 4:8].v(lambda a: a.unsqueeze(2).to_broadcast([128, 4, 64])), ALU.mult)
                self.cp("act", T_[:, 0, 3, :], T_[:, 0, 2, :])
                self.act(bt, zt[b][:, 256:264], AF.Sigmoid)
                self.ts("dve", nbt, bt, -1.0, ALU.mult)
                self.tt("dve", gg, zt[b][:, 264:272], dtB, ALU.add)
                self.act(gg, gg, AF.Exp)
                self.act(gg, gg, AF.Ln, bias=1.0)
                self.tt("dve", gg, gg, negA, ALU.mult)
                for d in range(2):
                    bc = lambda t_: t_[:, d * 4:(d + 1) * 4].v(lambda a: a.unsqueeze(2).to_broadcast([128, 4, 64]))
                    self.cp("pool", T_[:, d, 0, :].v(h3), bc(gg))
                    self.tt("dve", T_[:, d, 1, :].v(h3), T_[:, 0, 2, :].v(h3), bc(nbt), ALU.mult)
                    self.tt("pool", T_[:, d, 5, :].v(h3), cv[:, 512:768].v(h3), bc(bt), ALU.mult)
                self.act(aux[b], zt[b][:, 0:256], AF.Silu)
                self.st_(self.TM[r0:r0 + 128, 1], T_)
                self.st_(self.AUX[r0:r0 + 128, 2, :], aux[b])
        S.barrier()

    def stage_scan(self, l):
        nc, S = self.nc, self.S
        with ExitStack() as es:
            self.banks = [TB(self.ps(es, "bk%d" % i, [128, 512], F32), ("bk", i)) for i in range(8)]
            self.nbank = 0
            identf = TB(self.identf, "identf")
            tri = self.tb(es, "triS", [128, 2, 128])
            msk = self.tb(es, "mskS", [128, 2, 4, 128])
            onb = self.tb(es, "onbS", [128, 128])
            for d in range(2):
                self.ld(tri[:, d, :], self.c_tri[d])
                self.ld(msk[:, d, :, :], self.c_msk[d])
            self.ld(onb, self.c_onb)
            ST = [self.tb(es, "ST%d" % i, [64, 16, 64]) for i in range(2)]
            S.op("pool", lambda: nc.gpsimd.memset(ST[0].ap, 0.0), writes=[ST[0].key])
            U4 = range(4)
            BP = [self.tb(es, "BP%d" % u, [128, 256]) for u in U4]
            KP = [self.tb(es, "KP%d" % u, [128, 256]) for u in U4]
            VV = [self.tb(es, "VV%d" % u, [128, 256]) for u in U4]
            VP = [self.tb(es, "VP%d" % u, [128, 256]) for u in U4]
            G4 = [self.tb(es, "G4%d" % u, [128, 4, 4, 128]) for u in U4]
            APT = [self.tb(es, "APT%d" % u, [64, 4, 128]) for u in U4]
            RST = [self.tb(es, "RST%d" % u, [64, 4, 128]) for u in U4]
            PC = [self.tb(es, "PC%d" % u, [64, 4, 2]) for u in U4]
            def mk_lane(tg):
                lds = [self.tb(es, "ld%d_%s" % (q, tg), [128, 256]) for q in range(5)]
                c0t = self.tb(es, "c0t_" + tg, [128, 256])
                a1 = self.tb(es, "a1_" + tg, [128, 256])
                a1a = self.tb(es, "a1a_" + tg, [128, 256])
                X1 = self.tb(es, "X1_" + tg, [128, 256])
                X1a = self.tb(es, "X1a_" + tg, [128, 256])
                X2 = self.tb(es, "X2_" + tg, [128, 256])
                Ec = self.tb(es, "Ec_" + tg, [128, 256])
                Ag = self.tb(es, "Ag_" + tg, [128, 256])
                Rg = self.tb(es, "Rg_" + tg, [128, 256])
                Bg = self.tb(es, "Bg_" + tg, [128, 256])
                Kg = self.tb(es, "Kg_" + tg, [128, 256])
                As = self.tb(es, "As_" + tg, [128, 256])
                Rs = self.tb(es, "Rs_" + tg, [128, 256])
                FMar = self.tb(es, "FMar_" + tg, [64, 4, 2, 128])
                FMbg = self.tb(es, "FMbg_" + tg, [64, 4, 128])
                FMkg = self.tb(es, "FMkg_" + tg, [64, 4, 128])
                FMcum = self.tb(es, "FMcum_" + tg, [64, 4, 128])
                cumS = self.tb(es, "cumS_" + tg, [128, 256])
                Dm = self.tb(es, "DmS_" + tg, [128, 4, 128])
                mskD = self.tb(es, "mskD_" + tg, [128, 4, 2, 128])
                Xb = [self.tb(es, ("Xb%d_" % i) + tg, [128, 4, 128]) for i in range(2)]
                XTb = [self.tb(es, ("XTb%d_" % i) + tg, [128, 4, 128]) for i in range(2)]
                Wb = [self.tb(es, ("Wb%d_" % i) + tg, [128, 4, 128]) for i in range(2)]
                Zs = self.tb(es, "Zs_" + tg, [128, 256])
                return dict(lds=lds, c0t=c0t, a1=a1, a1a=a1a, X1=X1, X1a=X1a, X2=X2, Ec=Ec, Ag=Ag, Rg=Rg, Bg=Bg, Kg=Kg, As=As, Rs=Rs, FMar=FMar, FMbg=FMbg, FMkg=FMkg, FMcum=FMcum, cumS=cumS, Dm=Dm, mskD=mskD, Xb=Xb, XTb=XTb, Wb=Wb, Zs=Zs)
            avg64 = self.tb(es, "avg64", [64, 128])
            S.op("pool", lambda: nc.gpsimd.memset(avg64.ap, 1.0 / 64), writes=[avg64.key])
            lanes = [mk_lane("L0"), mk_lane("L1")]
            Usb = self.tb(es, "Usb", [128, 2, 512])
            tmpS = self.tb(es, "tmpS", [64, 16, 64])
            Y1s = self.tb(es, "Y1s", [128, 2, 512])
            yt = [self.tb(es, "ytS%d" % d, [128, 512]) for d in range(2)]
            flat = lambda a: a.rearrange("p h t -> p (h t)")
            nld = 0
            cur = 0
            def unit_gen(m, d, j, Lz):
                lds = Lz["lds"]
                c0t = Lz["c0t"]
                a1 = Lz["a1"]
                a1a = Lz["a1a"]
                X1 = Lz["X1"]
                X1a = Lz["X1a"]
                X2 = Lz["X2"]
                Ec = Lz["Ec"]
                Ag = Lz["Ag"]
                Rg = Lz["Rg"]
                Bg = Lz["Bg"]
                Kg = Lz["Kg"]
                As = Lz["As"]
                Rs = Lz["Rs"]
                FMar = Lz["FMar"]
                FMbg = Lz["FMbg"]
                FMkg = Lz["FMkg"]
                FMcum = Lz["FMcum"]
                cumS = Lz["cumS"]
                Dm = Lz["Dm"]
                mskD = Lz["mskD"]
                Xb = Lz["Xb"]
                XTb = Lz["XTb"]
                Wb = Lz["Wb"]
                Zs = Lz["Zs"]
                u = m * 2 + d
                _CUR_U[0] = u
                tile_ = j if d == 0 else NT - 1 - j
                r0 = tile_ * 128
                L_ = lds
                dsrc = {0: d, 1: (0 if m == 0 else d), 2: (d if m == 0 else 0), 3: (d if m == 0 else 0), 4: 0, 5: (0 if m == 0 else d)}
                LW, Aa, Bb, Kk, Rr = L_
                for q, dst in ((0, LW), (1, Aa), (2, Bb), (3, Kk), (4, Rr), (5, VV[u])):
                    self.ld(dst, self.TM[r0:r0 + 128, m, dsrc[q], q, :])
                bc = self.bank()
                self.mm(bc[:, 0:256], tri[:, d, :], LW)
                self.mm(bc[:, 256:512], onb, LW)
                self.ts("dve", c0t, bc[:, 256:512], 0.5, ALU.mult)
                if m == 0:
                    self.tt("dve", a1, bc[:, 0:256], c0t, ALU.subtract)
                    self.act(X1, a1, AF.Exp)
                    self.act(X2, a1, AF.Exp, scale=-1.0)
                    self.act(Ec, c0t, AF.Exp)
                    self.tt("pool", a1a, a1, LW, ALU.subtract)
                    self.act(X1a, a1a, AF.Exp)
                    xa = X1a
                    self.tt("dve", Ag, Aa, xa, ALU.mult)
                    self.tt("pool", Rg, Rr, X1, ALU.mult)
                    self.tt("dve", Bg, Bb, X2, ALU.mult)
                    self.tt("pool", Kg, Kk, X2, ALU.mult)
                    self.tt("dve", As, Ag, Ec, ALU.mult)
                    self.tt("pool", Rs, Rg, Ec, ALU.mult)
                    self.tt("dve", BP[u], Bg, Ec, ALU.mult)
                    self.tt("pool", KP[u], Kg, Ec, ALU.mult)
                    gA, gR, gB_ = Ag, Rg, Bg
                else:
                    cops = [
                        lambda: self.cp("dve", cumS, bc[:, 0:256]),
                        lambda: self.act(X1, cumS, AF.Exp),
                        lambda: self.tt("dve", a1, c0t, cumS, ALU.subtract),
                        lambda: self.tt("dve", a1, a1, c0t, ALU.add),
                        lambda: self.act(X2, a1, AF.Exp),
                        lambda: self.tt("dve", As, Aa, X1, ALU.mult),
                        lambda: self.tt("pool", Rs, Rr, X1, ALU.mult),
                        lambda: self.tt("dve", BP[u], Bb, X2, ALU.mult),
                        lambda: self.cp("pool", KP[u], BP[u]),
                    ]
                    import os as _os
                    for ci, cop in enumerate(cops):
                        if _os.environ.get("SCAN_STOP") == "cn" and ci == int(_os.environ.get("SCAN_N", "0")):
                            S.barrier()
                            return True
                        cop()
                    gA, gR, gB_ = Aa, Rr, Bb
                if _stop("a"):
                    S.barrier()
                    return True
                yield
                bpc = self.bank()
                on2 = onb.v(lambda a: a.rearrange("p (c t) -> p c t", t=64)[:, :, 0])
                for h in range(4):
                    self.mm(bpc[0:64, h * 2:(h + 1) * 2], LW[:, h * 64:(h + 1) * 64], on2)
                self.act(PC[u].v(lambda a: a.rearrange("p h c -> p (h c)")), bpc[0:64, 0:8], AF.Exp)
                if _stop("b"):
                    S.barrier()
                    return True
                yield
                fm_list = [(gA, FMar[:, :, 0, :]), (gR, FMar[:, :, 1, :]), (gB_, FMbg), (Rs, RST[u])]
                fm_list.append((Kg, FMkg) if m == 0 else (cumS, FMcum))
                for (src, dst) in fm_list:
                    bt_ = self.bank()
                    for h in range(4):
                        self.mm(bt_[0:64, h * 128:(h + 1) * 128], src[:, h * 64:(h + 1) * 64], identf)
                    self.cp("act", dst, bt_[0:64, :].v(lambda a: a.rearrange("p (h t) -> p h t", t=128)))
                if _stop("c"):
                    S.barrier()
                    return True
                yield
                nblk = 4 if m == 0 else 2
                if m == 1:
                    bd_ = self.bank()
                    for h in range(4):
                        self.mm(bd_[:, h * 128:(h + 1) * 128], avg64, FMcum[:, h, :])
                    for h in range(4):
                        self.ts("dve", Dm[:, h, :], bd_[:, h * 128:(h + 1) * 128], cumS[:, h * 64:h * 64 + 1], ALU.subtract)
                    self.ts("dve", Dm, Dm, 0.0, ALU.min)
                    if True:
                        pass
                    self.act(Dm, Dm, AF.Exp)
                    for h in range(4):
                        self.tt("pool", mskD[:, h, :, :], msk[:, d, 0:2, :], Dm[:, h, :].v(lambda a: a.unsqueeze(1).to_broadcast([128, 2, 128])), ALU.mult)
                for h in range(4):
                    bg_ = self.bank()
                    rhs = FMar[:, h, :, :].v(lambda a: a.rearrange("p c t -> p (c t)"))
                    self.mm(bg_[:, 0:256], FMbg[:, h, :], rhs)
                    if m == 0:
                        self.mm(bg_[:, 256:512], FMkg[:, h, :], rhs)
                    if _stop("c2"):
                        S.barrier()
                        return True
                    mk_ = msk[:, d, 0:nblk, :] if m == 0 else mskD[:, h, :, :]
                    self.tt("dve", G4[u][:, h, 0:nblk, :].v(lambda a: a.rearrange("p b t -> p (b t)")), bg_[:, 0:nblk * 128],
                            mk_.v(lambda a: a.rearrange("p b t -> p (b t)")), ALU.mult)
                    import os as _os
                    if _stop("c3") and h == int(_os.environ.get("SCAN_H", "0")):
                        S.barrier()
                        return True
                mak_i = 2 if m == 0 else 0
                nrk_i = 3 if m == 0 else 1
                if _stop("d"):
                    S.barrier()
                    return True
                yield
                xi = 0
                Xc, XTc, Wc = Xb[0], XTb[0], Wb[0]
                self.cp("pool", Xc, G4[u][:, :, 0, :])
                bt_ = self.bank()
                for h in range(4):
                    self.tp(bt_[:, h * 128:(h + 1) * 128], Xc[:, h, :], identf)
                self.cp("act", XTc.v(flat), bt_)
                self.tt("dve", Wc, Xc, identf.v(lambda a: a.unsqueeze(1).to_broadcast([128, 4, 128])), ALU.add)
                for lv in range(1, 6):
                    Xn, XTn, Wn = Xb[1 - xi], XTb[1 - xi], Wb[1 - xi]
                    bx = self.bank()
                    for h in range(4):
                        self.mm(bx[:, h * 128:(h + 1) * 128], Xc[:, h, :], XTc[:, h, :])
                    self.cp("act", XTn.v(flat), bx)
                    if lv < 5:
                        by = self.bank()
                        for h in range(4):
                            self.mm(by[:, h * 128:(h + 1) * 128], XTc[:, h, :], Xc[:, h, :])
                        self.cp("pool" if False else "dve", Xn.v(flat), by)
                    bw = self.bank()
                    for h in range(4):
                        self.mm(bw[:, h * 128:(h + 1) * 128], XTn[:, h, :], Wc[:, h, :])
                    self.tt("dve", Wn.v(flat), Wc.v(flat), bw, ALU.add)
                    xi = 1 - xi
                    Xc, XTc, Wc = Xn, XTn, Wn
                    yield
                if _stop("e"):
                    S.barrier()
                    return True
                yield
                bz = self.bank()
                for h in range(4):
                    self.mm(bz[:, h * 64:(h + 1) * 64], G4[u][:, h, mak_i, :], VV[u][:, h * 64:(h + 1) * 64])
                self.cp("act", Zs, bz[:, 0:256])
                yield
                bv = self.bank()
                for h in range(4):
                    self.mm(bv[:, h * 64:(h + 1) * 64], Wc[:, h, :], Zs[:, h * 64:(h + 1) * 64])
                self.cp("act", VP[u], bv[:, 0:256])
                yield
                ba = self.bank()
                for h in range(4):
                    self.mm(ba[0:64, h * 128:(h + 1) * 128], As[:, h * 64:(h + 1) * 64], Wc[:, h, :])
                self.cp("act", APT[u].v(flat), ba[0:64, :])
                import os as _os
                if _stop("u") and u == int(_os.environ.get("SCAN_U", "0")):
                    S.barrier()
                    return True
                yield
            for j in range(NT):
                for m in range(2):
                    gens = [unit_gen(m, 0, j, lanes[0]), unit_gen(m, 1, j, lanes[1])]
                    while gens:
                        for g_ in list(gens):
                            try:
                                next(g_)
                            except StopIteration:
                                gens.remove(g_)
                if _stop("f"):
                    S.barrier()
                    return True
                for sub in range(2):
                    STc, STn = ST[cur], ST[1 - cur]
                    par = [sub, 1 - sub]
                    rows = [slice(par[d] * 64, par[d] * 64 + 64) for d in range(2)]
                    bu = [self.bank(), self.bank()]
                    for d in range(2):
                        for m in range(2):
                            u = m * 2 + d
                            for h in range(4):
                                c0_ = (m * 4 + h) * 64
                                self.mm(bu[d][rows[d], c0_:c0_ + 64], APT[u][:, h, rows[d]], STc[:, d * 8 + m * 4 + h, :])
                    for d in range(2):
                        for m in range(2):
                            u = m * 2 + d
                            self.tt("dve", Usb[rows[d], d, m * 256:(m + 1) * 256], bu[d][rows[d], m * 256:(m + 1) * 256], VP[u][rows[d], :], ALU.add)
                    bs = [self.bank(), self.bank()]
                    for d in range(2):
                        for m in range(2):
                            u = m * 2 + d
                            for h in range(4):
                                c0_ = (m * 4 + h) * 64
                                self.mm(bs[d][0:64, c0_:c0_ + 64], BP[u][rows[d], h * 64:(h + 1) * 64], Usb[rows[d], d, c0_:c0_ + 64], start=True, stop=False)
                                self.mm(bs[d][0:64, c0_:c0_ + 64], KP[u][rows[d], h * 64:(h + 1) * 64], VV[u][rows[d], h * 64:(h + 1) * 64], start=False, stop=True)
                    by1 = [self.bank(), self.bank()]
                    by2 = [self.bank(), self.bank()]
                    for d in range(2):
                        for m in range(2):
                            u = m * 2 + d
                            nrk_i = 3 if m == 0 else 1
                            for h in range(4):
                                c0_ = (m * 4 + h) * 64
                                self.mm(by1[d][rows[d], c0_:c0_ + 64], RST[u][:, h, rows[d]], STc[:, d * 8 + m * 4 + h, :])
                                self.mm(by2[d][rows[d], c0_:c0_ + 64], G4[u][rows[d], h, 1, rows[d]], Usb[rows[d], d, c0_:c0_ + 64], start=True, stop=False)
                                self.mm(by2[d][rows[d], c0_:c0_ + 64], G4[u][rows[d], h, nrk_i, rows[d]], VV[u][rows[d], h * 64:(h + 1) * 64], start=False, stop=True)
                    for d in range(2):
                        for m in range(2):
                            u = m * 2 + d
                            sl_ = slice(d * 8 + m * 4, d * 8 + m * 4 + 4)
                            self.tt("pool", tmpS[:, sl_, :], STc[:, sl_, :], PC[u][:, :, par[d]].v(lambda a: a.unsqueeze(2).to_broadcast([64, 4, 64])), ALU.mult)
                        sl8 = slice(d * 8, d * 8 + 8)
                        self.tt("dve", STn[:, sl8, :].v(lambda a: a.rearrange("p s v -> p (s v)")), tmpS[:, sl8, :].v(lambda a: a.rearrange("p s v -> p (s v)")),
                                bs[d][0:64, :], ALU.add)
                        self.cp("act", Y1s[rows[d], d, :], by1[d][rows[d], :])
                        self.tt("dve", yt[d][rows[d], :], Y1s[rows[d], d, :], by2[d][rows[d], :], ALU.add)
                    cur = 1 - cur
                    if _stop("g"):
                        S.barrier()
                        return True
                for d in range(2):
                    tile_ = j if d == 0 else NT - 1 - j
                    self.st_(self.YAC[tile_ * 128:(tile_ + 1) * 128, :, d, :], yt[d].v(lambda a: a.rearrange("p (m c) -> p m c", m=2)))
        S.barrier()

    def stage_postAC(self, l):
        nc, S = self.nc, self.S
        with ExitStack() as es:
            lnw = self.bcast_row(es, "lnwB", self.rwkv_ln_w[l, :], 256)
            lnb = self.bcast_row(es, "lnbB", self.rwkv_ln_b[l, :], 256)
            gn = self.tb(es, "gnB", [128, 4, 64])
            for h in range(4):
                self.ld(gn[:, h, :], self.gdn_norm_g[l, :].partition_broadcast(128))
            ys = [self.tb(es, "ysP%d" % i, [128, 2, 2, 256]) for i in range(2)]
            ax = [self.tb(es, "axP%d" % i, [128, 3, 256]) for i in range(2)]
            y = self.tb(es, "yP", [128, 256])
            yc = self.tb(es, "ycP", [128, 256])
            sq = self.tb(es, "sqP", [128, 256])
            s4 = self.tb(es, "s4P", [128, 4])
            t4 = self.tb(es, "t4P", [128, 4])
            r4 = self.tb(es, "r4P", [128, 4])
            oa = [self.tb(es, "oaP%d" % i, [128, 256]) for i in range(2)]
            oc = [self.tb(es, "ocP%d" % i, [128, 256]) for i in range(2)]
            h3 = lambda a: a.rearrange("p (h d) -> p h d", d=64)
            b4 = lambda t_: t_.v(lambda a: a.unsqueeze(2).to_broadcast([128, 4, 64]))
            for i in range(NT):
                b = i % 2
                r0 = i * 128
                self.ld(ys[b], self.YAC[r0:r0 + 128])
                self.ld(ax[b], self.AUX[r0:r0 + 128])
                self.tt("dve", y, ys[b][:, 0, 0, :], ys[b][:, 0, 1, :], ALU.add)
                self.red(s4, y.v(h3))
                self.ts("dve", s4, s4, 1.0 / 64, ALU.mult)
                self.tt("dve", yc.v(h3), y.v(h3), b4(s4), ALU.subtract)
                self.tt("pool", sq, yc, yc, ALU.mult)
                self.red(s4, sq.v(h3))
                self.rstd(r4, s4, t4, 1.0 / 64, 64e-5)
                self.tt("dve", yc.v(h3), yc.v(h3), b4(r4), ALU.mult)
                self.tt("pool", yc, yc, lnw, ALU.mult)
                self.tt("dve", yc, yc, lnb, ALU.add)
                self.tt("pool", yc, yc, ax[b][:, 0, :], ALU.add)
                self.tt("dve", oa[b], yc, ax[b][:, 1, :], ALU.mult)
                self.st_(self.mixed[r0:r0 + 128, 0:256], oa[b])
                self.tt("dve", y, ys[b][:, 1, 0, :], ys[b][:, 1, 1, :], ALU.add)
                self.tt("pool", sq, y, y, ALU.mult)
                self.red(s4, sq.v(h3))
                self.rstd(r4, s4, t4, 1.0 / 64, EPS)
                self.tt("dve", yc.v(h3), y.v(h3), b4(r4), ALU.mult)
                self.tt("pool", yc.v(h3), yc.v(h3), gn, ALU.mult)
                self.tt("dve", oc[b], yc, ax[b][:, 2, :], ALU.mult)
                self.st_(self.mixed[r0:r0 + 128, 512:768], oc[b])
        S.barrier()

    def stage_final(self, xsrc):
        nc, S = self.nc, self.S
        with ExitStack() as es:
            gB = self.bcast_row(es, "gFB", self.norm_final_g[0, :], D)
            xt = [self.tb(es, "xF%d" % i, [128, D]) for i in range(2)]
            ot = [self.tb(es, "oF%d" % i, [128, D]) for i in range(2)]
            junk = self.tb(es, "junkF", [128, D])
            st = self.tb(es, "stF", [128, NT, 4])
            for i in range(NT):
                b = i % 2
                self.ld(xt[b], xsrc[i * 128:(i + 1) * 128, :])
                S.op("act", lambda: nc.scalar.activation(out=junk.ap, in_=xt[b].ap, func=AF.Square, accum_out=st.ap[:, i, 0:1]),
                     reads=[xt[b].key], writes=[junk.key, st.key])
                self.rstd(st[:, i, 2:3], st[:, i, 0:1], st[:, i, 1:2], 1.0 / D, EPS)
                self.stt(ot[b], xt[b], st[:, i, 2:3], gB, ALU.mult, ALU.mult)
                self.st_(self.out[i * 128:(i + 1) * 128, :], ot[b])
        S.barrier()

    def build(self):
        nc, S = self.nc, self.S
        self.stage_inproj(0, self.x)
        if self.stop_after == "inproj0":
            return self.finish_debug(self.P, [T, P_IN])
        if self.stop_after in ("moe_idx", "moe", "outproj"):
            self.mixed = self.inp("mixed_in", [T, MIXW])
            self.stage_outproj(0, self.x)
            if self.stop_after == "outproj":
                return self.finish_debug(self.X2, [T, D])
            if self.stop_after == "moe_idx":
                self.dbg_small = nc.dram_tensor("dbg", [128, 80], F32, kind="ExternalOutput").ap()
                self.stage_moe(0)
                self.es.close()
                return nc
            self.stage_moe(0)
            return self.finish_debug(self.X3, [T, D])
        if self.stop_after == "AC":
            import os
            la = int(os.environ.get("ACL", "0"))
            if la:
                self.stage_inproj(la, self.x)
            self.stage_prepA(la)
            self.stage_prepC(la)
            if self.stage_scan(la):
                return self.finish_debug(self.AUX.rearrange("t a c -> t (a c)"), [T, 768])
            if os.environ.get("SCAN_STOP") == "h":
                return self.finish_debug(self.YAC.rearrange("t a b c -> t (a b c)"), [T, 1024])
            self.stage_postAC(la)
            return self.finish_debug(self.mixed, [T, MIXW], cols=[(0, 256), (512, 768)])
        if self.stop_after == "prepAC":
            self.stage_prepA(0)
            self.stage_prepC(0)
            return self.finish_debug(self.TM.rearrange("t m d q c -> t (m d q c)"), [T, 2 * 2 * 6 * 256])
        if self.stop_after == "BD":
            self.stage_attnB(0)
            self.stage_attnD(0)
            return self.finish_debug(self.mixed, [T, MIXW], cols=[(256, 512), (768, 1152)])
        self.out = nc.dram_tensor("out", [T, D], F32, kind="ExternalOutput").ap()
        xsrc = self.x
        for l in range(L):
            if l > 0:
                self.stage_inproj(l, xsrc)
            self.X3 = self.X3s[l % 2]
            self.stage_prepA(l)
            self.stage_prepC(l)
            self.stage_scan(l)
            self.stage_postAC(l)
            self.stage_attnB(l)
            self.stage_attnD(l)
            self.stage_outproj(l, xsrc)
            self.stage_moe(l)
            xsrc = self.X3
        self.stage_final(xsrc)
        self.es.close()
        return nc

    def finish_debug(self, src, shape, cols=None):
        nc, S = self.nc, self.S
        dbg = nc.dram_tensor("dbg", list(shape), F32, kind="ExternalOutput").ap()
        with ExitStack() as es:
            tb = [self.sb(es, "dbgt%d" % i, [128, shape[1]], F32) for i in range(2)]
            for i in range(shape[0] // 128):
                for (c0, c1) in (cols or [(0, shape[1])]):
                    S.dma("sp", tb[i % 2][:, c0:c1], src[i * 128:(i + 1) * 128, c0:c1], writes=[("dbgt", i % 2)])
                    S.dma("sp", dbg[i * 128:(i + 1) * 128, c0:c1], tb[i % 2][:, c0:c1], reads=[("dbgt", i % 2)])
        S.barrier()
        self.es.close()
        return nc


def _t5_bucket(rel):
    nb = 16
    max_exact = 8
    n = np.abs(rel)
    large = max_exact + (np.log(np.maximum(n, 1).astype(np.float32) / np.float32(max_exact))
                         / np.float32(math.log(1024 / max_exact)) * np.float32(nb - max_exact)).astype(np.int32)
    large = np.minimum(large, nb - 1)
    return np.where(rel > 0, nb, 0) + np.where(n < max_exact, n, large)


def host_consts(inputs=None):
    c = {}
    c["ident_f"] = np.eye(128, dtype=np.float32)
    t = np.arange(T)
    inv = (np.float32(10000.0) ** (-np.arange(0, 32, 2, dtype=np.float32) / np.float32(32))).astype(np.float32)
    ar = (t // 64).astype(np.float32)[:, None] * inv
    ac = (t % 64).astype(np.float32)[:, None] * inv
    c["rope_cos"] = np.concatenate([np.cos(ar), np.cos(ar), np.cos(ac), np.cos(ac)], 1).astype(np.float32)
    c["rope_sin"] = np.concatenate([-np.sin(ar), np.sin(ar), -np.sin(ac), np.sin(ac)], 1).astype(np.float32)
    ch = np.arange(128) // 64
    same = (ch[:, None] == ch[None, :])
    sI, tI = np.arange(128)[:, None], np.arange(128)[None, :]
    c["c_onb"] = same.astype(np.float32)
    c["c_tri"] = np.stack([(same & (sI <= tI)), (same & (sI >= tI))]).astype(np.float32)
    mk = np.zeros((2, 128, 4, 128), np.float32)
    for blk in range(4):
        strict = blk in (0, 2)
        mk[0, :, blk, :] = same & ((sI < tI) if strict else (sI <= tI))
        mk[1, :, blk, :] = same & ((sI > tI) if strict else (sI >= tI))
    c["c_msk"] = mk
    c["ustrict"] = np.triu(np.ones((128, 128), np.float32), 1)
    c["iota_c"] = np.tile(np.arange(CAP, dtype=np.float32)[None, :], (128, 1))
    tv = np.zeros((128, NT, 2), np.float32)
    tv[:, :, 0] = np.arange(128)[:, None]
    tv[:, :, 1] = 128.0 * np.arange(NT)[None, :]
    c["tvals"] = tv
    c["tvals_"] = tv
    c["coef2"] = np.array([[1.0], [128.0]], np.float32)
    if inputs is not None:
        rb = np.asarray(inputs["rel_bias"], np.float32)
        kp = np.arange(128)[:, None, None]
        rel = np.arange(3)[None, :, None]
        qp = np.arange(128)[None, None, :]
        o = 128 * (rel - 1) + kp - qp
        valid = np.abs(o) <= 64
        db = np.full((3, 2, 128, 3, 128), NEG, np.float32)
        for br, dil in enumerate((1, 4, 16)):
            bk = _t5_bucket(o * dil)
            for j in range(2):
                db[br, j] = np.where(valid, rb[bk, br * 2 + j], np.float32(NEG))
        c["dbias"] = db
    return c


_PROG = None


def kernel(**inputs):
    global _PROG
    if _PROG is None:
        pr = Prog()
        _PROG = (pr, pr.build())
    pr, nc = _PROG
    consts = host_consts(inputs)
    x = np.asarray(inputs["x"], np.float32)
    nb = x.shape[0]
    in_maps = []
    for b in range(nb):
        m = {}
        for name, (shape, dt) in pr.inputs.items():
            if name == "x":
                m[name] = np.ascontiguousarray(x[b])
            elif name in consts:
                m[name] = consts[name]
            else:
                m[name] = np.ascontiguousarray(np.asarray(inputs[name], np.float32)).reshape(shape)
        in_maps.append(m)
    res = run_bass_kernel_spmd(nc, in_maps, core_ids=list(range(nb)))
    return np.stack([np.asarray(res.results[b]["out"], np.float32) for b in range(nb)])
```

```python
import math
from contextlib import ExitStack
import numpy as np
import concourse.bass as bass
import concourse.mybir as mybir
from concourse.bass_utils import run_bass_kernel_spmd

F32 = mybir.dt.float32
F32R = mybir.dt.float32r
BF16 = mybir.dt.bfloat16
U32 = mybir.dt.uint32
I32 = mybir.dt.int32
AF = mybir.ActivationFunctionType
ALU = mybir.AluOpType
AX = mybir.AxisListType

T = 4096
NT = 32
D = 1024
L = 2
HD = 64
EPS = 1e-6
A_W = 256
A_IN = 1024
B_IN = 512
C_IN = 1040
D_IN = 1152
P_IN = 3728
OFF_A = 0
OFF_B = 1024
OFF_C = 1536
OFF_D = 2576
MIXW = 1152
NE = 16
CAP = 512
NEG = -30000.0

NDMA = 44
NSW = 12


class Sched:
    def __init__(self, nc, same_engine_sync=True):
        self.nc = nc
        self.eng = {"pe": nc.tensor, "act": nc.scalar, "dve": nc.vector,
                    "pool": nc.gpsimd, "sp": nc.sync}
        self.sem = {}
        for k in self.eng:
            self.sem[k] = nc.alloc_semaphore("sem_" + k)
        self.cnt = {k: 0 for k in self.eng}
        for i in range(NDMA):
            self.sem[("d", i)] = nc.alloc_semaphore("sem_d%d" % i)
        self.dval = [0] * NDMA
        self.dnext = {"hw": 0, "sw": 0}
        self.known = {k: {} for k in self.eng}
        self.w = {}
        self.r = {}
        self.same = same_engine_sync
        self.nwaits = 0
        self.nops = 0

    def _wait(self, e, ev):
        if ev is None:
            return
        key, val = ev
        if key == e and (e == "pe" or not self.same):
            return
        if self.known[e].get(key, 0) >= val:
            return
        self.eng[e].wait_ge(self.sem[key], val)
        self.known[e][key] = val
        self.nwaits += 1

    def _deps(self, e, reads, writes):
        for b in reads:
            self._wait(e, self.w.get(b))
        for b in writes:
            self._wait(e, self.w.get(b))
            for ev in self.r.get(b, ()):
                self._wait(e, ev)

    def _commit(self, ev, reads, writes):
        for b in reads:
            self.r.setdefault(b, []).append(ev)
        for b in writes:
            self.w[b] = ev
            self.r[b] = []

    def op(self, e, fn, reads=(), writes=()):
        self._deps(e, reads, writes)
        ins = fn()
        self.cnt[e] += 1
        ins.then_inc(self.sem[e], 1)
        self._commit((e, self.cnt[e]), reads, writes)
        self.nops += 1
        return ins

    def dma_raw(self, q, fn, reads=(), writes=()):
        self._deps(q, reads, writes)
        if q == "pool":
            s = NDMA - NSW + self.dnext["sw"]
            self.dnext["sw"] = (self.dnext["sw"] + 1) % NSW
        else:
            s = self.dnext["hw"]
            self.dnext["hw"] = (self.dnext["hw"] + 1) % (NDMA - NSW)
        key = ("d", s)
        if self.dval[s] > 0:
            self._wait(q, (key, self.dval[s]))
        ins = fn()
        self.dval[s] += 16
        ins.then_inc(self.sem[key], 16)
        self._commit((key, self.dval[s]), reads, writes)
        self.nops += 1
        return ins

    def dma(self, q, out, in_, reads=(), writes=(), **kw):
        return self.dma_raw(q, lambda: self.eng[q].dma_start(out=out, in_=in_, **kw), reads, writes)

    def barrier(self):
        for e in self.eng:
            for s in range(NDMA):
                if self.dval[s] > 0:
                    self._wait(e, (("d", s), self.dval[s]))
            for o in self.eng:
                if o != e and self.cnt[o] > 0:
                    key, val = o, self.cnt[o]
                    if self.known[e].get(key, 0) < val:
                        self.eng[e].wait_ge(self.sem[key], val)
                        self.known[e][key] = val
                        self.nwaits += 1
        self.w = {}
        self.r = {}


class StopScan(Exception):
    pass


def _stop(tag):
    import os
    return os.environ.get("SCAN_STOP") == tag and _CUR_U[0] >= int(os.environ.get("SCAN_U", "0"))


_CUR_U = [0]


class TB:
    def __init__(self, ap, key):
        self.ap, self.key = ap, key

    def __getitem__(self, idx):
        return TB(self.ap[idx], self.key)

    def v(self, f):
        return TB(f(self.ap), self.key)


class Prog:
    def __init__(self, stop_after=None, dbg=None):
        self.nc = nc = bass.Bass("TRN2", target_bir_lowering=False)
        self.S = Sched(nc)
        self.stop_after = stop_after
        self.dbg = dbg
        dt = nc.dram_tensor
        self.inputs = {}
        self.x = self.inp("x", [T, D])
        self.norm_mix_g = self.inp("norm_mix_g", [L, D])
        self.w_in = self.inp("w_in", [L, D, P_IN])
        self.ident_f = self.inp("ident_f", [128, 128])
        self.P = dt("P_scr", [T, P_IN], F32, kind="Internal").ap()
        self.mixed = dt("mixed_scr", [T, MIXW], F32, kind="Internal").ap()
        self.Dnum = dt("Dnum_scr", [T, 6, 65], F32, kind="Internal").ap()
        self.X2 = dt("X2_scr", [T, D], F32, kind="Internal").ap()
        self.X3s = [dt("X3_scr%d" % i, [T, D], F32, kind="Internal").ap() for i in range(2)]
        self.X3 = self.X3s[0]
        self.norm_final_g = self.inp("norm_final_g", [1, D])
        self.H2 = dt("H2_scr", [T, D], BF16, kind="Internal").ap()
        self.AFFd = dt("AFF_scr", [T, NE], F32, kind="Internal").ap()
        self.IDXd = dt("IDX_scr", [NE, CAP], U32, kind="Internal").ap()
        self.w_out = self.inp("w_out", [L, MIXW, D])
        self.norm_ffn_g = self.inp("norm_ffn_g", [L, D])
        self.router_w = self.inp("router_w", [L, D, NE])
        self.router_b = self.inp("router_b", [L, NE])
        if stop_after not in ("inproj0", "BD", "outproj", "moe_idx", "AC", "prepAC"):
            self.w_gate = self.inp("expert_w_gate", [L, NE, D, D])
            self.w_up = self.inp("expert_w_up", [L, NE, D, D])
            self.w_down = self.inp("expert_w_down", [L, NE, D, D])
        self.ustrict = self.inp("ustrict", [128, 128])
        self.iota_c = self.inp("iota_c", [128, CAP])
        self.tvals = self.inp("tvals", [128, NT, 2])
        self.TM = dt("TM_scr", [T, 2, 2, 6, 256], F32, kind="Internal").ap()
        self.AUX = dt("AUX_scr", [T, 3, 256], F32, kind="Internal").ap()
        self.YAC = dt("YAC_scr", [T, 2, 2, 256], F32, kind="Internal").ap()
        self.rwkv_mu_prev = self.inp("rwkv_mu_prev", [L, 1024])
        self.rwkv_mu_next = self.inp("rwkv_mu_next", [L, 1024])
        self.rwkv_w0 = self.inp("rwkv_w0", [L, 2, 256])
        self.rwkv_w_up = self.inp("rwkv_w_up", [L, 2, 64, 256])
        self.rwkv_a0 = self.inp("rwkv_a0", [L, 2, 256])
        self.rwkv_a_up = self.inp("rwkv_a_up", [L, 2, 64, 256])
        self.rwkv_g_up = self.inp("rwkv_g_up", [L, 128, 256])
        self.rwkv_k_k = self.inp("rwkv_k_k", [L, 256])
        self.rwkv_k_a = self.inp("rwkv_k_a", [L, 256])
        self.rwkv_r_k = self.inp("rwkv_r_k", [L, 4, 64])
        self.rwkv_ln_w = self.inp("rwkv_ln_w", [L, 256])
        self.rwkv_ln_b = self.inp("rwkv_ln_b", [L, 256])
        self.gdn_conv = self.inp("gdn_conv", [L, 5, 768])
        self.gdn_a_log = self.inp("gdn_a_log", [L, 2, 4])
        self.gdn_dt_bias = self.inp("gdn_dt_bias", [L, 2, 4])
        self.gdn_norm_g = self.inp("gdn_norm_g", [L, 64])
        self.c_tri = self.inp("c_tri", [2, 128, 128])
        self.c_msk = self.inp("c_msk", [2, 128, 4, 128])
        self.c_onb = self.inp("c_onb", [128, 128])
        self.attn_q_norm = self.inp("attn_q_norm", [L, 64])
        self.attn_k_norm = self.inp("attn_k_norm", [L, 64])
        self.rope_cos = self.inp("rope_cos", [T, 64])
        self.rope_sin = self.inp("rope_sin", [T, 64])
        self.dbias = self.inp("dbias", [3, 2, 128, 3, 128])
        self.es = ExitStack()
        self.identf = self.sb(self.es, "identf", [128, 128], F32)
        self.identb = self.sb(self.es, "identb", [128, 128], BF16)
        self.AFF = self.sb(self.es, "AFF", [128, NT, NE], F32)
        S = self.S
        S.dma("sp", self.identf, self.ident_f, writes=["identf"])
        S.op("dve", lambda: nc.vector.tensor_copy(out=self.identb, in_=self.identf), reads=["identf"], writes=["identb"])

    def inp(self, name, shape, dtype=F32):
        self.inputs[name] = (tuple(shape), dtype)
        return self.nc.dram_tensor(name, list(shape), dtype, kind="ExternalInput").ap()

    def _uname(self, name):
        self.uid = getattr(self, "uid", 0) + 1
        return "%s_%d" % (name, self.uid)

    def sb(self, es, name, shape, dtype):
        return es.enter_context(self.nc.sbuf_tensor(self._uname(name), list(shape), dtype)).ap()

    def ps(self, es, name, shape, dtype=F32):
        return es.enter_context(self.nc.psum_tensor(self._uname(name), list(shape), dtype)).ap()

    def stage_inproj(self, l, xsrc):
        nc, S = self.nc, self.S
        with ExitStack() as es:
            W = self.sb(es, "Win", [128, 8, P_IN], BF16)
            gB = self.sb(es, "gB", [128, D], F32)
            xts = [self.sb(es, "xt%d" % i, [128, D], F32) for i in range(2)]
            junk = self.sb(es, "junk", [128, D], F32)
            hb = [self.sb(es, "hb%d" % i, [128, D], BF16) for i in range(2)]
            hT = [self.sb(es, "hT%d" % i, [128, 8, 128], BF16) for i in range(2)]
            pts = [self.sb(es, "pt%d" % i, [128, P_IN], F32) for i in range(2)]
            st = self.sb(es, "st", [128, NT, 4], F32)
            pT = self.ps(es, "pT", [128, 8, 128], BF16)
            pss = [self.ps(es, "psA%d" % i, [128, 512], F32) for i in range(4)]
            wv = self.w_in[l].rearrange("(k p) c -> p k c", p=128)
            for k in range(8):
                for c0 in (0, 1864):
                    S.dma("pool", W[:, k, c0:c0 + 1864], wv[:, k, c0:c0 + 1864], writes=[("W", k)])
            S.dma("sp", gB, self.norm_mix_g[l, :].partition_broadcast(128), writes=["gB"])
            nps = 0
            for i in range(NT):
                b = i % 2
                xt = xts[b]
                S.dma("sp", xt, xsrc[i * 128:(i + 1) * 128, :], writes=[("xt", b)])
                S.op("act", lambda: nc.scalar.activation(out=junk, in_=xt, func=AF.Square, accum_out=st[:, i, 0:1]),
                     reads=[("xt", b)], writes=["junk", ("st", i)])
                S.op("dve", lambda: nc.vector.tensor_scalar(out=st[:, i, 1:2], in0=st[:, i, 0:1], scalar1=1.0 / D, scalar2=EPS,
                                                            op0=ALU.mult, op1=ALU.add), reads=[("st", i)], writes=[("st", i)])
                S.op("act", lambda: nc.scalar.activation(out=st[:, i, 2:3], in_=st[:, i, 1:2], func=AF.Sqrt),
                     reads=[("st", i)], writes=[("st", i)])
                S.op("dve", lambda: nc.vector.reciprocal(out=st[:, i, 3:4], in_=st[:, i, 2:3]), reads=[("st", i)], writes=[("st", i)])
                S.op("dve", lambda: nc.vector.scalar_tensor_tensor(out=hb[b], in0=xt, scalar=st[:, i, 3:4], in1=gB,
                                                                   op0=ALU.mult, op1=ALU.mult),
                     reads=[("xt", b), ("st", i), "gB"], writes=[("hb", b)])
                for k in range(8):
                    S.op("pe", lambda: nc.tensor.transpose(out=pT[:, k, :], in_=hb[b][:, k * 128:(k + 1) * 128], identity=self.identb),
                         reads=[("hb", b), "identb"], writes=["pT"])
                S.op("act", lambda: nc.scalar.copy(out=hT[b], in_=pT), reads=["pT"], writes=[("hT", b)])
                for cg in range(8):
                    c0 = cg * 512
                    cw = min(512, P_IN - c0)
                    pb = nps % 4
                    nps += 1
                    for k in range(8):
                        S.op("pe", lambda: nc.tensor.matmul(pss[pb][:, :cw], lhsT=hT[b][:, k, :], rhs=W[:, k, c0:c0 + cw],
                                                            start=(k == 0), stop=(k == 7)),
                             reads=[("hT", b), ("W", k)], writes=[("psA", pb)])
                    if cg % 2 == 0:
                        S.op("dve", lambda: nc.vector.tensor_copy(out=pts[b][:, c0:c0 + cw], in_=pss[pb][:, :cw]),
                             reads=[("psA", pb)], writes=[("pt", b)])
                    else:
                        S.op("act", lambda: nc.scalar.copy(out=pts[b][:, c0:c0 + cw], in_=pss[pb][:, :cw]),
                             reads=[("psA", pb)], writes=[("pt", b)])
                S.dma("sp", self.P[i * 128:(i + 1) * 128, :], pts[b], reads=[("pt", b)])
        S.barrier()

    def rstd_ops(self, ss, tmp, rs, inv_n, eps, r, w):
        nc, S = self.nc, self.S
        S.op("dve", lambda: nc.vector.tensor_scalar(out=tmp, in0=ss, scalar1=inv_n, scalar2=eps, op0=ALU.mult, op1=ALU.add), reads=r, writes=w)
        S.op("act", lambda: nc.scalar.activation(out=tmp, in_=tmp, func=AF.Sqrt), reads=w, writes=w)
        S.op("dve", lambda: nc.vector.reciprocal(out=rs, in_=tmp), reads=w, writes=w)

    def stage_attnB(self, l):
        nc, S = self.nc, self.S
        with ExitStack() as es:
            qT = self.sb(es, "qT", [128, 2, T], BF16)
            kT = self.sb(es, "kT", [128, T], BF16)
            Va = self.sb(es, "Va", [128, NT, 2, 65], BF16)
            gqk = self.sb(es, "gqk", [128, 6, 64], F32)
            fbs = [self.sb(es, "fb%d" % i, [128, 512], F32) for i in range(2)]
            css = [self.sb(es, "cs%d" % i, [128, 2, 64], F32) for i in range(2)]
            sq = self.sb(es, "sqB", [128, 384], F32)
            ss = self.sb(es, "ssB", [128, 6], F32)
            tm = self.sb(es, "tmB", [128, 6], F32)
            rs = self.sb(es, "rsB", [128, 6], F32)
            qn = self.sb(es, "qnB", [128, 6, 64], F32)
            t1 = self.sb(es, "t1B", [128, 6, 64], F32)
            t2 = self.sb(es, "t2B", [128, 6, 64], F32)
            qkb = self.sb(es, "qkb", [128, 6, 64], BF16)
            pTb = [self.sb(es, "pTb%d" % i, [128, 512], BF16) for i in range(2)]
            oT = self.sb(es, "oT", [65, 512], F32)
            rc = self.sb(es, "rcB", [128, 4, 1], F32)
            ybt = [self.sb(es, "ybt%d" % i, [128, 4, 64], F32) for i in range(2)]
            pT3 = self.ps(es, "pT3", [128, 3, 128], BF16)
            ps_s = [self.ps(es, "ps_s%d" % i, [128, 512], F32) for i in range(2)]
            ps_o = self.ps(es, "ps_o", [65, 512], F32)
            ps_t = self.ps(es, "ps_t", [128, 4, 65], F32)
            for h in range(6):
                src = self.attn_q_norm if h < 4 else self.attn_k_norm
                S.dma("sp", gqk[:, h, :], src[l, :].partition_broadcast(128), writes=["gqk"])
            S.op("pool", lambda: nc.gpsimd.memset(Va[:, :, :, 64:65], 1.0), writes=["Va"])
            for i in range(NT):
                b = i % 2
                fb = fbs[b]
                S.dma("sp", fb, self.P[i * 128:(i + 1) * 128, OFF_B:OFF_B + 512], writes=[("fb", b)])
                S.dma("sp", css[b][:, 0, :], self.rope_cos[i * 128:(i + 1) * 128, :], writes=[("cs", b)])
                S.dma("sp", css[b][:, 1, :], self.rope_sin[i * 128:(i + 1) * 128, :], writes=[("cs", b)])
                S.op("dve", lambda: nc.vector.tensor_tensor(out=sq, in0=fb[:, 0:384], in1=fb[:, 0:384], op=ALU.mult), reads=[("fb", b)], writes=["sqB"])
                S.op("dve", lambda: nc.vector.tensor_reduce(out=ss, in_=sq.rearrange("p (h d) -> p h d", d=64), op=ALU.add, axis=AX.X), reads=["sqB"], writes=["ssB"])
                self.rstd_ops(ss, tm, rs, 1.0 / 64, EPS, ["ssB"], ["rsB"])
                f3 = fb[:, 0:384].rearrange("p (h d) -> p h d", d=64)
                S.op("dve", lambda: nc.vector.tensor_tensor(out=qn, in0=f3, in1=rs.unsqueeze(2).to_broadcast([128, 6, 64]), op=ALU.mult),
                     reads=[("fb", b), "rsB"], writes=["qnB"])
                S.op("pool", lambda: nc.gpsimd.tensor_tensor(out=qn, in0=qn, in1=gqk, op=ALU.mult), reads=["qnB", "gqk"], writes=["qnB"])
                cosb = css[b][:, 0, :].unsqueeze(1).to_broadcast([128, 6, 64])
                S.op("dve", lambda: nc.vector.tensor_tensor(out=t1, in0=qn, in1=cosb, op=ALU.mult), reads=["qnB", ("cs", b)], writes=["t1B"])
                q5 = qn.rearrange("p h (a f d) -> p h a f d", a=2, f=2)
                t5 = t2.rearrange("p h (a f d) -> p h a f d", a=2, f=2)
                s5 = css[b][:, 1, :].rearrange("p (a f d) -> p a f d", a=2, f=2)
                for hf in range(2):
                    for a in range(2):
                        S.op("pool", lambda: nc.gpsimd.tensor_tensor(out=t5[:, :, a, hf, :], in0=q5[:, :, a, 1 - hf, :],
                                                                     in1=s5[:, a, hf, :].unsqueeze(1).to_broadcast([128, 6, 16]), op=ALU.mult),
                             reads=["qnB", ("cs", b)], writes=["t2B"])
                S.op("dve", lambda: nc.vector.tensor_tensor(out=qkb[:, 0:4, :].rearrange("p (b a) d -> p a b d", b=2, a=2),
                                                            in0=t1[:, 0:4, :].rearrange("p (a b) d -> p a b d", a=2, b=2),
                                                            in1=t2[:, 0:4, :].rearrange("p (a b) d -> p a b d", a=2, b=2), op=ALU.add),
                     reads=["t1B", "t2B"], writes=["qkb"])
                S.op("dve", lambda: nc.vector.tensor_tensor(out=qkb[:, 4:6, :], in0=t1[:, 4:6, :], in1=t2[:, 4:6, :], op=ALU.add),
                     reads=["t1B", "t2B"], writes=["qkb"])
                S.op("act", lambda: nc.scalar.copy(out=Va[:, i, :, 0:64], in_=fb[:, 384:512].rearrange("p (h d) -> p h d", d=64)),
                     reads=[("fb", b)], writes=["Va"])
                for c in range(3):
                    S.op("pe", lambda: nc.tensor.transpose(out=pT3[:, c, :], in_=qkb[:, 2 * c:2 * c + 2, :], identity=self.identb),
                         reads=["qkb", "identb"], writes=["pT3"])
                S.op("act", lambda: nc.scalar.copy(out=qT[:, :, i * 128:(i + 1) * 128], in_=pT3[:, 0:2, :]), reads=["pT3"], writes=["qT"])
                S.op("act", lambda: nc.scalar.copy(out=kT[:, i * 128:(i + 1) * 128], in_=pT3[:, 2, :]), reads=["pT3"], writes=["kT"])
            n = 0
            for qh in range(4):
                kv = qh // 2
                base = 64 * kv
                ch = qh % 2
                for qc in range(8):
                    def s_mm(kb_):
                        pb_ = kb_ % 2
                        S.op("pe", lambda: nc.tensor.matmul(ps_s[pb_], lhsT=kT[base:base + 64, kb_ * 128:(kb_ + 1) * 128],
                                                            rhs=qT[base:base + 64, ch, qc * 512:(qc + 1) * 512], start=True, stop=True),
                             reads=["kT", "qT"], writes=[("ps_s", pb_)])
                    s_mm(0)
                    for kb in range(NT):
                        pb = kb % 2
                        if kb + 1 < NT:
                            s_mm(kb + 1)
                        S.op("act", lambda: nc.scalar.activation(out=pTb[pb], in_=ps_s[pb], func=AF.Exp, scale=0.125),
                             reads=[("ps_s", pb)], writes=[("pTb", pb)])
                        S.op("pe", lambda: nc.tensor.matmul(ps_o, lhsT=Va[:, kb, kv, :], rhs=pTb[pb], start=(kb == 0), stop=(kb == NT - 1)),
                             reads=["Va", ("pTb", pb)], writes=["ps_o"])
                    S.op("dve", lambda: nc.vector.tensor_copy(out=oT, in_=ps_o), reads=["ps_o"], writes=["oT"])
                    for j in range(4):
                        S.op("pe", lambda: nc.tensor.transpose(out=ps_t[:, j, :], in_=oT[:, j * 128:(j + 1) * 128], identity=self.identf[0:65, 0:65]),
                             reads=["oT", "identf"], writes=["ps_t"])
                    S.op("dve", lambda: nc.vector.reciprocal(out=rc, in_=ps_t[:, :, 64:65]), reads=["ps_t"], writes=["rcB"])
                    yb = ybt[qc % 2]
                    S.op("dve", lambda: nc.vector.tensor_tensor(out=yb, in0=ps_t[:, :, 0:64], in1=rc.to_broadcast([128, 4, 64]), op=ALU.mult),
                         reads=["ps_t", "rcB"], writes=[("ybt", qc % 2)])
                    S.dma("sp", self.mixed[qc * 512:(qc + 1) * 512, 256 + qh * 64:256 + (qh + 1) * 64].rearrange("(j p) d -> p j d", p=128),
                          yb, reads=[("ybt", qc % 2)])
        S.barrier()

    def stage_attnD(self, l):
        nc, S = self.nc, self.S
        with ExitStack() as es:
            dB = self.sb(es, "dB", [128, 6, 3, 128], F32)
            qTd = self.sb(es, "qTd", [128, T], BF16)
            kTd = self.sb(es, "kTd", [128, T], BF16)
            Vd = self.sb(es, "Vd", [128, NT, 2, 65], BF16)
            fds = [self.sb(es, "fd%d" % i, [128, 3, 128], F32) for i in range(2)]
            qkd = self.sb(es, "qkd", [128, 2, 128], BF16)
            sd = [self.sb(es, "sd%d" % i, [128, 3, 128], F32) for i in range(2)]
            pd = [self.sb(es, "pd%d" % i, [128, 3, 128], BF16) for i in range(2)]
            od = [self.sb(es, "od%d" % i, [128, 2, 65], F32) for i in range(2)]
            dn = [self.sb(es, "dn%d" % i, [128, 6, 65], F32) for i in range(2)]
            zs = self.sb(es, "zsD", [128, 2], F32)
            rz = self.sb(es, "rzD", [128, 2], F32)
            yd = [self.sb(es, "yd%d" % i, [128, 6, 64], F32) for i in range(2)]
            pTd = self.ps(es, "pTd", [128, 2, 128], BF16)
            ps_sd = [self.ps(es, "ps_sd%d" % i, [128, 3, 128], F32) for i in range(2)]
            ps_od = [self.ps(es, "ps_od%d" % i, [128, 2, 65], F32) for i in range(2)]
            for br in range(3):
                for j in range(2):
                    S.dma("sp", dB[:, br * 2 + j, :, :], self.dbias[br, j], writes=["dB"])
            S.op("pool", lambda: nc.gpsimd.memset(Vd[:, :, :, 64:65], 1.0), writes=["Vd"])
            Pd = self.P[:, OFF_D:OFF_D + D_IN].rearrange("r (t x) -> r t x", t=3)
            for br, dil in enumerate((1, 4, 16)):
                nb = NT // dil
                Pv = Pd.rearrange("(n m d) t x -> d n m t x", m=128, d=dil)
                Dv = self.Dnum.rearrange("(n m d) s c -> d n m s c", m=128, d=dil)
                for r in range(dil):
                    for b in range(nb):
                        ti = r * nb + b
                        fb = ti % 2
                        S.dma("sp", fds[fb], Pv[r, b][:, :, br * 128:(br + 1) * 128], writes=[("fd", fb)])
                        S.op("act", lambda: nc.scalar.copy(out=qkd, in_=fds[fb][:, 0:2, :]), reads=[("fd", fb)], writes=["qkd"])
                        S.op("pool", lambda: nc.gpsimd.tensor_copy(out=Vd[:, ti, :, 0:64], in_=fds[fb][:, 2, :].rearrange("p (h d) -> p h d", d=64)),
                             reads=[("fd", fb)], writes=["Vd"])
                        for c in range(2):
                            S.op("pe", lambda: nc.tensor.transpose(out=pTd[:, c, :], in_=qkd[:, c, :], identity=self.identb),
                                 reads=["qkd", "identb"], writes=["pTd"])
                        S.op("dve", lambda: nc.vector.tensor_copy(out=qTd[:, ti * 128:(ti + 1) * 128], in_=pTd[:, 0, :]), reads=["pTd"], writes=["qTd"])
                        S.op("dve", lambda: nc.vector.tensor_copy(out=kTd[:, ti * 128:(ti + 1) * 128], in_=pTd[:, 1, :]), reads=["pTd"], writes=["kTd"])
                for r in range(dil):
                    for b in range(nb):
                        ti = r * nb + b
                        ob = ti % 2
                        for j in range(2):
                            base = 64 * j
                            rels = [rel for rel in range(3) if 0 <= b + rel - 1 < nb]
                            r0, r1 = rels[0], rels[-1] + 1
                            for rel in rels:
                                kt = ti + rel - 1
                                S.op("pe", lambda: nc.tensor.matmul(ps_sd[j][:, rel, :], lhsT=kTd[base:base + 64, kt * 128:(kt + 1) * 128],
                                                                    rhs=qTd[base:base + 64, ti * 128:(ti + 1) * 128], start=True, stop=True),
                                     reads=["kTd", "qTd"], writes=[("ps_sd", j)])
                            S.op("dve", lambda: nc.vector.scalar_tensor_tensor(out=sd[j][:, r0:r1, :], in0=ps_sd[j][:, r0:r1, :], scalar=0.125,
                                                                               in1=dB[:, br * 2 + j, r0:r1, :], op0=ALU.mult, op1=ALU.add),
                                 reads=[("ps_sd", j), "dB"], writes=[("sd", j)])
                            S.op("act", lambda: nc.scalar.activation(out=pd[j][:, r0:r1, :], in_=sd[j][:, r0:r1, :], func=AF.Exp),
                                 reads=[("sd", j)], writes=[("pd", j)])
                            for rel in rels:
                                kt = ti + rel - 1
                                S.op("pe", lambda: nc.tensor.matmul(ps_od[ob][:, j, :], lhsT=pd[j][:, rel, :], rhs=Vd[:, kt, j, :],
                                                                    start=(rel == rels[0]), stop=(rel == rels[-1])),
                                     reads=[("pd", j), "Vd"], writes=[("ps_od", ob)])
                        S.op("dve", lambda: nc.vector.tensor_copy(out=od[ob], in_=ps_od[ob]), reads=[("ps_od", ob)], writes=[("od", ob)])
                        S.dma("sp", Dv[r, b][:, br * 2:br * 2 + 2, :], od[ob], reads=[("od", ob)])
            S.barrier()
            for i in range(NT):
                b = i % 2
                S.dma("sp", dn[b], self.Dnum[i * 128:(i + 1) * 128], writes=[("dn", b)])
                z3 = dn[b][:, :, 64].rearrange("p (r j) -> p r j", j=2)
                S.op("dve", lambda: nc.vector.tensor_tensor(out=zs, in0=z3[:, 0, :], in1=z3[:, 1, :], op=ALU.add), reads=[("dn", b)], writes=["zsD"])
                S.op("dve", lambda: nc.vector.tensor_tensor(out=zs, in0=zs, in1=z3[:, 2, :], op=ALU.add), reads=[("dn", b), "zsD"], writes=["zsD"])
                S.op("dve", lambda: nc.vector.reciprocal(out=rz, in_=zs), reads=["zsD"], writes=["rzD"])
                for br in range(3):
                    S.op("dve", lambda: nc.vector.tensor_tensor(out=yd[b][:, br * 2:br * 2 + 2, :], in0=dn[b][:, br * 2:br * 2 + 2, 0:64],
                                                                in1=rz.unsqueeze(2).to_broadcast([128, 2, 64]), op=ALU.mult),
                         reads=[("dn", b), "rzD"], writes=[("yd", b)])
                S.dma("sp", self.mixed[i * 128:(i + 1) * 128, 768:1152], yd[b], reads=[("yd", b)])
        S.barrier()

    def stage_outproj(self, l, xsrc):
        nc, S = self.nc, self.S
        with ExitStack() as es:
            Wo = self.sb(es, "Wo", [128, 9, D], BF16)
            g2 = self.sb(es, "g2B", [128, D], F32)
            Wr = self.sb(es, "Wr", [128, 8, NE], F32)
            rbB = self.sb(es, "rbB", [128, NE], F32)
            mts = [self.sb(es, "mt%d" % i, [128, MIXW], F32) for i in range(2)]
            mb = self.sb(es, "mb", [128, MIXW], BF16)
            mT = self.sb(es, "mT", [128, 9, 128], BF16)
            xts = [self.sb(es, "xo%d" % i, [128, D], F32) for i in range(2)]
            x2s = [self.sb(es, "x2t%d" % i, [128, D], F32) for i in range(2)]
            junk = self.sb(es, "junkO", [128, D], F32)
            st = self.sb(es, "stO", [128, NT, 4], F32)
            h2f = self.sb(es, "h2f", [128, D], F32)
            h2b = [self.sb(es, "h2b%d" % i, [128, D], BF16) for i in range(2)]
            h2T = self.sb(es, "h2T", [128, 8, 128], F32)
            lg = self.sb(es, "lgO", [128, NE], F32)
            mx = self.sb(es, "mxO", [128, 4], F32)
            ex = self.sb(es, "exO", [128, NE], F32)
            pTa = self.ps(es, "pTa", [128, 8, 128], BF16)
            pTc = self.ps(es, "pTc", [128, 1, 128], BF16)
            pso = [self.ps(es, "pso%d" % i, [128, 512], F32) for i in range(2)]
            pT4 = [self.ps(es, "pT4%d" % i, [128, 4, 128], F32) for i in range(2)]
            psr = self.ps(es, "psr", [128, NE], F32)
            wv = self.w_out[l].rearrange("(k p) c -> p k c", p=128)
            for k in range(9):
                S.dma("pool", Wo[:, k, :], wv[:, k, :], writes=["Wo"])
            S.dma("sp", g2, self.norm_ffn_g[l, :].partition_broadcast(128), writes=["g2B"])
            S.dma("sp", Wr, self.router_w[l].rearrange("(k p) e -> p k e", p=128), writes=["Wr"])
            S.dma("sp", rbB, self.router_b[l, :].partition_broadcast(128), writes=["rbB"])
            for i in range(NT):
                b = i % 2
                rows = slice(i * 128, (i + 1) * 128)
                S.dma("sp", mts[b], self.mixed[rows, :], writes=[("mt", b)])
                S.dma("sp", xts[b], xsrc[rows, :], writes=[("xo", b)])
                S.op("act", lambda: nc.scalar.copy(out=mb, in_=mts[b]), reads=[("mt", b)], writes=["mb"])
                for k in range(9):
                    dst = pTa[:, k, :] if k < 8 else pTc[:, 0, :]
                    S.op("pe", lambda: nc.tensor.transpose(out=dst, in_=mb[:, k * 128:(k + 1) * 128], identity=self.identb),
                         reads=["mb", "identb"], writes=["pTa" if k < 8 else "pTc"])
                S.op("act", lambda: nc.scalar.copy(out=mT[:, 0:8, :], in_=pTa), reads=["pTa"], writes=["mT"])
                S.op("dve", lambda: nc.vector.tensor_copy(out=mT[:, 8:9, :], in_=pTc), reads=["pTc"], writes=["mT"])
                x2 = x2s[b]
                for hf in range(2):
                    for k in range(9):
                        S.op("pe", lambda: nc.tensor.matmul(pso[hf], lhsT=mT[:, k, :], rhs=Wo[:, k, hf * 512:(hf + 1) * 512], start=(k == 0), stop=(k == 8)),
                             reads=["mT", "Wo"], writes=[("pso", hf)])
                    S.op("dve", lambda: nc.vector.tensor_tensor(out=x2[:, hf * 512:(hf + 1) * 512], in0=pso[hf], in1=xts[b][:, hf * 512:(hf + 1) * 512], op=ALU.add),
                         reads=[("pso", hf), ("xo", b)], writes=[("x2t", b)])
                S.dma("sp", self.X2[rows, :], x2, reads=[("x2t", b)])
                S.dma("sp", self.X3[rows, :], x2, reads=[("x2t", b)])
                S.op("act", lambda: nc.scalar.activation(out=junk, in_=x2, func=AF.Square, accum_out=st[:, i, 0:1]), reads=[("x2t", b)], writes=["junkO", ("stO", i)])
                self.rstd_ops(st[:, i, 0:1], st[:, i, 1:2], st[:, i, 2:3], 1.0 / D, EPS, [("stO", i)], [("stO", i)])
                S.op("dve", lambda: nc.vector.scalar_tensor_tensor(out=h2f, in0=x2, scalar=st[:, i, 2:3], in1=g2, op0=ALU.mult, op1=ALU.mult),
                     reads=[("x2t", b), ("stO", i), "g2B"], writes=["h2f"])
                S.op("act", lambda: nc.scalar.copy(out=h2b[b], in_=h2f), reads=["h2f"], writes=[("h2b", b)])
                S.dma("sp", self.H2[rows, :], h2b[b], reads=[("h2b", b)])
                for k in range(8):
                    S.op("pe", lambda: nc.tensor.transpose(out=pT4[k // 4][:, k % 4, :], in_=h2f[:, k * 128:(k + 1) * 128], identity=self.identf),
                         reads=["h2f", "identf"], writes=[("pT4", k // 4)])
                S.op("act", lambda: nc.scalar.copy(out=h2T[:, 0:4, :], in_=pT4[0]), reads=[("pT4", 0)], writes=["h2T"])
                S.op("dve", lambda: nc.vector.tensor_copy(out=h2T[:, 4:8, :], in_=pT4[1]), reads=[("pT4", 1)], writes=["h2T"])
                for k in range(8):
                    S.op("pe", lambda: nc.tensor.matmul(psr, lhsT=h2T[:, k, :], rhs=Wr[:, k, :], start=(k == 0), stop=(k == 7)),
                         reads=["h2T", "Wr"], writes=["psr"])
                S.op("dve", lambda: nc.vector.tensor_tensor(out=lg, in0=psr, in1=rbB, op=ALU.add), reads=["psr", "rbB"], writes=["lgO"])
                S.op("dve", lambda: nc.vector.tensor_reduce(out=mx[:, 0:1], in_=lg, op=ALU.max, axis=AX.X), reads=["lgO"], writes=["mxO"])
                S.op("dve", lambda: nc.vector.tensor_scalar(out=lg, in0=lg, scalar1=mx[:, 0:1], scalar2=None, op0=ALU.subtract), reads=["lgO", "mxO"], writes=["lgO"])
                S.op("act", lambda: nc.scalar.activation(out=ex, in_=lg, func=AF.Exp), reads=["lgO"], writes=["exO"])
                S.op("dve", lambda: nc.vector.tensor_reduce(out=mx[:, 2:3], in_=ex, op=ALU.add, axis=AX.X), reads=["exO"], writes=["mxO"])
                S.op("dve", lambda: nc.vector.reciprocal(out=mx[:, 3:4], in_=mx[:, 2:3]), reads=["mxO"], writes=["mxO"])
                S.op("dve", lambda: nc.vector.tensor_scalar(out=self.AFF[:, i, :], in0=ex, scalar1=mx[:, 3:4], scalar2=None, op0=ALU.mult),
                     reads=["exO", "mxO"], writes=["AFF"])
                S.dma("sp", self.AFFd[rows, :], self.AFF[:, i, :], reads=["AFF"])
        S.barrier()

    def stage_moe(self, l):
        nc, S = self.nc, self.S
        AFF = self.AFF
        with ExitStack() as es:
            IDX = self.sb(es, "IDX", [128, NE, 4], U32)
            es1 = ExitStack()
            es_outer = es
            es = es1
            lo = self.sb(es, "loM", [128, NE], F32)
            mid = self.sb(es, "midM", [128, NE], F32)
            cmp_ = self.sb(es, "cmpM", [128, NT, NE], F32)
            pc = self.sb(es, "pcM", [128, NE], F32)
            ge = self.sb(es, "geM", [128, NE], F32)
            onesb = self.sb(es, "onesb", [128, 128], BF16)
            onesf = self.sb(es, "onesf", [128, 128], F32)
            Ub = self.sb(es, "Ub", [128, 128], BF16)
            selb = self.sb(es, "selb", [128, NT, NE], BF16)
            Uf = self.sb(es, "Uf", [128, 128], F32)
            self_f = self.sb(es, "self", [128, NT, NE], F32)
            tot = [self.sb(es, "totM%d" % i, [128, NT, NE], F32) for i in range(2)]
            tot0 = self.sb(es, "tot0", [128, NT, NE], F32)
            rank = self.sb(es, "rankM", [128, NT, NE], F32)
            iotaC = self.sb(es, "iotaC", [128, CAP], F32)
            tvf = self.sb(es, "tvf", [128, NT, 2], F32)
            oh = [self.sb(es, "oh%d" % i, [128, CAP], F32) for i in range(3)]
            rws = self.sb(es, "rws", [2, CAP], F32)
            rwu = self.sb(es, "rwu", [2, CAP], U32)
            IDXf = self.sb(es, "IDXf", [128, NE, 4], F32)
            psc = self.ps(es, "psc", [128, NE], F32)
            psp = self.ps(es, "psp", [128, 512], F32)
            pst = self.ps(es, "pst", [128, 512], F32)
            psid = [self.ps(es, "psid%d" % i, [128, NE, 2], F32) for i in range(4)]
            IDX2 = self.sb(es, "IDX2", [128, NE, 4, 2], F32)
            S.dma("sp", Uf, self.ustrict, writes=["Uf"])
            S.op("dve", lambda: nc.vector.tensor_copy(out=Ub, in_=Uf), reads=["Uf"], writes=["Ub"])
            S.op("dve", lambda: nc.vector.memset(onesb, 1.0), writes=["onesb"])
            S.op("dve", lambda: nc.vector.memset(onesf, 1.0), writes=["onesf"])
            S.dma("sp", iotaC, self.iota_c, writes=["iotaC"])
            S.dma("sp", tvf, self.tvals, writes=["tvf"])
            S.op("dve", lambda: nc.vector.memset(lo, 0.0), writes=["lo"])
            for it in range(32):
                c = 2.0 ** -(it + 1)
                S.op("dve", lambda: nc.vector.tensor_scalar(out=mid, in0=lo, scalar1=c, scalar2=None, op0=ALU.add), reads=["lo"], writes=["mid"])
                S.op("dve", lambda: nc.vector.tensor_tensor(out=cmp_, in0=AFF, in1=mid.unsqueeze(1).to_broadcast([128, NT, NE]), op=ALU.is_ge),
                     reads=["AFF", "mid"], writes=["cmp"])
                S.op("dve", lambda: nc.vector.tensor_reduce(out=pc, in_=cmp_.rearrange("p t e -> p e t"), op=ALU.add, axis=AX.X), reads=["cmp"], writes=["pc"])
                S.op("pe", lambda: nc.tensor.matmul(psc, lhsT=onesf, rhs=pc, start=True, stop=True), reads=["onesf", "pc"], writes=["psc"])
                S.op("dve", lambda: nc.vector.tensor_single_scalar(out=ge, in_=psc, scalar=CAP - 0.5, op=ALU.is_ge), reads=["psc"], writes=["ge"])
                S.op("dve", lambda: nc.vector.scalar_tensor_tensor(out=lo, in0=ge, scalar=c, in1=lo, op0=ALU.mult, op1=ALU.add), reads=["ge", "lo"], writes=["lo"])
            import os
            if os.environ.get("MOE_STOP") == "1":
                S.dma("sp", self.dbg_small[:, 64:80], lo, reads=["lo"])
                S.barrier()
                es1.close()
                return
            S.op("dve", lambda: nc.vector.tensor_tensor(out=self_f, in0=AFF, in1=lo.unsqueeze(1).to_broadcast([128, NT, NE]), op=ALU.is_ge),
                 reads=["AFF", "lo"], writes=["self"])
            S.op("dve", lambda: nc.vector.tensor_tensor(out=selb, in0=AFF, in1=lo.unsqueeze(1).to_broadcast([128, NT, NE]), op=ALU.is_ge),
                 reads=["AFF", "lo"], writes=["selb"])
            sel2 = selb.rearrange("p t e -> p (t e)")
            S.op("pe", lambda: nc.tensor.matmul(psp, lhsT=Ub, rhs=sel2, start=True, stop=True), reads=["Ub", "selb"], writes=["psp"])
            S.op("pe", lambda: nc.tensor.matmul(pst, lhsT=onesb, rhs=sel2, start=True, stop=True), reads=["onesb", "selb"], writes=["pst"])
            S.op("dve", lambda: nc.vector.tensor_copy(out=tot0.rearrange("p t e -> p (t e)"), in_=pst), reads=["pst"], writes=["tot0"])
            S.op("dve", lambda: nc.vector.tensor_copy(out=tot[0].rearrange("p t e -> p (t e)"), in_=pst), reads=["pst"], writes=[("tot", 0)])
            if os.environ.get("MOE_STOP") == "1b":
                S.dma("sp", self.dbg_small[:, 0:16], tot[0][:, 5, :], reads=[("tot", 0)])
                S.barrier()
                es1.close()
                return
            cur = 0
            for sft in (1, 2, 4, 8, 16):
                a, bb = tot[cur], tot[1 - cur]
                S.op("pool", lambda: nc.gpsimd.tensor_copy(out=bb[:, 0:sft, :], in_=a[:, 0:sft, :]), reads=[("tot", cur)], writes=[("tot", 1 - cur)])
                S.op("dve", lambda: nc.vector.tensor_tensor(out=bb[:, sft:NT, :], in0=a[:, sft:NT, :], in1=a[:, 0:NT - sft, :], op=ALU.add),
                     reads=[("tot", cur)], writes=[("tot", 1 - cur)])
                cur = 1 - cur
            inc = tot[cur]
            if os.environ.get("MOE_STOP") == "1c":
                S.dma("sp", self.dbg_small[:, 0:16], inc[:, 5, :], reads=[("tot", cur)])
                S.barrier()
                es1.close()
                return
            S.op("dve", lambda: nc.vector.tensor_tensor(out=rank, in0=inc, in1=tot0, op=ALU.subtract), reads=[("tot", cur), "tot0"], writes=["rank"])
            S.op("dve", lambda: nc.vector.tensor_tensor(out=rank.rearrange("p t e -> p (t e)"), in0=rank.rearrange("p t e -> p (t e)"), in1=psp, op=ALU.add),
                 reads=["rank", "psp"], writes=["rank"])
            S.op("dve", lambda: nc.vector.scalar_tensor_tensor(out=rank, in0=rank, scalar=-9999.0, in1=self_f, op0=ALU.add, op1=ALU.mult),
                 reads=["rank", "self"], writes=["rank"])
            S.op("dve", lambda: nc.vector.tensor_scalar(out=rank, in0=rank, scalar1=9999.0, scalar2=None, op0=ALU.add), reads=["rank"], writes=["rank"])
            if os.environ.get("MOE_STOP") == "2":
                S.dma("sp", self.dbg_small[:, 0:16], rank[:, 5, :], reads=["rank"])
                S.barrier()
                es1.close()
                return
            n = 0
            for e in range(NE):
                for i in range(NT):
                    ob = n % 3
                    n += 1
                    S.op("dve", lambda: nc.vector.tensor_scalar(out=oh[ob], in0=iotaC, scalar1=rank[:, i, e:e + 1], scalar2=None, op0=ALU.is_equal),
                         reads=["iotaC", "rank"], writes=[("oh", ob)])
                    for cc in range(4):
                        S.op("pe", lambda: nc.tensor.matmul(psid[cc][:, e, :], lhsT=oh[ob][:, cc * 128:(cc + 1) * 128], rhs=tvf[:, i, :],
                                                            start=(i == 0), stop=(i == NT - 1)),
                             reads=["tvf", ("oh", ob)], writes=[("psid", cc)])
            for cc in range(4):
                S.op("dve", lambda: nc.vector.tensor_tensor(out=IDXf[:, :, cc], in0=psid[cc][:, :, 0], in1=psid[cc][:, :, 1], op=ALU.add) if False else
                     nc.vector.tensor_copy(out=IDX2[:, :, cc, :], in_=psid[cc]), reads=[("psid", cc)], writes=["IDX2"])
            S.op("dve", lambda: nc.vector.tensor_tensor(out=IDXf, in0=IDX2[:, :, :, 0], in1=IDX2[:, :, :, 1], op=ALU.add), reads=["IDX2"], writes=["IDXf"])
            S.op("dve", lambda: nc.vector.tensor_copy(out=IDX, in_=IDXf), reads=["IDXf"], writes=["IDX"])
            if self.stop_after == "moe_idx":
                S.dma("sp", self.dbg_small[:, 0:64], IDXf.rearrange("p e c -> p (e c)"), reads=["IDXf"])
                S.dma("sp", self.dbg_small[:, 64:80], lo, reads=["lo"])
                S.barrier()
                es1.close()
                return
            S.barrier()
            es1.close()
            es = es_outer
            Wg = [self.sb(es, "Wg%d" % i, [128, 8, D], BF16) for i in range(2)]
            Wu = [self.sb(es, "Wu%d" % i, [128, 8, D], BF16) for i in range(2)]
            Wd = [self.sb(es, "Wd%d" % i, [128, 8, D], BF16) for i in range(2)]
            xs = [self.sb(es, "xs%d" % i, [128, 4, D], BF16) for i in range(2)]
            gt = [self.sb(es, "gt%d" % i, [128, 4, NE], F32) for i in range(2)]
            xsT = self.sb(es, "xsT", [128, 8, CAP], BF16)
            sg = [self.sb(es, "sg%d" % i, [128, CAP], F32) for i in range(2)]
            hid = self.sb(es, "hid", [128, 8, CAP], BF16)
            yt = [self.sb(es, "yt%d" % i, [128, D], F32) for i in range(2)]
            psx = self.ps(es, "psx", [128, 8, 128], BF16)
            psg2 = [self.ps(es, "psg%d" % i, [128, CAP], F32) for i in range(2)]
            psu2 = [self.ps(es, "psu%d" % i, [128, CAP], F32) for i in range(2)]
            psy = [self.ps(es, "psy%d" % i, [128, 512], F32) for i in range(2)]

            def load_w(e):
                wb = e % 2
                for (dst, src, nm) in ((Wg[wb], self.w_gate, "Wg"), (Wu[wb], self.w_up, "Wu"), (Wd[wb], self.w_down, "Wd")):
                    sv = src[l, e].rearrange("(k p) f -> p k f", p=128)
                    for k0 in (0, 4):
                        S.dma("pool", dst[:, k0:k0 + 4, :], sv[:, k0:k0 + 4, :], writes=[(nm, wb)])

            def gather(e):
                wb = e % 2
                for cc in range(4):
                    S.dma_raw("pool", lambda: nc.gpsimd.indirect_dma_start(out=xs[wb][:, cc, :], out_offset=None, in_=self.H2,
                                                                           in_offset=bass.IndirectOffsetOnAxis(ap=IDX[:, e, cc:cc + 1], axis=0)),
                              reads=["IDX"], writes=[("xs", wb)])
                    S.dma_raw("pool", lambda: nc.gpsimd.indirect_dma_start(out=gt[wb][:, cc, :], out_offset=None, in_=self.AFFd,
                                                                           in_offset=bass.IndirectOffsetOnAxis(ap=IDX[:, e, cc:cc + 1], axis=0)),
                              reads=["IDX"], writes=[("gt", wb)])

            load_w(0)
            gather(0)
            ny = 0
            for e in range(NE):
                wb = e % 2
                if e + 1 < NE:
                    load_w(e + 1)
                    gather(e + 1)
                for cc in range(4):
                    for k in range(8):
                        S.op("pe", lambda: nc.tensor.transpose(out=psx[:, k, :], in_=xs[wb][:, cc, k * 128:(k + 1) * 128], identity=self.identb),
                             reads=[("xs", wb), "identb"], writes=["psx"])
                    S.op("act", lambda: nc.scalar.copy(out=xsT[:, :, cc * 128:(cc + 1) * 128], in_=psx), reads=["psx"], writes=["xsT"])
                for f in range(8):
                    psg, psu = psg2[f % 2], psu2[f % 2]
                    for k in range(8):
                        S.op("pe", lambda: nc.tensor.matmul(psg, lhsT=Wg[wb][:, k, f * 128:(f + 1) * 128], rhs=xsT[:, k, :], start=(k == 0), stop=(k == 7)),
                             reads=[("Wg", wb), "xsT"], writes=[("psg", f % 2)])
                    for k in range(8):
                        S.op("pe", lambda: nc.tensor.matmul(psu, lhsT=Wu[wb][:, k, f * 128:(f + 1) * 128], rhs=xsT[:, k, :], start=(k == 0), stop=(k == 7)),
                             reads=[("Wu", wb), "xsT"], writes=[("psu", f % 2)])
                    S.op("act", lambda: nc.scalar.activation(out=sg[f % 2], in_=psg, func=AF.Silu), reads=[("psg", f % 2)], writes=[("sg", f % 2)])
                    S.op("dve", lambda: nc.vector.tensor_tensor(out=hid[:, f, :], in0=sg[f % 2], in1=psu, op=ALU.mult), reads=[("sg", f % 2), ("psu", f % 2)], writes=[("hid", f)])
                for cc in range(4):
                    yb = ny % 2
                    ny += 1
                    for hf in range(2):
                        for f in range(8):
                            S.op("pe", lambda: nc.tensor.matmul(psy[hf], lhsT=hid[:, f, cc * 128:(cc + 1) * 128], rhs=Wd[wb][:, f, hf * 512:(hf + 1) * 512],
                                                                start=(f == 0), stop=(f == 7)), reads=[("hid", f), ("Wd", wb)], writes=[("psy", hf)])
                        S.op("dve", lambda: nc.vector.tensor_scalar(out=yt[yb][:, hf * 512:(hf + 1) * 512], in0=psy[hf], scalar1=gt[wb][:, cc, e:e + 1], scalar2=None, op0=ALU.mult),
                             reads=[("psy", hf), ("gt", wb)], writes=[("yt", yb)])
                    S.dma_raw("pool", lambda: nc.gpsimd.indirect_dma_start(out=self.X3, out_offset=bass.IndirectOffsetOnAxis(ap=IDX[:, e, cc:cc + 1], axis=0),
                                                                           in_=yt[yb], in_offset=None, compute_op=ALU.add),
                              reads=[("yt", yb), "IDX", "X3"], writes=["X3"])
        S.barrier()

    def _e(self, eng):
        return self.S.eng[eng]

    def tt(self, eng, out, a, b, op):
        self.S.op(eng, lambda: self._e(eng).tensor_tensor(out=out.ap, in0=a.ap, in1=b.ap, op=op), reads=[a.key, b.key], writes=[out.key])

    def ts(self, eng, out, a, s1, op0, s2=None, op1=None):
        rd = [a.key]
        v1 = s1
        if isinstance(s1, TB):
            rd.append(s1.key)
            v1 = s1.ap
        kw = {}
        if op1 is not None:
            kw["op1"] = op1
        self.S.op(eng, lambda: self._e(eng).tensor_scalar(out=out.ap, in0=a.ap, scalar1=v1, scalar2=s2, op0=op0, **kw), reads=rd, writes=[out.key])

    def stt(self, out, a, scalar, b, op0, op1):
        rd = [a.key, b.key]
        sv = scalar
        if isinstance(scalar, TB):
            rd.append(scalar.key)
            sv = scalar.ap
        self.S.op("dve", lambda: self.nc.vector.scalar_tensor_tensor(out=out.ap, in0=a.ap, scalar=sv, in1=b.ap, op0=op0, op1=op1), reads=rd, writes=[out.key])

    def act(self, out, a, func, scale=1.0, bias=None):
        rd = [a.key]
        kw = {}
        if isinstance(bias, TB):
            rd.append(bias.key)
            kw["bias"] = bias.ap
        elif bias is not None:
            kw["bias"] = bias
        self.S.op("act", lambda: self.nc.scalar.activation(out=out.ap, in_=a.ap, func=func, scale=scale, **kw), reads=rd, writes=[out.key])

    def cp(self, eng, out, a):
        if eng == "act":
            self.S.op("act", lambda: self.nc.scalar.copy(out=out.ap, in_=a.ap), reads=[a.key], writes=[out.key])
        else:
            self.S.op(eng, lambda: self._e(eng).tensor_copy(out=out.ap, in_=a.ap), reads=[a.key], writes=[out.key])

    def red(self, out, a, op=None):
        self.S.op("dve", lambda: self.nc.vector.tensor_reduce(out=out.ap, in_=a.ap, op=(op or ALU.add), axis=AX.X), reads=[a.key], writes=[out.key])

    def rcp(self, out, a):
        self.S.op("dve", lambda: self.nc.vector.reciprocal(out=out.ap, in_=a.ap), reads=[a.key], writes=[out.key])

    def mm(self, out, lhsT, rhs, start=True, stop=True):
        self.S.op("pe", lambda: self.nc.tensor.matmul(out.ap, lhsT=lhsT.ap, rhs=rhs.ap, start=start, stop=stop), reads=[lhsT.key, rhs.key], writes=[out.key])

    def tp(self, out, a, ident):
        self.S.op("pe", lambda: self.nc.tensor.transpose(out=out.ap, in_=a.ap, identity=ident.ap), reads=[a.key, ident.key], writes=[out.key])

    def ld(self, out, src, q="sp"):
        self.S.dma(q, out.ap, src, writes=[out.key])

    def st_(self, dst, a, q="sp"):
        self.S.dma(q, dst, a.ap, reads=[a.key])

    def tb(self, es, name, shape, dtype=F32):
        return TB(self.sb(es, name, shape, dtype), name)

    def rstd(self, out, ss, tmp, inv_n, eps):
        self.ts("dve", tmp, ss, inv_n, ALU.mult, eps, ALU.add)
        self.act(tmp, tmp, AF.Sqrt)
        self.rcp(out, tmp)

    def bank(self):
        b = self.banks[self.nbank % 8]
        self.nbank += 1
        return b

    def bcast_row(self, es, name, src_row, n):
        t = self.tb(es, name, [128, n])
        self.ld(t, src_row.partition_broadcast(128))
        return t

    def stage_prepA(self, l):
        nc, S = self.nc, self.S
        with ExitStack() as es:
            self.banks = [TB(self.ps(es, "bk%d" % i, [128, 512], F32), ("bk", i)) for i in range(8)]
            self.nbank = 0
            identf = TB(self.identf, "identf")
            mpB = self.bcast_row(es, "mpB", self.rwkv_mu_prev[l, :], 1024)
            mnB = self.bcast_row(es, "mnB", self.rwkv_mu_next[l, :], 1024)
            c0B = self.tb(es, "c0B", [128, 1024])
            self.tt("dve", c0B, mpB, mnB, ALU.add)
            self.ts("dve", c0B, c0B, -1.0, ALU.mult, 1.0, ALU.add)
            kkB = self.bcast_row(es, "kkB", self.rwkv_k_k[l, :], 256)
            kaB = self.bcast_row(es, "kaB", self.rwkv_k_a[l, :], 256)
            omk = self.tb(es, "omk", [128, 256])
            self.ts("dve", omk, kaB, -1.0, ALU.mult, 1.0, ALU.add)
            rkB = self.bcast_row(es, "rkB", self.rwkv_r_k[l].rearrange("h d -> (h d)"), 256)
            w0B = self.tb(es, "w0B", [128, 2, 256])
            a0B = self.tb(es, "a0B", [128, 2, 256])
            for d in range(2):
                self.ld(w0B[:, d, :], self.rwkv_w0[l, d, :].partition_broadcast(128))
                self.ld(a0B[:, d, :], self.rwkv_a0[l, d, :].partition_broadcast(128))
            Wl = self.tb(es, "Wl", [128, 2, 256])
            for d in range(2):
                self.ld(Wl[0:64, d, :], self.rwkv_w_up[l, d])
                self.ld(Wl[64:128, d, :], self.rwkv_a_up[l, d])
            Wg = self.tb(es, "WgA", [128, 256])
            self.ld(Wg, self.rwkv_g_up[l])
            cur = [self.tb(es, "curA%d" % i, [128, 1024]) for i in range(2)]
            prv = [self.tb(es, "prvA%d" % i, [128, 1024]) for i in range(2)]
            nxt = [self.tb(es, "nxtA%d" % i, [128, 1024]) for i in range(2)]
            f = self.tb(es, "fA", [128, 1024])
            t2 = self.tb(es, "t2A", [128, 1024])
            lin = self.tb(es, "linA", [128, 256])
            linT = self.tb(es, "linT", [128, 2, 128])
            zw = self.tb(es, "zwA", [128, 2, 256])
            al = self.tb(es, "alA", [128, 2, 256])
            kkr = self.tb(es, "kkr", [128, 256])
            sq = self.tb(es, "sqA", [128, 256])
            ss = self.tb(es, "ssA", [128, 4])
            tm4 = self.tb(es, "tm4A", [128, 4])
            rs4 = self.tb(es, "rs4A", [128, 4])
            kk = self.tb(es, "kkA", [128, 256])
            tk = self.tb(es, "tkA", [128, 256])
            km = self.tb(es, "kmA", [128, 256])
            TMt = [self.tb(es, "TMtA%d" % i, [128, 2, 6, 256]) for i in range(2)]
            for t_ in TMt:
                S.op("pool", lambda: nc.gpsimd.memset(t_.ap, 0.0), writes=[t_.key])
            aux = [self.tb(es, "auxA%d" % i, [128, 2, 256]) for i in range(2)]
            P = self.P
            for i in range(NT):
                b = i % 2
                r0 = i * 128
                self.ld(cur[b], P[r0:r0 + 128, 0:1024])
                if i == 0:
                    S.op("pool", lambda: nc.gpsimd.memset(prv[b].ap, 0.0), writes=[prv[b].key])
                    S.dma("sp", prv[b].ap[1:128, :], P[0:127, 0:1024], writes=[prv[b].key])
                else:
                    self.ld(prv[b], P[r0 - 1:r0 + 127, 0:1024])
                if i == NT - 1:
                    S.op("pool", lambda: nc.gpsimd.memset(nxt[b].ap, 0.0), writes=[nxt[b].key])
                    S.dma("sp", nxt[b].ap[0:127, :], P[r0 + 1:r0 + 128, 0:1024], writes=[nxt[b].key])
                else:
                    self.ld(nxt[b], P[r0 + 1:r0 + 129, 0:1024])
                self.tt("dve", f, cur[b], c0B, ALU.mult)
                self.tt("pool", t2, prv[b], mpB, ALU.mult)
                self.tt("dve", f, f, t2, ALU.add)
                self.tt("pool", t2, nxt[b], mnB, ALU.mult)
                self.tt("dve", f, f, t2, ALU.add)
                T_ = TMt[b]
                r_, k_, v_ = f[:, 0:256], f[:, 256:512], f[:, 512:768]
                self.act(lin[:, 0:64], f[:, 768:832], AF.Tanh)
                self.cp("act", lin[:, 64:128], f[:, 832:896])
                self.act(lin[:, 128:256], f[:, 896:1024], AF.Sigmoid)
                bkT = self.bank()
                for c in range(2):
                    self.tp(bkT[:, c * 128:(c + 1) * 128], lin[:, c * 128:(c + 1) * 128], identf)
                self.cp("dve", linT.v(lambda a: a.rearrange("p c t -> p (c t)")), bkT[:, 0:256])
                bw = self.bank()
                ba = self.bank()
                for d in range(2):
                    self.mm(bw[:, d * 256:(d + 1) * 256], linT[0:64, 0, :], Wl[0:64, d, :])
                for d in range(2):
                    self.mm(ba[:, d * 256:(d + 1) * 256], linT[64:128, 0, :], Wl[64:128, d, :])
                bg = self.bank()
                self.mm(bg[:, 0:256], linT[:, 1, :], Wg)
                self.tt("dve", zw.v(lambda a: a.rearrange("p d c -> p (d c)")), bw, w0B.v(lambda a: a.rearrange("p d c -> p (d c)")), ALU.add)
                self.tt("dve", al.v(lambda a: a.rearrange("p d c -> p (d c)")), ba, a0B.v(lambda a: a.rearrange("p d c -> p (d c)")), ALU.add)
                self.act(zw, zw, AF.Sigmoid)
                self.act(al, al, AF.Sigmoid)
                self.cp("act", aux[b][:, 1, :], bg[:, 0:256])
                self.tt("dve", kkr, k_, kkB, ALU.mult)
                self.tt("pool", sq, kkr, kkr, ALU.mult)
                self.red(ss, sq.v(lambda a: a.rearrange("p (h d) -> p h d", d=64)))
                self.rstd(rs4, ss, tm4, 1.0, 1e-12)
                self.tt("dve", kk.v(lambda a: a.rearrange("p (h d) -> p h d", d=64)), kkr.v(lambda a: a.rearrange("p (h d) -> p h d", d=64)),
                        rs4.v(lambda a: a.unsqueeze(2).to_broadcast([128, 4, 64])), ALU.mult)
                for d in range(2):
                    self.ts("dve", T_[:, d, 0, :], zw[:, d, :], -0.6065306597126334, ALU.mult)
                    self.tt("pool", T_[:, d, 2, :], kk, al[:, d, :], ALU.mult)
                    self.tt("dve", tk, al[:, d, :], kaB, ALU.mult)
                    self.tt("pool", tk, tk, omk, ALU.add)
                    self.tt("dve", T_[:, d, 3, :], k_, tk, ALU.mult)
                self.ts("dve", T_[:, 0, 1, :], kk, -1.0, ALU.mult)
                self.cp("act", T_[:, 0, 4, :], r_)
                self.cp("act", T_[:, 0, 5, :], v_)
                self.tt("pool", km, T_[:, 0, 3, :], T_[:, 1, 3, :], ALU.add)
                self.tt("dve", km, km, r_, ALU.mult)
                self.tt("pool", km, km, rkB, ALU.mult)
                self.red(ss, km.v(lambda a: a.rearrange("p (h d) -> p h d", d=64)))
                self.ts("dve", ss, ss, 0.5, ALU.mult)
                self.tt("dve", aux[b][:, 0, :].v(lambda a: a.rearrange("p (h d) -> p h d", d=64)), v_.v(lambda a: a.rearrange("p (h d) -> p h d", d=64)),
                        ss.v(lambda a: a.unsqueeze(2).to_broadcast([128, 4, 64])), ALU.mult)
                self.st_(self.TM[r0:r0 + 128, 0], T_)
                self.st_(self.AUX[r0:r0 + 128, 0:2, :], aux[b])
        S.barrier()

    def stage_prepC(self, l):
        nc, S = self.nc, self.S
        with ExitStack() as es:
            cwB = self.tb(es, "cwB", [128, 5, 768])
            for j in range(5):
                self.ld(cwB[:, j, :], self.gdn_conv[l, j, :].partition_broadcast(128))
            alB = self.bcast_row(es, "alogB", self.gdn_a_log[l].rearrange("d h -> (d h)"), 8)
            dtB = self.bcast_row(es, "dtB", self.gdn_dt_bias[l].rearrange("d h -> (d h)"), 8)
            negA = self.tb(es, "negA", [128, 8])
            self.act(negA, alB, AF.Exp)
            self.ts("dve", negA, negA, -1.0, ALU.mult)
            xs = [[self.tb(es, "xc%d_%d" % (j, i), [128, 768]) for j in range(5)] for i in range(2)]
            zt = [self.tb(es, "ztC%d" % i, [128, 272]) for i in range(2)]
            cv = self.tb(es, "cvC", [128, 768])
            t2 = self.tb(es, "t2C", [128, 768])
            sq = self.tb(es, "sqC", [128, 512])
            ss = self.tb(es, "ssC", [128, 8])
            tm8 = self.tb(es, "tm8C", [128, 8])
            rs8 = self.tb(es, "rs8C", [128, 8])
            bt = self.tb(es, "btC", [128, 8])
            nbt = self.tb(es, "nbtC", [128, 8])
            gg = self.tb(es, "ggC", [128, 8])
            TMt = [self.tb(es, "TMtC%d" % i, [128, 2, 6, 256]) for i in range(2)]
            for t_ in TMt:
                S.op("pool", lambda: nc.gpsimd.memset(t_.ap, 0.0), writes=[t_.key])
            aux = [self.tb(es, "auxC%d" % i, [128, 256]) for i in range(2)]
            P = self.P
            h3 = lambda a: a.rearrange("p (h d) -> p h d", d=64)
            for i in range(NT):
                b = i % 2
                r0 = i * 128
                for j in range(5):
                    sh = j - 2
                    lo_, hi_ = r0 + sh, r0 + sh + 128
                    x = xs[b][j]
                    if lo_ < 0 or hi_ > T:
                        S.op("pool", lambda: nc.gpsimd.memset(x.ap, 0.0), writes=[x.key])
                        a0, a1 = max(lo_, 0), min(hi_, T)
                        S.dma("sp", x.ap[a0 - lo_:a1 - lo_, :], P[a0:a1, OFF_C:OFF_C + 768], writes=[x.key])
                    else:
                        self.ld(x, P[lo_:hi_, OFF_C:OFF_C + 768])
                self.ld(zt[b], P[r0:r0 + 128, OFF_C + 768:OFF_C + 1040])
                self.tt("dve", cv, xs[b][0], cwB[:, 0, :], ALU.mult)
                for j in range(1, 5):
                    self.tt("pool", t2, xs[b][j], cwB[:, j, :], ALU.mult)
                    self.tt("dve", cv, cv, t2, ALU.add)
                self.act(cv, cv, AF.Silu)
                T_ = TMt[b]
                self.tt("pool", sq, cv[:, 0:512], cv[:, 0:512], ALU.mult)
                self.red(ss, sq.v(h3))
                self.rstd(rs8, ss, tm8, 1.0, 1e-12)
                self.ts("dve", rs8[:, 0:4], rs8[:, 0:4], 0.125, ALU.mult)
                self.tt("dve", T_[:, 0, 4, :].v(h3), cv[:, 0:256].v(h3), rs8[:, 0:4].v(lambda a: a.unsqueeze(2).to_broadcast([128, 4, 64])), ALU.mult)
                self.tt("dve", T_[:, 0, 2, :].v(h3), cv[:, 256:512].v(h3), rs8[:, 4:8].v(lambda a: a.unsqueeze(2).to_broadcast([128, 4, 64])), ALU.mult)
                self.cp("act", T_[:, 0, 3, :], T_[:, 0, 2, :])
                self.act(bt, zt[b][:, 256:264], AF.Sigmoid)
                self.ts("dve", nbt, bt, -1.0, ALU.mult)
                self.tt("dve", gg, zt[b][:, 264:272], dtB, ALU.add)
                self.act(gg, gg, AF.Exp)
                self.act(gg, gg, AF.Ln, bias=1.0)
                self.tt("dve", gg, gg, negA, ALU.mult)
                for d in range(2):
                    bc = lambda t_: t_[:, d * 4:(d + 1) * 4].v(lambda a: a.unsqueeze(2).to_broadcast([128, 4, 64]))
                    self.cp("pool", T_[:, d, 0, :].v(h3), bc(gg))
                    self.tt("dve", T_[:, d, 1, :].v(h3), T_[:, 0, 2, :].v(h3), bc(nbt), ALU.mult)
                    self.tt("pool", T_[:, d, 5, :].v(h3), cv[:, 512:768].v(h3), bc(bt), ALU.mult)
                self.act(aux[b], zt[b][:, 0:256], AF.Silu)
                self.st_(self.TM[r0:r0 + 128, 1], T_)
                self.st_(self.AUX[r0:r0 + 128, 2, :], aux[b])
        S.barrier()

    def stage_scan(self, l):
        nc, S = self.nc, self.S
        with ExitStack() as es:
            self.banks = [TB(self.ps(es, "bk%d" % i, [128, 512], F32), ("bk", i)) for i in range(8)]
            self.nbank = 0
            identf = TB(self.identf, "identf")
            tri = self.tb(es, "triS", [128, 2, 128])
            msk = self.tb(es, "mskS", [128, 2, 4, 128])
            onb = self.tb(es, "onbS", [128, 128])
            for d in range(2):
                self.ld(tri[:, d, :], self.c_tri[d])
                self.ld(msk[:, d, :, :], self.c_msk[d])
            self.ld(onb, self.c_onb)
            ST = [self.tb(es, "ST%d" % i, [64, 16, 64]) for i in range(2)]
            S.op("pool", lambda: nc.gpsimd.memset(ST[0].ap, 0.0), writes=[ST[0].key])
            U4 = range(4)
            BP = [self.tb(es, "BP%d" % u, [128, 256]) for u in U4]
            KP = [self.tb(es, "KP%d" % u, [128, 256]) for u in U4]
            VV = [self.tb(es, "VV%d" % u, [128, 256]) for u in U4]
            VP = [self.tb(es, "VP%d" % u, [128, 256]) for u in U4]
            G4 = [self.tb(es, "G4%d" % u, [128, 4, 4, 128]) for u in U4]
            APT = [self.tb(es, "APT%d" % u, [64, 4, 128]) for u in U4]
            RST = [self.tb(es, "RST%d" % u, [64, 4, 128]) for u in U4]
            PC = [self.tb(es, "PC%d" % u, [64, 4, 2]) for u in U4]
            def mk_lane(tg):
                lds = [self.tb(es, "ld%d_%s" % (q, tg), [128, 256]) for q in range(5)]
                c0t = self.tb(es, "c0t_" + tg, [128, 256])
                a1 = self.tb(es, "a1_" + tg, [128, 256])
                a1a = self.tb(es, "a1a_" + tg, [128, 256])
                X1 = self.tb(es, "X1_" + tg, [128, 256])
                X1a = self.tb(es, "X1a_" + tg, [128, 256])
                X2 = self.tb(es, "X2_" + tg, [128, 256])
                Ec = self.tb(es, "Ec_" + tg, [128, 256])
                Ag = self.tb(es, "Ag_" + tg, [128, 256])
                Rg = self.tb(es, "Rg_" + tg, [128, 256])
                Bg = self.tb(es, "Bg_" + tg, [128, 256])
                Kg = self.tb(es, "Kg_" + tg, [128, 256])
                As = self.tb(es, "As_" + tg, [128, 256])
                Rs = self.tb(es, "Rs_" + tg, [128, 256])
                FMar = self.tb(es, "FMar_" + tg, [64, 4, 2, 128])
                FMbg = self.tb(es, "FMbg_" + tg, [64, 4, 128])
                FMkg = self.tb(es, "FMkg_" + tg, [64, 4, 128])
                FMcum = self.tb(es, "FMcum_" + tg, [64, 4, 128])
                cumS = self.tb(es, "cumS_" + tg, [128, 256])
                Dm = self.tb(es, "DmS_" + tg, [128, 4, 128])
                mskD = self.tb(es, "mskD_" + tg, [128, 4, 2, 128])
                Xb = [self.tb(es, ("Xb%d_" % i) + tg, [128, 4, 128]) for i in range(2)]
                XTb = [self.tb(es, ("XTb%d_" % i) + tg, [128, 4, 128]) for i in range(2)]
                Wb = [self.tb(es, ("Wb%d_" % i) + tg, [128, 4, 128]) for i in range(2)]
                Zs = self.tb(es, "Zs_" + tg, [128, 256])
                return dict(lds=lds, c0t=c0t, a1=a1, a1a=a1a, X1=X1, X1a=X1a, X2=X2, Ec=Ec, Ag=Ag, Rg=Rg, Bg=Bg, Kg=Kg, As=As, Rs=Rs, FMar=FMar, FMbg=FMbg, FMkg=FMkg, FMcum=FMcum, cumS=cumS, Dm=Dm, mskD=mskD, Xb=Xb, XTb=XTb, Wb=Wb, Zs=Zs)
            avg64 = self.tb(es, "avg64", [64, 128])
            S.op("pool", lambda: nc.gpsimd.memset(avg64.ap, 1.0 / 64), writes=[avg64.key])
            lanes = [mk_lane("L0"), mk_lane("L1")]
            Usb = self.tb(es, "Usb", [128, 2, 512])
            tmpS = self.tb(es, "tmpS", [64, 16, 64])
            Y1s = self.tb(es, "Y1s", [128, 2, 512])
            yt = [self.tb(es, "ytS%d" % d, [128, 512]) for d in range(2)]
            flat = lambda a: a.rearrange("p h t -> p (h t)")
            nld = 0
            cur = 0
            def unit_gen(m, d, j, Lz):
                lds = Lz["lds"]
                c0t = Lz["c0t"]
                a1 = Lz["a1"]
                a1a = Lz["a1a"]
                X1 = Lz["X1"]
                X1a = Lz["X1a"]
                X2 = Lz["X2"]
                Ec = Lz["Ec"]
                Ag = Lz["Ag"]
                Rg = Lz["Rg"]
                Bg = Lz["Bg"]
                Kg = Lz["Kg"]
                As = Lz["As"]
                Rs = Lz["Rs"]
                FMar = Lz["FMar"]
                FMbg = Lz["FMbg"]
                FMkg = Lz["FMkg"]
                FMcum = Lz["FMcum"]
                cumS = Lz["cumS"]
                Dm = Lz["Dm"]
                mskD = Lz["mskD"]
                Xb = Lz["Xb"]
                XTb = Lz["XTb"]
                Wb = Lz["Wb"]
                Zs = Lz["Zs"]
                u = m * 2 + d
                _CUR_U[0] = u
                tile_ = j if d == 0 else NT - 1 - j
                r0 = tile_ * 128
                L_ = lds
                dsrc = {0: d, 1: (0 if m == 0 else d), 2: (d if m == 0 else 0), 3: (d if m == 0 else 0), 4: 0, 5: (0 if m == 0 else d)}
                LW, Aa, Bb, Kk, Rr = L_
                for q, dst in ((0, LW), (1, Aa), (2, Bb), (3, Kk), (4, Rr), (5, VV[u])):
                    self.ld(dst, self.TM[r0:r0 + 128, m, dsrc[q], q, :])
                bc = self.bank()
                self.mm(bc[:, 0:256], tri[:, d, :], LW)
                self.mm(bc[:, 256:512], onb, LW)
                self.ts("dve", c0t, bc[:, 256:512], 0.5, ALU.mult)
                if m == 0:
                    self.tt("dve", a1, bc[:, 0:256], c0t, ALU.subtract)
                    self.act(X1, a1, AF.Exp)
                    self.act(X2, a1, AF.Exp, scale=-1.0)
                    self.act(Ec, c0t, AF.Exp)
                    self.tt("pool", a1a, a1, LW, ALU.subtract)
                    self.act(X1a, a1a, AF.Exp)
                    xa = X1a
                    self.tt("dve", Ag, Aa, xa, ALU.mult)
                    self.tt("pool", Rg, Rr, X1, ALU.mult)
                    self.tt("dve", Bg, Bb, X2, ALU.mult)
                    self.tt("pool", Kg, Kk, X2, ALU.mult)
                    self.tt("dve", As, Ag, Ec, ALU.mult)
                    self.tt("pool", Rs, Rg, Ec, ALU.mult)
                    self.tt("dve", BP[u], Bg, Ec, ALU.mult)
                    self.tt("pool", KP[u], Kg, Ec, ALU.mult)
                    gA, gR, gB_ = Ag, Rg, Bg
                else:
                    cops = [
                        lambda: self.cp("dve", cumS, bc[:, 0:256]),
                        lambda: self.act(X1, cumS, AF.Exp),
                        lambda: self.tt("dve", a1, c0t, cumS, ALU.subtract),
                        lambda: self.tt("dve", a1, a1, c0t, ALU.add),
                        lambda: self.act(X2, a1, AF.Exp),
                        lambda: self.tt("dve", As, Aa, X1, ALU.mult),
                        lambda: self.tt("pool", Rs, Rr, X1, ALU.mult),
                        lambda: self.tt("dve", BP[u], Bb, X2, ALU.mult),
                        lambda: self.cp("pool", KP[u], BP[u]),
                    ]
                    import os as _os
                    for ci, cop in enumerate(cops):
                        if _os.environ.get("SCAN_STOP") == "cn" and ci == int(_os.environ.get("SCAN_N", "0")):
                            S.barrier()
                            return True
                        cop()
                    gA, gR, gB_ = Aa, Rr, Bb
                if _stop("a"):
                    S.barrier()
                    return True
                yield
                bpc = self.bank()
                on2 = onb.v(lambda a: a.rearrange("p (c t) -> p c t", t=64)[:, :, 0])
                for h in range(4):
                    self.mm(bpc[0:64, h * 2:(h + 1) * 2], LW[:, h * 64:(h + 1) * 64], on2)
                self.act(PC[u].v(lambda a: a.rearrange("p h c -> p (h c)")), bpc[0:64, 0:8], AF.Exp)
                if _stop("b"):
                    S.barrier()
                    return True
                yield
                fm_list = [(gA, FMar[:, :, 0, :]), (gR, FMar[:, :, 1, :]), (gB_, FMbg), (Rs, RST[u])]
                fm_list.append((Kg, FMkg) if m == 0 else (cumS, FMcum))
                for (src, dst) in fm_list:
                    bt_ = self.bank()
                    for h in range(4):
                        self.mm(bt_[0:64, h * 128:(h + 1) * 128], src[:, h * 64:(h + 1) * 64], identf)
                    self.cp("act", dst, bt_[0:64, :].v(lambda a: a.rearrange("p (h t) -> p h t", t=128)))
                if _stop("c"):
                    S.barrier()
                    return True
                yield
                nblk = 4 if m == 0 else 2
                if m == 1:
                    bd_ = self.bank()
                    for h in range(4):
                        self.mm(bd_[:, h * 128:(h + 1) * 128], avg64, FMcum[:, h, :])
                    for h in range(4):
                        self.ts("dve", Dm[:, h, :], bd_[:, h * 128:(h + 1) * 128], cumS[:, h * 64:h * 64 + 1], ALU.subtract)
                    self.ts("dve", Dm, Dm, 0.0, ALU.min)
                    if True:
                        pass
                    self.act(Dm, Dm, AF.Exp)
                    for h in range(4):
                        self.tt("pool", mskD[:, h, :, :], msk[:, d, 0:2, :], Dm[:, h, :].v(lambda a: a.unsqueeze(1).to_broadcast([128, 2, 128])), ALU.mult)
                for h in range(4):
                    bg_ = self.bank()
                    rhs = FMar[:, h, :, :].v(lambda a: a.rearrange("p c t -> p (c t)"))
                    self.mm(bg_[:, 0:256], FMbg[:, h, :], rhs)
                    if m == 0:
                        self.mm(bg_[:, 256:512], FMkg[:, h, :], rhs)
                    if _stop("c2"):
                        S.barrier()
                        return True
                    mk_ = msk[:, d, 0:nblk, :] if m == 0 else mskD[:, h, :, :]
                    self.tt("dve", G4[u][:, h, 0:nblk, :].v(lambda a: a.rearrange("p b t -> p (b t)")), bg_[:, 0:nblk * 128],
                            mk_.v(lambda a: a.rearrange("p b t -> p (b t)")), ALU.mult)
                    import os as _os
                    if _stop("c3") and h == int(_os.environ.get("SCAN_H", "0")):
                        S.barrier()
                        return True
                mak_i = 2 if m == 0 else 0
                nrk_i = 3 if m == 0 else 1
                if _stop("d"):
                    S.barrier()
                    return True
                yield
                xi = 0
                Xc, XTc, Wc = Xb[0], XTb[0], Wb[0]
                self.cp("pool", Xc, G4[u][:, :, 0, :])
                bt_ = self.bank()
                for h in range(4):
                    self.tp(bt_[:, h * 128:(h + 1) * 128], Xc[:, h, :], identf)
                self.cp("act", XTc.v(flat), bt_)
                self.tt("dve", Wc, Xc, identf.v(lambda a: a.unsqueeze(1).to_broadcast([128, 4, 128])), ALU.add)
                for lv in range(1, 6):
                    Xn, XTn, Wn = Xb[1 - xi], XTb[1 - xi], Wb[1 - xi]
                    bx = self.bank()
                    for h in range(4):
                        self.mm(bx[:, h * 128:(h + 1) * 128], Xc[:, h, :], XTc[:, h, :])
                    self.cp("act", XTn.v(flat), bx)
                    if lv < 5:
                        by = self.bank()
                        for h in range(4):
                            self.mm(by[:, h * 128:(h + 1) * 128], XTc[:, h, :], Xc[:, h, :])
                        self.cp("pool" if False else "dve", Xn.v(flat), by)
                    bw = self.bank()
                    for h in range(4):
                        self.mm(bw[:, h * 128:(h + 1) * 128], XTn[:, h, :], Wc[:, h, :])
                    self.tt("dve", Wn.v(flat), Wc.v(flat), bw, ALU.add)
                    xi = 1 - xi
                    Xc, XTc, Wc = Xn, XTn, Wn
                    yield
                if _stop("e"):
                    S.barrier()
                    return True
                yield
                bz = self.bank()
                for h in range(4):
                    self.mm(bz[:, h * 64:(h + 1) * 64], G4[u][:, h, mak_i, :], VV[u][:, h * 64:(h + 1) * 64])
                self.cp("act", Zs, bz[:, 0:256])
                yield
                bv = self.bank()
                for h in range(4):
                    self.mm(bv[:, h * 64:(h + 1) * 64], Wc[:, h, :], Zs[:, h * 64:(h + 1) * 64])
                self.cp("act", VP[u], bv[:, 0:256])
                yield
                ba = self.bank()
                for h in range(4):
                    self.mm(ba[0:64, h * 128:(h + 1) * 128], As[:, h * 64:(h + 1) * 64], Wc[:, h, :])
                self.cp("act", APT[u].v(flat), ba[0:64, :])
                import os as _os
                if _stop("u") and u == int(_os.environ.get("SCAN_U", "0")):
                    S.barrier()
                    return True
                yield
            for j in range(NT):
                for m in range(2):
                    gens = [unit_gen(m, 0, j, lanes[0]), unit_gen(m, 1, j, lanes[1])]
                    while gens:
                        for g_ in list(gens):
                            try:
                                next(g_)
                            except StopIteration:
                                gens.remove(g_)
                if _stop("f"):
                    S.barrier()
                    return True
                for sub in range(2):
                    STc, STn = ST[cur], ST[1 - cur]
                    par = [sub, 1 - sub]
                    rows = [slice(par[d] * 64, par[d] * 64 + 64) for d in range(2)]
                    bu = [self.bank(), self.bank()]
                    for d in range(2):
                        for m in range(2):
                            u = m * 2 + d
                            for h in range(4):
                                c0_ = (m * 4 + h) * 64
                                self.mm(bu[d][rows[d], c0_:c0_ + 64], APT[u][:, h, rows[d]], STc[:, d * 8 + m * 4 + h, :])
                    for d in range(2):
                        for m in range(2):
                            u = m * 2 + d
                            self.tt("dve", Usb[rows[d], d, m * 256:(m + 1) * 256], bu[d][rows[d], m * 256:(m + 1) * 256], VP[u][rows[d], :], ALU.add)
                    bs = [self.bank(), self.bank()]
                    for d in range(2):
                        for m in range(2):
                            u = m * 2 + d
                            for h in range(4):
                                c0_ = (m * 4 + h) * 64
                                self.mm(bs[d][0:64, c0_:c0_ + 64], BP[u][rows[d], h * 64:(h + 1) * 64], Usb[rows[d], d, c0_:c0_ + 64], start=True, stop=False)
                                self.mm(bs[d][0:64, c0_:c0_ + 64], KP[u][rows[d], h * 64:(h + 1) * 64], VV[u][rows[d], h * 64:(h + 1) * 64], start=False, stop=True)
                    by1 = [self.bank(), self.bank()]
                    by2 = [self.bank(), self.bank()]
                    for d in range(2):
                        for m in range(2):
                            u = m * 2 + d
                            nrk_i = 3 if m == 0 else 1
                            for h in range(4):
                                c0_ = (m * 4 + h) * 64
                                self.mm(by1[d][rows[d], c0_:c0_ + 64], RST[u][:, h, rows[d]], STc[:, d * 8 + m * 4 + h, :])
                                self.mm(by2[d][rows[d], c0_:c0_ + 64], G4[u][rows[d], h, 1, rows[d]], Usb[rows[d], d, c0_:c0_ + 64], start=True, stop=False)
                                self.mm(by2[d][rows[d], c0_:c0_ + 64], G4[u][rows[d], h, nrk_i, rows[d]], VV[u][rows[d], h * 64:(h + 1) * 64], start=False, stop=True)
                    for d in range(2):
                        for m in range(2):
                            u = m * 2 + d
                            sl_ = slice(d * 8 + m * 4, d * 8 + m * 4 + 4)
                            self.tt("pool", tmpS[:, sl_, :], STc[:, sl_, :], PC[u][:, :, par[d]].v(lambda a: a.unsqueeze(2).to_broadcast([64, 4, 64])), ALU.mult)
                        sl8 = slice(d * 8, d * 8 + 8)
                        self.tt("dve", STn[:, sl8, :].v(lambda a: a.rearrange("p s v -> p (s v)")), tmpS[:, sl8, :].v(lambda a: a.rearrange("p s v -> p (s v)")),
                                bs[d][0:64, :], ALU.add)
                        self.cp("act", Y1s[rows[d], d, :], by1[d][rows[d], :])
                        self.tt("dve", yt[d][rows[d], :], Y1s[rows[d], d, :], by2[d][rows[d], :], ALU.add)
                    cur = 1 - cur
                    if _stop("g"):
                        S.barrier()
                        return True
                for d in range(2):
                    tile_ = j if d == 0 else NT - 1 - j
                    self.st_(self.YAC[tile_ * 128:(tile_ + 1) * 128, :, d, :], yt[d].v(lambda a: a.rearrange("p (m c) -> p m c", m=2)))
        S.barrier()

    def stage_postAC(self, l):
        nc, S = self.nc, self.S
        with ExitStack() as es:
            lnw = self.bcast_row(es, "lnwB", self.rwkv_ln_w[l, :], 256)
            lnb = self.bcast_row(es, "lnbB", self.rwkv_ln_b[l, :], 256)
            gn = self.tb(es, "gnB", [128, 4, 64])
            for h in range(4):
                self.ld(gn[:, h, :], self.gdn_norm_g[l, :].partition_broadcast(128))
            ys = [self.tb(es, "ysP%d" % i, [128, 2, 2, 256]) for i in range(2)]
            ax = [self.tb(es, "axP%d" % i, [128, 3, 256]) for i in range(2)]
            y = self.tb(es, "yP", [128, 256])
            yc = self.tb(es, "ycP", [128, 256])
            sq = self.tb(es, "sqP", [128, 256])
            s4 = self.tb(es, "s4P", [128, 4])
            t4 = self.tb(es, "t4P", [128, 4])
            r4 = self.tb(es, "r4P", [128, 4])
            oa = [self.tb(es, "oaP%d" % i, [128, 256]) for i in range(2)]
            oc = [self.tb(es, "ocP%d" % i, [128, 256]) for i in range(2)]
            h3 = lambda a: a.rearrange("p (h d) -> p h d", d=64)
            b4 = lambda t_: t_.v(lambda a: a.unsqueeze(2).to_broadcast([128, 4, 64]))
            for i in range(NT):
                b = i % 2
                r0 = i * 128
                self.ld(ys[b], self.YAC[r0:r0 + 128])
                self.ld(ax[b], self.AUX[r0:r0 + 128])
                self.tt("dve", y, ys[b][:, 0, 0, :], ys[b][:, 0, 1, :], ALU.add)
                self.red(s4, y.v(h3))
                self.ts("dve", s4, s4, 1.0 / 64, ALU.mult)
                self.tt("dve", yc.v(h3), y.v(h3), b4(s4), ALU.subtract)
                self.tt("pool", sq, yc, yc, ALU.mult)
                self.red(s4, sq.v(h3))
                self.rstd(r4, s4, t4, 1.0 / 64, 64e-5)
                self.tt("dve", yc.v(h3), yc.v(h3), b4(r4), ALU.mult)
                self.tt("pool", yc, yc, lnw, ALU.mult)
                self.tt("dve", yc, yc, lnb, ALU.add)
                self.tt("pool", yc, yc, ax[b][:, 0, :], ALU.add)
                self.tt("dve", oa[b], yc, ax[b][:, 1, :], ALU.mult)
                self.st_(self.mixed[r0:r0 + 128, 0:256], oa[b])
                self.tt("dve", y, ys[b][:, 1, 0, :], ys[b][:, 1, 1, :], ALU.add)
                self.tt("pool", sq, y, y, ALU.mult)
                self.red(s4, sq.v(h3))
                self.rstd(r4, s4, t4, 1.0 / 64, EPS)
                self.tt("dve", yc.v(h3), y.v(h3), b4(r4), ALU.mult)
                self.tt("pool", yc.v(h3), yc.v(h3), gn, ALU.mult)
                self.tt("dve", oc[b], yc, ax[b][:, 2, :], ALU.mult)
                self.st_(self.mixed[r0:r0 + 128, 512:768], oc[b])
        S.barrier()

    def stage_final(self, xsrc):
        nc, S = self.nc, self.S
        with ExitStack() as es:
            gB = self.bcast_row(es, "gFB", self.norm_final_g[0, :], D)
            xt = [self.tb(es, "xF%d" % i, [128, D]) for i in range(2)]
            ot = [self.tb(es, "oF%d" % i, [128, D]) for i in range(2)]
            junk = self.tb(es, "junkF", [128, D])
            st = self.tb(es, "stF", [128, NT, 4])
            for i in range(NT):
                b = i % 2
                self.ld(xt[b], xsrc[i * 128:(i + 1) * 128, :])
                S.op("act", lambda: nc.scalar.activation(out=junk.ap, in_=xt[b].ap, func=AF.Square, accum_out=st.ap[:, i, 0:1]),
                     reads=[xt[b].key], writes=[junk.key, st.key])
                self.rstd(st[:, i, 2:3], st[:, i, 0:1], st[:, i, 1:2], 1.0 / D, EPS)
                self.stt(ot[b], xt[b], st[:, i, 2:3], gB, ALU.mult, ALU.mult)
                self.st_(self.out[i * 128:(i + 1) * 128, :], ot[b])
        S.barrier()

    def build(self):
        nc, S = self.nc, self.S
        self.stage_inproj(0, self.x)
        if self.stop_after == "inproj0":
            return self.finish_debug(self.P, [T, P_IN])
        if self.stop_after in ("moe_idx", "moe", "outproj"):
            self.mixed = self.inp("mixed_in", [T, MIXW])
            self.stage_outproj(0, self.x)
            if self.stop_after == "outproj":
                return self.finish_debug(self.X2, [T, D])
            if self.stop_after == "moe_idx":
                self.dbg_small = nc.dram_tensor("dbg", [128, 80], F32, kind="ExternalOutput").ap()
                self.stage_moe(0)
                self.es.close()
                return nc
            self.stage_moe(0)
            return self.finish_debug(self.X3, [T, D])
        if self.stop_after == "AC":
            import os
            la = int(os.environ.get("ACL", "0"))
            if la:
                self.stage_inproj(la, self.x)
            self.stage_prepA(la)
            self.stage_prepC(la)
            if self.stage_scan(la):
                return self.finish_debug(self.AUX.rearrange("t a c -> t (a c)"), [T, 768])
            if os.environ.get("SCAN_STOP") == "h":
                return self.finish_debug(self.YAC.rearrange("t a b c -> t (a b c)"), [T, 1024])
            self.stage_postAC(la)
            return self.finish_debug(self.mixed, [T, MIXW], cols=[(0, 256), (512, 768)])
        if self.stop_after == "prepAC":
            self.stage_prepA(0)
            self.stage_prepC(0)
            return self.finish_debug(self.TM.rearrange("t m d q c -> t (m d q c)"), [T, 2 * 2 * 6 * 256])
        if self.stop_after == "BD":
            self.stage_attnB(0)
            self.stage_attnD(0)
            return self.finish_debug(self.mixed, [T, MIXW], cols=[(256, 512), (768, 1152)])
        self.out = nc.dram_tensor("out", [T, D], F32, kind="ExternalOutput").ap()
        xsrc = self.x
        for l in range(L):
            if l > 0:
                self.stage_inproj(l, xsrc)
            self.X3 = self.X3s[l % 2]
            self.stage_prepA(l)
            self.stage_prepC(l)
            self.stage_scan(l)
            self.stage_postAC(l)
            self.stage_attnB(l)
            self.stage_attnD(l)
            self.stage_outproj(l, xsrc)
            self.stage_moe(l)
            xsrc = self.X3
        self.stage_final(xsrc)
        self.es.close()
        return nc

    def finish_debug(self, src, shape, cols=None):
        nc, S = self.nc, self.S
        dbg = nc.dram_tensor("dbg", list(shape), F32, kind="ExternalOutput").ap()
        with ExitStack() as es:
            tb = [self.sb(es, "dbgt%d" % i, [128, shape[1]], F32) for i in range(2)]
            for i in range(shape[0] // 128):
                for (c0, c1) in (cols or [(0, shape[1])]):
                    S.dma("sp", tb[i % 2][:, c0:c1], src[i * 128:(i + 1) * 128, c0:c1], writes=[("dbgt", i % 2)])
                    S.dma("sp", dbg[i * 128:(i + 1) * 128, c0:c1], tb[i % 2][:, c0:c1], reads=[("dbgt", i % 2)])
        S.barrier()
        self.es.close()
        return nc


def _t5_bucket(rel):
    nb = 16
    max_exact = 8
    n = np.abs(rel)
    large = max_exact + (np.log(np.maximum(n, 1).astype(np.float32) / np.float32(max_exact))
                         / np.float32(math.log(1024 / max_exact)) * np.float32(nb - max_exact)).astype(np.int32)
    large = np.minimum(large, nb - 1)
    return np.where(rel > 0, nb, 0) + np.where(n < max_exact, n, large)


def host_consts(inputs=None):
    c = {}
    c["ident_f"] = np.eye(128, dtype=np.float32)
    t = np.arange(T)
    inv = (np.float32(10000.0) ** (-np.arange(0, 32, 2, dtype=np.float32) / np.float32(32))).astype(np.float32)
    ar = (t // 64).astype(np.float32)[:, None] * inv
    ac = (t % 64).astype(np.float32)[:, None] * inv
    c["rope_cos"] = np.concatenate([np.cos(ar), np.cos(ar), np.cos(ac), np.cos(ac)], 1).astype(np.float32)
    c["rope_sin"] = np.concatenate([-np.sin(ar), np.sin(ar), -np.sin(ac), np.sin(ac)], 1).astype(np.float32)
    ch = np.arange(128) // 64
    same = (ch[:, None] == ch[None, :])
    sI, tI = np.arange(128)[:, None], np.arange(128)[None, :]
    c["c_onb"] = same.astype(np.float32)
    c["c_tri"] = np.stack([(same & (sI <= tI)), (same & (sI >= tI))]).astype(np.float32)
    mk = np.zeros((2, 128, 4, 128), np.float32)
    for blk in range(4):
        strict = blk in (0, 2)
        mk[0, :, blk, :] = same & ((sI < tI) if strict else (sI <= tI))
        mk[1, :, blk, :] = same & ((sI > tI) if strict else (sI >= tI))
    c["c_msk"] = mk
    c["ustrict"] = np.triu(np.ones((128, 128), np.float32), 1)
    c["iota_c"] = np.tile(np.arange(CAP, dtype=np.float32)[None, :], (128, 1))
    tv = np.zeros((128, NT, 2), np.float32)
    tv[:, :, 0] = np.arange(128)[:, None]
    tv[:, :, 1] = 128.0 * np.arange(NT)[None, :]
    c["tvals"] = tv
    c["tvals_"] = tv
    c["coef2"] = np.array([[1.0], [128.0]], np.float32)
    if inputs is not None:
        rb = np.asarray(inputs["rel_bias"], np.float32)
        kp = np.arange(128)[:, None, None]
        rel = np.arange(3)[None, :, None]
        qp = np.arange(128)[None, None, :]
        o = 128 * (rel - 1) + kp - qp
        valid = np.abs(o) <= 64
        db = np.full((3, 2, 128, 3, 128), NEG, np.float32)
        for br, dil in enumerate((1, 4, 16)):
            bk = _t5_bucket(o * dil)
            for j in range(2):
                db[br, j] = np.where(valid, rb[bk, br * 2 + j], np.float32(NEG))
        c["dbias"] = db
    return c


_PROG = None


def kernel(**inputs):
    global _PROG
    if _PROG is None:
        pr = Prog()
        _PROG = (pr, pr.build())
    pr, nc = _PROG
    consts = host_consts(inputs)
    x = np.asarray(inputs["x"], np.float32)
    nb = x.shape[0]
    in_maps = []
    for b in range(nb):
        m = {}
        for name, (shape, dt) in pr.inputs.items():
            if name == "x":
                m[name] = np.ascontiguousarray(x[b])
            elif name in consts:
                m[name] = consts[name]
            else:
                m[name] = np.ascontiguousarray(np.asarray(inputs[name], np.float32)).reshape(shape)
        in_maps.append(m)
    res = run_bass_kernel_spmd(nc, in_maps, core_ids=list(range(nb)))
    return np.stack([np.asarray(res.results[b]["out"], np.float32) for b in range(nb)])
```

```python
import math
from contextlib import ExitStack
import numpy as np
import concourse.bass as bass
import concourse.mybir as mybir
from concourse.bass_utils import run_bass_kernel_spmd

F32 = mybir.dt.float32
F32R = mybir.dt.float32r
BF16 = mybir.dt.bfloat16
U32 = mybir.dt.uint32
I32 = mybir.dt.int32
AF = mybir.ActivationFunctionType
ALU = mybir.AluOpType
AX = mybir.AxisListType

T = 4096
NT = 32
D = 1024
L = 2
HD = 64
EPS = 1e-6
A_W = 256
A_IN = 1024
B_IN = 512
C_IN = 1040
D_IN = 1152
P_IN = 3728
OFF_A = 0
OFF_B = 1024
OFF_C = 1536
OFF_D = 2576
MIXW = 1152
NE = 16
CAP = 512
NEG = -30000.0

NDMA = 44
NSW = 12


class Sched:
    def __init__(self, nc, same_engine_sync=True):
        self.nc = nc
        self.eng = {"pe": nc.tensor, "act": nc.scalar, "dve": nc.vector,
                    "pool": nc.gpsimd, "sp": nc.sync}
        self.sem = {}
        for k in self.eng:
            self.sem[k] = nc.alloc_semaphore("sem_" + k)
        self.cnt = {k: 0 for k in self.eng}
        for i in range(NDMA):
            self.sem[("d", i)] = nc.alloc_semaphore("sem_d%d" % i)
        self.dval = [0] * NDMA
        self.dnext = {"hw": 0, "sw": 0}
        self.known = {k: {} for k in self.eng}
        self.w = {}
        self.r = {}
        self.same = same_engine_sync
        self.nwaits = 0
        self.nops = 0

    def _wait(self, e, ev):
        if ev is None:
            return
        key, val = ev
        if key == e and (e == "pe" or not self.same):
            return
        if self.known[e].get(key, 0) >= val:
            return
        self.eng[e].wait_ge(self.sem[key], val)
        self.known[e][key] = val
        self.nwaits += 1

    def _deps(self, e, reads, writes):
        for b in reads:
            self._wait(e, self.w.get(b))
        for b in writes:
            self._wait(e, self.w.get(b))
            for ev in self.r.get(b, ()):
                self._wait(e, ev)

    def _commit(self, ev, reads, writes):
        for b in reads:
            self.r.setdefault(b, []).append(ev)
        for b in writes:
            self.w[b] = ev
            self.r[b] = []

    def op(self, e, fn, reads=(), writes=()):
        self._deps(e, reads, writes)
        ins = fn()
        self.cnt[e] += 1
        ins.then_inc(self.sem[e], 1)
        self._commit((e, self.cnt[e]), reads, writes)
        self.nops += 1
        return ins

    def dma_raw(self, q, fn, reads=(), writes=()):
        self._deps(q, reads, writes)
        if q == "pool":
            s = NDMA - NSW + self.dnext["sw"]
            self.dnext["sw"] = (self.dnext["sw"] + 1) % NSW
        else:
            s = self.dnext["hw"]
            self.dnext["hw"] = (self.dnext["hw"] + 1) % (NDMA - NSW)
        key = ("d", s)
        if self.dval[s] > 0:
            self._wait(q, (key, self.dval[s]))
        ins = fn()
        self.dval[s] += 16
        ins.then_inc(self.sem[key], 16)
        self._commit((key, self.dval[s]), reads, writes)
        self.nops += 1
        return ins

    def dma(self, q, out, in_, reads=(), writes=(), **kw):
        return self.dma_raw(q, lambda: self.eng[q].dma_start(out=out, in_=in_, **kw), reads, writes)

    def barrier(self):
        for e in self.eng:
            for s in range(NDMA):
                if self.dval[s] > 0:
                    self._wait(e, (("d", s), self.dval[s]))
            for o in self.eng:
                if o != e and self.cnt[o] > 0:
                    key, val = o, self.cnt[o]
                    if self.known[e].get(key, 0) < val:
                        self.eng[e].wait_ge(self.sem[key], val)
                        self.known[e][key] = val
                        self.nwaits += 1
        self.w = {}
        self.r = {}


class StopScan(Exception):
    pass


def _stop(tag):
    import os
    return os.environ.get("SCAN_STOP") == tag and _CUR_U[0] >= int(os.environ.get("SCAN_U", "0"))


_CUR_U = [0]


class TB:
    def __init__(self, ap, key):
        self.ap, self.key = ap, key

    def __getitem__(self, idx):
        return TB(self.ap[idx], self.key)

    def v(self, f):
        return TB(f(self.ap), self.key)


class Prog:
    def __init__(self, stop_after=None, dbg=None):
        self.nc = nc = bass.Bass("TRN2", target_bir_lowering=False)
        self.S = Sched(nc)
        self.stop_after = stop_after
        self.dbg = dbg
        dt = nc.dram_tensor
        self.inputs = {}
        self.x = self.inp("x", [T, D])
        self.norm_mix_g = self.inp("norm_mix_g", [L, D])
        self.w_in = self.inp("w_in", [L, D, P_IN])
        self.ident_f = self.inp("ident_f", [128, 128])
        self.P = dt("P_scr", [T, P_IN], F32, kind="Internal").ap()
        self.mixed = dt("mixed_scr", [T, MIXW], F32, kind="Internal").ap()
        self.Dnum = dt("Dnum_scr", [T, 6, 65], F32, kind="Internal").ap()
        self.X2 = dt("X2_scr", [T, D], F32, kind="Internal").ap()
        self.X3s = [dt("X3_scr%d" % i, [T, D], F32, kind="Internal").ap() for i in range(2)]
        self.X3 = self.X3s[0]
        self.norm_final_g = self.inp("norm_final_g", [1, D])
        self.H2 = dt("H2_scr", [T, D], BF16, kind="Internal").ap()
        self.AFFd = dt("AFF_scr", [T, NE], F32, kind="Internal").ap()
        self.IDXd = dt("IDX_scr", [NE, CAP], U32, kind="Internal").ap()
        self.w_out = self.inp("w_out", [L, MIXW, D])
        self.norm_ffn_g = self.inp("norm_ffn_g", [L, D])
        self.router_w = self.inp("router_w", [L, D, NE])
        self.router_b = self.inp("router_b", [L, NE])
        if stop_after not in ("inproj0", "BD", "outproj", "moe_idx", "AC", "prepAC"):
            self.w_gate = self.inp("expert_w_gate", [L, NE, D, D])
            self.w_up = self.inp("expert_w_up", [L, NE, D, D])
            self.w_down = self.inp("expert_w_down", [L, NE, D, D])
        self.ustrict = self.inp("ustrict", [128, 128])
        self.iota_c = self.inp("iota_c", [128, CAP])
        self.tvals = self.inp("tvals", [128, NT, 2])
        self.TM = dt("TM_scr", [T, 2, 2, 6, 256], F32, kind="Internal").ap()
        self.AUX = dt("AUX_scr", [T, 3, 256], F32, kind="Internal").ap()
        self.YAC = dt("YAC_scr", [T, 2, 2, 256], F32, kind="Internal").ap()
        self.rwkv_mu_prev = self.inp("rwkv_mu_prev", [L, 1024])
        self.rwkv_mu_next = self.inp("rwkv_mu_next", [L, 1024])
        self.rwkv_w0 = self.inp("rwkv_w0", [L, 2, 256])
        self.rwkv_w_up = self.inp("rwkv_w_up", [L, 2, 64, 256])
        self.rwkv_a0 = self.inp("rwkv_a0", [L, 2, 256])
        self.rwkv_a_up = self.inp("rwkv_a_up", [L, 2, 64, 256])
        self.rwkv_g_up = self.inp("rwkv_g_up", [L, 128, 256])
        self.rwkv_k_k = self.inp("rwkv_k_k", [L, 256])
        self.rwkv_k_a = self.inp("rwkv_k_a", [L, 256])
        self.rwkv_r_k = self.inp("rwkv_r_k", [L, 4, 64])
        self.rwkv_ln_w = self.inp("rwkv_ln_w", [L, 256])
        self.rwkv_ln_b = self.inp("rwkv_ln_b", [L, 256])
        self.gdn_conv = self.inp("gdn_conv", [L, 5, 768])
        self.gdn_a_log = self.inp("gdn_a_log", [L, 2, 4])
        self.gdn_dt_bias = self.inp("gdn_dt_bias", [L, 2, 4])
        self.gdn_norm_g = self.inp("gdn_norm_g", [L, 64])
        self.c_tri = self.inp("c_tri", [2, 128, 128])
        self.c_msk = self.inp("c_msk", [2, 128, 4, 128])
        self.c_onb = self.inp("c_onb", [128, 128])
        self.attn_q_norm = self.inp("attn_q_norm", [L, 64])
        self.attn_k_norm = self.inp("attn_k_norm", [L, 64])
        self.rope_cos = self.inp("rope_cos", [T, 64])
        self.rope_sin = self.inp("rope_sin", [T, 64])
        self.dbias = self.inp("dbias", [3, 2, 128, 3, 128])
        self.es = ExitStack()
        self.identf = self.sb(self.es, "identf", [128, 128], F32)
        self.identb = self.sb(self.es, "identb", [128, 128], BF16)
        self.AFF = self.sb(self.es, "AFF", [128, NT, NE], F32)
        S = self.S
        S.dma("sp", self.identf, self.ident_f, writes=["identf"])
        S.op("dve", lambda: nc.vector.tensor_copy(out=self.identb, in_=self.identf), reads=["identf"], writes=["identb"])

    def inp(self, name, shape, dtype=F32):
        self.inputs[name] = (tuple(shape), dtype)
        return self.nc.dram_tensor(name, list(shape), dtype, kind="ExternalInput").ap()

    def _uname(self, name):
        self.uid = getattr(self, "uid", 0) + 1
        return "%s_%d" % (name, self.uid)

    def sb(self, es, name, shape, dtype):
        return es.enter_context(self.nc.sbuf_tensor(self._uname(name), list(shape), dtype)).ap()

    def ps(self, es, name, shape, dtype=F32):
        return es.enter_context(self.nc.psum_tensor(self._uname(name), list(shape), dtype)).ap()

    def stage_inproj(self, l, xsrc):
        nc, S = self.nc, self.S
        with ExitStack() as es:
            W = self.sb(es, "Win", [128, 8, P_IN], BF16)
            gB = self.sb(es, "gB", [128, D], F32)
            xts = [self.sb(es, "xt%d" % i, [128, D], F32) for i in range(2)]
            junk = self.sb(es, "junk", [128, D], F32)
            hb = [self.sb(es, "hb%d" % i, [128, D], BF16) for i in range(2)]
            hT = [self.sb(es, "hT%d" % i, [128, 8, 128], BF16) for i in range(2)]
            pts = [self.sb(es, "pt%d" % i, [128, P_IN], F32) for i in range(2)]
            st = self.sb(es, "st", [128, NT, 4], F32)
            pT = self.ps(es, "pT", [128, 8, 128], BF16)
            pss = [self.ps(es, "psA%d" % i, [128, 512], F32) for i in range(4)]
            wv = self.w_in[l].rearrange("(k p) c -> p k c", p=128)
            for k in range(8):
                for c0 in (0, 1864):
                    S.dma("pool", W[:, k, c0:c0 + 1864], wv[:, k, c0:c0 + 1864], writes=[("W", k)])
            S.dma("sp", gB, self.norm_mix_g[l, :].partition_broadcast(128), writes=["gB"])
            nps = 0
            for i in range(NT):
                b = i % 2
                xt = xts[b]
                S.dma("sp", xt, xsrc[i * 128:(i + 1) * 128, :], writes=[("xt", b)])
                S.op("act", lambda: nc.scalar.activation(out=junk, in_=xt, func=AF.Square, accum_out=st[:, i, 0:1]),
                     reads=[("xt", b)], writes=["junk", ("st", i)])
                S.op("dve", lambda: nc.vector.tensor_scalar(out=st[:, i, 1:2], in0=st[:, i, 0:1], scalar1=1.0 / D, scalar2=EPS,
                                                            op0=ALU.mult, op1=ALU.add), reads=[("st", i)], writes=[("st", i)])
                S.op("act", lambda: nc.scalar.activation(out=st[:, i, 2:3], in_=st[:, i, 1:2], func=AF.Sqrt),
                     reads=[("st", i)], writes=[("st", i)])
                S.op("dve", lambda: nc.vector.reciprocal(out=st[:, i, 3:4], in_=st[:, i, 2:3]), reads=[("st", i)], writes=[("st", i)])
                S.op("dve", lambda: nc.vector.scalar_tensor_tensor(out=hb[b], in0=xt, scalar=st[:, i, 3:4], in1=gB,
                                                                   op0=ALU.mult, op1=ALU.mult),
                     reads=[("xt", b), ("st", i), "gB"], writes=[("hb", b)])
                for k in range(8):
                    S.op("pe", lambda: nc.tensor.transpose(out=pT[:, k, :], in_=hb[b][:, k * 128:(k + 1) * 128], identity=self.identb),
                         reads=[("hb", b), "identb"], writes=["pT"])
                S.op("act", lambda: nc.scalar.copy(out=hT[b], in_=pT), reads=["pT"], writes=[("hT", b)])
                for cg in range(8):
                    c0 = cg * 512
                    cw = min(512, P_IN - c0)
                    pb = nps % 4
                    nps += 1
                    for k in range(8):
                        S.op("pe", lambda: nc.tensor.matmul(pss[pb][:, :cw], lhsT=hT[b][:, k, :], rhs=W[:, k, c0:c0 + cw],
                                                            start=(k == 0), stop=(k == 7)),
                             reads=[("hT", b), ("W", k)], writes=[("psA", pb)])
                    if cg % 2 == 0:
                        S.op("dve", lambda: nc.vector.tensor_copy(out=pts[b][:, c0:c0 + cw], in_=pss[pb][:, :cw]),
                             reads=[("psA", pb)], writes=[("pt", b)])
                    else:
                        S.op("act", lambda: nc.scalar.copy(out=pts[b][:, c0:c0 + cw], in_=pss[pb][:, :cw]),
                             reads=[("psA", pb)], writes=[("pt", b)])
                S.dma("sp", self.P[i * 128:(i + 1) * 128, :], pts[b], reads=[("pt", b)])
        S.barrier()

    def rstd_ops(self, ss, tmp, rs, inv_n, eps, r, w):
        nc, S = self.nc, self.S
        S.op("dve", lambda: nc.vector.tensor_scalar(out=tmp, in0=ss, scalar1=inv_n, scalar2=eps, op0=ALU.mult, op1=ALU.add), reads=r, writes=w)
        S.op("act", lambda: nc.scalar.activation(out=tmp, in_=tmp, func=AF.Sqrt), reads=w, writes=w)
        S.op("dve", lambda: nc.vector.reciprocal(out=rs, in_=tmp), reads=w, writes=w)

    def stage_attnB(self, l):
        nc, S = self.nc, self.S
        with ExitStack() as es:
            qT = self.sb(es, "qT", [128, 2, T], BF16)
            kT = self.sb(es, "kT", [128, T], BF16)
            Va = self.sb(es, "Va", [128, NT, 2, 65], BF16)
            gqk = self.sb(es, "gqk", [128, 6, 64], F32)
            fbs = [self.sb(es, "fb%d" % i, [128, 512], F32) for i in range(2)]
            css = [self.sb(es, "cs%d" % i, [128, 2, 64], F32) for i in range(2)]
            sq = self.sb(es, "sqB", [128, 384], F32)
            ss = self.sb(es, "ssB", [128, 6], F32)
            tm = self.sb(es, "tmB", [128, 6], F32)
            rs = self.sb(es, "rsB", [128, 6], F32)
            qn = self.sb(es, "qnB", [128, 6, 64], F32)
            t1 = self.sb(es, "t1B", [128, 6, 64], F32)
            t2 = self.sb(es, "t2B", [128, 6, 64], F32)
            qkb = self.sb(es, "qkb", [128, 6, 64], BF16)
            pTb = [self.sb(es, "pTb%d" % i, [128, 512], BF16) for i in range(2)]
            oT = self.sb(es, "oT", [65, 512], F32)
            rc = self.sb(es, "rcB", [128, 4, 1], F32)
            ybt = [self.sb(es, "ybt%d" % i, [128, 4, 64], F32) for i in range(2)]
            pT3 = self.ps(es, "pT3", [128, 3, 128], BF16)
            ps_s = [self.ps(es, "ps_s%d" % i, [128, 512], F32) for i in range(2)]
            ps_o = self.ps(es, "ps_o", [65, 512], F32)
            ps_t = self.ps(es, "ps_t", [128, 4, 65], F32)
            for h in range(6):
                src = self.attn_q_norm if h < 4 else self.attn_k_norm
                S.dma("sp", gqk[:, h, :], src[l, :].partition_broadcast(128), writes=["gqk"])
            S.op("pool", lambda: nc.gpsimd.memset(Va[:, :, :, 64:65], 1.0), writes=["Va"])
            for i in range(NT):
                b = i % 2
                fb = fbs[b]
                S.dma("sp", fb, self.P[i * 128:(i + 1) * 128, OFF_B:OFF_B + 512], writes=[("fb", b)])
                S.dma("sp", css[b][:, 0, :], self.rope_cos[i * 128:(i + 1) * 128, :], writes=[("cs", b)])
                S.dma("sp", css[b][:, 1, :], self.rope_sin[i * 128:(i + 1) * 128, :], writes=[("cs", b)])
                S.op("dve", lambda: nc.vector.tensor_tensor(out=sq, in0=fb[:, 0:384], in1=fb[:, 0:384], op=ALU.mult), reads=[("fb", b)], writes=["sqB"])
                S.op("dve", lambda: nc.vector.tensor_reduce(out=ss, in_=sq.rearrange("p (h d) -> p h d", d=64), op=ALU.add, axis=AX.X), reads=["sqB"], writes=["ssB"])
                self.rstd_ops(ss, tm, rs, 1.0 / 64, EPS, ["ssB"], ["rsB"])
                f3 = fb[:, 0:384].rearrange("p (h d) -> p h d", d=64)
                S.op("dve", lambda: nc.vector.tensor_tensor(out=qn, in0=f3, in1=rs.unsqueeze(2).to_broadcast([128, 6, 64]), op=ALU.mult),
                     reads=[("fb", b), "rsB"], writes=["qnB"])
                S.op("pool", lambda: nc.gpsimd.tensor_tensor(out=qn, in0=qn, in1=gqk, op=ALU.mult), reads=["qnB", "gqk"], writes=["qnB"])
                cosb = css[b][:, 0, :].unsqueeze(1).to_broadcast([128, 6, 64])
                S.op("dve", lambda: nc.vector.tensor_tensor(out=t1, in0=qn, in1=cosb, op=ALU.mult), reads=["qnB", ("cs", b)], writes=["t1B"])
                q5 = qn.rearrange("p h (a f d) -> p h a f d", a=2, f=2)
                t5 = t2.rearrange("p h (a f d) -> p h a f d", a=2, f=2)
                s5 = css[b][:, 1, :].rearrange("p (a f d) -> p a f d", a=2, f=2)
                for hf in range(2):
                    for a in range(2):
                        S.op("pool", lambda: nc.gpsimd.tensor_tensor(out=t5[:, :, a, hf, :], in0=q5[:, :, a, 1 - hf, :],
                                                                     in1=s5[:, a, hf, :].unsqueeze(1).to_broadcast([128, 6, 16]), op=ALU.mult),
                             reads=["qnB", ("cs", b)], writes=["t2B"])
                S.op("dve", lambda: nc.vector.tensor_tensor(out=qkb[:, 0:4, :].rearrange("p (b a) d -> p a b d", b=2, a=2),
                                                            in0=t1[:, 0:4, :].rearrange("p (a b) d -> p a b d", a=2, b=2),
                                                            in1=t2[:, 0:4, :].rearrange("p (a b) d -> p a b d", a=2, b=2), op=ALU.add),
                     reads=["t1B", "t2B"], writes=["qkb"])
                S.op("dve", lambda: nc.vector.tensor_tensor(out=qkb[:, 4:6, :], in0=t1[:, 4:6, :], in1=t2[:, 4:6, :], op=ALU.add),
                     reads=["t1B", "t2B"], writes=["qkb"])
                S.op("act", lambda: nc.scalar.copy(out=Va[:, i, :, 0:64], in_=fb[:, 384:512].rearrange("p (h d) -> p h d", d=64)),
                     reads=[("fb", b)], writes=["Va"])
                for c in range(3):
                    S.op("pe", lambda: nc.tensor.transpose(out=pT3[:, c, :], in_=qkb[:, 2 * c:2 * c + 2, :], identity=self.identb),
                         reads=["qkb", "identb"], writes=["pT3"])
                S.op("act", lambda: nc.scalar.copy(out=qT[:, :, i * 128:(i + 1) * 128], in_=pT3[:, 0:2, :]), reads=["pT3"], writes=["qT"])
                S.op("act", lambda: nc.scalar.copy(out=kT[:, i * 128:(i + 1) * 128], in_=pT3[:, 2, :]), reads=["pT3"], writes=["kT"])
            n = 0
            for qh in range(4):
                kv = qh // 2
                base = 64 * kv
                ch = qh % 2
                for qc in range(8):
                    def s_mm(kb_):
                        pb_ = kb_ % 2
                        S.op("pe", lambda: nc.tensor.matmul(ps_s[pb_], lhsT=kT[base:base + 64, kb_ * 128:(kb_ + 1) * 128],
                                                            rhs=qT[base:base + 64, ch, qc * 512:(qc + 1) * 512], start=True, stop=True),
                             reads=["kT", "qT"], writes=[("ps_s", pb_)])
                    s_mm(0)
                    for kb in range(NT):
                        pb = kb % 2
                        if kb + 1 < NT:
                            s_mm(kb + 1)
                        S.op("act", lambda: nc.scalar.activation(out=pTb[pb], in_=ps_s[pb], func=AF.Exp, scale=0.125),
                             reads=[("ps_s", pb)], writes=[("pTb", pb)])
                        S.op("pe", lambda: nc.tensor.matmul(ps_o, lhsT=Va[:, kb, kv, :], rhs=pTb[pb], start=(kb == 0), stop=(kb == NT - 1)),
                             reads=["Va", ("pTb", pb)], writes=["ps_o"])
                    S.op("dve", lambda: nc.vector.tensor_copy(out=oT, in_=ps_o), reads=["ps_o"], writes=["oT"])
                    for j in range(4):
                        S.op("pe", lambda: nc.tensor.transpose(out=ps_t[:, j, :], in_=oT[:, j * 128:(j + 1) * 128], identity=self.identf[0:65, 0:65]),
                             reads=["oT", "identf"], writes=["ps_t"])
                    S.op("dve", lambda: nc.vector.reciprocal(out=rc, in_=ps_t[:, :, 64:65]), reads=["ps_t"], writes=["rcB"])
                    yb = ybt[qc % 2]
                    S.op("dve", lambda: nc.vector.tensor_tensor(out=yb, in0=ps_t[:, :, 0:64], in1=rc.to_broadcast([128, 4, 64]), op=ALU.mult),
                         reads=["ps_t", "rcB"], writes=[("ybt", qc % 2)])
                    S.dma("sp", self.mixed[qc * 512:(qc + 1) * 512, 256 + qh * 64:256 + (qh + 1) * 64].rearrange("(j p) d -> p j d", p=128),
                          yb, reads=[("ybt", qc % 2)])
        S.barrier()

    def stage_attnD(self, l):
        nc, S = self.nc, self.S
        with ExitStack() as es:
            dB = self.sb(es, "dB", [128, 6, 3, 128], F32)
            qTd = self.sb(es, "qTd", [128, T], BF16)
            kTd = self.sb(es, "kTd", [128, T], BF16)
            Vd = self.sb(es, "Vd", [128, NT, 2, 65], BF16)
            fds = [self.sb(es, "fd%d" % i, [128, 3, 128], F32) for i in range(2)]
            qkd = self.sb(es, "qkd", [128, 2, 128], BF16)
            sd = [self.sb(es, "sd%d" % i, [128, 3, 128], F32) for i in range(2)]
            pd = [self.sb(es, "pd%d" % i, [128, 3, 128], BF16) for i in range(2)]
            od = [self.sb(es, "od%d" % i, [128, 2, 65], F32) for i in range(2)]
            dn = [self.sb(es, "dn%d" % i, [128, 6, 65], F32) for i in range(2)]
            zs = self.sb(es, "zsD", [128, 2], F32)
            rz = self.sb(es, "rzD", [128, 2], F32)
            yd = [self.sb(es, "yd%d" % i, [128, 6, 64], F32) for i in range(2)]
            pTd = self.ps(es, "pTd", [128, 2, 128], BF16)
            ps_sd = [self.ps(es, "ps_sd%d" % i, [128, 3, 128], F32) for i in range(2)]
            ps_od = [self.ps(es, "ps_od%d" % i, [128, 2, 65], F32) for i in range(2)]
            for br in range(3):
                for j in range(2):
                    S.dma("sp", dB[:, br * 2 + j, :, :], self.dbias[br, j], writes=["dB"])
            S.op("pool", lambda: nc.gpsimd.memset(Vd[:, :, :, 64:65], 1.0), writes=["Vd"])
            Pd = self.P[:, OFF_D:OFF_D + D_IN].rearrange("r (t x) -> r t x", t=3)
            for br, dil in enumerate((1, 4, 16)):
                nb = NT // dil
                Pv = Pd.rearrange("(n m d) t x -> d n m t x", m=128, d=dil)
                Dv = self.Dnum.rearrange("(n m d) s c -> d n m s c", m=128, d=dil)
                for r in range(dil):
                    for b in range(nb):
                        ti = r * nb + b
                        fb = ti % 2
                        S.dma("sp", fds[fb], Pv[r, b][:, :, br * 128:(br + 1) * 128], writes=[("fd", fb)])
                        S.op("act", lambda: nc.scalar.copy(out=qkd, in_=fds[fb][:, 0:2, :]), reads=[("fd", fb)], writes=["qkd"])
                        S.op("pool", lambda: nc.gpsimd.tensor_copy(out=Vd[:, ti, :, 0:64], in_=fds[fb][:, 2, :].rearrange("p (h d) -> p h d", d=64)),
                             reads=[("fd", fb)], writes=["Vd"])
                        for c in range(2):
                            S.op("pe", lambda: nc.tensor.transpose(out=pTd[:, c, :], in_=qkd[:, c, :], identity=self.identb),
                                 reads=["qkd", "identb"], writes=["pTd"])
                        S.op("dve", lambda: nc.vector.tensor_copy(out=qTd[:, ti * 128:(ti + 1) * 128], in_=pTd[:, 0, :]), reads=["pTd"], writes=["qTd"])
                        S.op("dve", lambda: nc.vector.tensor_copy(out=kTd[:, ti * 128:(ti + 1) * 128], in_=pTd[:, 1, :]), reads=["pTd"], writes=["kTd"])
                items = [(r, b, j) for r in range(dil) for b in range(nb) for j in range(2)]

                def d_scores(it):
                    r, b, j = it
                    ti = r * nb + b
                    base = 64 * j
                    for rel in [rel for rel in range(3) if 0 <= b + rel - 1 < nb]:
                        kt = ti + rel - 1
                        S.op("pe", lambda: nc.tensor.matmul(ps_sd[j][:, rel, :], lhsT=kTd[base:base + 64, kt * 128:(kt + 1) * 128],
                                                            rhs=qTd[base:base + 64, ti * 128:(ti + 1) * 128], start=True, stop=True),
                             reads=["kTd", "qTd"], writes=[("ps_sd", j)])

                d_scores(items[0])
                for n_, (r, b, j) in enumerate(items):
                    ti = r * nb + b
                    ob = ti % 2
                    rels = [rel for rel in range(3) if 0 <= b + rel - 1 < nb]
                    r0, r1 = rels[0], rels[-1] + 1
                    if n_ + 1 < len(items):
                        d_scores(items[n_ + 1])
                    S.op("dve", lambda: nc.vector.scalar_tensor_tensor(out=sd[j][:, r0:r1, :], in0=ps_sd[j][:, r0:r1, :], scalar=0.125,
                                                                       in1=dB[:, br * 2 + j, r0:r1, :], op0=ALU.mult, op1=ALU.add),
                         reads=[("ps_sd", j), "dB"], writes=[("sd", j)])
                    S.op("act", lambda: nc.scalar.activation(out=pd[j][:, r0:r1, :], in_=sd[j][:, r0:r1, :], func=AF.Exp),
                         reads=[("sd", j)], writes=[("pd", j)])
                    for rel in rels:
                        kt = ti + rel - 1
                        S.op("pe", lambda: nc.tensor.matmul(ps_od[ob][:, j, :], lhsT=pd[j][:, rel, :], rhs=Vd[:, kt, j, :],
                                                            start=(rel == rels[0]), stop=(rel == rels[-1])),
                             reads=[("pd", j), "Vd"], writes=[("ps_od", ob)])
                    if j == 1:
                        S.op("dve", lambda: nc.vector.tensor_copy(out=od[ob], in_=ps_od[ob]), reads=[("ps_od", ob)], writes=[("od", ob)])
                        S.dma("sp", Dv[r, b][:, br * 2:br * 2 + 2, :], od[ob], reads=[("od", ob)])
            S.barrier()
            for i in range(NT):
                b = i % 2
                S.dma("sp", dn[b], self.Dnum[i * 128:(i + 1) * 128], writes=[("dn", b)])
                z3 = dn[b][:, :, 64].rearrange("p (r j) -> p r j", j=2)
                S.op("dve", lambda: nc.vector.tensor_tensor(out=zs, in0=z3[:, 0, :], in1=z3[:, 1, :], op=ALU.add), reads=[("dn", b)], writes=["zsD"])
                S.op("dve", lambda: nc.vector.tensor_tensor(out=zs, in0=zs, in1=z3[:, 2, :], op=ALU.add), reads=[("dn", b), "zsD"], writes=["zsD"])
                S.op("dve", lambda: nc.vector.reciprocal(out=rz, in_=zs), reads=["zsD"], writes=["rzD"])
                for br in range(3):
                    S.op("dve", lambda: nc.vector.tensor_tensor(out=yd[b][:, br * 2:br * 2 + 2, :], in0=dn[b][:, br * 2:br * 2 + 2, 0:64],
                                                                in1=rz.unsqueeze(2).to_broadcast([128, 2, 64]), op=ALU.mult),
                         reads=[("dn", b), "rzD"], writes=[("yd", b)])
                S.dma("sp", self.mixed[i * 128:(i + 1) * 128, 768:1152], yd[b], reads=[("yd", b)])
        S.barrier()

    def stage_outproj(self, l, xsrc):
        nc, S = self.nc, self.S
        with ExitStack() as es:
            Wo = self.sb(es, "Wo", [128, 9, D], BF16)
            g2 = self.sb(es, "g2B", [128, D], F32)
            Wr = self.sb(es, "Wr", [128, 8, NE], F32)
            rbB = self.sb(es, "rbB", [128, NE], F32)
            mts = [self.sb(es, "mt%d" % i, [128, MIXW], F32) for i in range(2)]
            mb = self.sb(es, "mb", [128, MIXW], BF16)
            mT = self.sb(es, "mT", [128, 9, 128], BF16)
            xts = [self.sb(es, "xo%d" % i, [128, D], F32) for i in range(2)]
            x2s = [self.sb(es, "x2t%d" % i, [128, D], F32) for i in range(2)]
            junk = self.sb(es, "junkO", [128, D], F32)
            st = self.sb(es, "stO", [128, NT, 4], F32)
            h2f = self.sb(es, "h2f", [128, D], F32)
            h2b = [self.sb(es, "h2b%d" % i, [128, D], BF16) for i in range(2)]
            h2T = self.sb(es, "h2T", [128, 8, 128], F32)
            lg = self.sb(es, "lgO", [128, NE], F32)
            mx = self.sb(es, "mxO", [128, 4], F32)
            ex = self.sb(es, "exO", [128, NE], F32)
            pTa = self.ps(es, "pTa", [128, 8, 128], BF16)
            pTc = self.ps(es, "pTc", [128, 1, 128], BF16)
            pso = [self.ps(es, "pso%d" % i, [128, 512], F32) for i in range(2)]
            pT4 = [self.ps(es, "pT4%d" % i, [128, 4, 128], F32) for i in range(2)]
            psr = self.ps(es, "psr", [128, NE], F32)
            wv = self.w_out[l].rearrange("(k p) c -> p k c", p=128)
            for k in range(9):
                S.dma("pool", Wo[:, k, :], wv[:, k, :], writes=["Wo"])
            S.dma("sp", g2, self.norm_ffn_g[l, :].partition_broadcast(128), writes=["g2B"])
            S.dma("sp", Wr, self.router_w[l].rearrange("(k p) e -> p k e", p=128), writes=["Wr"])
            S.dma("sp", rbB, self.router_b[l, :].partition_broadcast(128), writes=["rbB"])
            for i in range(NT):
                b = i % 2
                rows = slice(i * 128, (i + 1) * 128)
                S.dma("sp", mts[b], self.mixed[rows, :], writes=[("mt", b)])
                S.dma("sp", xts[b], xsrc[rows, :], writes=[("xo", b)])
                S.op("act", lambda: nc.scalar.copy(out=mb, in_=mts[b]), reads=[("mt", b)], writes=["mb"])
                for k in range(9):
                    dst = pTa[:, k, :] if k < 8 else pTc[:, 0, :]
                    S.op("pe", lambda: nc.tensor.transpose(out=dst, in_=mb[:, k * 128:(k + 1) * 128], identity=self.identb),
                         reads=["mb", "identb"], writes=["pTa" if k < 8 else "pTc"])
                S.op("act", lambda: nc.scalar.copy(out=mT[:, 0:8, :], in_=pTa), reads=["pTa"], writes=["mT"])
                S.op("dve", lambda: nc.vector.tensor_copy(out=mT[:, 8:9, :], in_=pTc), reads=["pTc"], writes=["mT"])
                x2 = x2s[b]
                for hf in range(2):
                    for k in range(9):
                        S.op("pe", lambda: nc.tensor.matmul(pso[hf], lhsT=mT[:, k, :], rhs=Wo[:, k, hf * 512:(hf + 1) * 512], start=(k == 0), stop=(k == 8)),
                             reads=["mT", "Wo"], writes=[("pso", hf)])
                    S.op("dve", lambda: nc.vector.tensor_tensor(out=x2[:, hf * 512:(hf + 1) * 512], in0=pso[hf], in1=xts[b][:, hf * 512:(hf + 1) * 512], op=ALU.add),
                         reads=[("pso", hf), ("xo", b)], writes=[("x2t", b)])
                S.dma("sp", self.X2[rows, :], x2, reads=[("x2t", b)])
                S.dma("sp", self.X3[rows, :], x2, reads=[("x2t", b)])
                S.op("act", lambda: nc.scalar.activation(out=junk, in_=x2, func=AF.Square, accum_out=st[:, i, 0:1]), reads=[("x2t", b)], writes=["junkO", ("stO", i)])
                self.rstd_ops(st[:, i, 0:1], st[:, i, 1:2], st[:, i, 2:3], 1.0 / D, EPS, [("stO", i)], [("stO", i)])
                S.op("dve", lambda: nc.vector.scalar_tensor_tensor(out=h2f, in0=x2, scalar=st[:, i, 2:3], in1=g2, op0=ALU.mult, op1=ALU.mult),
                     reads=[("x2t", b), ("stO", i), "g2B"], writes=["h2f"])
                S.op("act", lambda: nc.scalar.copy(out=h2b[b], in_=h2f), reads=["h2f"], writes=[("h2b", b)])
                S.dma("sp", self.H2[rows, :], h2b[b], reads=[("h2b", b)])
                for k in range(8):
                    S.op("pe", lambda: nc.tensor.transpose(out=pT4[k // 4][:, k % 4, :], in_=h2f[:, k * 128:(k + 1) * 128], identity=self.identf),
                         reads=["h2f", "identf"], writes=[("pT4", k // 4)])
                S.op("act", lambda: nc.scalar.copy(out=h2T[:, 0:4, :], in_=pT4[0]), reads=[("pT4", 0)], writes=["h2T"])
                S.op("dve", lambda: nc.vector.tensor_copy(out=h2T[:, 4:8, :], in_=pT4[1]), reads=[("pT4", 1)], writes=["h2T"])
                for k in range(8):
                    S.op("pe", lambda: nc.tensor.matmul(psr, lhsT=h2T[:, k, :], rhs=Wr[:, k, :], start=(k == 0), stop=(k == 7)),
                         reads=["h2T", "Wr"], writes=["psr"])
                S.op("dve", lambda: nc.vector.tensor_tensor(out=lg, in0=psr, in1=rbB, op=ALU.add), reads=["psr", "rbB"], writes=["lgO"])
                S.op("dve", lambda: nc.vector.tensor_reduce(out=mx[:, 0:1], in_=lg, op=ALU.max, axis=AX.X), reads=["lgO"], writes=["mxO"])
                S.op("dve", lambda: nc.vector.tensor_scalar(out=lg, in0=lg, scalar1=mx[:, 0:1], scalar2=None, op0=ALU.subtract), reads=["lgO", "mxO"], writes=["lgO"])
                S.op("act", lambda: nc.scalar.activation(out=ex, in_=lg, func=AF.Exp), reads=["lgO"], writes=["exO"])
                S.op("dve", lambda: nc.vector.tensor_reduce(out=mx[:, 2:3], in_=ex, op=ALU.add, axis=AX.X), reads=["exO"], writes=["mxO"])
                S.op("dve", lambda: nc.vector.reciprocal(out=mx[:, 3:4], in_=mx[:, 2:3]), reads=["mxO"], writes=["mxO"])
                S.op("dve", lambda: nc.vector.tensor_scalar(out=self.AFF[:, i, :], in0=ex, scalar1=mx[:, 3:4], scalar2=None, op0=ALU.mult),
                     reads=["exO", "mxO"], writes=["AFF"])
                S.dma("sp", self.AFFd[rows, :], self.AFF[:, i, :], reads=["AFF"])
        S.barrier()

    def stage_moe(self, l):
        nc, S = self.nc, self.S
        AFF = self.AFF
        with ExitStack() as es:
            IDX = self.sb(es, "IDX", [128, NE, 4], U32)
            es1 = ExitStack()
            es_outer = es
            es = es1
            lo = self.sb(es, "loM", [128, NE], F32)
            mid = self.sb(es, "midM", [128, NE], F32)
            cmp_ = self.sb(es, "cmpM", [128, NT, NE], F32)
            pc = self.sb(es, "pcM", [128, NE], F32)
            ge = self.sb(es, "geM", [128, NE], F32)
            onesb = self.sb(es, "onesb", [128, 128], BF16)
            onesf = self.sb(es, "onesf", [128, 128], F32)
            Ub = self.sb(es, "Ub", [128, 128], BF16)
            selb = self.sb(es, "selb", [128, NT, NE], BF16)
            Uf = self.sb(es, "Uf", [128, 128], F32)
            self_f = self.sb(es, "self", [128, NT, NE], F32)
            tot = [self.sb(es, "totM%d" % i, [128, NT, NE], F32) for i in range(2)]
            tot0 = self.sb(es, "tot0", [128, NT, NE], F32)
            rank = self.sb(es, "rankM", [128, NT, NE], F32)
            iotaC = self.sb(es, "iotaC", [128, CAP], F32)
            tvf = self.sb(es, "tvf", [128, NT, 2], F32)
            oh = [self.sb(es, "oh%d" % i, [128, CAP], F32) for i in range(3)]
            rws = self.sb(es, "rws", [2, CAP], F32)
            rwu = self.sb(es, "rwu", [2, CAP], U32)
            IDXf = self.sb(es, "IDXf", [128, NE, 4], F32)
            psc = self.ps(es, "psc", [128, NE], F32)
            psp = self.ps(es, "psp", [128, 512], F32)
            pst = self.ps(es, "pst", [128, 512], F32)
            psid = [self.ps(es, "psid%d" % i, [128, NE, 2], F32) for i in range(4)]
            IDX2 = self.sb(es, "IDX2", [128, NE, 4, 2], F32)
            S.dma("sp", Uf, self.ustrict, writes=["Uf"])
            S.op("dve", lambda: nc.vector.tensor_copy(out=Ub, in_=Uf), reads=["Uf"], writes=["Ub"])
            S.op("dve", lambda: nc.vector.memset(onesb, 1.0), writes=["onesb"])
            S.op("dve", lambda: nc.vector.memset(onesf, 1.0), writes=["onesf"])
            S.dma("sp", iotaC, self.iota_c, writes=["iotaC"])
            S.dma("sp", tvf, self.tvals, writes=["tvf"])
            S.op("dve", lambda: nc.vector.memset(lo, 0.0), writes=["lo"])
            for it in range(32):
                c = 2.0 ** -(it + 1)
                S.op("dve", lambda: nc.vector.tensor_scalar(out=mid, in0=lo, scalar1=c, scalar2=None, op0=ALU.add), reads=["lo"], writes=["mid"])
                S.op("dve", lambda: nc.vector.tensor_tensor(out=cmp_, in0=AFF, in1=mid.unsqueeze(1).to_broadcast([128, NT, NE]), op=ALU.is_ge),
                     reads=["AFF", "mid"], writes=["cmp"])
                S.op("dve", lambda: nc.vector.tensor_reduce(out=pc, in_=cmp_.rearrange("p t e -> p e t"), op=ALU.add, axis=AX.X), reads=["cmp"], writes=["pc"])
                S.op("pe", lambda: nc.tensor.matmul(psc, lhsT=onesf, rhs=pc, start=True, stop=True), reads=["onesf", "pc"], writes=["psc"])
                S.op("dve", lambda: nc.vector.tensor_single_scalar(out=ge, in_=psc, scalar=CAP - 0.5, op=ALU.is_ge), reads=["psc"], writes=["ge"])
                S.op("dve", lambda: nc.vector.scalar_tensor_tensor(out=lo, in0=ge, scalar=c, in1=lo, op0=ALU.mult, op1=ALU.add), reads=["ge", "lo"], writes=["lo"])
            import os
            if os.environ.get("MOE_STOP") == "1":
                S.dma("sp", self.dbg_small[:, 64:80], lo, reads=["lo"])
                S.barrier()
                es1.close()
                return
            S.op("dve", lambda: nc.vector.tensor_tensor(out=self_f, in0=AFF, in1=lo.unsqueeze(1).to_broadcast([128, NT, NE]), op=ALU.is_ge),
                 reads=["AFF", "lo"], writes=["self"])
            S.op("dve", lambda: nc.vector.tensor_tensor(out=selb, in0=AFF, in1=lo.unsqueeze(1).to_broadcast([128, NT, NE]), op=ALU.is_ge),
                 reads=["AFF", "lo"], writes=["selb"])
            sel2 = selb.rearrange("p t e -> p (t e)")
            S.op("pe", lambda: nc.tensor.matmul(psp, lhsT=Ub, rhs=sel2, start=True, stop=True), reads=["Ub", "selb"], writes=["psp"])
            S.op("pe", lambda: nc.tensor.matmul(pst, lhsT=onesb, rhs=sel2, start=True, stop=True), reads=["onesb", "selb"], writes=["pst"])
            S.op("dve", lambda: nc.vector.tensor_copy(out=tot0.rearrange("p t e -> p (t e)"), in_=pst), reads=["pst"], writes=["tot0"])
            S.op("dve", lambda: nc.vector.tensor_copy(out=tot[0].rearrange("p t e -> p (t e)"), in_=pst), reads=["pst"], writes=[("tot", 0)])
            if os.environ.get("MOE_STOP") == "1b":
                S.dma("sp", self.dbg_small[:, 0:16], tot[0][:, 5, :], reads=[("tot", 0)])
                S.barrier()
                es1.close()
                return
            cur = 0
            for sft in (1, 2, 4, 8, 16):
                a, bb = tot[cur], tot[1 - cur]
                S.op("pool", lambda: nc.gpsimd.tensor_copy(out=bb[:, 0:sft, :], in_=a[:, 0:sft, :]), reads=[("tot", cur)], writes=[("tot", 1 - cur)])
                S.op("dve", lambda: nc.vector.tensor_tensor(out=bb[:, sft:NT, :], in0=a[:, sft:NT, :], in1=a[:, 0:NT - sft, :], op=ALU.add),
                     reads=[("tot", cur)], writes=[("tot", 1 - cur)])
                cur = 1 - cur
            inc = tot[cur]
            if os.environ.get("MOE_STOP") == "1c":
                S.dma("sp", self.dbg_small[:, 0:16], inc[:, 5, :], reads=[("tot", cur)])
                S.barrier()
                es1.close()
                return
            S.op("dve", lambda: nc.vector.tensor_tensor(out=rank, in0=inc, in1=tot0, op=ALU.subtract), reads=[("tot", cur), "tot0"], writes=["rank"])
            S.op("dve", lambda: nc.vector.tensor_tensor(out=rank.rearrange("p t e -> p (t e)"), in0=rank.rearrange("p t e -> p (t e)"), in1=psp, op=ALU.add),
                 reads=["rank", "psp"], writes=["rank"])
            S.op("dve", lambda: nc.vector.scalar_tensor_tensor(out=rank, in0=rank, scalar=-9999.0, in1=self_f, op0=ALU.add, op1=ALU.mult),
                 reads=["rank", "self"], writes=["rank"])
            S.op("dve", lambda: nc.vector.tensor_scalar(out=rank, in0=rank, scalar1=9999.0, scalar2=None, op0=ALU.add), reads=["rank"], writes=["rank"])
            if os.environ.get("MOE_STOP") == "2":
                S.dma("sp", self.dbg_small[:, 0:16], rank[:, 5, :], reads=["rank"])
                S.barrier()
                es1.close()
                return
            n = 0
            for e in range(NE):
                for i in range(NT):
                    ob = n % 3
                    n += 1
                    S.op("dve", lambda: nc.vector.tensor_scalar(out=oh[ob], in0=iotaC, scalar1=rank[:, i, e:e + 1], scalar2=None, op0=ALU.is_equal),
                         reads=["iotaC", "rank"], writes=[("oh", ob)])
                    for cc in range(4):
                        S.op("pe", lambda: nc.tensor.matmul(psid[cc][:, e, :], lhsT=oh[ob][:, cc * 128:(cc + 1) * 128], rhs=tvf[:, i, :],
                                                            start=(i == 0), stop=(i == NT - 1)),
                             reads=["tvf", ("oh", ob)], writes=[("psid", cc)])
            for cc in range(4):
                S.op("dve", lambda: nc.vector.tensor_tensor(out=IDXf[:, :, cc], in0=psid[cc][:, :, 0], in1=psid[cc][:, :, 1], op=ALU.add) if False else
                     nc.vector.tensor_copy(out=IDX2[:, :, cc, :], in_=psid[cc]), reads=[("psid", cc)], writes=["IDX2"])
            S.op("dve", lambda: nc.vector.tensor_tensor(out=IDXf, in0=IDX2[:, :, :, 0], in1=IDX2[:, :, :, 1], op=ALU.add), reads=["IDX2"], writes=["IDXf"])
            S.op("dve", lambda: nc.vector.tensor_copy(out=IDX, in_=IDXf), reads=["IDXf"], writes=["IDX"])
            if self.stop_after == "moe_idx":
                S.dma("sp", self.dbg_small[:, 0:64], IDXf.rearrange("p e c -> p (e c)"), reads=["IDXf"])
                S.dma("sp", self.dbg_small[:, 64:80], lo, reads=["lo"])
                S.barrier()
                es1.close()
                return
            S.barrier()
            es1.close()
            es = es_outer
            Wg = [self.sb(es, "Wg%d" % i, [128, 8, D], BF16) for i in range(2)]
            Wu = [self.sb(es, "Wu%d" % i, [128, 8, D], BF16) for i in range(2)]
            Wd = [self.sb(es, "Wd%d" % i, [128, 8, D], BF16) for i in range(2)]
            xs = [self.sb(es, "xs%d" % i, [128, 4, D], BF16) for i in range(2)]
            gt = [self.sb(es, "gt%d" % i, [128, 4, NE], F32) for i in range(2)]
            xsT = self.sb(es, "xsT", [128, 8, CAP], BF16)
            sg = [self.sb(es, "sg%d" % i, [128, CAP], F32) for i in range(2)]
            hid = self.sb(es, "hid", [128, 8, CAP], BF16)
            yt = [self.sb(es, "yt%d" % i, [128, D], F32) for i in range(2)]
            psx = self.ps(es, "psx", [128, 8, 128], BF16)
            psg2 = [self.ps(es, "psg%d" % i, [128, CAP], F32) for i in range(2)]
            psu2 = [self.ps(es, "psu%d" % i, [128, CAP], F32) for i in range(2)]
            psy = [self.ps(es, "psy%d" % i, [128, 512], F32) for i in range(2)]

            def load_w(e):
                wb = e % 2
                for (dst, src, nm) in ((Wg[wb], self.w_gate, "Wg"), (Wu[wb], self.w_up, "Wu"), (Wd[wb], self.w_down, "Wd")):
                    sv = src[l, e].rearrange("(k p) f -> p k f", p=128)
                    for k0 in (0, 4):
                        S.dma("pool", dst[:, k0:k0 + 4, :], sv[:, k0:k0 + 4, :], writes=[(nm, wb)])

            def gather(e):
                wb = e % 2
                for cc in range(4):
                    S.dma_raw("pool", lambda: nc.gpsimd.indirect_dma_start(out=xs[wb][:, cc, :], out_offset=None, in_=self.H2,
                                                                           in_offset=bass.IndirectOffsetOnAxis(ap=IDX[:, e, cc:cc + 1], axis=0)),
                              reads=["IDX"], writes=[("xs", wb)])
                    S.dma_raw("pool", lambda: nc.gpsimd.indirect_dma_start(out=gt[wb][:, cc, :], out_offset=None, in_=self.AFFd,
                                                                           in_offset=bass.IndirectOffsetOnAxis(ap=IDX[:, e, cc:cc + 1], axis=0)),
                              reads=["IDX"], writes=[("gt", wb)])

            load_w(0)
            gather(0)
            ny = 0
            for e in range(NE):
                wb = e % 2
                if e + 1 < NE:
                    load_w(e + 1)
                    gather(e + 1)
                for cc in range(4):
                    for k in range(8):
                        S.op("pe", lambda: nc.tensor.transpose(out=psx[:, k, :], in_=xs[wb][:, cc, k * 128:(k + 1) * 128], identity=self.identb),
                             reads=[("xs", wb), "identb"], writes=["psx"])
                    S.op("act", lambda: nc.scalar.copy(out=xsT[:, :, cc * 128:(cc + 1) * 128], in_=psx), reads=["psx"], writes=["xsT"])
                for f in range(8):
                    psg, psu = psg2[f % 2], psu2[f % 2]
                    for k in range(8):
                        S.op("pe", lambda: nc.tensor.matmul(psg, lhsT=Wg[wb][:, k, f * 128:(f + 1) * 128], rhs=xsT[:, k, :], start=(k == 0), stop=(k == 7)),
                             reads=[("Wg", wb), "xsT"], writes=[("psg", f % 2)])
                    for k in range(8):
                        S.op("pe", lambda: nc.tensor.matmul(psu, lhsT=Wu[wb][:, k, f * 128:(f + 1) * 128], rhs=xsT[:, k, :], start=(k == 0), stop=(k == 7)),
                             reads=[("Wu", wb), "xsT"], writes=[("psu", f % 2)])
                    S.op("act", lambda: nc.scalar.activation(out=sg[f % 2], in_=psg, func=AF.Silu), reads=[("psg", f % 2)], writes=[("sg", f % 2)])
                    S.op("dve", lambda: nc.vector.tensor_tensor(out=hid[:, f, :], in0=sg[f % 2], in1=psu, op=ALU.mult), reads=[("sg", f % 2), ("psu", f % 2)], writes=[("hid", f)])
                for cc in range(4):
                    yb = ny % 2
                    ny += 1
                    for hf in range(2):
                        for f in range(8):
                            S.op("pe", lambda: nc.tensor.matmul(psy[hf], lhsT=hid[:, f, cc * 128:(cc + 1) * 128], rhs=Wd[wb][:, f, hf * 512:(hf + 1) * 512],
                                                                start=(f == 0), stop=(f == 7)), reads=[("hid", f), ("Wd", wb)], writes=[("psy", hf)])
                        S.op("dve", lambda: nc.vector.tensor_scalar(out=yt[yb][:, hf * 512:(hf + 1) * 512], in0=psy[hf], scalar1=gt[wb][:, cc, e:e + 1], scalar2=None, op0=ALU.mult),
                             reads=[("psy", hf), ("gt", wb)], writes=[("yt", yb)])
                    S.dma_raw("pool", lambda: nc.gpsimd.indirect_dma_start(out=self.X3, out_offset=bass.IndirectOffsetOnAxis(ap=IDX[:, e, cc:cc + 1], axis=0),
                                                                           in_=yt[yb], in_offset=None, compute_op=ALU.add),
                              reads=[("yt", yb), "IDX", "X3"], writes=["X3"])
        S.barrier()

    def _e(self, eng):
        return self.S.eng[eng]

    def tt(self, eng, out, a, b, op):
        self.S.op(eng, lambda: self._e(eng).tensor_tensor(out=out.ap, in0=a.ap, in1=b.ap, op=op), reads=[a.key, b.key], writes=[out.key])

    def ts(self, eng, out, a, s1, op0, s2=None, op1=None):
        rd = [a.key]
        v1 = s1
        if isinstance(s1, TB):
            rd.append(s1.key)
            v1 = s1.ap
        kw = {}
        if op1 is not None:
            kw["op1"] = op1
        self.S.op(eng, lambda: self._e(eng).tensor_scalar(out=out.ap, in0=a.ap, scalar1=v1, scalar2=s2, op0=op0, **kw), reads=rd, writes=[out.key])

    def stt(self, out, a, scalar, b, op0, op1):
        rd = [a.key, b.key]
        sv = scalar
        if isinstance(scalar, TB):
            rd.append(scalar.key)
            sv = scalar.ap
        self.S.op("dve", lambda: self.nc.vector.scalar_tensor_tensor(out=out.ap, in0=a.ap, scalar=sv, in1=b.ap, op0=op0, op1=op1), reads=rd, writes=[out.key])

    def act(self, out, a, func, scale=1.0, bias=None):
        rd = [a.key]
        kw = {}
        if isinstance(bias, TB):
            rd.append(bias.key)
            kw["bias"] = bias.ap
        elif bias is not None:
            kw["bias"] = bias
        self.S.op("act", lambda: self.nc.scalar.activation(out=out.ap, in_=a.ap, func=func, scale=scale, **kw), reads=rd, writes=[out.key])

    def cp(self, eng, out, a):
        if eng == "act":
            self.S.op("act", lambda: self.nc.scalar.copy(out=out.ap, in_=a.ap), reads=[a.key], writes=[out.key])
        else:
            self.S.op(eng, lambda: self._e(eng).tensor_copy(out=out.ap, in_=a.ap), reads=[a.key], writes=[out.key])

    def red(self, out, a, op=None):
        self.S.op("dve", lambda: self.nc.vector.tensor_reduce(out=out.ap, in_=a.ap, op=(op or ALU.add), axis=AX.X), reads=[a.key], writes=[out.key])

    def rcp(self, out, a):
        self.S.op("dve", lambda: self.nc.vector.reciprocal(out=out.ap, in_=a.ap), reads=[a.key], writes=[out.key])

    def mm(self, out, lhsT, rhs, start=True, stop=True):
        self.S.op("pe", lambda: self.nc.tensor.matmul(out.ap, lhsT=lhsT.ap, rhs=rhs.ap, start=start, stop=stop), reads=[lhsT.key, rhs.key], writes=[out.key])

    def tp(self, out, a, ident):
        self.S.op("pe", lambda: self.nc.tensor.transpose(out=out.ap, in_=a.ap, identity=ident.ap), reads=[a.key, ident.key], writes=[out.key])

    def ld(self, out, src, q="sp"):
        self.S.dma(q, out.ap, src, writes=[out.key])

    def st_(self, dst, a, q="sp"):
        self.S.dma(q, dst, a.ap, reads=[a.key])

    def tb(self, es, name, shape, dtype=F32):
        return TB(self.sb(es, name, shape, dtype), name)

    def rstd(self, out, ss, tmp, inv_n, eps):
        self.ts("dve", tmp, ss, inv_n, ALU.mult, eps, ALU.add)
        self.act(tmp, tmp, AF.Sqrt)
        self.rcp(out, tmp)

    def bank(self):
        b = self.banks[self.nbank % 8]
        self.nbank += 1
        return b

    def bcast_row(self, es, name, src_row, n):
        t = self.tb(es, name, [128, n])
        self.ld(t, src_row.partition_broadcast(128))
        return t

    def stage_prepA(self, l):
        nc, S = self.nc, self.S
        with ExitStack() as es:
            self.banks = [TB(self.ps(es, "bk%d" % i, [128, 512], F32), ("bk", i)) for i in range(8)]
            self.nbank = 0
            identf = TB(self.identf, "identf")
            mpB = self.bcast_row(es, "mpB", self.rwkv_mu_prev[l, :], 1024)
            mnB = self.bcast_row(es, "mnB", self.rwkv_mu_next[l, :], 1024)
            c0B = self.tb(es, "c0B", [128, 1024])
            self.tt("dve", c0B, mpB, mnB, ALU.add)
            self.ts("dve", c0B, c0B, -1.0, ALU.mult, 1.0, ALU.add)
            kkB = self.bcast_row(es, "kkB", self.rwkv_k_k[l, :], 256)
            kaB = self.bcast_row(es, "kaB", self.rwkv_k_a[l, :], 256)
            omk = self.tb(es, "omk", [128, 256])
            self.ts("dve", omk, kaB, -1.0, ALU.mult, 1.0, ALU.add)
            rkB = self.bcast_row(es, "rkB", self.rwkv_r_k[l].rearrange("h d -> (h d)"), 256)
            w0B = self.tb(es, "w0B", [128, 2, 256])
            a0B = self.tb(es, "a0B", [128, 2, 256])
            for d in range(2):
                self.ld(w0B[:, d, :], self.rwkv_w0[l, d, :].partition_broadcast(128))
                self.ld(a0B[:, d, :], self.rwkv_a0[l, d, :].partition_broadcast(128))
            Wl = self.tb(es, "Wl", [128, 2, 256])
            for d in range(2):
                self.ld(Wl[0:64, d, :], self.rwkv_w_up[l, d])
                self.ld(Wl[64:128, d, :], self.rwkv_a_up[l, d])
            Wg = self.tb(es, "WgA", [128, 256])
            self.ld(Wg, self.rwkv_g_up[l])
            cur = [self.tb(es, "curA%d" % i, [128, 1024]) for i in range(2)]
            prv = [self.tb(es, "prvA%d" % i, [128, 1024]) for i in range(2)]
            nxt = [self.tb(es, "nxtA%d" % i, [128, 1024]) for i in range(2)]
            f = self.tb(es, "fA", [128, 1024])
            t2 = self.tb(es, "t2A", [128, 1024])
            lin = self.tb(es, "linA", [128, 256])
            linT = self.tb(es, "linT", [128, 2, 128])
            zw = self.tb(es, "zwA", [128, 2, 256])
            al = self.tb(es, "alA", [128, 2, 256])
            kkr = self.tb(es, "kkr", [128, 256])
            sq = self.tb(es, "sqA", [128, 256])
            ss = self.tb(es, "ssA", [128, 4])
            tm4 = self.tb(es, "tm4A", [128, 4])
            rs4 = self.tb(es, "rs4A", [128, 4])
            kk = self.tb(es, "kkA", [128, 256])
            tk = self.tb(es, "tkA", [128, 256])
            km = self.tb(es, "kmA", [128, 256])
            TMt = [self.tb(es, "TMtA%d" % i, [128, 2, 6, 256]) for i in range(2)]
            for t_ in TMt:
                S.op("pool", lambda: nc.gpsimd.memset(t_.ap, 0.0), writes=[t_.key])
            aux = [self.tb(es, "auxA%d" % i, [128, 2, 256]) for i in range(2)]
            P = self.P
            for i in range(NT):
                b = i % 2
                r0 = i * 128
                self.ld(cur[b], P[r0:r0 + 128, 0:1024])
                if i == 0:
                    S.op("pool", lambda: nc.gpsimd.memset(prv[b].ap, 0.0), writes=[prv[b].key])
                    S.dma("sp", prv[b].ap[1:128, :], P[0:127, 0:1024], writes=[prv[b].key])
                else:
                    self.ld(prv[b], P[r0 - 1:r0 + 127, 0:1024])
                if i == NT - 1:
                    S.op("pool", lambda: nc.gpsimd.memset(nxt[b].ap, 0.0), writes=[nxt[b].key])
                    S.dma("sp", nxt[b].ap[0:127, :], P[r0 + 1:r0 + 128, 0:1024], writes=[nxt[b].key])
                else:
                    self.ld(nxt[b], P[r0 + 1:r0 + 129, 0:1024])
                self.tt("dve", f, cur[b], c0B, ALU.mult)
                self.tt("pool", t2, prv[b], mpB, ALU.mult)
                self.tt("dve", f, f, t2, ALU.add)
                self.tt("pool", t2, nxt[b], mnB, ALU.mult)
                self.tt("dve", f, f, t2, ALU.add)
                T_ = TMt[b]
                r_, k_, v_ = f[:, 0:256], f[:, 256:512], f[:, 512:768]
                self.act(lin[:, 0:64], f[:, 768:832], AF.Tanh)
                self.cp("act", lin[:, 64:128], f[:, 832:896])
                self.act(lin[:, 128:256], f[:, 896:1024], AF.Sigmoid)
                bkT = self.bank()
                for c in range(2):
                    self.tp(bkT[:, c * 128:(c + 1) * 128], lin[:, c * 128:(c + 1) * 128], identf)
                self.cp("dve", linT.v(lambda a: a.rearrange("p c t -> p (c t)")), bkT[:, 0:256])
                bw = self.bank()
                ba = self.bank()
                for d in range(2):
                    self.mm(bw[:, d * 256:(d + 1) * 256], linT[0:64, 0, :], Wl[0:64, d, :])
                for d in range(2):
                    self.mm(ba[:, d * 256:(d + 1) * 256], linT[64:128, 0, :], Wl[64:128, d, :])
                bg = self.bank()
                self.mm(bg[:, 0:256], linT[:, 1, :], Wg)
                self.tt("dve", zw.v(lambda a: a.rearrange("p d c -> p (d c)")), bw, w0B.v(lambda a: a.rearrange("p d c -> p (d c)")), ALU.add)
                self.tt("dve", al.v(lambda a: a.rearrange("p d c -> p (d c)")), ba, a0B.v(lambda a: a.rearrange("p d c -> p (d c)")), ALU.add)
                self.act(zw, zw, AF.Sigmoid)
                self.act(al, al, AF.Sigmoid)
                self.cp("act", aux[b][:, 1, :], bg[:, 0:256])
                self.tt("dve", kkr, k_, kkB, ALU.mult)
                self.tt("pool", sq, kkr, kkr, ALU.mult)
                self.red(ss, sq.v(lambda a: a.rearrange("p (h d) -> p h d", d=64)))
                self.rstd(rs4, ss, tm4, 1.0, 1e-12)
                self.tt("dve", kk.v(lambda a: a.rearrange("p (h d) -> p h d", d=64)), kkr.v(lambda a: a.rearrange("p (h d) -> p h d", d=64)),
                        rs4.v(lambda a: a.unsqueeze(2).to_broadcast([128, 4, 64])), ALU.mult)
                for d in range(2):
                    self.ts("dve", T_[:, d, 0, :], zw[:, d, :], -0.6065306597126334, ALU.mult)
                    self.tt("pool", T_[:, d, 2, :], kk, al[:, d, :], ALU.mult)
                    self.tt("dve", tk, al[:, d, :], kaB, ALU.mult)
                    self.tt("pool", tk, tk, omk, ALU.add)
                    self.tt("dve", T_[:, d, 3, :], k_, tk, ALU.mult)
                self.ts("dve", T_[:, 0, 1, :], kk, -1.0, ALU.mult)
                self.cp("act", T_[:, 0, 4, :], r_)
                self.cp("act", T_[:, 0, 5, :], v_)
                self.tt("pool", km, T_[:, 0, 3, :], T_[:, 1, 3, :], ALU.add)
                self.tt("dve", km, km, r_, ALU.mult)
                self.tt("pool", km, km, rkB, ALU.mult)
                self.red(ss, km.v(lambda a: a.rearrange("p (h d) -> p h d", d=64)))
                self.ts("dve", ss, ss, 0.5, ALU.mult)
                self.tt("dve", aux[b][:, 0, :].v(lambda a: a.rearrange("p (h d) -> p h d", d=64)), v_.v(lambda a: a.rearrange("p (h d) -> p h d", d=64)),
                        ss.v(lambda a: a.unsqueeze(2).to_broadcast([128, 4, 64])), ALU.mult)
                self.st_(self.TM[r0:r0 + 128, 0], T_)
                self.st_(self.AUX[r0:r0 + 128, 0:2, :], aux[b])
        S.barrier()

    def stage_prepC(self, l):
        nc, S = self.nc, self.S
        with ExitStack() as es:
            cwB = self.tb(es, "cwB", [128, 5, 768])
            for j in range(5):
                self.ld(cwB[:, j, :], self.gdn_conv[l, j, :].partition_broadcast(128))
            alB = self.bcast_row(es, "alogB", self.gdn_a_log[l].rearrange("d h -> (d h)"), 8)
            dtB = self.bcast_row(es, "dtB", self.gdn_dt_bias[l].rearrange("d h -> (d h)"), 8)
            negA = self.tb(es, "negA", [128, 8])
            self.act(negA, alB, AF.Exp)
            self.ts("dve", negA, negA, -1.0, ALU.mult)
            xs = [[self.tb(es, "xc%d_%d" % (j, i), [128, 768]) for j in range(5)] for i in range(2)]
            zt = [self.tb(es, "ztC%d" % i, [128, 272]) for i in range(2)]
            cv = self.tb(es, "cvC", [128, 768])
            t2 = self.tb(es, "t2C", [128, 768])
            sq = self.tb(es, "sqC", [128, 512])
            ss = self.tb(es, "ssC", [128, 8])
            tm8 = self.tb(es, "tm8C", [128, 8])
            rs8 = self.tb(es, "rs8C", [128, 8])
            bt = self.tb(es, "btC", [128, 8])
            nbt = self.tb(es, "nbtC", [128, 8])
            gg = self.tb(es, "ggC", [128, 8])
            TMt = [self.tb(es, "TMtC%d" % i, [128, 2, 6, 256]) for i in range(2)]
            for t_ in TMt:
                S.op("pool", lambda: nc.gpsimd.memset(t_.ap, 0.0), writes=[t_.key])
            aux = [self.tb(es, "auxC%d" % i, [128, 256]) for i in range(2)]
            P = self.P
            h3 = lambda a: a.rearrange("p (h d) -> p h d", d=64)
            for i in range(NT):
                b = i % 2
                r0 = i * 128
                for j in range(5):
                    sh = j - 2
                    lo_, hi_ = r0 + sh, r0 + sh + 128
                    x = xs[b][j]
                    if lo_ < 0 or hi_ > T:
                        S.op("pool", lambda: nc.gpsimd.memset(x.ap, 0.0), writes=[x.key])
                        a0, a1 = max(lo_, 0), min(hi_, T)
                        S.dma("sp", x.ap[a0 - lo_:a1 - lo_, :], P[a0:a1, OFF_C:OFF_C + 768], writes=[x.key])
                    else:
                        self.ld(x, P[lo_:hi_, OFF_C:OFF_C + 768])
                self.ld(zt[b], P[r0:r0 + 128, OFF_C + 768:OFF_C + 1040])
                self.tt("dve", cv, xs[b][0], cwB[:, 0, :], ALU.mult)
                for j in range(1, 5):
                    self.tt("pool", t2, xs[b][j], cwB[:, j, :], ALU.mult)
                    self.tt("dve", cv, cv, t2, ALU.add)
                self.act(cv, cv, AF.Silu)
                T_ = TMt[b]
                self.tt("pool", sq, cv[:, 0:512], cv[:, 0:512], ALU.mult)
                self.red(ss, sq.v(h3))
                self.rstd(rs8, ss, tm8, 1.0, 1e-12)
                self.ts("dve", rs8[:, 0:4], rs8[:, 0:4], 0.125, ALU.mult)
                self.tt("dve", T_[:, 0, 4, :].v(h3), cv[:, 0:256].v(h3), rs8[:, 0:4].v(lambda a: a.unsqueeze(2).to_broadcast([128, 4, 64])), ALU.mult)
                self.tt("dve", T_[:, 0, 2, :].v(h3), cv[:, 256:512].v(h3), rs8[:, 4:8].v(lambda a: a.unsqueeze(2).to_broadcast([128, 4, 64])), ALU.mult)
                self.cp("act", T_[:, 0, 3, :], T_[:, 0, 2, :])
                self.act(bt, zt[b][:, 256:264], AF.Sigmoid)
                self.ts("dve", nbt, bt, -1.0, ALU.mult)
                self.tt("dve", gg, zt[b][:, 264:272], dtB, ALU.add)
                self.act(gg, gg, AF.Exp)
                self.act(gg, gg, AF.Ln, bias=1.0)
                self.tt("dve", gg, gg, negA, ALU.mult)
                for d in range(2):
                    bc = lambda t_: t_[:, d * 4:(d + 1) * 4].v(lambda a: a.unsqueeze(2).to_broadcast([128, 4, 64]))
                    self.cp("pool", T_[:, d, 0, :].v(h3), bc(gg))
                    self.tt("dve", T_[:, d, 1, :].v(h3), T_[:, 0, 2, :].v(h3), bc(nbt), ALU.mult)
                    self.tt("pool", T_[:, d, 5, :].v(h3), cv[:, 512:768].v(h3), bc(bt), ALU.mult)
                self.act(aux[b], zt[b][:, 0:256], AF.Silu)
                self.st_(self.TM[r0:r0 + 128, 1], T_)
                self.st_(self.AUX[r0:r0 + 128, 2, :], aux[b])
        S.barrier()

    def stage_scan(self, l):
        nc, S = self.nc, self.S
        with ExitStack() as es:
            self.banks = [TB(self.ps(es, "bk%d" % i, [128, 512], F32), ("bk", i)) for i in range(8)]
            self.nbank = 0
            identf = TB(self.identf, "identf")
            tri = self.tb(es, "triS", [128, 2, 128])
            msk = self.tb(es, "mskS", [128, 2, 4, 128])
            onb = self.tb(es, "onbS", [128, 128])
            for d in range(2):
                self.ld(tri[:, d, :], self.c_tri[d])
                self.ld(msk[:, d, :, :], self.c_msk[d])
            self.ld(onb, self.c_onb)
            ST = [self.tb(es, "ST%d" % i, [64, 16, 64]) for i in range(2)]
            S.op("pool", lambda: nc.gpsimd.memset(ST[0].ap, 0.0), writes=[ST[0].key])
            U4 = range(4)
            BP = [self.tb(es, "BP%d" % u, [128, 256]) for u in U4]
            KP = [self.tb(es, "KP%d" % u, [128, 256]) for u in U4]
            VV = [self.tb(es, "VV%d" % u, [128, 256]) for u in U4]
            VP = [self.tb(es, "VP%d" % u, [128, 256]) for u in U4]
            G4 = [self.tb(es, "G4%d" % u, [128, 4, 4, 128]) for u in U4]
            APT = [self.tb(es, "APT%d" % u, [64, 4, 128]) for u in U4]
            RST = [self.tb(es, "RST%d" % u, [64, 4, 128]) for u in U4]
            PC = [self.tb(es, "PC%d" % u, [64, 4, 2]) for u in U4]
            def mk_lane(tg):
                lds = [self.tb(es, "ld%d_%s" % (q, tg), [128, 256]) for q in range(5)]
                c0t = self.tb(es, "c0t_" + tg, [128, 256])
                a1 = self.tb(es, "a1_" + tg, [128, 256])
                a1a = self.tb(es, "a1a_" + tg, [128, 256])
                X1 = self.tb(es, "X1_" + tg, [128, 256])
                X1a = self.tb(es, "X1a_" + tg, [128, 256])
                X2 = self.tb(es, "X2_" + tg, [128, 256])
                Ec = self.tb(es, "Ec_" + tg, [128, 256])
                Ag = self.tb(es, "Ag_" + tg, [128, 256])
                Rg = self.tb(es, "Rg_" + tg, [128, 256])
                Bg = self.tb(es, "Bg_" + tg, [128, 256])
                Kg = self.tb(es, "Kg_" + tg, [128, 256])
                As = self.tb(es, "As_" + tg, [128, 256])
                Rs = self.tb(es, "Rs_" + tg, [128, 256])
                FMar = self.tb(es, "FMar_" + tg, [64, 4, 2, 128])
                FMbg = self.tb(es, "FMbg_" + tg, [64, 4, 128])
                FMkg = self.tb(es, "FMkg_" + tg, [64, 4, 128])
                FMcum = self.tb(es, "FMcum_" + tg, [64, 4, 128])
                cumS = self.tb(es, "cumS_" + tg, [128, 256])
                Dm = self.tb(es, "DmS_" + tg, [128, 4, 128])
                mskD = self.tb(es, "mskD_" + tg, [128, 4, 2, 128])
                Xb = [self.tb(es, ("Xb%d_" % i) + tg, [128, 4, 128]) for i in range(2)]
                XTb = [self.tb(es, ("XTb%d_" % i) + tg, [128, 4, 128]) for i in range(2)]
                Wb = [self.tb(es, ("Wb%d_" % i) + tg, [128, 4, 128]) for i in range(2)]
                Zs = self.tb(es, "Zs_" + tg, [128, 256])
                return dict(lds=lds, c0t=c0t, a1=a1, a1a=a1a, X1=X1, X1a=X1a, X2=X2, Ec=Ec, Ag=Ag, Rg=Rg, Bg=Bg, Kg=Kg, As=As, Rs=Rs, FMar=FMar, FMbg=FMbg, FMkg=FMkg, FMcum=FMcum, cumS=cumS, Dm=Dm, mskD=mskD, Xb=Xb, XTb=XTb, Wb=Wb, Zs=Zs)
            avg64 = self.tb(es, "avg64", [64, 128])
            S.op("pool", lambda: nc.gpsimd.memset(avg64.ap, 1.0 / 64), writes=[avg64.key])
            lanes = [mk_lane("L0"), mk_lane("L1")]
            Usb = self.tb(es, "Usb", [128, 2, 512])
            tmpS = self.tb(es, "tmpS", [64, 16, 64])
            Y1s = self.tb(es, "Y1s", [128, 2, 512])
            yt = [self.tb(es, "ytS%d" % d, [128, 512]) for d in range(2)]
            flat = lambda a: a.rearrange("p h t -> p (h t)")
            nld = 0
            cur = 0
            def unit_gen(m, d, j, Lz):
                lds = Lz["lds"]
                c0t = Lz["c0t"]
                a1 = Lz["a1"]
                a1a = Lz["a1a"]
                X1 = Lz["X1"]
                X1a = Lz["X1a"]
                X2 = Lz["X2"]
                Ec = Lz["Ec"]
                Ag = Lz["Ag"]
                Rg = Lz["Rg"]
                Bg = Lz["Bg"]
                Kg = Lz["Kg"]
                As = Lz["As"]
                Rs = Lz["Rs"]
                FMar = Lz["FMar"]
                FMbg = Lz["FMbg"]
                FMkg = Lz["FMkg"]
                FMcum = Lz["FMcum"]
                cumS = Lz["cumS"]
                Dm = Lz["Dm"]
                mskD = Lz["mskD"]
                Xb = Lz["Xb"]
                XTb = Lz["XTb"]
                Wb = Lz["Wb"]
                Zs = Lz["Zs"]
                u = m * 2 + d
                _CUR_U[0] = u
                tile_ = j if d == 0 else NT - 1 - j
                r0 = tile_ * 128
                L_ = lds
                dsrc = {0: d, 1: (0 if m == 0 else d), 2: (d if m == 0 else 0), 3: (d if m == 0 else 0), 4: 0, 5: (0 if m == 0 else d)}
                LW, Aa, Bb, Kk, Rr = L_
                for q, dst in ((0, LW), (1, Aa), (2, Bb), (3, Kk), (4, Rr), (5, VV[u])):
                    self.ld(dst, self.TM[r0:r0 + 128, m, dsrc[q], q, :])
                bc = self.bank()
                self.mm(bc[:, 0:256], tri[:, d, :], LW)
                self.mm(bc[:, 256:512], onb, LW)
                self.ts("dve", c0t, bc[:, 256:512], 0.5, ALU.mult)
                if m == 0:
                    self.tt("dve", a1, bc[:, 0:256], c0t, ALU.subtract)
                    self.act(X1, a1, AF.Exp)
                    self.act(X2, a1, AF.Exp, scale=-1.0)
                    self.act(Ec, c0t, AF.Exp)
                    self.tt("pool", a1a, a1, LW, ALU.subtract)
                    self.act(X1a, a1a, AF.Exp)
                    xa = X1a
                    self.tt("dve", Ag, Aa, xa, ALU.mult)
                    self.tt("pool", Rg, Rr, X1, ALU.mult)
                    self.tt("dve", Bg, Bb, X2, ALU.mult)
                    self.tt("pool", Kg, Kk, X2, ALU.mult)
                    self.tt("dve", As, Ag, Ec, ALU.mult)
                    self.tt("pool", Rs, Rg, Ec, ALU.mult)
                    self.tt("dve", BP[u], Bg, Ec, ALU.mult)
                    self.tt("pool", KP[u], Kg, Ec, ALU.mult)
                    gA, gR, gB_ = Ag, Rg, Bg
                else:
                    cops = [
                        lambda: self.cp("dve", cumS, bc[:, 0:256]),
                        lambda: self.act(X1, cumS, AF.Exp),
                        lambda: self.tt("dve", a1, c0t, cumS, ALU.subtract),
                        lambda: self.tt("dve", a1, a1, c0t, ALU.add),
                        lambda: self.act(X2, a1, AF.Exp),
                        lambda: self.tt("dve", As, Aa, X1, ALU.mult),
                        lambda: self.tt("pool", Rs, Rr, X1, ALU.mult),
                        lambda: self.tt("dve", BP[u], Bb, X2, ALU.mult),
                        lambda: self.cp("pool", KP[u], BP[u]),
                    ]
                    import os as _os
                    for ci, cop in enumerate(cops):
                        if _os.environ.get("SCAN_STOP") == "cn" and ci == int(_os.environ.get("SCAN_N", "0")):
                            S.barrier()
                            return True
                        cop()
                    gA, gR, gB_ = Aa, Rr, Bb
                if _stop("a"):
                    S.barrier()
                    return True
                yield
                bpc = self.bank()
                on2 = onb.v(lambda a: a.rearrange("p (c t) -> p c t", t=64)[:, :, 0])
                for h in range(4):
                    self.mm(bpc[0:64, h * 2:(h + 1) * 2], LW[:, h * 64:(h + 1) * 64], on2)
                self.act(PC[u].v(lambda a: a.rearrange("p h c -> p (h c)")), bpc[0:64, 0:8], AF.Exp)
                if _stop("b"):
                    S.barrier()
                    return True
                yield
                fm_list = [(gA, FMar[:, :, 0, :]), (gR, FMar[:, :, 1, :]), (gB_, FMbg), (Rs, RST[u])]
                fm_list.append((Kg, FMkg) if m == 0 else (cumS, FMcum))
                for (src, dst) in fm_list:
                    bt_ = self.bank()
                    for h in range(4):
                        self.mm(bt_[0:64, h * 128:(h + 1) * 128], src[:, h * 64:(h + 1) * 64], identf)
                    self.cp("act", dst, bt_[0:64, :].v(lambda a: a.rearrange("p (h t) -> p h t", t=128)))
                if _stop("c"):
                    S.barrier()
                    return True
                yield
                nblk = 4 if m == 0 else 2
                if m == 1:
                    bd_ = self.bank()
                    for h in range(4):
                        self.mm(bd_[:, h * 128:(h + 1) * 128], avg64, FMcum[:, h, :])
                    for h in range(4):
                        self.ts("dve", Dm[:, h, :], bd_[:, h * 128:(h + 1) * 128], cumS[:, h * 64:h * 64 + 1], ALU.subtract)
                    self.ts("dve", Dm, Dm, 0.0, ALU.min)
                    if True:
                        pass
                    self.act(Dm, Dm, AF.Exp)
                    for h in range(4):
                        self.tt("pool", mskD[:, h, :, :], msk[:, d, 0:2, :], Dm[:, h, :].v(lambda a: a.unsqueeze(1).to_broadcast([128, 2, 128])), ALU.mult)
                for h in range(4):
                    bg_ = self.bank()
                    rhs = FMar[:, h, :, :].v(lambda a: a.rearrange("p c t -> p (c t)"))
                    self.mm(bg_[:, 0:256], FMbg[:, h, :], rhs)
                    if m == 0:
                        self.mm(bg_[:, 256:512], FMkg[:, h, :], rhs)
                    if _stop("c2"):
                        S.barrier()
                        return True
                    mk_ = msk[:, d, 0:nblk, :] if m == 0 else mskD[:, h, :, :]
                    self.tt("dve", G4[u][:, h, 0:nblk, :].v(lambda a: a.rearrange("p b t -> p (b t)")), bg_[:, 0:nblk * 128],
                            mk_.v(lambda a: a.rearrange("p b t -> p (b t)")), ALU.mult)
                    import os as _os
                    if _stop("c3") and h == int(_os.environ.get("SCAN_H", "0")):
                        S.barrier()
                        return True
                mak_i = 2 if m == 0 else 0
                nrk_i = 3 if m == 0 else 1
                if _stop("d"):
                    S.barrier()
                    return True
                yield
                xi = 0
                Xc, XTc, Wc = Xb[0], XTb[0], Wb[0]
                self.cp("pool", Xc, G4[u][:, :, 0, :])
                bt_ = self.bank()
                for h in range(4):
                    self.tp(bt_[:, h * 128:(h + 1) * 128], Xc[:, h, :], identf)
                self.cp("act", XTc.v(flat), bt_)
                self.tt("dve", Wc, Xc, identf.v(lambda a: a.unsqueeze(1).to_broadcast([128, 4, 128])), ALU.add)
                for lv in range(1, 6):
                    Xn, XTn, Wn = Xb[1 - xi], XTb[1 - xi], Wb[1 - xi]
                    bx = self.bank()
                    for h in range(4):
                        self.mm(bx[:, h * 128:(h + 1) * 128], Xc[:, h, :], XTc[:, h, :])
                    self.cp("act", XTn.v(flat), bx)
                    if lv < 5:
                        by = self.bank()
                        for h in range(4):
                            self.mm(by[:, h * 128:(h + 1) * 128], XTc[:, h, :], Xc[:, h, :])
                        self.cp("pool" if False else "dve", Xn.v(flat), by)
                    bw = self.bank()
                    for h in range(4):
                        self.mm(bw[:, h * 128:(h + 1) * 128], XTn[:, h, :], Wc[:, h, :])
                    self.tt("dve", Wn.v(flat), Wc.v(flat), bw, ALU.add)
                    xi = 1 - xi
                    Xc, XTc, Wc = Xn, XTn, Wn
                    yield
                if _stop("e"):
                    S.barrier()
                    return True
                yield
                bz = self.bank()
                for h in range(4):
                    self.mm(bz[:, h * 64:(h + 1) * 64], G4[u][:, h, mak_i, :], VV[u][:, h * 64:(h + 1) * 64])
                self.cp("act", Zs, bz[:, 0:256])
                yield
                bv = self.bank()
                for h in range(4):
                    self.mm(bv[:, h * 64:(h + 1) * 64], Wc[:, h, :], Zs[:, h * 64:(h + 1) * 64])
                self.cp("act", VP[u], bv[:, 0:256])
                yield
                ba = self.bank()
                for h in range(4):
                    self.mm(ba[0:64, h * 128:(h + 1) * 128], As[:, h * 64:(h + 1) * 64], Wc[:, h, :])
                self.cp("act", APT[u].v(flat), ba[0:64, :])
                import os as _os
                if _stop("u") and u == int(_os.environ.get("SCAN_U", "0")):
                    S.barrier()
                    return True
                yield
            for j in range(NT):
                for m in range(2):
                    gens = [unit_gen(m, 0, j, lanes[0]), unit_gen(m, 1, j, lanes[1])]
                    while gens:
                        for g_ in list(gens):
                            try:
                                next(g_)
                            except StopIteration:
                                gens.remove(g_)
                if _stop("f"):
                    S.barrier()
                    return True
                for sub in range(2):
                    STc, STn = ST[cur], ST[1 - cur]
                    par = [sub, 1 - sub]
                    rows = [slice(par[d] * 64, par[d] * 64 + 64) for d in range(2)]
                    bu = [self.bank(), self.bank()]
                    for d in range(2):
                        for m in range(2):
                            u = m * 2 + d
                            for h in range(4):
                                c0_ = (m * 4 + h) * 64
                                self.mm(bu[d][rows[d], c0_:c0_ + 64], APT[u][:, h, rows[d]], STc[:, d * 8 + m * 4 + h, :])
                    for d in range(2):
                        for m in range(2):
                            u = m * 2 + d
                            self.tt("dve", Usb[rows[d], d, m * 256:(m + 1) * 256], bu[d][rows[d], m * 256:(m + 1) * 256], VP[u][rows[d], :], ALU.add)
                    bs = [self.bank(), self.bank()]
                    for d in range(2):
                        for m in range(2):
                            u = m * 2 + d
                            for h in range(4):
                                c0_ = (m * 4 + h) * 64
                                self.mm(bs[d][0:64, c0_:c0_ + 64], BP[u][rows[d], h * 64:(h + 1) * 64], Usb[rows[d], d, c0_:c0_ + 64], start=True, stop=False)
                                self.mm(bs[d][0:64, c0_:c0_ + 64], KP[u][rows[d], h * 64:(h + 1) * 64], VV[u][rows[d], h * 64:(h + 1) * 64], start=False, stop=True)
                    by1 = [self.bank(), self.bank()]
                    by2 = [self.bank(), self.bank()]
                    for d in range(2):
                        for m in range(2):
                            u = m * 2 + d
                            nrk_i = 3 if m == 0 else 1
                            for h in range(4):
                                c0_ = (m * 4 + h) * 64
                                self.mm(by1[d][rows[d], c0_:c0_ + 64], RST[u][:, h, rows[d]], STc[:, d * 8 + m * 4 + h, :])
                                self.mm(by2[d][rows[d], c0_:c0_ + 64], G4[u][rows[d], h, 1, rows[d]], Usb[rows[d], d, c0_:c0_ + 64], start=True, stop=False)
                                self.mm(by2[d][rows[d], c0_:c0_ + 64], G4[u][rows[d], h, nrk_i, rows[d]], VV[u][rows[d], h * 64:(h + 1) * 64], start=False, stop=True)
                    for d in range(2):
                        for m in range(2):
                            u = m * 2 + d
                            sl_ = slice(d * 8 + m * 4, d * 8 + m * 4 + 4)
                            self.tt("pool", tmpS[:, sl_, :], STc[:, sl_, :], PC[u][:, :, par[d]].v(lambda a: a.unsqueeze(2).to_broadcast([64, 4, 64])), ALU.mult)
                        sl8 = slice(d * 8, d * 8 + 8)
                        self.tt("dve", STn[:, sl8, :].v(lambda a: a.rearrange("p s v -> p (s v)")), tmpS[:, sl8, :].v(lambda a: a.rearrange("p s v -> p (s v)")),
                                bs[d][0:64, :], ALU.add)
                        self.cp("act", Y1s[rows[d], d, :], by1[d][rows[d], :])
                        self.tt("dve", yt[d][rows[d], :], Y1s[rows[d], d, :], by2[d][rows[d], :], ALU.add)
                    cur = 1 - cur
                    if _stop("g"):
                        S.barrier()
                        return True
                for d in range(2):
                    tile_ = j if d == 0 else NT - 1 - j
                    self.st_(self.YAC[tile_ * 128:(tile_ + 1) * 128, :, d, :], yt[d].v(lambda a: a.rearrange("p (m c) -> p m c", m=2)))
        S.barrier()

    def stage_postAC(self, l):
        nc, S = self.nc, self.S
        with ExitStack() as es:
            lnw = self.bcast_row(es, "lnwB", self.rwkv_ln_w[l, :], 256)
            lnb = self.bcast_row(es, "lnbB", self.rwkv_ln_b[l, :], 256)
            gn = self.tb(es, "gnB", [128, 4, 64])
            for h in range(4):
                self.ld(gn[:, h, :], self.gdn_norm_g[l, :].partition_broadcast(128))
            ys = [self.tb(es, "ysP%d" % i, [128, 2, 2, 256]) for i in range(2)]
            ax = [self.tb(es, "axP%d" % i, [128, 3, 256]) for i in range(2)]
            y = self.tb(es, "yP", [128, 256])
            yc = self.tb(es, "ycP", [128, 256])
            sq = self.tb(es, "sqP", [128, 256])
            s4 = self.tb(es, "s4P", [128, 4])
            t4 = self.tb(es, "t4P", [128, 4])
            r4 = self.tb(es, "r4P", [128, 4])
            oa = [self.tb(es, "oaP%d" % i, [128, 256]) for i in range(2)]
            oc = [self.tb(es, "ocP%d" % i, [128, 256]) for i in range(2)]
            h3 = lambda a: a.rearrange("p (h d) -> p h d", d=64)
            b4 = lambda t_: t_.v(lambda a: a.unsqueeze(2).to_broadcast([128, 4, 64]))
            for i in range(NT):
                b = i % 2
                r0 = i * 128
                self.ld(ys[b], self.YAC[r0:r0 + 128])
                self.ld(ax[b], self.AUX[r0:r0 + 128])
                self.tt("dve", y, ys[b][:, 0, 0, :], ys[b][:, 0, 1, :], ALU.add)
                self.red(s4, y.v(h3))
                self.ts("dve", s4, s4, 1.0 / 64, ALU.mult)
                self.tt("dve", yc.v(h3), y.v(h3), b4(s4), ALU.subtract)
                self.tt("pool", sq, yc, yc, ALU.mult)
                self.red(s4, sq.v(h3))
                self.rstd(r4, s4, t4, 1.0 / 64, 64e-5)
                self.tt("dve", yc.v(h3), yc.v(h3), b4(r4), ALU.mult)
                self.tt("pool", yc, yc, lnw, ALU.mult)
                self.tt("dve", yc, yc, lnb, ALU.add)
                self.tt("pool", yc, yc, ax[b][:, 0, :], ALU.add)
                self.tt("dve", oa[b], yc, ax[b][:, 1, :], ALU.mult)
                self.st_(self.mixed[r0:r0 + 128, 0:256], oa[b])
                self.tt("dve", y, ys[b][:, 1, 0, :], ys[b][:, 1, 1, :], ALU.add)
                self.tt("pool", sq, y, y, ALU.mult)
                self.red(s4, sq.v(h3))
                self.rstd(r4, s4, t4, 1.0 / 64, EPS)
                self.tt("dve", yc.v(h3), y.v(h3), b4(r4), ALU.mult)
                self.tt("pool", yc.v(h3), yc.v(h3), gn, ALU.mult)
                self.tt("dve", oc[b], yc, ax[b][:, 2, :], ALU.mult)
                self.st_(self.mixed[r0:r0 + 128, 512:768], oc[b])
        S.barrier()

    def stage_final(self, xsrc):
        nc, S = self.nc, self.S
        with ExitStack() as es:
            gB = self.bcast_row(es, "gFB", self.norm_final_g[0, :], D)
            xt = [self.tb(es, "xF%d" % i, [128, D]) for i in range(2)]
            ot = [self.tb(es, "oF%d" % i, [128, D]) for i in range(2)]
            junk = self.tb(es, "junkF", [128, D])
            st = self.tb(es, "stF", [128, NT, 4])
            for i in range(NT):
                b = i % 2
                self.ld(xt[b], xsrc[i * 128:(i + 1) * 128, :])
                S.op("act", lambda: nc.scalar.activation(out=junk.ap, in_=xt[b].ap, func=AF.Square, accum_out=st.ap[:, i, 0:1]),
                     reads=[xt[b].key], writes=[junk.key, st.key])
                self.rstd(st[:, i, 2:3], st[:, i, 0:1], st[:, i, 1:2], 1.0 / D, EPS)
                self.stt(ot[b], xt[b], st[:, i, 2:3], gB, ALU.mult, ALU.mult)
                self.st_(self.out[i * 128:(i + 1) * 128, :], ot[b])
        S.barrier()

    def build(self):
        nc, S = self.nc, self.S
        self.stage_inproj(0, self.x)
        if self.stop_after == "inproj0":
            return self.finish_debug(self.P, [T, P_IN])
        if self.stop_after in ("moe_idx", "moe", "outproj"):
            self.mixed = self.inp("mixed_in", [T, MIXW])
            self.stage_outproj(0, self.x)
            if self.stop_after == "outproj":
                return self.finish_debug(self.X2, [T, D])
            if self.stop_after == "moe_idx":
                self.dbg_small = nc.dram_tensor("dbg", [128, 80], F32, kind="ExternalOutput").ap()
                self.stage_moe(0)
                self.es.close()
                return nc
            self.stage_moe(0)
            return self.finish_debug(self.X3, [T, D])
        if self.stop_after == "AC":
            import os
            la = int(os.environ.get("ACL", "0"))
            if la:
                self.stage_inproj(la, self.x)
            self.stage_prepA(la)
            self.stage_prepC(la)
            if self.stage_scan(la):
                return self.finish_debug(self.AUX.rearrange("t a c -> t (a c)"), [T, 768])
            if os.environ.get("SCAN_STOP") == "h":
                return self.finish_debug(self.YAC.rearrange("t a b c -> t (a b c)"), [T, 1024])
            self.stage_postAC(la)
            return self.finish_debug(self.mixed, [T, MIXW], cols=[(0, 256), (512, 768)])
        if self.stop_after == "prepAC":
            self.stage_prepA(0)
            self.stage_prepC(0)
            return self.finish_debug(self.TM.rearrange("t m d q c -> t (m d q c)"), [T, 2 * 2 * 6 * 256])
        if self.stop_after == "BD":
            self.stage_attnB(0)
            self.stage_attnD(0)
            return self.finish_debug(self.mixed, [T, MIXW], cols=[(256, 512), (768, 1152)])
        self.out = nc.dram_tensor("out", [T, D], F32, kind="ExternalOutput").ap()
        xsrc = self.x
        for l in range(L):
            if l > 0:
                self.stage_inproj(l, xsrc)
            self.X3 = self.X3s[l % 2]
            self.stage_prepA(l)
            self.stage_prepC(l)
            self.stage_scan(l)
            self.stage_postAC(l)
            self.stage_attnB(l)
            self.stage_attnD(l)
            self.stage_outproj(l, xsrc)
            self.stage_moe(l)
            xsrc = self.X3
        self.stage_final(xsrc)
        self.es.close()
        return nc

    def finish_debug(self, src, shape, cols=None):
        nc, S = self.nc, self.S
        dbg = nc.dram_tensor("dbg", list(shape), F32, kind="ExternalOutput").ap()
        with ExitStack() as es:
            tb = [self.sb(es, "dbgt%d" % i, [128, shape[1]], F32) for i in range(2)]
            for i in range(shape[0] // 128):
                for (c0, c1) in (cols or [(0, shape[1])]):
                    S.dma("sp", tb[i % 2][:, c0:c1], src[i * 128:(i + 1) * 128, c0:c1], writes=[("dbgt", i % 2)])
                    S.dma("sp", dbg[i * 128:(i + 1) * 128, c0:c1], tb[i % 2][:, c0:c1], reads=[("dbgt", i % 2)])
        S.barrier()
        self.es.close()
        return nc


def _t5_bucket(rel):
    nb = 16
    max_exact = 8
    n = np.abs(rel)
    large = max_exact + (np.log(np.maximum(n, 1).astype(np.float32) / np.float32(max_exact))
                         / np.float32(math.log(1024 / max_exact)) * np.float32(nb - max_exact)).astype(np.int32)
    large = np.minimum(large, nb - 1)
    return np.where(rel > 0, nb, 0) + np.where(n < max_exact, n, large)


def host_consts(inputs=None):
    c = {}
    c["ident_f"] = np.eye(128, dtype=np.float32)
    t = np.arange(T)
    inv = (np.float32(10000.0) ** (-np.arange(0, 32, 2, dtype=np.float32) / np.float32(32))).astype(np.float32)
    ar = (t // 64).astype(np.float32)[:, None] * inv
    ac = (t % 64).astype(np.float32)[:, None] * inv
    c["rope_cos"] = np.concatenate([np.cos(ar), np.cos(ar), np.cos(ac), np.cos(ac)], 1).astype(np.float32)
    c["rope_sin"] = np.concatenate([-np.sin(ar), np.sin(ar), -np.sin(ac), np.sin(ac)], 1).astype(np.float32)
    ch = np.arange(128) // 64
    same = (ch[:, None] == ch[None, :])
    sI, tI = np.arange(128)[:, None], np.arange(128)[None, :]
    c["c_onb"] = same.astype(np.float32)
    c["c_tri"] = np.stack([(same & (sI <= tI)), (same & (sI >= tI))]).astype(np.float32)
    mk = np.zeros((2, 128, 4, 128), np.float32)
    for blk in range(4):
        strict = blk in (0, 2)
        mk[0, :, blk, :] = same & ((sI < tI) if strict else (sI <= tI))
        mk[1, :, blk, :] = same & ((sI > tI) if strict else (sI >= tI))
    c["c_msk"] = mk
    c["ustrict"] = np.triu(np.ones((128, 128), np.float32), 1)
    c["iota_c"] = np.tile(np.arange(CAP, dtype=np.float32)[None, :], (128, 1))
    tv = np.zeros((128, NT, 2), np.float32)
    tv[:, :, 0] = np.arange(128)[:, None]
    tv[:, :, 1] = 128.0 * np.arange(NT)[None, :]
    c["tvals"] = tv
    c["tvals_"] = tv
    c["coef2"] = np.array([[1.0], [128.0]], np.float32)
    if inputs is not None:
        rb = np.asarray(inputs["rel_bias"], np.float32)
        kp = np.arange(128)[:, None, None]
        rel = np.arange(3)[None, :, None]
        qp = np.arange(128)[None, None, :]
        o = 128 * (rel - 1) + kp - qp
        valid = np.abs(o) <= 64
        db = np.full((3, 2, 128, 3, 128), NEG, np.float32)
        for br, dil in enumerate((1, 4, 16)):
            bk = _t5_bucket(o * dil)
            for j in range(2):
                db[br, j] = np.where(valid, rb[bk, br * 2 + j], np.float32(NEG))
        c["dbias"] = db
    return c


_PROG = None


def kernel(**inputs):
    global _PROG
    if _PROG is None:
        pr = Prog()
        _PROG = (pr, pr.build())
    pr, nc = _PROG
    consts = host_consts(inputs)
    x = np.asarray(inputs["x"], np.float32)
    nb = x.shape[0]
    in_maps = []
    for b in range(nb):
        m = {}
        for name, (shape, dt) in pr.inputs.items():
            if name == "x":
                m[name] = np.ascontiguousarray(x[b])
            elif name in consts:
                m[name] = consts[name]
            else:
                m[name] = np.ascontiguousarray(np.asarray(inputs[name], np.float32)).reshape(shape)
        in_maps.append(m)
    res = run_bass_kernel_spmd(nc, in_maps, core_ids=list(range(nb)))
    return np.stack([np.asarray(res.results[b]["out"], np.float32) for b in range(nb)])
```
